# Optimizing a Trainium2 kernel written in Bass

```python
import jax, jax.numpy as jnp
from jax import lax
import numpy as np

D_MODEL = 1024
BATCH = 16
SEQ = 2048
DEPTH = 4

GRID_W = 64
CTX_LEN = 256
HEAD_DIM = 64
D_RWKV = D_MODEL // 2
D_NA = D_MODEL - D_RWKV
H_RWKV = D_RWKV // HEAD_DIM
H_NA = D_NA // HEAD_DIM
NA_ROWS_MAX = 8
NA_COLS = 16
R_DECAY = 64
R_AAA = 64
R_GATE = 128
D_SHIFT = 3 * D_RWKV + 2 * R_DECAY + 2 * R_AAA + R_GATE
D_IN = 3 * D_NA + D_SHIFT
D_FF = 2816
N_EXPERTS = 8
TOP_K = 2
D_FF_EXPERT = 3584
MOE_BLOCK = 256
RMS_EPS = 1e-6
GN_EPS = 64e-5
L2_EPS = 1e-12

kernel_name = "hybrid_rwkv7_natten_moe_dit"


def _rms_norm(x, g):
    xf = x.astype(jnp.float32)
    y = xf * lax.rsqrt(jnp.mean(xf * xf, axis=-1, keepdims=True) + RMS_EPS)
    return (y * g.astype(jnp.float32)).astype(x.dtype)


def _centred_shift(p, mu):
    z = jnp.zeros_like(p[:, :1])
    prev = jnp.concatenate([z, p[:, :-1]], axis=1)
    nxt = jnp.concatenate([p[:, 1:], z], axis=1)
    return p + mu * (0.5 * (prev + nxt) - p)


def _rwkv_features(u, w0, w2, a0, a2, g2, k_k, k_a):
    B, T, _ = u.shape
    C = D_RWKV
    r = u[..., :C]
    k = u[..., C:2 * C]
    v = u[..., 2 * C:3 * C]
    o = 3 * C
    wl = u[..., o:o + 2 * R_DECAY].reshape(B, T, 2, R_DECAY)
    o += 2 * R_DECAY
    al = u[..., o:o + 2 * R_AAA].reshape(B, T, 2, R_AAA)
    o += 2 * R_AAA
    gl = u[..., o:o + R_GATE]
    w_log = -jax.nn.softplus(-(w0 + jnp.einsum('btzr,zrc->btzc', jnp.tanh(wl), w2))) - 0.5
    decay = jnp.exp(-jnp.exp(w_log.astype(jnp.float32)))
    a = jax.nn.sigmoid(a0 + jnp.einsum('btzr,zrc->btzc', al, a2))
    g = jax.nn.sigmoid(gl) @ g2
    kk = (k * k_k).reshape(B, T, H_RWKV, HEAD_DIM).astype(jnp.float32)
    kk = kk / jnp.maximum(jnp.sqrt(jnp.sum(kk * kk, axis=-1, keepdims=True)), L2_EPS)
    k_dir = k[:, :, None, :] * (1.0 + (a - 1.0) * k_a)
    return r, k_dir, v, kk, decay, a, g


def _rwkv_scan(feats, d, s0, reverse):
    r, k_dir, v, kk, decay, a, g = feats
    B, T, _ = r.shape

    def hs(t):
        return t.reshape(B, T, H_RWKV, HEAD_DIM).astype(jnp.float32)

    seqs = (hs(r), hs(decay[:, :, d]), hs(k_dir[:, :, d]), hs(v), kk, kk * hs(a[:, :, d]))
    xs = tuple(jnp.moveaxis(t, 1, 0) for t in seqs)

    def step(s, inp):
        r_t, w_t, k_t, v_t, kk_t, b_t = inp
        sa = jnp.einsum('bhvk,bhk->bhv', s, kk_t)
        s = s * w_t[:, :, None, :] - sa[..., None] * b_t[:, :, None, :] + v_t[..., None] * k_t[:, :, None, :]
        return s, jnp.einsum('bhvk,bhk->bhv', s, r_t)

    s_fin, ys = lax.scan(step, s0, xs, reverse=reverse)
    return jnp.moveaxis(ys, 0, 1), s_fin


def _rwkv_readout(y, feats, r_k, ln_w, ln_b):
    r, k_dir, v, kk, decay, a, g = feats
    B, T, _ = r.shape
    mu = jnp.mean(y, axis=-1, keepdims=True)
    var = jnp.mean(jnp.square(y - mu), axis=-1, keepdims=True)
    yn = ((y - mu) * lax.rsqrt(var + GN_EPS)).reshape(B, T, D_RWKV) * ln_w + ln_b
    rh = r.reshape(B, T, H_RWKV, HEAD_DIM)
    kh = jnp.mean(k_dir, axis=2).reshape(B, T, H_RWKV, HEAD_DIM)
    vh = v.reshape(B, T, H_RWKV, HEAD_DIM)
    bonus = (jnp.sum(rh * kh * r_k, axis=-1, keepdims=True) * vh).reshape(B, T, D_RWKV)
    return (yn.astype(r.dtype) + bonus) * g


def _rwkv_group(u_lat, u_ctx, w0, w2, a0, a2, g2, k_k, k_a, r_k, ln_w, ln_b, need_ctx_out):
    f_lat = _rwkv_features(u_lat, w0, w2, a0, a2, g2, k_k, k_a)
    f_ctx = _rwkv_features(u_ctx, w0, w2, a0, a2, g2, k_k, k_a)
    B = u_lat.shape[0]
    ys_lat, ys_ctx = [], []
    for d, rev in enumerate((False, True)):
        s0 = jnp.zeros((B, H_RWKV, HEAD_DIM, HEAD_DIM), jnp.float32)
        y_c, s_c = _rwkv_scan(f_ctx, d, s0, rev)
        y_l, _ = _rwkv_scan(f_lat, d, s_c, rev)
        ys_lat.append(y_l)
        ys_ctx.append(y_c)
    out_lat = _rwkv_readout(ys_lat[0] + ys_lat[1], f_lat, r_k, ln_w, ln_b)
    out_ctx = _rwkv_readout(ys_ctx[0] + ys_ctx[1], f_ctx, r_k, ln_w, ln_b) if need_ctx_out else None
    return out_lat, out_ctx


def _neighbourhood_attention(q, k, v, qc, kc, vc, rpb, need_ctx_out):
    B, T = q.shape[0], q.shape[1]
    rows = T // GRID_W
    kh = min(NA_ROWS_MAX, rows)
    kw = NA_COLS
    scale = HEAD_DIM ** -0.5
    qg = q.reshape(B, rows, GRID_W, H_NA, HEAD_DIM)
    kg = k.reshape(B, rows, GRID_W, H_NA, HEAD_DIM)
    vg = v.reshape(B, rows, GRID_W, H_NA, HEAD_DIM)
    cols = np.arange(GRID_W)
    col_idx = np.clip(cols - kw // 2, 0, GRID_W - kw)[:, None] + np.arange(kw)[None, :]
    col_off = col_idx - cols[:, None] + (NA_COLS - 1)
    rpb_cols = rpb[:, :, col_off]

    def one_row(r):
        r0 = jnp.clip(r - kh // 2, 0, rows - kh)
        q_row = lax.dynamic_index_in_dim(qg, r, axis=1, keepdims=False)
        k_win = lax.dynamic_slice_in_dim(kg, r0, kh, axis=1)[:, :, col_idx]
        v_win = lax.dynamic_slice_in_dim(vg, r0, kh, axis=1)[:, :, col_idx]
        row_off = r0 + jnp.arange(kh) - r + (NA_ROWS_MAX - 1)
        bias = jnp.take(rpb_cols, row_off, axis=1).transpose(0, 2, 1, 3)
        s_loc = jnp.einsum('bqhd,biqjhd->bhqij', q_row, k_win).astype(jnp.float32) * scale + bias.astype(jnp.float32)
        s_ctx = jnp.einsum('bqhd,blhd->bhql', q_row, kc).astype(jnp.float32) * scale
        s = jnp.concatenate([s_loc.reshape(B, H_NA, GRID_W, kh * kw), s_ctx], axis=-1)
        p = jax.nn.softmax(s, axis=-1).astype(v.dtype)
        p_loc = p[..., :kh * kw].reshape(B, H_NA, GRID_W, kh, kw)
        p_ctx = p[..., kh * kw:]
        return jnp.einsum('bhqij,biqjhd->bqhd', p_loc, v_win) + jnp.einsum('bhql,blhd->bqhd', p_ctx, vc)

    y = lax.map(one_row, jnp.arange(rows))
    y_lat = jnp.moveaxis(y, 0, 1).reshape(B, T, D_NA)
    y_ctx = None
    if need_ctx_out:
        s = jnp.einsum('blhd,bmhd->bhlm', qc, kc).astype(jnp.float32) * scale
        p = jax.nn.softmax(s, axis=-1).astype(vc.dtype)
        y_ctx = jnp.einsum('bhlm,bmhd->blhd', p, vc).reshape(B, qc.shape[1], D_NA)
    return y_lat, y_ctx


def _mixer(h_lat, h_ctx, w_in, shift_mu, w0, w2, a0, a2, g2, k_k, k_a, r_k, ln_w, ln_b, rpb, w_out, need_ctx_out):
    p_lat = h_lat @ w_in
    p_ctx = h_ctx @ w_in
    n_na = 3 * D_NA

    def heads(t):
        return t.reshape(t.shape[0], t.shape[1], H_NA, HEAD_DIM)

    q, k, v = [heads(t) for t in jnp.split(p_lat[..., :n_na], 3, axis=-1)]
    qc, kc, vc = [heads(t) for t in jnp.split(p_ctx[..., :n_na], 3, axis=-1)]
    y_na_lat, y_na_ctx = _neighbourhood_attention(q, k, v, qc, kc, vc, rpb, need_ctx_out)
    u_lat = _centred_shift(p_lat[..., n_na:], shift_mu)
    u_ctx = _centred_shift(p_ctx[..., n_na:], shift_mu)
    y_rw_lat, y_rw_ctx = _rwkv_group(u_lat, u_ctx, w0, w2, a0, a2, g2, k_k, k_a, r_k, ln_w, ln_b, need_ctx_out)
    out_lat = jnp.concatenate([y_rw_lat, y_na_lat], axis=-1) @ w_out
    out_ctx = jnp.concatenate([y_rw_ctx, y_na_ctx], axis=-1) @ w_out if need_ctx_out else None
    return out_lat, out_ctx


def _swiglu(h, w1, w3, w2):
    return (jax.nn.silu(h @ w1) * (h @ w3)) @ w2


def _moe_swiglu(h, router, w1, w3, w2):
    shp = h.shape
    x = h.reshape(-1, shp[-1])
    n, d = x.shape
    logits = (x @ router).astype(jnp.float32)
    top_logit, top_idx = lax.top_k(logits, TOP_K)
    gate = jax.nn.softmax(top_logit, axis=-1)
    a = n * TOP_K
    flat_e = top_idx.reshape(a)
    flat_tok = jnp.broadcast_to(jnp.arange(n, dtype=jnp.int32)[:, None], (n, TOP_K)).reshape(a)
    flat_g = gate.reshape(a)
    order = jnp.argsort(flat_e)
    e_sorted = flat_e[order]
    counts = jnp.bincount(flat_e, length=N_EXPERTS)
    starts = jnp.cumsum(counts) - counts
    padded = (counts + MOE_BLOCK - 1) // MOE_BLOCK * MOE_BLOCK
    pad_ends = jnp.cumsum(padded)
    pad_starts = pad_ends - padded
    dest = pad_starts[e_sorted] + (jnp.arange(a) - starts[e_sorted])
    n_blocks = -(-a // MOE_BLOCK) + N_EXPERTS
    n_slots = n_blocks * MOE_BLOCK
    slot_tok = jnp.full((n_slots,), n, jnp.int32).at[dest].set(flat_tok[order])
    slot_g = jnp.zeros((n_slots,), jnp.float32).at[dest].set(flat_g[order])
    block_e = jnp.minimum(jnp.searchsorted(pad_ends, jnp.arange(n_blocks) * MOE_BLOCK, side='right'), N_EXPERTS - 1)
    x_pad = jnp.concatenate([x, jnp.zeros((1, d), x.dtype)], axis=0)
    xs = x_pad[slot_tok].reshape(n_blocks, MOE_BLOCK, d)

    def run_block(args):
        xb, e = args
        return _swiglu(xb, w1[e], w3[e], w2[e])

    yb = lax.map(run_block, (xs, block_e)).reshape(n_slots, d)
    y = jnp.zeros((n + 1, d), x.dtype).at[slot_tok].add(yb * slot_g[:, None].astype(x.dtype))
    return y[:n].reshape(shp)


def setup_inputs(seed: int = 0) -> dict:
    key = jax.random.key(seed)
    ks = jax.random.split(key, 32)
    f32 = jnp.float32
    n_dense = (DEPTH + 1) // 2
    n_moe = DEPTH // 2

    def nrm(k, shape, s):
        return jax.random.normal(k, shape, f32) * s

    return {
        'x': nrm(ks[0], (BATCH, SEQ, D_MODEL), 1.0),
        'c': nrm(ks[1], (BATCH, D_MODEL), 1.0),
        'ctx': nrm(ks[2], (BATCH, CTX_LEN, D_MODEL), 1.0),
        'c_ctx': nrm(ks[3], (D_MODEL,), 1.0),
        'ada_w': nrm(ks[4], (DEPTH, D_MODEL, 6 * D_MODEL), 0.5 * D_MODEL ** -0.5),
        'ada_b': nrm(ks[5], (DEPTH, 6 * D_MODEL), 0.01),
        'norm_mix_g': 1.0 + nrm(ks[6], (DEPTH, D_MODEL), 0.05),
        'norm_ffn_g': 1.0 + nrm(ks[7], (DEPTH, D_MODEL), 0.05),
        'w_in': nrm(ks[8], (DEPTH, D_MODEL, D_IN), D_MODEL ** -0.5),
        'shift_mu': jax.random.uniform(ks[9], (DEPTH, D_SHIFT), f32),
        'w0': jax.random.uniform(ks[10], (DEPTH, 2, D_RWKV), f32, -6.0, -1.0),
        'w2': nrm(ks[11], (DEPTH, 2, R_DECAY, D_RWKV), 0.5 * R_DECAY ** -0.5),
        'a0': nrm(ks[12], (DEPTH, 2, D_RWKV), 0.1),
        'a2': nrm(ks[13], (DEPTH, 2, R_AAA, D_RWKV), 0.5 * R_AAA ** -0.5),
        'g2': nrm(ks[14], (DEPTH, R_GATE, D_RWKV), R_GATE ** -0.5),
        'k_k': 0.85 + nrm(ks[15], (DEPTH, D_RWKV), 0.05),
        'k_a': 1.0 + nrm(ks[16], (DEPTH, D_RWKV), 0.05),
        'r_k': nrm(ks[17], (DEPTH, H_RWKV, HEAD_DIM), 0.1),
        'ln_x_w': 1.0 + nrm(ks[18], (DEPTH, D_RWKV), 0.05),
        'ln_x_b': nrm(ks[19], (DEPTH, D_RWKV), 0.01),
        'na_rpb': nrm(ks[20], (DEPTH, H_NA, 2 * NA_ROWS_MAX - 1, 2 * NA_COLS - 1), 0.1),
        'w_out': nrm(ks[21], (DEPTH, D_MODEL, D_MODEL), D_MODEL ** -0.5),
        'ffn_w1': nrm(ks[22], (n_dense, D_MODEL, D_FF), D_MODEL ** -0.5),
        'ffn_w3': nrm(ks[23], (n_dense, D_MODEL, D_FF), D_MODEL ** -0.5),
        'ffn_w2': nrm(ks[24], (n_dense, D_FF, D_MODEL), D_FF ** -0.5),
        'router': nrm(ks[25], (n_moe, D_MODEL, N_EXPERTS), D_MODEL ** -0.5),
        'moe_w1': nrm(ks[26], (n_moe, N_EXPERTS, D_MODEL, D_FF_EXPERT), D_MODEL ** -0.5),
        'moe_w3': nrm(ks[27], (n_moe, N_EXPERTS, D_MODEL, D_FF_EXPERT), D_MODEL ** -0.5),
        'moe_w2': nrm(ks[28], (n_moe, N_EXPERTS, D_FF_EXPERT, D_MODEL), D_FF_EXPERT ** -0.5),
        'final_g': 1.0 + nrm(ks[29], (D_MODEL,), 0.05),
    }


def reference(x, c, ctx, c_ctx, ada_w, ada_b, norm_mix_g, norm_ffn_g, w_in, shift_mu, w0, w2, a0, a2, g2,
              k_k, k_a, r_k, ln_x_w, ln_x_b, na_rpb, w_out, ffn_w1, ffn_w3, ffn_w2, router, moe_w1, moe_w3,
              moe_w2, final_g):
    L = ctx.shape[1]
    s_c = jax.nn.silu(c)
    s_cc = jax.nn.silu(c_ctx)
    for l in range(DEPTH):
        last = l == DEPTH - 1
        mod = jnp.split((s_c @ ada_w[l] + ada_b[l])[:, None, :], 6, axis=-1)
        mod_c = jnp.split(s_cc @ ada_w[l] + ada_b[l], 6, axis=-1)
        h = _rms_norm(x, norm_mix_g[l]) * (1.0 + mod[1]) + mod[0]
        hc = _rms_norm(ctx, norm_mix_g[l]) * (1.0 + mod_c[1]) + mod_c[0]
        o, oc = _mixer(h, hc, w_in[l], shift_mu[l], w0[l], w2[l], a0[l], a2[l], g2[l], k_k[l], k_a[l], r_k[l],
                       ln_x_w[l], ln_x_b[l], na_rpb[l], w_out[l], not last)
        x = x + mod[2] * o
        h = _rms_norm(x, norm_ffn_g[l]) * (1.0 + mod[4]) + mod[3]
        if not last:
            ctx = ctx + mod_c[2] * oc
            hc = _rms_norm(ctx, norm_ffn_g[l]) * (1.0 + mod_c[4]) + mod_c[3]
            tokens = jnp.concatenate([hc, h], axis=1)
        else:
            tokens = h
        i = l // 2
        if l % 2 == 0:
            y = _swiglu(tokens, ffn_w1[i], ffn_w3[i], ffn_w2[i])
        else:
            y = _moe_swiglu(tokens, router[i], moe_w1[i], moe_w3[i], moe_w2[i])
        if not last:
            ctx = ctx + mod_c[5] * y[:, :L]
            x = x + mod[5] * y[:, L:]
        else:
            x = x + mod[5] * y
    return _rms_norm(x, final_g)
```

```python
import numpy as np
import concourse.bass as bass
import concourse.mybir as mybir
from concourse.bass_utils import run_bass_kernel_spmd
from contextlib import ExitStack

F32 = mybir.dt.float32
BF16 = mybir.dt.bfloat16
AF = mybir.ActivationFunctionType
ALU = mybir.AluOpType
AX = mybir.AxisListType

SAME_ENGINE_SYNC = True
DMA_RING = {"sp": 16, "act": 4, "pool": 16}
SEM_LIMIT = 60000
MAX_EPOCHS = 7


class Res:
    __slots__ = ("name", "w", "rd")

    def __init__(self, name=""):
        self.name = name
        self.w = None
        self.rd = []


class K:
    ENG = ("pe", "act", "dve", "pool", "sp")
    CE = ("pe", "act", "dve", "pool")

    def __init__(self, nc, stack):
        self.nc = nc
        self.stack = stack
        self.recs = []
        self.cnt = {e: 0 for e in self.ENG}
        self.slots = []
        self.dq = {}
        for q in ("sp", "act", "pool"):
            idx = []
            for i in range(DMA_RING[q]):
                self.slots.append(0)
                idx.append(len(self.slots) - 1)
            self.dq[q] = {"idx": idx, "n": 0}
        self.waited_e = {e: {} for e in self.ENG}
        self.waited_d = {e: {} for e in self.ENG}
        self.n_inst = 0

    def _need(self, eng, tok, waits, force=False):
        if tok is None:
            return
        if tok[0] == "e":
            _, x, seq = tok
            if x == eng and (not SAME_ENGINE_SYNC or eng == "pe") and not force:
                return
            if self.waited_e[eng].get(x, 0) >= seq:
                return
            self.waited_e[eng][x] = seq
            waits.append(tok)
        else:
            _, si, use = tok
            if self.waited_d[eng].get(si, 0) >= use:
                return
            self.waited_d[eng][si] = use
            waits.append(tok)

    def _deps(self, eng, reads, writes):
        waits = []
        for r in reads:
            self._need(eng, r.w, waits)
        for w in writes:
            self._need(eng, w.w, waits)
            best = {}
            for t in w.rd:
                key = (t[0], t[1])
                if key not in best or best[key][2] < t[2]:
                    best[key] = t
            for t in best.values():
                self._need(eng, t, waits)
        return waits

    def _commit(self, tok, reads, writes):
        for r in reads:
            if r.rd and r.rd[-1][0] == tok[0] and r.rd[-1][1] == tok[1]:
                r.rd[-1] = tok
            else:
                r.rd.append(tok)
        for w in writes:
            w.w = tok
            w.rd = []

    def op(self, eng, fn, reads=(), writes=()):
        waits = self._deps(eng, reads, writes)
        self.cnt[eng] += 1
        tok = ("e", eng, self.cnt[eng])
        self.recs.append({"eng": eng, "waits": waits, "fn": fn, "tok": tok})
        self._commit(tok, reads, writes)
        self.n_inst += 1
        return tok

    def dma(self, q, out, in_, reads=(), writes=(), **kw):
        waits = self._deps(q, reads, writes)
        d = self.dq[q]
        si = d["idx"][d["n"] % len(d["idx"])]
        d["n"] += 1
        prev = self.slots[si]
        if prev > 0:
            self._need(q, ("d", si, prev), waits)
        self.slots[si] = prev + 1
        tok = ("d", si, prev + 1)
        fn = (lambda e, o=out, i=in_, kw=kw: e.dma_start(out=o, in_=i, **kw))
        self.recs.append({"eng": q, "waits": waits, "fn": fn, "tok": tok})
        self._commit(tok, reads, writes)
        self.n_inst += 1
        return tok

    def _all_waits(self, eng):
        waits = []
        for x in self.CE:
            if self.cnt[x] > 0:
                self._need(eng, ("e", x, self.cnt[x]), waits, force=True)
        for si, v in enumerate(self.slots):
            if v > 0:
                self._need(eng, ("d", si, v), waits)
        return waits

    def barrier(self):
        for eng in self.ENG:
            self.recs.append({"eng": eng, "waits": self._all_waits(eng), "fn": None, "tok": None})

    def final_wait(self, eng, toks):
        waits = []
        for t in toks:
            self._need(eng, t, waits)
        self.recs.append({"eng": eng, "waits": waits, "fn": None, "tok": None})

    def emit(self):
        nc, st = self.nc, self.stack
        recs = self.recs
        needed = set()
        for r in recs:
            for w in r["waits"]:
                if w[0] == "e":
                    needed.add((w[1], w[2]))
        out = []
        ep = 0
        ms = {e: 0 for e in self.CE}
        du = [0] * len(self.slots)
        last_e = {e: 0 for e in self.CE}
        last_d = [0] * len(self.slots)
        val = {}
        pend_last = {}

        def close_epoch():
            nonlocal ep, ms, du
            bw = {}
            for e in self.CE:
                if e in pend_last:
                    rr = out[pend_last[e]]
                    t = rr["tok"]
                    if t not in val:
                        ms[e] += 1
                        val[t] = (ep, ms[e])
                        rr["inc"] = True
            allw = []
            for e in self.CE:
                if e in pend_last:
                    allw.append(out[pend_last[e]]["tok"])
            for si in range(len(self.slots)):
                if du[si] > 0:
                    allw.append(("d", si, last_d[si]))
            for eng in self.ENG:
                out.append({"eng": eng, "waits": list(allw), "fn": None, "tok": None, "ep": ep})
            ep += 1
            ms = {e: 0 for e in self.CE}
            du = [0] * len(self.slots)
            pend_last.clear()

        for r in recs:
            t = r["tok"]
            if t is not None:
                if t[0] == "e":
                    if ms[t[1]] + 2 > SEM_LIMIT:
                        close_epoch()
                else:
                    if (du[t[1]] + 2) * 16 > SEM_LIMIT:
                        close_epoch()
            r = dict(r)
            r["ep"] = ep
            out.append(r)
            if t is not None:
                if t[0] == "e":
                    pend_last[t[1]] = len(out) - 1
                    if (t[1], t[2]) in needed:
                        ms[t[1]] += 1
                        val[t] = (ep, ms[t[1]])
                        r["inc"] = True
                    else:
                        r["inc"] = False
                else:
                    du[t[1]] += 1
                    last_d[t[1]] = t[2]
                    val[t] = (ep, du[t[1]] * 16)
        n_ep = ep + 1
        print("epochs needed", n_ep, "milestones", ms, "dma uses", max(du))
        assert n_ep <= MAX_EPOCHS, "too many epochs %d" % n_ep
        self.n_epochs = n_ep
        esem = [{e: st.enter_context(nc.semaphore("c%d_%s" % (i, e))) for e in self.CE} for i in range(n_ep)]
        dsem = [[st.enter_context(nc.semaphore("d%d_%d" % (i, j))) for j in range(len(self.slots))] for i in range(n_ep)]
        streams = {e: [] for e in self.ENG}
        for r in out:
            streams[r["eng"]].append(r)

        with nc.Block() as block:
            def run(engname, e):
                for ep_ in range(1, n_ep):
                    if engname in self.CE:
                        e.sem_clear(esem[ep_][engname])
                    if engname in self.dq:
                        for si in self.dq[engname]["idx"]:
                            e.sem_clear(dsem[ep_][si])
                for r in streams[engname]:
                    for w in r["waits"]:
                        v = val.get(w)
                        if v is None or v[0] != r["ep"]:
                            continue
                        if w[0] == "e":
                            e.wait_ge(esem[v[0]][w[1]], v[1])
                        else:
                            e.wait_ge(dsem[v[0]][w[1]], v[1])
                    if r["fn"] is None:
                        continue
                    ins = r["fn"](e)
                    t = r["tok"]
                    if t[0] == "e":
                        if r.get("inc"):
                            ins.then_inc(esem[r["ep"]][t[1]], 1)
                    else:
                        ins.then_inc(dsem[r["ep"]][t[1]], 16)

            @block.sync
            def _(e):
                run("sp", e)

            @block.tensor
            def _(e):
                run("pe", e)

            @block.vector
            def _(e):
                run("dve", e)

            @block.scalar
            def _(e):
                run("act", e)

            @block.gpsimd
            def _(e):
                run("pool", e)


D = 1024
DEPTH = 4
NB = 2
LC = 256
T = 2048
S = LC + T
NT = S // 128
D_IN = 3456
DFF = 2816
DFE = 3584
NE = 8
NEG = -30000.0

DEBUG_OUT = []
RW_DBG = {"nch": None, "upto": "Z", "nb": None, "nd": 2, "readout": True}
N_LAYERS_RUN = DEPTH


def build_program(n_layers=DEPTH, stages=None, debug=(), na_kw={}):
    nc = bass.Bass("TRN2", target_bir_lowering=False)
    st = ExitStack()
    with st:
        k = K(nc, st)

        def din(name, shape, dt=F32):
            return nc.dram_tensor(name, list(shape), dt, kind="ExternalInput").ap()

        def dscr(name, shape, dt=F32):
            kind = "ExternalOutput" if name in debug else "Internal"
            return nc.dram_tensor(name, list(shape), dt, kind=kind).ap()

        I = {}
        I["xT"] = din("xT", [NB, 128, 8, S])
        I["cT"] = din("cT", [128, 8, 3])
        I["ada_w"] = din("ada_w", [DEPTH, 128, 8, 6 * D])
        I["ada_bT"] = din("ada_bT", [DEPTH, 128, 48])
        I["g_mix"] = din("g_mix", [DEPTH, 128, 8])
        I["g_ffn"] = din("g_ffn", [DEPTH, 128, 8])
        I["w_in"] = din("w_in", [DEPTH, 128, 8, D_IN])
        I["mu"] = din("mu", [DEPTH, 128, 15])
        I["ident"] = din("ident", [128, 128])
        I["final_g"] = din("final_g", [128, 8])
        I["bfull"] = din("bfull", [DEPTH, 64, 8, 960])
        I["w_out"] = din("w_out", [DEPTH, 128, 8, D])
        I["rw_w2"] = din("rw_w2", [DEPTH, 128, 512]); I["rw_a2"] = din("rw_a2", [DEPTH, 128, 512]); I["rw_g2"] = din("rw_g2", [DEPTH, 128, 512])
        I["rw_a0T"] = din("rw_a0T", [DEPTH, 128, 2, 4]); I["rw_kk"] = din("rw_kk", [DEPTH, 128, 4]); I["rw_ka"] = din("rw_ka", [DEPTH, 128, 4]); I["rw_rk"] = din("rw_rk", [DEPTH, 128, 4])
        I["rw_lnw"] = din("rw_lnw", [DEPTH, 128, 512]); I["rw_lnb"] = din("rw_lnb", [DEPTH, 128, 512]); I["rw_w0b"] = din("rw_w0b", [DEPTH, 64, 2, 512])
        I["rw_m2"] = din("rw_m2", [64, 2, 128]); I["rw_m3"] = din("rw_m3", [64, 2, 128]); I["rw_mL"] = din("rw_mL", [64, 2, 64])
        I["rw_bones"] = din("rw_bones", [128, 128]); I["rw_sel"] = din("rw_sel", [128, 2]); I["rw_c1"] = din("rw_c1", [128, 4])
        I["ffn_w1"] = din("ffn_w1", [2, 128, 8, DFF]); I["ffn_w3"] = din("ffn_w3", [2, 128, 8, DFF]); I["ffn_w2"] = din("ffn_w2", [2, 128, DFF // 128, D])
        I["router"] = din("router", [2, 128, 8, NE])
        I["moe_w1"] = din("moe_w1", [2, NE, 128, 8, DFE]); I["moe_w3"] = din("moe_w3", [2, NE, 128, 8, DFE]); I["moe_w2"] = din("moe_w2", [2, NE, 128, DFE // 128, D])
        out = nc.dram_tensor("out", [NB, 128, 8, T], F32, kind="ExternalOutput").ap()

        XT = [dscr("xt%d" % b, [128, 8, S]) for b in range(NB)]
        HT = [dscr("ht%d" % b, [128, 8, S], BF16) for b in range(NB)]
        QT = [dscr("qt%d" % b, [128, 4, S], BF16) for b in range(NB)]
        KT = [dscr("kt%d" % b, [128, 4, S], BF16) for b in range(NB)]
        VN = [dscr("vn%d" % b, [S, 512], BF16) for b in range(NB)]
        UT = [dscr("ut%d" % b, [128, 15, S]) for b in range(NB)]
        R_XT = [Res() for _ in range(NB)]
        R_HT = [Res() for _ in range(NB)]
        R_QK = [Res() for _ in range(NB)]
        R_VN = [Res() for _ in range(NB)]
        R_UT = [Res() for _ in range(NB)]
        YT = [dscr("yt%d" % b, [128, 8, S], BF16) for b in range(NB)]
        H2T = [dscr("h2t%d" % b, [128, 8, S], BF16) for b in range(NB)]
        H2F = [dscr("h2f%d" % b, [128, 8, S]) for b in range(NB)]
        GB = [dscr("gb%d" % b, [128, NE, S]) for b in range(NB)]
        YD = [[dscr("yd%d_%d" % (d, b), [S, 512]) for b in range(NB)] for d in range(2)]
        R_YD = [[Res() for b in range(NB)] for d in range(2)]
        BON = [dscr("bon%d" % b, [S, 512]) for b in range(NB)]; R_BON = [Res() for _ in range(NB)]
        GG = [dscr("gg%d" % b, [S, 512]) for b in range(NB)]; R_GG = [Res() for _ in range(NB)]
        R_H2T = [Res() for _ in range(NB)]; R_H2F = [Res() for _ in range(NB)]; R_GB = [Res() for _ in range(NB)]
        R_YT = [Res() for _ in range(NB)]

        uid = [0]

        def sb(name, shape, dt=F32, stack=st):
            uid[0] += 1
            return stack.enter_context(nc.sbuf_tensor("%s_%d" % (name, uid[0]), list(shape), dt))

        ident = sb("ident_sb", [128, 128]); R_id = Res()
        ones = sb("ones_sb", [128, 128]); R_ones = Res()
        MOD = sb("mod_sb", [128, DEPTH, 48, 3]); R_mod = Res()
        GS = sb("gs_sb", [128, DEPTH, 2, 8, 3]); R_gs = Res()
        gmix = sb("gmix_sb", [128, DEPTH, 8]); gffn = sb("gffn_sb", [128, DEPTH, 8]); R_g = Res()
        eps_t = sb("eps_sb", [128, 1]); R_eps = Res()
        psum = [st.enter_context(nc.psum_tensor("ps%d" % i, [128, 512], F32)) for i in range(8)]
        R_ps = [Res() for _ in range(8)]
        pctr = [0]

        def ps_next():
            i = pctr[0] % 8
            pctr[0] += 1
            return psum[i], R_ps[i]

        k.dma("sp", ident[:], I["ident"][:, :], writes=[R_id])
        k.op("dve", lambda e: e.memset(ones[:], 1.0), writes=[R_ones])
        k.op("dve", lambda e: e.memset(eps_t[:], 1e-6), writes=[R_eps])
        for l in range(DEPTH):
            k.dma("sp", gmix[:, l, :], I["g_mix"][l], writes=[R_g])
            k.dma("sp", gffn[:, l, :], I["g_ffn"][l], writes=[R_g])

        with ExitStack() as s1:
            cT = sb("cT_sb", [128, 8, 3], F32, s1); R_c = Res()
            sT = sb("sT", [128, 8, 3], F32, s1); R_s = Res()
            abT = sb("abT", [128, 48], F32, s1); R_ab = Res()
            aw = [sb("aw%d" % i, [128, 8, 512], F32, s1) for i in range(2)]
            R_aw = [Res(), Res()]
            xcp = [sb("xcp%d" % i, [128, 8, 256], F32, s1) for i in range(2)]
            R_xcp = [Res(), Res()]
            k.dma("sp", cT[:], I["cT"][:, :, :], writes=[R_c])
            k.op("act", lambda e: e.activation(out=sT[:], in_=cT[:], func=AF.Silu), reads=[R_c], writes=[R_s])
            n = 0
            for b in range(NB):
                for t0 in range(0, S, 256):
                    j = n % 2; n += 1
                    k.dma("sp", xcp[j][:], I["xT"][b, :, :, t0:t0 + 256], writes=[R_xcp[j]])
                    k.dma("sp", XT[b][:, :, t0:t0 + 256], xcp[j][:], reads=[R_xcp[j]], writes=[R_XT[b]])
            n = 0
            for l in range(n_layers):
                k.dma("sp", abT[:], I["ada_bT"][l], writes=[R_ab])
                for cc in range(12):
                    j = n % 2; n += 1
                    k.dma("sp", aw[j][:], I["ada_w"][l, :, :, cc * 512:(cc + 1) * 512], writes=[R_aw[j]])
                    pt, rp = ps_next()
                    for c4 in range(4):
                        for kc in range(8):
                            k.op("pe", lambda e, pt=pt, j=j, c4=c4, kc=kc: e.matmul(
                                pt[:, c4 * 3:c4 * 3 + 3], aw[j][:, kc, c4 * 128:(c4 + 1) * 128], sT[:, kc, :],
                                start=(kc == 0), stop=(kc == 7)), reads=[R_aw[j], R_s], writes=[rp])
                    k.op("dve", lambda e, pt=pt, l=l, cc=cc: e.tensor_tensor(
                        out=MOD[:, l, cc * 4:(cc + 1) * 4, :], in0=pt[:, 0:12].rearrange("p (a b) -> p a b", b=3),
                        in1=abT[:, cc * 4:(cc + 1) * 4].unsqueeze(2).to_broadcast([128, 4, 3]), op=ALU.add),
                        reads=[rp, R_ab], writes=[R_mod])
                for which, (gt, mi) in enumerate(((gmix, 1), (gffn, 4))):
                    k.op("dve", lambda e, l=l, which=which, gt=gt, mi=mi: e.scalar_tensor_tensor(
                        out=GS[:, l, which, :, :], in0=MOD[:, l, mi * 8:(mi + 1) * 8, :], scalar=1.0,
                        in1=gt[:, l, :].unsqueeze(2).to_broadcast([128, 8, 3]), op0=ALU.add, op1=ALU.mult),
                        reads=[R_mod, R_g], writes=[R_gs])
        k.barrier()

        def which_of(b, t0):
            return 2 if t0 < LC else b

        def stage_norm(l, which, shift_idx, dst, R_dst, gsel=None, dst32=None, R_dst32=None, bsel=range(NB), tsel=None):
            with ExitStack() as s1:
                xb = [sb("nx%d" % i, [128, 8, 256], F32, s1) for i in range(2)]; R_xb = [Res(), Res()]
                sq = [sb("nsq%d" % i, [128, 8, 256], F32, s1) for i in range(2)]; R_sq = [Res(), Res()]
                rs = [sb("nrs%d" % i, [128, 256], F32, s1) for i in range(2)]; R_rs = [Res(), Res()]
                tm = [sb("ntm%d" % i, [128, 8, 256], F32, s1) for i in range(2)]; R_tm = [Res(), Res()]
                hb = [sb("nhb%d" % i, [128, 8, 256], BF16, s1) for i in range(2)]; R_hb = [Res(), Res()]
                n = 0
                for b in bsel:
                    for t0 in (tsel if tsel is not None else range(0, S, 256)):
                        j = n % 2; n += 1
                        w = which_of(b, t0)
                        k.dma("sp", xb[j][:], XT[b][:, :, t0:t0 + 256], reads=[R_XT[b]], writes=[R_xb[j]])
                        k.op("act", lambda e, j=j: e.activation(out=sq[j][:], in_=xb[j][:], func=AF.Square), reads=[R_xb[j]], writes=[R_sq[j]])
                        pt, rp = ps_next()
                        for c in range(8):
                            k.op("pe", lambda e, pt=pt, j=j, c=c: e.matmul(pt[:, 0:256], ones[:], sq[j][:, c, :], start=(c == 0), stop=(c == 7)),
                                 reads=[R_ones, R_sq[j]], writes=[rp])
                        k.op("act", lambda e, pt=pt, j=j: e.activation(out=rs[j][:], in_=pt[:, 0:256], func=AF.Sqrt, bias=eps_t[:, 0:1], scale=1.0 / D),
                             reads=[rp, R_eps], writes=[R_rs[j]])
                        k.op("dve", lambda e, j=j: e.reciprocal(out=rs[j][:], in_=rs[j][:]), reads=[R_rs[j]], writes=[R_rs[j]])
                        for c in range(8):
                            k.op("dve", lambda e, j=j, c=c, w=w: e.scalar_tensor_tensor(
                                out=tm[j][:, c, :], in0=xb[j][:, c, :], scalar=GS[:, l, which, c, w:w + 1], in1=rs[j][:],
                                op0=ALU.mult, op1=ALU.mult), reads=[R_xb[j], R_gs, R_rs[j]], writes=[R_tm[j]])
                            k.op("act", lambda e, j=j, c=c, w=w: e.activation(
                                out=hb[j][:, c, :], in_=tm[j][:, c, :], func=AF.Identity, bias=MOD[:, l, shift_idx * 8 + c, w:w + 1], scale=1.0),
                                reads=[R_tm[j], R_mod], writes=[R_hb[j]])
                            if dst32 is not None:
                                k.op("pool", lambda e, j=j, c=c, w=w: e.tensor_scalar(
                                    out=tm[j][:, c, :], in0=tm[j][:, c, :], scalar1=MOD[:, l, shift_idx * 8 + c, w:w + 1], scalar2=None, op0=ALU.add),
                                    reads=[R_tm[j], R_mod, R_hb[j]], writes=[R_tm[j]])
                        k.dma("sp", dst[b][:, :, t0:t0 + 256], hb[j][:], reads=[R_hb[j]], writes=[R_dst[b]])
                        if dst32 is not None:
                            k.dma("sp", dst32[b][:, :, t0:t0 + 256], tm[j][:], reads=[R_tm[j]], writes=[R_dst32[b]])
            k.barrier()

        def stage_proj(l):
            with ExitStack() as s1:
                w = sb("pw", [128, 8, D_IN], BF16, s1); R_w = Res()
                mu = sb("pmu", [128, 15], F32, s1); R_mu = Res()
                mu1 = sb("pmu1", [128, 15], F32, s1)
                muh = sb("pmuh", [128, 15], F32, s1)
                hb = [sb("ph%d" % i, [128, 8, 258], BF16, s1) for i in range(2)]; R_hb = [Res(), Res()]
                hs = [sb("phs%d" % i, [128, 8, 256], BF16, s1) for i in range(2)]; R_hs = [Res(), Res()]
                oq = [sb("poq%d" % i, [128, 8, 256], BF16, s1) for i in range(2)]; R_oq = [Res(), Res()]
                ov = [sb("pov%d" % i, [128, 2, 512], BF16, s1) for i in range(2)]; R_ov = [Res(), Res()]
                ou = [sb("pou%d" % i, [128, 15, 256], F32, s1) for i in range(2)]; R_ou = [Res(), Res()]
                t2 = [sb("pt2%d" % i, [128, 256], F32, s1) for i in range(2)]; R_t2 = [Res(), Res()]
                for kc in range(8):
                    k.dma("pool", w[:, kc, :], I["w_in"][l, :, kc, :], writes=[R_w])
                k.dma("sp", mu[:], I["mu"][l], writes=[R_mu])
                k.op("dve", lambda e: e.tensor_scalar(out=mu1[:], in0=mu[:], scalar1=-1.0, scalar2=1.0, op0=ALU.mult, op1=ALU.add), reads=[R_mu], writes=[R_mu])
                k.op("dve", lambda e: e.tensor_scalar(out=muh[:], in0=mu[:], scalar1=0.5, scalar2=None, op0=ALU.mult), reads=[R_mu], writes=[R_mu])
                n = 0
                for b in range(NB):
                    for t0 in range(0, S, 256):
                        j = n % 2; n += 1
                        seg0, seg1 = (0, LC) if t0 < LC else (LC, S)
                        lo, hi = max(t0 - 1, seg0), min(t0 + 257, seg1)
                        if lo == t0 or hi == t0 + 256:
                            k.op("dve", lambda e, j=j: e.memset(hb[j][:], 0.0), writes=[R_hb[j]])
                        k.dma("sp", hb[j][:, :, lo - (t0 - 1):hi - (t0 - 1)], HT[b][:, :, lo:hi], reads=[R_HT[b]], writes=[R_hb[j]])
                        k.op("dve", lambda e, j=j: e.tensor_tensor(out=hs[j][:], in0=hb[j][:, :, 0:256], in1=hb[j][:, :, 2:258], op=ALU.add),
                             reads=[R_hb[j]], writes=[R_hs[j]])
                        for cj in range(8):
                            pt, rp = ps_next()
                            for kc in range(8):
                                k.op("pe", lambda e, pt=pt, j=j, cj=cj, kc=kc: e.matmul(pt[:, 0:256], w[:, kc, cj * 128:(cj + 1) * 128], hb[j][:, kc, 1:257],
                                     start=(kc == 0), stop=(kc == 7)), reads=[R_w, R_hb[j]], writes=[rp])
                            k.op("act", lambda e, pt=pt, j=j, cj=cj: e.activation(out=oq[j][:, cj, :], in_=pt[:, 0:256], func=AF.Copy), reads=[rp], writes=[R_oq[j]])
                        k.dma("sp", QT[b][:, :, t0:t0 + 256], oq[j][:, 0:4, :], reads=[R_oq[j]], writes=[R_QK[b]])
                        k.dma("sp", KT[b][:, :, t0:t0 + 256], oq[j][:, 4:8, :], reads=[R_oq[j]], writes=[R_QK[b]])
                        for tt in range(2):
                            pt, rp = ps_next()
                            for kc in range(8):
                                k.op("pe", lambda e, pt=pt, j=j, tt=tt, kc=kc: e.matmul(pt[:, :], hb[j][:, kc, 1 + tt * 128:1 + (tt + 1) * 128], w[:, kc, 1024:1536],
                                     start=(kc == 0), stop=(kc == 7)), reads=[R_w, R_hb[j]], writes=[rp])
                            k.op("act", lambda e, pt=pt, j=j, tt=tt: e.activation(out=ov[j][:, tt, :], in_=pt[:, :], func=AF.Copy), reads=[rp], writes=[R_ov[j]])
                        k.dma("sp", VN[b][t0:t0 + 256, :].rearrange("(a p) c -> p a c", p=128), ov[j][:], reads=[R_ov[j]], writes=[R_VN[b]])
                        for cj in range(15):
                            c0 = 1536 + cj * 128
                            pa, ra = ps_next()
                            for kc in range(8):
                                k.op("pe", lambda e, pa=pa, j=j, c0=c0, kc=kc: e.matmul(pa[:, 0:256], w[:, kc, c0:c0 + 128], hb[j][:, kc, 1:257],
                                     start=(kc == 0), stop=(kc == 7)), reads=[R_w, R_hb[j]], writes=[ra])
                            pb, rb = ps_next()
                            for kc in range(8):
                                k.op("pe", lambda e, pb=pb, j=j, c0=c0, kc=kc: e.matmul(pb[:, 0:256], w[:, kc, c0:c0 + 128], hs[j][:, kc, :],
                                     start=(kc == 0), stop=(kc == 7)), reads=[R_w, R_hs[j]], writes=[rb])
                            k.op("act", lambda e, pb=pb, j=j, cj=cj: e.activation(out=t2[j][:], in_=pb[:, 0:256], func=AF.Copy, scale=muh[:, cj:cj + 1]),
                                 reads=[rb, R_mu], writes=[R_t2[j]])
                            k.op("dve", lambda e, pa=pa, j=j, cj=cj: e.scalar_tensor_tensor(out=ou[j][:, cj, :], in0=pa[:, 0:256], scalar=mu1[:, cj:cj + 1], in1=t2[j][:],
                                 op0=ALU.mult, op1=ALU.add), reads=[ra, R_mu, R_t2[j]], writes=[R_ou[j]])
                        k.dma("sp", UT[b][:, :, t0:t0 + 256], ou[j][:], reads=[R_ou[j]], writes=[R_UT[b]])
            k.barrier()

        def stage_na(l, need_ctx, hsel=range(8), bsel=range(NB)):
            with ExitStack() as s1:
                bias = sb("na_bias", [64, 8, 960], F32, s1); R_bias = Res()
                k.dma("sp", bias[:], I["bfull"][l], writes=[R_bias])
                qh = [sb("na_q%d" % i, [64, S], BF16, s1) for i in range(2)]
                kh = [sb("na_k%d" % i, [64, S], BF16, s1) for i in range(2)]
                VE = [sb("na_ve%d" % i, [128, 18, 64], BF16, s1) for i in range(2)]
                VO = [sb("na_vo%d" % i, [128, 17, 64], BF16, s1) for i in range(2)]
                yh = [sb("na_y%d" % i, [64, S], BF16, s1) for i in range(2)]
                R_in = [Res(), Res()]; R_y = [Res(), Res()]
                ssb = [sb("na_s%d" % i, [128, 768], F32, s1) for i in range(2)]; R_s = [Res(), Res()]
                psb = [sb("na_p%d" % i, [128, 768], F32, s1) for i in range(2)]; R_p = [Res(), Res()]
                pn = [sb("na_pn%d" % i, [128, 768], F32, s1) for i in range(2)]; R_pn = [Res(), Res()]
                pT = [sb("na_pT%d" % i, [128, 384], BF16, s1) for i in range(2)]; R_pT = [Res(), Res()]
                st4 = [sb("na_st%d" % i, [128, 4], F32, s1) for i in range(2)]; R_st = [Res(), Res()]
                if need_ctx == "zero":
                    pass
                n = 0
                u = 0
                for b in bsel:
                    for h in hsel:
                        j = n % 2; n += 1
                        p0 = (h % 2) * 64
                        k.dma("sp", qh[j][:], QT[b][p0:p0 + 64, h // 2, :], reads=[R_QK[b]], writes=[R_in[j]])
                        k.dma("sp", kh[j][:], KT[b][p0:p0 + 64, h // 2, :], reads=[R_QK[b]], writes=[R_in[j]])
                        k.dma("sp", VE[j][:], VN[b][:, h * 64:(h + 1) * 64].rearrange("(a p) d -> p a d", p=128), reads=[R_VN[b]], writes=[R_in[j]])
                        k.dma("sp", VO[j][:], VN[b][64:64 + 17 * 128, h * 64:(h + 1) * 64].rearrange("(a p) d -> p a d", p=128), reads=[R_VN[b]], writes=[R_in[j]])
                        units = [("lat", r) for r in range(32)] + ([("ctx", 0), ("ctx", 1)] if need_ctx else [])
                        if not need_ctx:
                            k.op("pool", lambda e, j=j: e.memset(yh[j][:, 0:LC], 0.0), writes=[R_y[j]])
                        for kind, r in units:
                            i = u % 2; u += 1
                            if kind == "lat":
                                M = 64
                                r0 = min(max(r - 4, 0), 24); dl = r0 - r
                                tq = LC + r * 64; w0 = LC + r0 * 64
                                pa, ra = ps_next()
                                k.op("pe", lambda e, pa=pa, j=j, tq=tq, w0=w0: e.matmul(pa[0:64, :], qh[j][:, tq:tq + 64], kh[j][:, w0:w0 + 512], start=True, stop=True),
                                     reads=[R_in[j]], writes=[ra])
                                pb, rb = ps_next()
                                k.op("pe", lambda e, pb=pb, j=j, tq=tq: e.matmul(pb[0:64, 0:256], qh[j][:, tq:tq + 64], kh[j][:, 0:256], start=True, stop=True),
                                     reads=[R_in[j]], writes=[rb])
                                k.op("dve", lambda e, pa=pa, i=i, h=h, dl=dl: e.scalar_tensor_tensor(out=ssb[i][0:64, 0:512], in0=pa[0:64, :], scalar=0.125,
                                     in1=bias[:, h, (dl + 7) * 64:(dl + 15) * 64], op0=ALU.mult, op1=ALU.add), reads=[ra, R_bias], writes=[R_s[i]])
                                k.op("act", lambda e, pb=pb, i=i: e.activation(out=ssb[i][0:64, 512:768], in_=pb[0:64, 0:256], func=AF.Copy, scale=0.125),
                                     reads=[rb], writes=[R_s[i]])
                                W = 768
                            else:
                                M = 128
                                pa, ra = ps_next()
                                k.op("pe", lambda e, pa=pa, j=j, r=r: e.matmul(pa[:, 0:256], qh[j][:, r * 128:(r + 1) * 128], kh[j][:, 0:256], start=True, stop=True),
                                     reads=[R_in[j]], writes=[ra])
                                k.op("act", lambda e, pa=pa, i=i: e.activation(out=ssb[i][:, 0:256], in_=pa[:, 0:256], func=AF.Copy, scale=0.125),
                                     reads=[ra], writes=[R_s[i]])
                                W = 256
                            k.op("dve", lambda e, i=i, M=M, W=W: e.tensor_reduce(out=st4[i][0:M, 0:1], in_=ssb[i][0:M, 0:W], axis=AX.X, op=ALU.max),
                                 reads=[R_s[i]], writes=[R_st[i]])
                            k.op("dve", lambda e, i=i, M=M: e.tensor_scalar(out=st4[i][0:M, 1:2], in0=st4[i][0:M, 0:1], scalar1=-1.0, scalar2=None, op0=ALU.mult),
                                 reads=[R_st[i]], writes=[R_st[i]])
                            k.op("act", lambda e, i=i, M=M, W=W: e.activation(out=psb[i][0:M, 0:W], in_=ssb[i][0:M, 0:W], func=AF.Exp, bias=st4[i][0:M, 1:2], scale=1.0,
                                 accum_out=st4[i][0:M, 2:3]), reads=[R_s[i], R_st[i]], writes=[R_p[i], R_st[i]])
                            k.op("dve", lambda e, i=i, M=M: e.reciprocal(out=st4[i][0:M, 3:4], in_=st4[i][0:M, 2:3]), reads=[R_st[i]], writes=[R_st[i]])
                            k.op("pool", lambda e, i=i, M=M, W=W: e.tensor_scalar(out=pn[i][0:M, 0:W], in0=psb[i][0:M, 0:W], scalar1=st4[i][0:M, 3:4], scalar2=None, op0=ALU.mult),
                                 reads=[R_p[i], R_st[i]], writes=[R_pn[i]])
                            pc, rc = ps_next()
                            nch = W // 128
                            for c in range(nch):
                                k.op("pe", lambda e, pc=pc, i=i, c=c, M=M: e.transpose(pc[:, c * M:(c + 1) * M], pn[i][0:M, c * 128:(c + 1) * 128], ident[0:M, 0:M]),
                                     reads=[R_pn[i], R_id], writes=[rc])
                            k.op("act", lambda e, pc=pc, i=i, M=M, nch=nch: e.activation(out=pT[i][:, 0:nch * M], in_=pc[:, 0:nch * M], func=AF.Copy), reads=[rc], writes=[R_pT[i]])
                            pd, rd = ps_next()
                            for c in range(nch):
                                if kind == "lat" and c < 4:
                                    g = w0 // 64
                                    vt = VE[j][:, g // 2 + c, :] if g % 2 == 0 else VO[j][:, (g - 1) // 2 + c, :]
                                elif kind == "lat":
                                    vt = VE[j][:, c - 4, :]
                                else:
                                    vt = VE[j][:, c, :]
                                k.op("pe", lambda e, pd=pd, i=i, c=c, M=M, vt=vt, nch=nch: e.matmul(pd[0:64, 0:M], vt, pT[i][:, c * M:(c + 1) * M], start=(c == 0), stop=(c == nch - 1)),
                                     reads=[R_in[j], R_pT[i]], writes=[rd])
                            tq2 = tq if kind == "lat" else r * 128
                            k.op("act", lambda e, pd=pd, j=j, tq2=tq2, M=M: e.activation(out=yh[j][:, tq2:tq2 + M], in_=pd[0:64, 0:M], func=AF.Copy), reads=[rd], writes=[R_y[j]])
                        k.dma("sp", YT[b][p0:p0 + 64, 4 + h // 2, :], yh[j][:], reads=[R_y[j]], writes=[R_YT[b]])
            k.barrier()

        def stage_wout(l, last):
            with ExitStack() as s1:
                w = sb("wo_w", [128, 8, D], BF16, s1); R_w = Res()
                for kc in range(8):
                    k.dma("pool", w[:, kc, :], I["w_out"][l, :, kc, :], writes=[R_w])
                yb = [sb("wo_y%d" % i, [128, 8, 256], BF16, s1) for i in range(2)]; R_yb = [Res(), Res()]
                xb = [sb("wo_x%d" % i, [128, 8, 256], F32, s1) for i in range(2)]; R_xb = [Res(), Res()]
                n = 0
                for b in range(NB):
                    for t0 in range(LC if last else 0, S, 256):
                        j = n % 2; n += 1
                        wq = which_of(b, t0)
                        k.dma("sp", yb[j][:], YT[b][:, :, t0:t0 + 256], reads=[R_YT[b]], writes=[R_yb[j]])
                        k.dma("sp", xb[j][:], XT[b][:, :, t0:t0 + 256], reads=[R_XT[b]], writes=[R_xb[j]])
                        for oc in range(8):
                            pt, rp = ps_next()
                            for kc in range(8):
                                k.op("pe", lambda e, pt=pt, j=j, oc=oc, kc=kc: e.matmul(pt[:, 0:256], w[:, kc, oc * 128:(oc + 1) * 128], yb[j][:, kc, :],
                                     start=(kc == 0), stop=(kc == 7)), reads=[R_w, R_yb[j]], writes=[rp])
                            k.op("dve", lambda e, pt=pt, j=j, oc=oc, wq=wq: e.scalar_tensor_tensor(out=xb[j][:, oc, :], in0=pt[:, 0:256],
                                 scalar=MOD[:, l, 16 + oc, wq:wq + 1], in1=xb[j][:, oc, :], op0=ALU.mult, op1=ALU.add), reads=[rp, R_mod, R_xb[j]], writes=[R_xb[j]])
                        k.dma("sp", XT[b][:, :, t0:t0 + 256], xb[j][:], reads=[R_xb[j]], writes=[R_XT[b]])
            k.barrier()

        def stage_router(l, last):
            i_moe = l // 2
            with ExitStack() as s1:
                rw = sb("rt_w", [128, 8, NE], F32, s1); R_rw = Res()
                k.dma("sp", rw[:], I["router"][i_moe], writes=[R_rw])
                hb = [sb("rt_h%d" % i, [128, 8, 128], F32, s1) for i in range(2)]; R_hb = [Res(), Res()]
                lg = [sb("rt_l%d" % i, [128, 40], F32, s1) for i in range(2)]; R_lg = [Res(), Res()]
                ge = [sb("rt_ge%d" % i, [128, 128], F32, s1) for i in range(2)]; R_ge = [Res(), Res()]
                gb = [sb("rt_gb%d" % i, [128, NE, 128], F32, s1) for i in range(2)]; R_gb = [Res(), Res()]
                n = 0; m = 0
                for b in range(NB):
                    for tt in range(2 if last else 0, NT):
                        j = n % 2; n += 1
                        t0 = tt * 128
                        k.dma("sp", hb[j][:], H2F[b][:, :, t0:t0 + 128], reads=[R_H2F[b]], writes=[R_hb[j]])
                        pt, rp = ps_next()
                        for kc in range(8):
                            k.op("pe", lambda e, pt=pt, j=j, kc=kc: e.matmul(pt[:, 0:NE], hb[j][:, kc, :], rw[:, kc, :], start=(kc == 0), stop=(kc == 7)),
                                 reads=[R_hb[j], R_rw], writes=[rp])
                        L = lg[j]
                        k.op("dve", lambda e, pt=pt, L=L: e.tensor_copy(out=L[:, 0:8], in_=pt[:, 0:8]), reads=[rp], writes=[R_lg[j]])
                        k.op("dve", lambda e, L=L: e.tensor_reduce(out=L[:, 8:9], in_=L[:, 0:8], axis=AX.X, op=ALU.max), reads=[R_lg[j]], writes=[R_lg[j]])
                        k.op("dve", lambda e, L=L: e.tensor_scalar(out=L[:, 9:17], in0=L[:, 0:8], scalar1=L[:, 8:9], scalar2=None, op0=ALU.is_ge), reads=[R_lg[j]], writes=[R_lg[j]])
                        k.op("dve", lambda e, L=L: e.scalar_tensor_tensor(out=L[:, 17:25], in0=L[:, 9:17], scalar=-1e30, in1=L[:, 0:8], op0=ALU.mult, op1=ALU.add), reads=[R_lg[j]], writes=[R_lg[j]])
                        k.op("dve", lambda e, L=L: e.tensor_reduce(out=L[:, 25:26], in_=L[:, 17:25], axis=AX.X, op=ALU.max), reads=[R_lg[j]], writes=[R_lg[j]])
                        k.op("dve", lambda e, L=L: e.tensor_scalar(out=L[:, 26:34], in0=L[:, 17:25], scalar1=L[:, 25:26], scalar2=None, op0=ALU.is_ge), reads=[R_lg[j]], writes=[R_lg[j]])
                        k.op("dve", lambda e, L=L: e.tensor_tensor(out=L[:, 34:35], in0=L[:, 8:9], in1=L[:, 25:26], op=ALU.subtract), reads=[R_lg[j]], writes=[R_lg[j]])
                        k.op("act", lambda e, L=L: e.activation(out=L[:, 35:36], in_=L[:, 34:35], func=AF.Sigmoid), reads=[R_lg[j]], writes=[R_lg[j]])
                        k.op("dve", lambda e, L=L: e.tensor_scalar(out=L[:, 36:37], in0=L[:, 35:36], scalar1=-1.0, scalar2=1.0, op0=ALU.mult, op1=ALU.add), reads=[R_lg[j]], writes=[R_lg[j]])
                        k.op("dve", lambda e, L=L: e.tensor_scalar(out=L[:, 9:17], in0=L[:, 9:17], scalar1=L[:, 35:36], scalar2=None, op0=ALU.mult), reads=[R_lg[j]], writes=[R_lg[j]])
                        k.op("dve", lambda e, L=L: e.scalar_tensor_tensor(out=L[:, 9:17], in0=L[:, 26:34], scalar=L[:, 36:37], in1=L[:, 9:17], op0=ALU.mult, op1=ALU.add), reads=[R_lg[j]], writes=[R_lg[j]])
                        for ex in range(NE):
                            i2 = m % 2; m += 1
                            k.op("pool", lambda e, L=L, i2=i2, ex=ex: e.tensor_copy(out=ge[i2][:], in_=L[:, 9 + ex:10 + ex].to_broadcast([128, 128])), reads=[R_lg[j]], writes=[R_ge[i2]])
                            pg, rg = ps_next()
                            k.op("pe", lambda e, pg=pg, i2=i2: e.matmul(pg[:, 0:128], ge[i2][:], ident[:], start=True, stop=True), reads=[R_ge[i2], R_id], writes=[rg])
                            k.op("act", lambda e, pg=pg, j=j, ex=ex: e.activation(out=gb[j][:, ex, :], in_=pg[:, 0:128], func=AF.Copy), reads=[rg], writes=[R_gb[j]])
                        k.dma("sp", GB[b][:, :, t0:t0 + 128], gb[j][:], reads=[R_gb[j]], writes=[R_GB[b]])
            k.barrier()

        def stage_ffn(l, last):
            moe = (l % 2 == 1)
            i_w = l // 2
            F = DFE if moe else DFF
            GW = 512 if moe else 256
            ng = F // GW
            nfc = GW // 128
            tstart = LC if last else 0
            with ExitStack() as s1:
                hT = sb("ff_h", [128, 8, S], BF16, s1); R_h = Res()
                yacc = sb("ff_y", [128, 8, S], F32, s1); R_ya = [Res() for _ in range(9)]
                w1 = [sb("ff_w1%d" % i, [128, 8, GW], BF16, s1) for i in range(2)]
                w3 = [sb("ff_w3%d" % i, [128, 8, GW], BF16, s1) for i in range(2)]
                w2 = [sb("ff_w2%d" % i, [128, nfc, D], BF16, s1) for i in range(2)]
                R_wg = [Res(), Res()]
                sg = [sb("ff_s%d" % i, [128, 256], F32, s1) for i in range(2)]; R_sg = [Res(), Res()]
                ac = [sb("ff_a%d" % i, [128, nfc, 256], BF16, s1) for i in range(2)]; R_ac = [Res(), Res()]
                gt = [sb("ff_g%d" % i, [128, 256], F32, s1) for i in range(2)]; R_gt = [Res(), Res()]
                xb = [sb("ff_x%d" % i, [128, 8, 256], F32, s1) for i in range(2)]; R_xb = [Res(), Res()]
                nw = 0; na_ = 0; ns = 0; ngt = 0
                for b in range(NB):
                    k.dma("sp", hT[:, :, tstart:S], H2T[b][:, :, tstart:S], reads=[R_H2T[b]], writes=[R_h])
                    first = True
                    for ex in range(NE if moe else 1):
                        for g in range(ng):
                            jw = nw % 2; nw += 1
                            if moe:
                                s_w1, s_w3, s_w2 = I["moe_w1"][i_w, ex], I["moe_w3"][i_w, ex], I["moe_w2"][i_w, ex]
                            else:
                                s_w1, s_w3, s_w2 = I["ffn_w1"][i_w], I["ffn_w3"][i_w], I["ffn_w2"][i_w]
                            k.dma("pool", w1[jw][:], s_w1[:, :, g * GW:(g + 1) * GW], writes=[R_wg[jw]])
                            k.dma("pool", w3[jw][:], s_w3[:, :, g * GW:(g + 1) * GW], writes=[R_wg[jw]])
                            k.dma("pool", w2[jw][:], s_w2[:, g * nfc:(g + 1) * nfc, :], writes=[R_wg[jw]])
                            for tb in range(tstart // 256, S // 256):
                                t0 = tb * 256
                                ja = na_ % 2; na_ += 1
                                if moe:
                                    jg = ngt % 2; ngt += 1
                                    k.dma("sp", gt[jg][:], GB[b][:, ex, t0:t0 + 256], reads=[R_GB[b]], writes=[R_gt[jg]])
                                for fc in range(nfc):
                                    p1, r1 = ps_next()
                                    for kc in range(8):
                                        k.op("pe", lambda e, p1=p1, jw=jw, fc=fc, kc=kc, t0=t0: e.matmul(p1[:, 0:256], w1[jw][:, kc, fc * 128:(fc + 1) * 128], hT[:, kc, t0:t0 + 256],
                                             start=(kc == 0), stop=(kc == 7)), reads=[R_wg[jw], R_h], writes=[r1])
                                    p3, r3 = ps_next()
                                    for kc in range(8):
                                        k.op("pe", lambda e, p3=p3, jw=jw, fc=fc, kc=kc, t0=t0: e.matmul(p3[:, 0:256], w3[jw][:, kc, fc * 128:(fc + 1) * 128], hT[:, kc, t0:t0 + 256],
                                             start=(kc == 0), stop=(kc == 7)), reads=[R_wg[jw], R_h], writes=[r3])
                                    js = ns % 2; ns += 1
                                    k.op("act", lambda e, p1=p1, js=js: e.activation(out=sg[js][:], in_=p1[:, 0:256], func=AF.Silu), reads=[r1], writes=[R_sg[js]])
                                    if moe:
                                        k.op("pool", lambda e, js=js, jg=jg: e.tensor_tensor(out=sg[js][:], in0=sg[js][:], in1=gt[jg][:], op=ALU.mult), reads=[R_sg[js], R_gt[jg]], writes=[R_sg[js]])
                                    k.op("dve", lambda e, p3=p3, js=js, ja=ja, fc=fc: e.tensor_tensor(out=ac[ja][:, fc, :], in0=sg[js][:], in1=p3[:, 0:256], op=ALU.mult),
                                         reads=[R_sg[js], r3], writes=[R_ac[ja]])
                                for oc in range(8):
                                    po, ro = ps_next()
                                    for fc in range(nfc):
                                        k.op("pe", lambda e, po=po, jw=jw, ja=ja, fc=fc, oc=oc: e.matmul(po[:, 0:256], w2[jw][:, fc, oc * 128:(oc + 1) * 128], ac[ja][:, fc, :],
                                             start=(fc == 0), stop=(fc == nfc - 1)), reads=[R_wg[jw], R_ac[ja]], writes=[ro])
                                    if first:
                                        k.op("act", lambda e, po=po, oc=oc, t0=t0: e.activation(out=yacc[:, oc, t0:t0 + 256], in_=po[:, 0:256], func=AF.Copy), reads=[ro], writes=[R_ya[tb]])
                                    else:
                                        k.op("dve", lambda e, po=po, oc=oc, t0=t0: e.tensor_tensor(out=yacc[:, oc, t0:t0 + 256], in0=yacc[:, oc, t0:t0 + 256], in1=po[:, 0:256], op=ALU.add),
                                             reads=[ro, R_ya[tb]], writes=[R_ya[tb]])
                            first = False
                    for tb in range(tstart // 256, S // 256):
                        t0 = tb * 256
                        j = tb % 2
                        wq = which_of(b, t0)
                        k.dma("sp", xb[j][:], XT[b][:, :, t0:t0 + 256], reads=[R_XT[b]], writes=[R_xb[j]])
                        for oc in range(8):
                            k.op("dve", lambda e, j=j, oc=oc, t0=t0, wq=wq: e.scalar_tensor_tensor(out=xb[j][:, oc, :], in0=yacc[:, oc, t0:t0 + 256],
                                 scalar=MOD[:, l, 40 + oc, wq:wq + 1], in1=xb[j][:, oc, :], op0=ALU.mult, op1=ALU.add), reads=[R_ya[tb], R_mod, R_xb[j]], writes=[R_xb[j]])
                        k.dma("sp", XT[b][:, :, t0:t0 + 256], xb[j][:], reads=[R_xb[j]], writes=[R_XT[b]])
            k.barrier()

        def stage_final():
            with ExitStack() as s1:
                fg = sb("fn_g", [128, 8], F32, s1); R_fg = Res()
                k.dma("sp", fg[:], I["final_g"][:, :], writes=[R_fg])
                xb = [sb("fx%d" % i, [128, 8, 256], F32, s1) for i in range(2)]; R_xb = [Res(), Res()]
                sq = [sb("fsq%d" % i, [128, 8, 256], F32, s1) for i in range(2)]; R_sq = [Res(), Res()]
                rs = [sb("frs%d" % i, [128, 256], F32, s1) for i in range(2)]; R_rs = [Res(), Res()]
                ob = [sb("fo%d" % i, [128, 8, 256], F32, s1) for i in range(2)]; R_ob = [Res(), Res()]
                n = 0
                outs = []
                for b in range(NB):
                    for t0 in range(LC, S, 256):
                        j = n % 2; n += 1
                        k.dma("sp", xb[j][:], XT[b][:, :, t0:t0 + 256], reads=[R_XT[b]], writes=[R_xb[j]])
                        k.op("act", lambda e, j=j: e.activation(out=sq[j][:], in_=xb[j][:], func=AF.Square), reads=[R_xb[j]], writes=[R_sq[j]])
                        pt, rp = ps_next()
                        for c in range(8):
                            k.op("pe", lambda e, pt=pt, j=j, c=c: e.matmul(pt[:, 0:256], ones[:], sq[j][:, c, :], start=(c == 0), stop=(c == 7)), reads=[R_ones, R_sq[j]], writes=[rp])
                        k.op("act", lambda e, pt=pt, j=j: e.activation(out=rs[j][:], in_=pt[:, 0:256], func=AF.Sqrt, bias=eps_t[:, 0:1], scale=1.0 / D), reads=[rp, R_eps], writes=[R_rs[j]])
                        k.op("dve", lambda e, j=j: e.reciprocal(out=rs[j][:], in_=rs[j][:]), reads=[R_rs[j]], writes=[R_rs[j]])
                        for c in range(8):
                            k.op("dve", lambda e, j=j, c=c: e.scalar_tensor_tensor(out=ob[j][:, c, :], in0=xb[j][:, c, :], scalar=fg[:, c:c + 1], in1=rs[j][:],
                                 op0=ALU.mult, op1=ALU.mult), reads=[R_xb[j], R_fg, R_rs[j]], writes=[R_ob[j]])
                        outs.append(k.dma("sp", out[b, :, :, t0 - LC:t0 - LC + 256], ob[j][:], reads=[R_ob[j]]))
                return outs

        def stage_rwkv(l, need_ctx):
            with ExitStack() as s1:
                def T_(name, shape, dt=F32, n=2):
                    return [sb(name + str(i), shape, dt, s1) for i in range(n)], [Res() for _ in range(n)]
                w2 = sb("rw_w2", [128, 512], F32, s1); a2 = sb("rw_a2", [128, 512], F32, s1); g2 = sb("rw_g2", [128, 512], F32, s1)
                w0b = sb("rw_w0b", [64, 2, 512], F32, s1); a0T = sb("rw_a0T", [128, 2, 4], F32, s1)
                kkp = sb("rw_kkp", [128, 4], F32, s1); kap = sb("rw_kap", [128, 4], F32, s1); okap = sb("rw_okap", [128, 4], F32, s1)
                rkp = sb("rw_rkp", [128, 4], F32, s1)
                lnw = sb("rw_lnw", [128, 512], F32, s1); lnb = sb("rw_lnb", [128, 512], F32, s1)
                m2 = sb("rw_m2", [64, 2, 128], F32, s1); m3 = sb("rw_m3", [64, 2, 128], F32, s1); mL = sb("rw_mL", [64, 2, 64], F32, s1)
                bones = sb("rw_bones", [128, 128], F32, s1); sel = sb("rw_sel", [128, 2], F32, s1)
                c1 = sb("rw_c1", [128, 4], F32, s1)
                R_par = Res()
                for dst, src in ((w2, I["rw_w2"][l]), (a2, I["rw_a2"][l]), (g2, I["rw_g2"][l]), (a0T, I["rw_a0T"][l]),
                                 (kkp, I["rw_kk"][l]), (kap, I["rw_ka"][l]), (rkp, I["rw_rk"][l]), (lnw, I["rw_lnw"][l]), (lnb, I["rw_lnb"][l]),
                                 (m2, I["rw_m2"]), (m3, I["rw_m3"]), (mL, I["rw_mL"]), (bones, I["rw_bones"]), (sel, I["rw_sel"]), (c1, I["rw_c1"]),
                                 (w0b, I["rw_w0b"][l])):
                    k.dma("sp", dst[:], src, writes=[R_par])
                k.op("dve", lambda e: e.tensor_scalar(out=okap[:], in0=kap[:], scalar1=-1.0, scalar2=1.0, op0=ALU.mult, op1=ALU.add), reads=[R_par], writes=[R_par])
                Hst = sb("rw_H", [128, 4, 64], F32, s1); R_H = Res()
                ub, R_ub = T_("rw_u", [128, 15, 64])
                twl, R_twl = T_("rw_twl", [128, 64])
                sgl, R_sgl = T_("rw_sgl", [128, 64])
                e2a, R_e2a = T_("rw_e2a", [64, 512])
                e2b, R_e2b = T_("rw_e2b", [64, 512])
                a_sb, R_a = T_("rw_a", [128, 4, 64])
                a1_sb, R_a1 = T_("rw_a1", [128, 4, 64])
                kr, R_kr = T_("rw_kr", [128, 4, 64])
                sq, R_sq = T_("rw_sq", [128, 4, 64])
                kk_t, R_kk = T_("rw_kkt", [128, 4, 64])
                ff, R_ff = T_("rw_ff", [128, 4, 64])
                kd, R_kd = T_("rw_kd", [128, 4, 64])
                bb, R_bb = T_("rw_bb", [128, 4, 64])
                EG, R_EG = T_("rw_EG", [128, 4, 2, 64])
                IEG, R_IEG = T_("rw_IEG", [128, 4, 64])
                fm, R_fm = T_("rw_fm", [128, 4, 4, 64])
                tm, R_tm = T_("rw_tm", [64, 4, 4, 128])
                LA, R_LA = T_("rw_LA", [64, 8, 128])
                NBt, R_NB = T_("rw_NB", [64, 8, 128])
                Lm, R_Lm = T_("rw_Lm", [64, 8, 64])
                Pm, R_Pm = T_("rw_Pm", [64, 8, 64])
                Nl, R_Nl = T_("rw_Nl", [64, 8, 64], n=4)
                Ll, R_Ll = T_("rw_Ll", [64, 8, 64], n=4)
                WT, R_WT = T_("rw_WT", [128, 4, 64])
                Xa, R_Xa = T_("rw_Xa", [64, 8, 64])
                Uta, R_Uta = T_("rw_Uta", [64, 8, 64])
                Ua, R_Ua = T_("rw_Ua", [64, 8, 64])
                ysb, R_ysb = T_("rw_ysb", [64, 512])
                htmp, R_htmp = T_("rw_htmp", [128, 4, 64])
                rk_t, R_rk = T_("rw_rkt", [128, 4, 64])
                rkh, R_rkh = T_("rw_rkh", [64, 8])
                bon, R_bon = T_("rw_bon", [64, 512])
                gsb, R_gsb = T_("rw_gsb", [64, 512])
                n = 0
                nl4 = [0]
                for b in range(NB if RW_DBG["nb"] is None else RW_DBG["nb"]):
                    for d in range(RW_DBG["nd"]):
                        k.op("pool", lambda e: e.memset(Hst[:], 0.0), writes=[R_H])
                        order = list(range(36)) if d == 0 else [3, 2, 1, 0] + list(range(35, 3, -1))
                        if RW_DBG["nch"] is not None:
                            order = order[:RW_DBG["nch"]]
                        last_col = 63 if d == 0 else 0
                        for ch in order:
                            if (not need_ctx) and False:
                                pass
                            j = n % 2; n += 1
                            t0 = ch * 64
                            U_ = ub[j]
                            k.dma("sp", U_[:], UT[b][:, :, t0:t0 + 64], reads=[R_UT[b]], writes=[R_ub[j]])
                            k.op("act", lambda e, j=j, U_=U_: e.activation(out=twl[j][:], in_=U_[:, 12, :], func=AF.Tanh), reads=[R_ub[j]], writes=[R_twl[j]])
                            pt, rp = ps_next()
                            k.op("pe", lambda e, pt=pt, j=j, d=d: e.matmul(pt[0:64, :], twl[j][d * 64:(d + 1) * 64, :], w2[d * 64:(d + 1) * 64, :], start=True, stop=True),
                                 reads=[R_twl[j], R_par], writes=[rp])
                            k.op("dve", lambda e, pt=pt, j=j, d=d: e.tensor_tensor(out=e2a[j][:], in0=pt[0:64, :], in1=w0b[:, d, :], op=ALU.add), reads=[rp, R_par], writes=[R_e2a[j]])
                            k.op("act", lambda e, j=j: e.activation(out=e2b[j][:], in_=e2a[j][:], func=AF.Exp, scale=-1.0), reads=[R_e2a[j]], writes=[R_e2b[j]])
                            k.op("act", lambda e, j=j: e.activation(out=e2a[j][:], in_=e2b[j][:], func=AF.Ln, bias=c1[0:64, 0:1], scale=1.0), reads=[R_e2b[j], R_par], writes=[R_e2a[j]])
                            k.op("act", lambda e, j=j: e.activation(out=e2b[j][:], in_=e2a[j][:], func=AF.Exp, bias=c1[0:64, 1:2], scale=-1.0), reads=[R_e2a[j], R_par], writes=[R_e2b[j]])
                            def a_path(dd, dst, R_dst):
                                pa, ra = ps_next()
                                for q in range(4):
                                    k.op("pe", lambda e, pa=pa, q=q, dd=dd, U_=U_: e.matmul(pa[:, q * 64:(q + 1) * 64], a2[dd * 64:(dd + 1) * 64, q * 128:(q + 1) * 128], U_[dd * 64:(dd + 1) * 64, 13, :], start=True, stop=True),
                                         reads=[R_par, R_ub[j]], writes=[ra])
                                for q in range(4):
                                    k.op("act", lambda e, pa=pa, q=q, dd=dd, dst=dst: e.activation(out=dst[:, q, :], in_=pa[:, q * 64:(q + 1) * 64], func=AF.Sigmoid, bias=a0T[:, dd, q:q + 1], scale=1.0),
                                         reads=[ra, R_par], writes=[R_dst])
                            a_path(d, a_sb[j], R_a[j])
                            k.op("dve", lambda e, j=j, U_=U_: e.tensor_tensor(out=kr[j][:], in0=U_[:, 4:8, :], in1=kkp[:, :].unsqueeze(2).to_broadcast([128, 4, 64]), op=ALU.mult), reads=[R_ub[j], R_par], writes=[R_kr[j]])
                            k.op("pool", lambda e, j=j: e.tensor_tensor(out=sq[j][:], in0=kr[j][:], in1=kr[j][:], op=ALU.mult), reads=[R_kr[j]], writes=[R_sq[j]])
                            pn_, rn = ps_next()
                            k.op("pe", lambda e, pn_=pn_, j=j: e.matmul(pn_[:, 0:256], bones[:], sq[j][:].rearrange("p q t -> p (q t)"), start=True, stop=True), reads=[R_par, R_sq[j]], writes=[rn])
                            k.op("act", lambda e, pn_=pn_, j=j: e.activation(out=sq[j][:].rearrange("p q t -> p (q t)"), in_=pn_[:, 0:256], func=AF.Sqrt), reads=[rn], writes=[R_sq[j]])
                            k.op("dve", lambda e, j=j: e.tensor_scalar(out=sq[j][:], in0=sq[j][:], scalar1=1e-12, scalar2=None, op0=ALU.max), reads=[R_sq[j]], writes=[R_sq[j]])
                            k.op("dve", lambda e, j=j: e.reciprocal(out=sq[j][:], in_=sq[j][:]), reads=[R_sq[j]], writes=[R_sq[j]])
                            k.op("dve", lambda e, j=j: e.tensor_tensor(out=kk_t[j][:], in0=kr[j][:], in1=sq[j][:], op=ALU.mult), reads=[R_kr[j], R_sq[j]], writes=[R_kk[j]])
                            k.op("pool", lambda e, j=j: e.tensor_tensor(out=ff[j][:], in0=a_sb[j][:], in1=kap[:, :].unsqueeze(2).to_broadcast([128, 4, 64]), op=ALU.mult), reads=[R_a[j], R_par], writes=[R_ff[j]])
                            k.op("pool", lambda e, j=j: e.tensor_tensor(out=ff[j][:], in0=ff[j][:], in1=okap[:, :].unsqueeze(2).to_broadcast([128, 4, 64]), op=ALU.add), reads=[R_ff[j], R_par], writes=[R_ff[j]])
                            k.op("pool", lambda e, j=j, U_=U_: e.tensor_tensor(out=kd[j][:], in0=U_[:, 4:8, :], in1=ff[j][:], op=ALU.mult), reads=[R_ub[j], R_ff[j]], writes=[R_kd[j]])
                            k.op("pool", lambda e, j=j: e.tensor_tensor(out=bb[j][:], in0=kk_t[j][:], in1=a_sb[j][:], op=ALU.mult), reads=[R_kk[j], R_a[j]], writes=[R_bb[j]])
                            pc, rc = ps_next()
                            for q in range(4):
                                k.op("pe", lambda e, pc=pc, q=q, j=j, d=d: e.matmul(pc[:, q * 128:(q + 1) * 128], e2b[j][:, q * 128:(q + 1) * 128], m2[:, d, :], start=True, stop=True),
                                     reads=[R_e2b[j], R_par], writes=[rc])
                            k.op("act", lambda e, pc=pc, j=j: e.activation(out=EG[j][:].rearrange("p q s t -> p (q s t)"), in_=pc[:, :], func=AF.Exp, scale=-1.0), reads=[rc], writes=[R_EG[j]])
                            k.op("act", lambda e, pc=pc, j=j: e.activation(out=IEG[j][:], in_=pc[:, :].rearrange("p (q s t) -> p q s t", q=4, s=2)[:, :, 1, :], func=AF.Exp, scale=1.0), reads=[rc], writes=[R_IEG[j]])
                            F_ = fm[j]
                            k.op("dve", lambda e, j=j, F_=F_: e.tensor_tensor(out=F_[:, :, 0, :], in0=kd[j][:], in1=IEG[j][:], op=ALU.mult), reads=[R_kd[j], R_IEG[j]], writes=[R_fm[j]])
                            k.op("dve", lambda e, j=j, F_=F_: e.tensor_tensor(out=F_[:, :, 1, :], in0=bb[j][:], in1=IEG[j][:], op=ALU.mult), reads=[R_bb[j], R_IEG[j]], writes=[R_fm[j]])
                            k.op("pool", lambda e, j=j, F_=F_: e.tensor_tensor(out=F_[:, :, 2, :], in0=kk_t[j][:], in1=EG[j][:, :, 0, :], op=ALU.mult), reads=[R_kk[j], R_EG[j]], writes=[R_fm[j]])
                            k.op("pool", lambda e, j=j, F_=F_, U_=U_: e.tensor_tensor(out=F_[:, :, 3, :], in0=U_[:, 0:4, :], in1=EG[j][:, :, 1, :], op=ALU.mult), reads=[R_ub[j], R_EG[j]], writes=[R_fm[j]])
                            T_m = tm[j]
                            for kind, (srcf, sc) in enumerate(((lambda q, F_=F_: F_[:, q, 2, :], 1.0), (lambda q, F_=F_: F_[:, q, 0, :], 1.0), (lambda q, F_=F_: F_[:, q, 1, :], -1.0), (lambda q, U_=U_: U_[:, 8 + q, :], 1.0))):
                                ptx, rtx = ps_next()
                                for q in range(4):
                                    k.op("pe", lambda e, ptx=ptx, q=q, srcf=srcf: e.transpose(ptx[0:64, q * 128:(q + 1) * 128], srcf(q), ident[:, :]),
                                         reads=[R_fm[j], R_ub[j], R_id], writes=[rtx])
                                eng = "act" if kind % 2 == 0 else "dve"
                                if eng == "act":
                                    k.op("act", lambda e, ptx=ptx, kind=kind, sc=sc, T_m=T_m: e.activation(out=T_m[:, :, kind, :], in_=ptx[0:64, :].rearrange("p (q c) -> p q c", q=4), func=AF.Copy, scale=sc),
                                         reads=[rtx], writes=[R_tm[j]])
                                else:
                                    k.op("dve", lambda e, ptx=ptx, kind=kind, sc=sc, T_m=T_m: e.tensor_scalar(out=T_m[:, :, kind, :], in0=ptx[0:64, :].rearrange("p (q c) -> p q c", q=4), scalar1=sc, scalar2=None, op0=ALU.mult),
                                         reads=[rtx], writes=[R_tm[j]])
                            if RW_DBG["upto"] < "A2":
                                continue
                            if d == 0 and (need_ctx or ch >= 4):
                                a_path(1, a1_sb[j], R_a1[j])
                                k.op("dve", lambda e, j=j: e.tensor_tensor(out=a1_sb[j][:], in0=a1_sb[j][:], in1=a_sb[j][:], op=ALU.add), reads=[R_a1[j], R_a[j]], writes=[R_a1[j]])
                                k.op("dve", lambda e, j=j: e.scalar_tensor_tensor(out=a1_sb[j][:], in0=a1_sb[j][:], scalar=0.5, in1=kap[:, :].unsqueeze(2).to_broadcast([128, 4, 64]), op0=ALU.mult, op1=ALU.mult), reads=[R_a1[j], R_par], writes=[R_a1[j]])
                                k.op("dve", lambda e, j=j: e.tensor_tensor(out=a1_sb[j][:], in0=a1_sb[j][:], in1=okap[:, :].unsqueeze(2).to_broadcast([128, 4, 64]), op=ALU.add), reads=[R_a1[j], R_par], writes=[R_a1[j]])
                                k.op("dve", lambda e, j=j, U_=U_: e.tensor_tensor(out=rk_t[j][:], in0=a1_sb[j][:], in1=U_[:, 4:8, :], op=ALU.mult), reads=[R_a1[j], R_ub[j]], writes=[R_rk[j]])
                                k.op("dve", lambda e, j=j, U_=U_: e.tensor_tensor(out=rk_t[j][:], in0=rk_t[j][:], in1=U_[:, 0:4, :], op=ALU.mult), reads=[R_rk[j], R_ub[j]], writes=[R_rk[j]])
                                k.op("dve", lambda e, j=j: e.tensor_tensor(out=rk_t[j][:], in0=rk_t[j][:], in1=rkp[:, :].unsqueeze(2).to_broadcast([128, 4, 64]), op=ALU.mult), reads=[R_rk[j], R_par], writes=[R_rk[j]])
                                pr, rr = ps_next()
                                for q in range(4):
                                    k.op("pe", lambda e, pr=pr, q=q, j=j: e.matmul(pr[0:64, q * 2:(q + 1) * 2], rk_t[j][:, q, :], sel[:, :], start=True, stop=True), reads=[R_rk[j], R_par], writes=[rr])
                                k.op("act", lambda e, pr=pr, j=j: e.activation(out=rkh[j][:], in_=pr[0:64, 0:8], func=AF.Copy), reads=[rr], writes=[R_rkh[j]])
                                k.op("dve", lambda e, j=j, T_m=T_m: e.tensor_tensor(out=bon[j][:].rearrange("p (q h v) -> p q h v", q=4, h=2), in0=T_m[:, :, 3, :].rearrange("p q (h v) -> p q h v", h=2),
                                     in1=rkh[j][:, :].rearrange("p (q h) -> p q h", q=4).unsqueeze(3).to_broadcast([64, 4, 2, 64]), op=ALU.mult), reads=[R_tm[j], R_rkh[j]], writes=[R_bon[j]])
                                k.dma("sp", BON[b][t0:t0 + 64, :], bon[j][:], reads=[R_bon[j]], writes=[R_BON[b]])
                                k.op("act", lambda e, j=j, U_=U_: e.activation(out=sgl[j][:], in_=U_[:, 14, :], func=AF.Sigmoid), reads=[R_ub[j]], writes=[R_sgl[j]])
                                pg, rg = ps_next()
                                k.op("pe", lambda e, pg=pg, j=j: e.matmul(pg[0:64, :], sgl[j][:], g2[:], start=True, stop=True), reads=[R_sgl[j], R_par], writes=[rg])
                                k.op("act", lambda e, pg=pg, j=j: e.activation(out=gsb[j][:], in_=pg[0:64, :], func=AF.Copy), reads=[rg], writes=[R_gsb[j]])
                                k.dma("sp", GG[b][t0:t0 + 64, :], gsb[j][:], reads=[R_gsb[j]], writes=[R_GG[b]])
                            if RW_DBG["upto"] < "B":
                                continue
                            def hp(h):
                                return h // 2, (h % 2) * 64
                            hv = lambda t, h2: t[:].rearrange("p (q h) c -> p q h c", q=4, h=2)[:, :, h2, :]
                            for h2 in range(2):
                                p0 = h2 * 64
                                p1, r1 = ps_next()
                                p2, r2 = ps_next()
                                p3, r3 = ps_next()
                                for q in range(4):
                                    k.op("pe", lambda e, p1=p1, q=q, p0=p0, F_=F_: e.matmul(p1[0:64, q * 128:(q + 1) * 128], F_[p0:p0 + 64, q, 0, :], F_[p0:p0 + 64, q, 2:4, :].rearrange("p s t -> p (s t)"), start=True, stop=True),
                                         reads=[R_fm[j]], writes=[r1])
                                    k.op("pe", lambda e, p2=p2, q=q, p0=p0, F_=F_: e.matmul(p2[0:64, q * 128:(q + 1) * 128], F_[p0:p0 + 64, q, 1, :], F_[p0:p0 + 64, q, 2:4, :].rearrange("p s t -> p (s t)"), start=True, stop=True),
                                         reads=[R_fm[j]], writes=[r2])
                                    k.op("pe", lambda e, p3=p3, q=q, p0=p0, F_=F_: e.matmul(p3[0:64, q * 64:(q + 1) * 64], F_[p0:p0 + 64, q, 2, :], F_[p0:p0 + 64, q, 1, :], start=True, stop=True), reads=[R_fm[j]], writes=[r3])
                                k.op("dve", lambda e, p1=p1, h2=h2, j=j, d=d: e.tensor_tensor(out=hv(LA[j], h2), in0=p1[0:64, :].rearrange("p (q c) -> p q c", q=4),
                                     in1=m2[:, d, :].unsqueeze(1).to_broadcast([64, 4, 128]), op=ALU.mult), reads=[r1, R_par], writes=[R_LA[j]])
                                k.op("dve", lambda e, p2=p2, h2=h2, j=j, d=d: e.tensor_tensor(out=hv(NBt[j], h2), in0=p2[0:64, :].rearrange("p (q c) -> p q c", q=4),
                                     in1=m3[:, d, :].unsqueeze(1).to_broadcast([64, 4, 128]), op=ALU.mult), reads=[r2, R_par], writes=[R_NB[j]])
                                k.op("dve", lambda e, p3=p3, h2=h2, j=j, d=d: e.tensor_tensor(out=hv(Lm[j], h2), in0=p3[0:64, 0:256].rearrange("p (q c) -> p q c", q=4),
                                     in1=mL[:, d, :].unsqueeze(1).to_broadcast([64, 4, 64]), op=ALU.mult), reads=[r3, R_par], writes=[R_Lm[j]])
                            if RW_DBG["upto"] < "B0":
                                continue
                            k.op("dve", lambda e, j=j: e.scalar_tensor_tensor(out=Pm[j][:], in0=NBt[j][:, :, 0:64], scalar=-1.0, in1=ident[0:64, 0:64].unsqueeze(1).to_broadcast([64, 8, 64]), op0=ALU.mult, op1=ALU.add),
                                 reads=[R_NB[j], R_id], writes=[R_Pm[j]])
                            Ncur = lambda h, j=j: NBt[j][:, h, 0:64]
                            Lcur = lambda h, j=j: Lm[j][:, h, :]
                            R_Nc, R_Lc = R_NB[j], R_Lm[j]
                            for lev in range(RW_DBG.get("nlev", 5)):
                                i4 = nl4[0] % 4; nl4[0] += 1
                                pL, rL = ps_next()
                                for h in range(8):
                                    k.op("pe", lambda e, pL=pL, h=h, Ncur=Ncur, Lcur=Lcur: e.matmul(pL[0:64, h * 64:(h + 1) * 64], Ncur(h), Lcur(h), start=True, stop=True), reads=[R_Nc, R_Lc], writes=[rL])
                                k.op("act", lambda e, pL=pL, i4=i4: e.activation(out=Ll[i4][:].rearrange("p h c -> p (h c)"), in_=pL[0:64, :], func=AF.Copy), reads=[rL], writes=[R_Ll[i4]])
                                if lev < 4:
                                    pN, rN = ps_next()
                                    for h in range(8):
                                        k.op("pe", lambda e, pN=pN, h=h, Ncur=Ncur, Lcur=Lcur: e.matmul(pN[0:64, h * 64:(h + 1) * 64], Lcur(h), Ncur(h), start=True, stop=True), reads=[R_Nc, R_Lc], writes=[rN])
                                    k.op("act", lambda e, pN=pN, i4=i4: e.activation(out=Nl[i4][:].rearrange("p h c -> p (h c)"), in_=pN[0:64, :], func=AF.Copy), reads=[rN], writes=[R_Nl[i4]])
                                pP, rP = ps_next()
                                for h in range(8):
                                    k.op("pe", lambda e, pP=pP, h=h, i4=i4, j=j: e.matmul(pP[0:64, h * 64:(h + 1) * 64], Ll[i4][:, h, :], Pm[j][:, h, :], start=True, stop=True), reads=[R_Ll[i4], R_Pm[j]], writes=[rP])
                                k.op("dve", lambda e, pP=pP, j=j: e.tensor_tensor(out=Pm[j][:].rearrange("p h c -> p (h c)"), in0=Pm[j][:].rearrange("p h c -> p (h c)"), in1=pP[0:64, :], op=ALU.add), reads=[rP, R_Pm[j]], writes=[R_Pm[j]])
                                Ncur = lambda h, i4=i4: Nl[i4][:, h, :]
                                Lcur = lambda h, i4=i4: Ll[i4][:, h, :]
                                R_Nc, R_Lc = R_Nl[i4], R_Ll[i4]
                            if RW_DBG["upto"] < "B2":
                                continue
                            pW, rW = ps_next()
                            for h in range(8):
                                q, p0 = hp(h)
                                k.op("pe", lambda e, pW=pW, h=h, q=q, j=j, T_m=T_m: e.matmul(pW[:, h * 64:(h + 1) * 64], T_m[:, q, 0, :], Pm[j][:, h, :], start=True, stop=True), reads=[R_tm[j], R_Pm[j]], writes=[rW])
                            for h2 in range(2):
                                k.op("act" if h2 == 0 else "dve",
                                     (lambda e, pW=pW, j=j, h2=h2: e.activation(out=WT[j][h2 * 64:(h2 + 1) * 64, :, :], in_=pW[h2 * 64:(h2 + 1) * 64, :].rearrange("p (q h i) -> p q h i", q=4, h=2)[:, :, h2, :], func=AF.Copy)) if h2 == 0 else
                                     (lambda e, pW=pW, j=j, h2=h2: e.tensor_copy(out=WT[j][h2 * 64:(h2 + 1) * 64, :, :], in_=pW[h2 * 64:(h2 + 1) * 64, :].rearrange("p (q h i) -> p q h i", q=4, h=2)[:, :, h2, :])),
                                     reads=[rW], writes=[R_WT[j]])
                            pX, rX = ps_next()
                            for h in range(8):
                                q, p0 = hp(h)
                                k.op("pe", lambda e, pX=pX, h=h, q=q, p0=p0, j=j, T_m=T_m: e.matmul(pX[0:64, h * 64:(h + 1) * 64], LA[j][:, h, 0:64], T_m[:, q, 3, p0:p0 + 64], start=True, stop=True), reads=[R_LA[j], R_tm[j]], writes=[rX])
                            k.op("act", lambda e, pX=pX, j=j: e.activation(out=Xa[j][:].rearrange("p h c -> p (h c)"), in_=pX[0:64, :], func=AF.Copy), reads=[rX], writes=[R_Xa[j]])
                            pU, rU = ps_next()
                            for h in range(8):
                                k.op("pe", lambda e, pU=pU, h=h, j=j: e.matmul(pU[0:64, h * 64:(h + 1) * 64], Pm[j][:, h, :], Xa[j][:, h, :], start=True, stop=True), reads=[R_Pm[j], R_Xa[j]], writes=[rU])
                            k.op("act", lambda e, pU=pU, j=j: e.activation(out=Uta[j][:].rearrange("p h c -> p (h c)"), in_=pU[0:64, :], func=AF.Copy), reads=[rU], writes=[R_Uta[j]])
                            if RW_DBG["upto"] < "C":
                                continue
                            for h2 in range(2):
                                p0 = h2 * 64
                                pS, rS = ps_next()
                                for q in range(4):
                                    k.op("pe", lambda e, pS=pS, q=q, p0=p0, j=j: e.matmul(pS[0:64, q * 64:(q + 1) * 64], WT[j][p0:p0 + 64, q, :], Hst[p0:p0 + 64, q, :], start=True, stop=True), reads=[R_WT[j], R_H], writes=[rS])
                                k.op("dve", lambda e, pS=pS, j=j, h2=h2: e.tensor_tensor(out=hv(Ua[j], h2), in0=hv(Uta[j], h2), in1=pS[0:64, 0:256].rearrange("p (q c) -> p q c", q=4), op=ALU.add),
                                     reads=[rS, R_Uta[j]], writes=[R_Ua[j]])
                            if RW_DBG["upto"] < "C1":
                                continue
                            pY, rY = ps_next()
                            for h in range(8):
                                q, p0 = hp(h)
                                k.op("pe", lambda e, pY=pY, h=h, q=q, p0=p0, j=j, T_m=T_m: e.matmul(pY[0:64, h * 64:(h + 1) * 64], LA[j][:, h, 64:128], T_m[:, q, 3, p0:p0 + 64], start=True, stop=False), reads=[R_LA[j], R_tm[j]], writes=[rY])
                                k.op("pe", lambda e, pY=pY, h=h, j=j: e.matmul(pY[0:64, h * 64:(h + 1) * 64], NBt[j][:, h, 64:128], Ua[j][:, h, :], start=False, stop=True), reads=[R_NB[j], R_Ua[j]], writes=[rY])
                            k.op("act", lambda e, pY=pY, j=j: e.activation(out=ysb[j][:], in_=pY[0:64, :], func=AF.Copy), reads=[rY], writes=[R_ysb[j]])
                            for h2 in range(2):
                                p0 = h2 * 64
                                pR, rR = ps_next()
                                for q in range(4):
                                    k.op("pe", lambda e, pR=pR, q=q, p0=p0, j=j, F_=F_: e.matmul(pR[0:64, q * 64:(q + 1) * 64], F_[p0:p0 + 64, q, 3, :], Hst[p0:p0 + 64, q, :], start=True, stop=True), reads=[R_fm[j], R_H], writes=[rR])
                                yv = lambda t, h2: t[:].rearrange("p (q h c) -> p q h c", q=4, h=2)[:, :, h2, :]
                                k.op("dve", lambda e, pR=pR, j=j, h2=h2, yv=yv: e.tensor_tensor(out=yv(ysb[j], h2), in0=yv(ysb[j], h2), in1=pR[0:64, 0:256].rearrange("p (q c) -> p q c", q=4), op=ALU.add),
                                     reads=[rR, R_ysb[j]], writes=[R_ysb[j]])
                            k.dma("sp", YD[d][b][t0:t0 + 64, :], ysb[j][:], reads=[R_ysb[j]], writes=[R_YD[d][b]])
                            if RW_DBG["upto"] < "C2":
                                continue
                            pH, rH = ps_next()
                            for q in range(4):
                                k.op("pe", lambda e, pH=pH, q=q, T_m=T_m: e.matmul(pH[:, q * 128:(q + 1) * 128], T_m[:, q, 1, :], T_m[:, q, 3, :], start=True, stop=False), reads=[R_tm[j]], writes=[rH])
                                k.op("pe", lambda e, pH=pH, q=q, j=j, T_m=T_m: e.matmul(pH[:, q * 128:(q + 1) * 128], T_m[:, q, 2, :], Ua[j][:, 2 * q:2 * q + 2, :].rearrange("p h c -> p (h c)"), start=False, stop=True), reads=[R_tm[j], R_Ua[j]], writes=[rH])
                            for h2 in range(2):
                                ps_ = slice(h2 * 64, (h2 + 1) * 64)
                                k.op("dve", lambda e, pH=pH, j=j, h2=h2, ps_=ps_: e.tensor_tensor(out=htmp[j][ps_, :, :], in0=pH[ps_, :].rearrange("p (q h v) -> p q h v", q=4, h=2)[:, :, h2, :], in1=Hst[ps_, :, :], op=ALU.add),
                                     reads=[rH, R_H], writes=[R_htmp[j]])
                                k.op("dve", lambda e, j=j, ps_=ps_, last_col=last_col: e.tensor_tensor(out=Hst[ps_, :, :], in0=htmp[j][ps_, :, :], in1=EG[j][ps_, :, 1, last_col:last_col + 1].to_broadcast([64, 4, 64]), op=ALU.mult),
                                     reads=[R_htmp[j], R_EG[j]], writes=[R_H])
            k.barrier()
            if not RW_DBG["readout"]:
                return
            with ExitStack() as s1:
                lnw_ro = sb("ro_lnw", [128, 512], F32, s1); lnb_ro = sb("ro_lnb", [128, 512], F32, s1); c1_ro = sb("ro_c1", [128, 4], F32, s1); R_par_ro = Res()
                k.dma("sp", lnw_ro[:], I["rw_lnw"][l], writes=[R_par_ro]); k.dma("sp", lnb_ro[:], I["rw_lnb"][l], writes=[R_par_ro]); k.dma("sp", c1_ro[:], I["rw_c1"], writes=[R_par_ro])
                def T2(name, shape, dt=F32):
                    return [sb(name + str(i), shape, dt, s1) for i in range(2)], [Res(), Res()]
                y0_ro, R_y0_ro = T2("ro_y0", [128, 512]); y1_ro, R_y1_ro = T2("ro_y1", [128, 512]); bo_ro, R_bo_ro = T2("ro_bo", [128, 512]); gg_ro, R_gg_ro = T2("ro_gg", [128, 512])
                stt__ro, R_stt_ro = T2("ro_st", [128, 32]); yc_ro, R_yc_ro = T2("ro_yc", [128, 512]); sq_ro, R_sq_ro = T2("ro_sq", [128, 512]); ot_ro, R_ot_ro = T2("ro_ot", [128, 4, 128], BF16)
                n = 0
                for b in range(NB):
                    for tt in range(0 if need_ctx else 2, NT):
                        j = n % 2; n += 1
                        t0 = tt * 128
                        k.dma("sp", y0_ro[j][:], YD[0][b][t0:t0 + 128, :], reads=[R_YD[0][b]], writes=[R_y0_ro[j]])
                        k.dma("sp", y1_ro[j][:], YD[1][b][t0:t0 + 128, :], reads=[R_YD[1][b]], writes=[R_y1_ro[j]])
                        k.dma("sp", bo_ro[j][:], BON[b][t0:t0 + 128, :], reads=[R_BON[b]], writes=[R_bo_ro[j]])
                        k.dma("sp", gg_ro[j][:], GG[b][t0:t0 + 128, :], reads=[R_GG[b]], writes=[R_gg_ro[j]])
                        S_ = stt__ro[j]
                        v3 = lambda t: t[:].rearrange("p (h v) -> p h v", h=8)
                        bc = lambda a: a.unsqueeze(2).to_broadcast([128, 8, 64])
                        k.op("dve", lambda e, j=j: e.tensor_tensor(out=y0_ro[j][:], in0=y0_ro[j][:], in1=y1_ro[j][:], op=ALU.add), reads=[R_y0_ro[j], R_y1_ro[j]], writes=[R_y0_ro[j]])
                        k.op("dve", lambda e, j=j, S_=S_: e.tensor_reduce(out=S_[:, 0:8], in_=v3(y0_ro[j]), axis=AX.X, op=ALU.add), reads=[R_y0_ro[j]], writes=[R_stt_ro[j]])
                        k.op("dve", lambda e, S_=S_: e.tensor_scalar(out=S_[:, 8:16], in0=S_[:, 0:8], scalar1=1.0 / 64, scalar2=None, op0=ALU.mult), reads=[R_stt_ro[j]], writes=[R_stt_ro[j]])
                        k.op("dve", lambda e, j=j, S_=S_: e.tensor_tensor(out=v3(yc_ro[j]), in0=v3(y0_ro[j]), in1=bc(S_[:, 8:16]), op=ALU.subtract), reads=[R_y0_ro[j], R_stt_ro[j]], writes=[R_yc_ro[j]])
                        k.op("pool", lambda e, j=j: e.tensor_tensor(out=sq_ro[j][:], in0=yc_ro[j][:], in1=yc_ro[j][:], op=ALU.mult), reads=[R_yc_ro[j]], writes=[R_sq_ro[j]])
                        k.op("dve", lambda e, j=j, S_=S_: e.tensor_reduce(out=S_[:, 16:24], in_=v3(sq_ro[j]), axis=AX.X, op=ALU.add), reads=[R_sq_ro[j]], writes=[R_stt_ro[j]])
                        k.op("act", lambda e, S_=S_: e.activation(out=S_[:, 24:32], in_=S_[:, 16:24], func=AF.Sqrt, bias=c1_ro[:, 2:3], scale=1.0 / 64), reads=[R_stt_ro[j], R_par_ro], writes=[R_stt_ro[j]])
                        k.op("dve", lambda e, S_=S_: e.reciprocal(out=S_[:, 24:32], in_=S_[:, 24:32]), reads=[R_stt_ro[j]], writes=[R_stt_ro[j]])
                        k.op("dve", lambda e, j=j, S_=S_: e.tensor_tensor(out=v3(yc_ro[j]), in0=v3(yc_ro[j]), in1=bc(S_[:, 24:32]), op=ALU.mult), reads=[R_yc_ro[j], R_stt_ro[j]], writes=[R_yc_ro[j]])
                        k.op("pool", lambda e, j=j: e.tensor_tensor(out=yc_ro[j][:], in0=yc_ro[j][:], in1=lnw_ro[:], op=ALU.mult), reads=[R_yc_ro[j], R_par_ro], writes=[R_yc_ro[j]])
                        k.op("pool", lambda e, j=j: e.tensor_tensor(out=yc_ro[j][:], in0=yc_ro[j][:], in1=lnb_ro[:], op=ALU.add), reads=[R_yc_ro[j], R_par_ro], writes=[R_yc_ro[j]])
                        k.op("dve", lambda e, j=j: e.tensor_tensor(out=yc_ro[j][:], in0=yc_ro[j][:], in1=bo_ro[j][:], op=ALU.add), reads=[R_yc_ro[j], R_bo_ro[j]], writes=[R_yc_ro[j]])
                        k.op("dve", lambda e, j=j: e.tensor_tensor(out=yc_ro[j][:], in0=yc_ro[j][:], in1=gg_ro[j][:], op=ALU.mult), reads=[R_yc_ro[j], R_gg_ro[j]], writes=[R_yc_ro[j]])
                        pt, rp = ps_next()
                        for q in range(4):
                            k.op("pe", lambda e, pt=pt, q=q, j=j: e.transpose(pt[:, q * 128:(q + 1) * 128], yc_ro[j][:, q * 128:(q + 1) * 128], ident[:, :]), reads=[R_yc_ro[j], R_id], writes=[rp])
                        k.op("act", lambda e, pt=pt, j=j: e.activation(out=ot_ro[j][:].rearrange("p q t -> p (q t)"), in_=pt[:, :], func=AF.Copy), reads=[rp], writes=[R_ot_ro[j]])
                        k.dma("sp", YT[b][:, 0:4, t0:t0 + 128], ot_ro[j][:], reads=[R_ot_ro[j]], writes=[R_YT[b]])
            k.barrier()

        toks = []
        for l in range(n_layers):
            if stages is None or "norm1" in stages:
                stage_norm(l, 0, 0, HT, R_HT)
            if stages is None or "proj" in stages:
                stage_proj(l)
            last = (l == DEPTH - 1)
            if stages is None or "na" in stages:
                stage_na(l, not last, **na_kw)
            if stages is None or "rwkv" in stages:
                stage_rwkv(l, not last)
            if stages is None or "wout" in stages:
                stage_wout(l, last)
            if stages is None or "ffn" in stages:
                moe = (l % 2 == 1)
                tsel = range(LC, S, 256) if last else None
                if moe:
                    stage_norm(l, 1, 3, H2T, R_H2T, dst32=H2F, R_dst32=R_H2F, tsel=tsel)
                    stage_router(l, last)
                else:
                    stage_norm(l, 1, 3, H2T, R_H2T, tsel=tsel)
                stage_ffn(l, last)
        if stages is None or "final" in stages:
            fin_out = stage_final()
        else:
            fin_out = []

        fin = list(fin_out)
        for r in R_XT + R_HT + R_QK + R_VN + R_UT + R_YT + R_YD[0] + R_YD[1] + R_BON + R_GG:
            if r.w is not None:
                fin.append(r.w)
        k.final_wait("sp", fin)
        k.emit()
        print("insts", k.n_inst, "epochs", k.n_epochs)
    return nc


def _fm(a, nchunk):
    return np.ascontiguousarray(a.reshape(nchunk, 128, -1).transpose(1, 0, 2))


def _build_bfull(rpb):
    L, H = rpb.shape[:2]
    q = np.arange(64)[:, None]; kc = np.arange(64)[None, :]
    lo = np.clip(q - 8, 0, 48)
    valid = (kc >= lo) & (kc < lo + 16)
    idx = np.clip(kc - q + 15, 0, 30)
    g = rpb[:, :, :, idx]
    g = np.where(valid[None, None, None], g, np.float32(NEG)).astype(np.float32)
    return np.ascontiguousarray(g.transpose(0, 3, 1, 2, 4).reshape(L, 64, H, 960))


def _prep_shared(inp):
    m = {}
    m["ada_w"] = np.stack([_fm(inp["ada_w"][l], 8) for l in range(4)])
    m["ada_bT"] = np.stack([inp["ada_b"][l].reshape(48, 128).T.copy() for l in range(4)])
    m["g_mix"] = np.stack([inp["norm_mix_g"][l].reshape(8, 128).T.copy() for l in range(4)])
    m["g_ffn"] = np.stack([inp["norm_ffn_g"][l].reshape(8, 128).T.copy() for l in range(4)])
    m["w_in"] = np.stack([_fm(inp["w_in"][l], 8) for l in range(4)])
    m["mu"] = np.stack([inp["shift_mu"][l].reshape(15, 128).T.copy() for l in range(4)])
    m["ident"] = np.eye(128, dtype=np.float32)
    m["final_g"] = inp["final_g"].reshape(8, 128).T.copy()
    m["bfull"] = _build_bfull(inp["na_rpb"])
    m["w_out"] = np.stack([_fm(inp["w_out"][l], 8) for l in range(4)])
    m["rw_w2"] = inp["w2"].reshape(4, 128, 512); m["rw_a2"] = inp["a2"].reshape(4, 128, 512); m["rw_g2"] = inp["g2"]
    m["rw_a0T"] = np.stack([inp["a0"][l].reshape(2, 4, 128).transpose(2, 0, 1) for l in range(4)])
    c4 = lambda a: np.stack([a[l].reshape(4, 128).T for l in range(4)])
    m["rw_kk"] = c4(inp["k_k"]); m["rw_ka"] = c4(inp["k_a"]); m["rw_rk"] = c4(inp["r_k"].reshape(4, 512))
    m["rw_lnw"] = np.broadcast_to(inp["ln_x_w"][:, None, :], (4, 128, 512)); m["rw_lnb"] = np.broadcast_to(inp["ln_x_b"][:, None, :], (4, 128, 512))
    m["rw_w0b"] = np.broadcast_to(inp["w0"][:, None, :, :], (4, 64, 2, 512))
    idx = np.arange(64)
    strict = [(idx[:, None] < idx[None, :]), (idx[:, None] > idx[None, :])]
    incl = [(idx[:, None] <= idx[None, :]), (idx[:, None] >= idx[None, :])]
    m["rw_m2"] = np.stack([np.concatenate([strict[d], incl[d]], 1) for d in range(2)], 1).astype(np.float32)
    m["rw_m3"] = np.stack([np.concatenate([strict[d].astype(np.float32), -incl[d].astype(np.float32)], 1) for d in range(2)], 1)
    m["rw_mL"] = np.stack([strict[d].T for d in range(2)], 1).astype(np.float32)
    bo = np.zeros((128, 128), np.float32); bo[:64, :64] = 1; bo[64:, 64:] = 1
    m["rw_bones"] = bo
    se = np.zeros((128, 2), np.float32); se[:64, 0] = 1; se[64:, 1] = 1
    m["rw_sel"] = se
    m["rw_c1"] = np.tile(np.array([1.0, -0.5, 64e-5, 1e-12], np.float32)[None], (128, 1))
    m["ffn_w1"] = np.stack([_fm(inp["ffn_w1"][i], 8) for i in range(2)])
    m["ffn_w3"] = np.stack([_fm(inp["ffn_w3"][i], 8) for i in range(2)])
    m["ffn_w2"] = np.stack([_fm(inp["ffn_w2"][i], DFF // 128) for i in range(2)])
    m["router"] = np.stack([_fm(inp["router"][i], 8) for i in range(2)])
    m["moe_w1"] = np.stack([np.stack([_fm(inp["moe_w1"][i, e], 8) for e in range(NE)]) for i in range(2)])
    m["moe_w3"] = np.stack([np.stack([_fm(inp["moe_w3"][i, e], 8) for e in range(NE)]) for i in range(2)])
    m["moe_w2"] = np.stack([np.stack([_fm(inp["moe_w2"][i, e], DFE // 128) for e in range(NE)]) for i in range(2)])
    return {k_: np.ascontiguousarray(v, dtype=np.float32) for k_, v in m.items()}


def _prep_core(inp, core):
    bs = [2 * core, 2 * core + 1]
    m = {}
    xcat = [np.concatenate([inp["ctx"][b], inp["x"][b]], 0) for b in bs]
    m["xT"] = np.stack([_fm(np.ascontiguousarray(xc.T), 8) for xc in xcat]).astype(np.float32)
    cc = np.stack([inp["c"][bs[0]], inp["c"][bs[1]], inp["c_ctx"]], 1)
    m["cT"] = _fm(cc, 8).astype(np.float32)
    return m


def kernel(**inputs):
    inp = {k_: np.asarray(v) for k_, v in inputs.items()}
    n = 8
    shared = _prep_shared(inp)
    in_maps = []
    for core in range(n):
        m = dict(shared)
        m.update(_prep_core(inp, core))
        in_maps.append(m)
    nc = build_program()
    res = run_bass_kernel_spmd(nc, in_maps, core_ids=list(range(n)))
    outs = []
    for core in range(n):
        o = np.asarray(res.results[core]["out"])
        for b in range(NB):
            outs.append(o[b].transpose(1, 0, 2).reshape(D, T).T)
    return np.ascontiguousarray(np.stack(outs, 0)).astype(np.float32)
```

```python
import numpy as np
import concourse.bass as bass
import concourse.mybir as mybir
from concourse.bass_utils import run_bass_kernel_spmd
from contextlib import ExitStack

F32 = mybir.dt.float32
BF16 = mybir.dt.bfloat16
AF = mybir.ActivationFunctionType
ALU = mybir.AluOpType
AX = mybir.AxisListType

SAME_ENGINE_SYNC = True
DMA_RING = {"sp": 16, "act": 4, "pool": 16}
SEM_LIMIT = 60000
MAX_EPOCHS = 7


class Res:
    __slots__ = ("name", "w", "rd")

    def __init__(self, name=""):
        self.name = name
        self.w = None
        self.rd = []


class K:
    ENG = ("pe", "act", "dve", "pool", "sp")
    CE = ("pe", "act", "dve", "pool")

    def __init__(self, nc, stack):
        self.nc = nc
        self.stack = stack
        self.recs = []
        self.cnt = {e: 0 for e in self.ENG}
        self.slots = []
        self.dq = {}
        for q in ("sp", "act", "pool"):
            idx = []
            for i in range(DMA_RING[q]):
                self.slots.append(0)
                idx.append(len(self.slots) - 1)
            self.dq[q] = {"idx": idx, "n": 0}
        self.waited_e = {e: {} for e in self.ENG}
        self.waited_d = {e: {} for e in self.ENG}
        self.n_inst = 0

    def _need(self, eng, tok, waits, force=False):
        if tok is None:
            return
        if tok[0] == "e":
            _, x, seq = tok
            if x == eng and (not SAME_ENGINE_SYNC or eng == "pe") and not force:
                return
            if self.waited_e[eng].get(x, 0) >= seq:
                return
            self.waited_e[eng][x] = seq
            waits.append(tok)
        else:
            _, si, use = tok
            if self.waited_d[eng].get(si, 0) >= use:
                return
            self.waited_d[eng][si] = use
            waits.append(tok)

    def _deps(self, eng, reads, writes):
        waits = []
        for r in reads:
            self._need(eng, r.w, waits)
        for w in writes:
            self._need(eng, w.w, waits)
            best = {}
            for t in w.rd:
                key = (t[0], t[1])
                if key not in best or best[key][2] < t[2]:
                    best[key] = t
            for t in best.values():
                self._need(eng, t, waits)
        return waits

    def _commit(self, tok, reads, writes):
        for r in reads:
            if r.rd and r.rd[-1][0] == tok[0] and r.rd[-1][1] == tok[1]:
                r.rd[-1] = tok
            else:
                r.rd.append(tok)
        for w in writes:
            w.w = tok
            w.rd = []

    def op(self, eng, fn, reads=(), writes=()):
        waits = self._deps(eng, reads, writes)
        self.cnt[eng] += 1
        tok = ("e", eng, self.cnt[eng])
        self.recs.append({"eng": eng, "waits": waits, "fn": fn, "tok": tok})
        self._commit(tok, reads, writes)
        self.n_inst += 1
        return tok

    def dma(self, q, out, in_, reads=(), writes=(), **kw):
        waits = self._deps(q, reads, writes)
        d = self.dq[q]
        si = d["idx"][d["n"] % len(d["idx"])]
        d["n"] += 1
        prev = self.slots[si]
        if prev > 0:
            self._need(q, ("d", si, prev), waits)
        self.slots[si] = prev + 1
        tok = ("d", si, prev + 1)
        fn = (lambda e, o=out, i=in_, kw=kw: e.dma_start(out=o, in_=i, **kw))
        self.recs.append({"eng": q, "waits": waits, "fn": fn, "tok": tok})
        self._commit(tok, reads, writes)
        self.n_inst += 1
        return tok

    def _all_waits(self, eng):
        waits = []
        for x in self.CE:
            if self.cnt[x] > 0:
                self._need(eng, ("e", x, self.cnt[x]), waits, force=True)
        for si, v in enumerate(self.slots):
            if v > 0:
                self._need(eng, ("d", si, v), waits)
        return waits

    def barrier(self):
        for eng in self.ENG:
            self.recs.append({"eng": eng, "waits": self._all_waits(eng), "fn": None, "tok": None})

    def final_wait(self, eng, toks):
        waits = []
        for t in toks:
            self._need(eng, t, waits)
        self.recs.append({"eng": eng, "waits": waits, "fn": None, "tok": None})

    def emit(self):
        nc, st = self.nc, self.stack
        recs = self.recs
        needed = set()
        for r in recs:
            for w in r["waits"]:
                if w[0] == "e":
                    needed.add((w[1], w[2]))
        out = []
        ep = 0
        ms = {e: 0 for e in self.CE}
        du = [0] * len(self.slots)
        last_e = {e: 0 for e in self.CE}
        last_d = [0] * len(self.slots)
        val = {}
        pend_last = {}

        def close_epoch():
            nonlocal ep, ms, du
            bw = {}
            for e in self.CE:
                if e in pend_last:
                    rr = out[pend_last[e]]
                    t = rr["tok"]
                    if t not in val:
                        ms[e] += 1
                        val[t] = (ep, ms[e])
                        rr["inc"] = True
            allw = []
            for e in self.CE:
                if e in pend_last:
                    allw.append(out[pend_last[e]]["tok"])
            for si in range(len(self.slots)):
                if du[si] > 0:
                    allw.append(("d", si, last_d[si]))
            for eng in self.ENG:
                out.append({"eng": eng, "waits": list(allw), "fn": None, "tok": None, "ep": ep})
            ep += 1
            ms = {e: 0 for e in self.CE}
            du = [0] * len(self.slots)
            pend_last.clear()

        for r in recs:
            t = r["tok"]
            if t is not None:
                if t[0] == "e":
                    if ms[t[1]] + 2 > SEM_LIMIT:
                        close_epoch()
                else:
                    if (du[t[1]] + 2) * 16 > SEM_LIMIT:
                        close_epoch()
            r = dict(r)
            r["ep"] = ep
            out.append(r)
            if t is not None:
                if t[0] == "e":
                    pend_last[t[1]] = len(out) - 1
                    if (t[1], t[2]) in needed:
                        ms[t[1]] += 1
                        val[t] = (ep, ms[t[1]])
                        r["inc"] = True
                    else:
                        r["inc"] = False
                else:
                    du[t[1]] += 1
                    last_d[t[1]] = t[2]
                    val[t] = (ep, du[t[1]] * 16)
        n_ep = ep + 1
        print("epochs needed", n_ep, "milestones", ms, "dma uses", max(du))
        assert n_ep <= MAX_EPOCHS, "too many epochs %d" % n_ep
        self.n_epochs = n_ep
        esem = [{e: st.enter_context(nc.semaphore("c%d_%s" % (i, e))) for e in self.CE} for i in range(n_ep)]
        dsem = [[st.enter_context(nc.semaphore("d%d_%d" % (i, j))) for j in range(len(self.slots))] for i in range(n_ep)]
        streams = {e: [] for e in self.ENG}
        for r in out:
            streams[r["eng"]].append(r)

        with nc.Block() as block:
            def run(engname, e):
                for ep_ in range(1, n_ep):
                    if engname in self.CE:
                        e.sem_clear(esem[ep_][engname])
                    if engname in self.dq:
                        for si in self.dq[engname]["idx"]:
                            e.sem_clear(dsem[ep_][si])
                for r in streams[engname]:
                    for w in r["waits"]:
                        v = val.get(w)
                        if v is None or v[0] != r["ep"]:
                            continue
                        if w[0] == "e":
                            e.wait_ge(esem[v[0]][w[1]], v[1])
                        else:
                            e.wait_ge(dsem[v[0]][w[1]], v[1])
                    if r["fn"] is None:
                        continue
                    ins = r["fn"](e)
                    t = r["tok"]
                    if t[0] == "e":
                        if r.get("inc"):
                            ins.then_inc(esem[r["ep"]][t[1]], 1)
                    else:
                        ins.then_inc(dsem[r["ep"]][t[1]], 16)

            @block.sync
            def _(e):
                run("sp", e)

            @block.tensor
            def _(e):
                run("pe", e)

            @block.vector
            def _(e):
                run("dve", e)

            @block.scalar
            def _(e):
                run("act", e)

            @block.gpsimd
            def _(e):
                run("pool", e)


D = 1024
DEPTH = 4
NB = 2
LC = 256
T = 2048
S = LC + T
NT = S // 128
D_IN = 3456
DFF = 2816
DFE = 3584
NE = 8
NEG = -30000.0

DEBUG_OUT = []
RW_DBG = {"nch": None, "upto": "Z", "nb": None, "nd": 2, "readout": True}
N_LAYERS_RUN = DEPTH


def build_program(n_layers=DEPTH, stages=None, debug=(), na_kw={}):
    nc = bass.Bass("TRN2", target_bir_lowering=False)
    st = ExitStack()
    with st:
        k = K(nc, st)

        def din(name, shape, dt=F32):
            return nc.dram_tensor(name, list(shape), dt, kind="ExternalInput").ap()

        def dscr(name, shape, dt=F32):
            kind = "ExternalOutput" if name in debug else "Internal"
            return nc.dram_tensor(name, list(shape), dt, kind=kind).ap()

        I = {}
        I["xT"] = din("xT", [NB, 128, 8, S])
        I["cT"] = din("cT", [128, 8, 3])
        I["ada_w"] = din("ada_w", [DEPTH, 128, 8, 6 * D])
        I["ada_bT"] = din("ada_bT", [DEPTH, 128, 48])
        I["g_mix"] = din("g_mix", [DEPTH, 128, 8])
        I["g_ffn"] = din("g_ffn", [DEPTH, 128, 8])
        I["w_in"] = din("w_in", [DEPTH, 128, 8, D_IN])
        I["mu"] = din("mu", [DEPTH, 128, 15])
        I["ident"] = din("ident", [128, 128])
        I["final_g"] = din("final_g", [128, 8])
        I["bfull"] = din("bfull", [DEPTH, 64, 8, 960])
        I["w_out"] = din("w_out", [DEPTH, 128, 8, D])
        I["rw_w2"] = din("rw_w2", [DEPTH, 128, 512]); I["rw_a2"] = din("rw_a2", [DEPTH, 128, 512]); I["rw_g2"] = din("rw_g2", [DEPTH, 128, 512])
        I["rw_a0T"] = din("rw_a0T", [DEPTH, 128, 2, 4]); I["rw_kk"] = din("rw_kk", [DEPTH, 128, 4]); I["rw_ka"] = din("rw_ka", [DEPTH, 128, 4]); I["rw_rk"] = din("rw_rk", [DEPTH, 128, 4])
        I["rw_lnw"] = din("rw_lnw", [DEPTH, 128, 512]); I["rw_lnb"] = din("rw_lnb", [DEPTH, 128, 512]); I["rw_w0b"] = din("rw_w0b", [DEPTH, 64, 2, 512])
        I["rw_m2"] = din("rw_m2", [64, 2, 128]); I["rw_m3"] = din("rw_m3", [64, 2, 128]); I["rw_mL"] = din("rw_mL", [64, 2, 64])
        I["rw_bones"] = din("rw_bones", [128, 128]); I["rw_sel"] = din("rw_sel", [128, 2]); I["rw_c1"] = din("rw_c1", [128, 4])
        I["ffn_w1"] = din("ffn_w1", [2, 128, 8, DFF]); I["ffn_w3"] = din("ffn_w3", [2, 128, 8, DFF]); I["ffn_w2"] = din("ffn_w2", [2, 128, DFF // 128, D])
        I["router"] = din("router", [2, 128, 8, NE])
        I["moe_w1"] = din("moe_w1", [2, NE, 128, 8, DFE]); I["moe_w3"] = din("moe_w3", [2, NE, 128, 8, DFE]); I["moe_w2"] = din("moe_w2", [2, NE, 128, DFE // 128, D])
        out = nc.dram_tensor("out", [NB, 128, 8, T], F32, kind="ExternalOutput").ap()

        XT = [dscr("xt%d" % b, [128, 8, S]) for b in range(NB)]
        HT = [dscr("ht%d" % b, [128, 8, S], BF16) for b in range(NB)]
        QT = [dscr("qt%d" % b, [128, 4, S], BF16) for b in range(NB)]
        KT = [dscr("kt%d" % b, [128, 4, S], BF16) for b in range(NB)]
        VN = [dscr("vn%d" % b, [S, 512], BF16) for b in range(NB)]
        UT = [dscr("ut%d" % b, [128, 15, S]) for b in range(NB)]
        R_XT = [Res() for _ in range(NB)]
        R_HT = [Res() for _ in range(NB)]
        R_QK = [Res() for _ in range(NB)]
        R_VN = [Res() for _ in range(NB)]
        R_UT = [Res() for _ in range(NB)]
        YT = [dscr("yt%d" % b, [128, 8, S], BF16) for b in range(NB)]
        H2T = [dscr("h2t%d" % b, [128, 8, S], BF16) for b in range(NB)]
        H2F = [dscr("h2f%d" % b, [128, 8, S]) for b in range(NB)]
        GB = [dscr("gb%d" % b, [128, NE, S]) for b in range(NB)]
        YD = [[dscr("yd%d_%d" % (d, b), [S, 512]) for b in range(NB)] for d in range(2)]
        R_YD = [[Res() for b in range(NB)] for d in range(2)]
        BON = [dscr("bon%d" % b, [S, 512]) for b in range(NB)]; R_BON = [Res() for _ in range(NB)]
        GG = [dscr("gg%d" % b, [S, 512]) for b in range(NB)]; R_GG = [Res() for _ in range(NB)]
        R_H2T = [Res() for _ in range(NB)]; R_H2F = [Res() for _ in range(NB)]; R_GB = [Res() for _ in range(NB)]
        R_YT = [Res() for _ in range(NB)]

        uid = [0]

        def sb(name, shape, dt=F32, stack=st):
            uid[0] += 1
            return stack.enter_context(nc.sbuf_tensor("%s_%d" % (name, uid[0]), list(shape), dt))

        ident = sb("ident_sb", [128, 128]); R_id = Res()
        ones = sb("ones_sb", [128, 128]); R_ones = Res()
        MOD = sb("mod_sb", [128, DEPTH, 48, 3]); R_mod = Res()
        GS = sb("gs_sb", [128, DEPTH, 2, 8, 3]); R_gs = Res()
        gmix = sb("gmix_sb", [128, DEPTH, 8]); gffn = sb("gffn_sb", [128, DEPTH, 8]); R_g = Res()
        eps_t = sb("eps_sb", [128, 1]); R_eps = Res()
        psum = [st.enter_context(nc.psum_tensor("ps%d" % i, [128, 512], F32)) for i in range(8)]
        R_ps = [Res() for _ in range(8)]
        pctr = [0]

        def ps_next():
            i = pctr[0] % 8
            pctr[0] += 1
            return psum[i], R_ps[i]

        k.dma("sp", ident[:], I["ident"][:, :], writes=[R_id])
        ident_bf = sb("identbf_sb", [128, 128], BF16)
        k.op("dve", lambda e: e.tensor_copy(out=ident_bf[:], in_=ident[:]), reads=[R_id], writes=[R_id])
        k.op("dve", lambda e: e.memset(ones[:], 1.0), writes=[R_ones])
        k.op("dve", lambda e: e.memset(eps_t[:], 1e-6), writes=[R_eps])
        for l in range(DEPTH):
            k.dma("sp", gmix[:, l, :], I["g_mix"][l], writes=[R_g])
            k.dma("sp", gffn[:, l, :], I["g_ffn"][l], writes=[R_g])

        with ExitStack() as s1:
            cT = sb("cT_sb", [128, 8, 3], F32, s1); R_c = Res()
            sT = sb("sT", [128, 8, 3], F32, s1); R_s = Res()
            abT = sb("abT", [128, 48], F32, s1); R_ab = Res()
            aw = [sb("aw%d" % i, [128, 8, 512], F32, s1) for i in range(2)]
            R_aw = [Res(), Res()]
            xcp = [sb("xcp%d" % i, [128, 8, 256], F32, s1) for i in range(2)]
            R_xcp = [Res(), Res()]
            k.dma("sp", cT[:], I["cT"][:, :, :], writes=[R_c])
            k.op("act", lambda e: e.activation(out=sT[:], in_=cT[:], func=AF.Silu), reads=[R_c], writes=[R_s])
            n = 0
            for b in range(NB):
                for t0 in range(0, S, 256):
                    j = n % 2; n += 1
                    k.dma("sp", xcp[j][:], I["xT"][b, :, :, t0:t0 + 256], writes=[R_xcp[j]])
                    k.dma("sp", XT[b][:, :, t0:t0 + 256], xcp[j][:], reads=[R_xcp[j]], writes=[R_XT[b]])
            n = 0
            for l in range(n_layers):
                k.dma("sp", abT[:], I["ada_bT"][l], writes=[R_ab])
                for cc in range(12):
                    j = n % 2; n += 1
                    k.dma("sp", aw[j][:], I["ada_w"][l, :, :, cc * 512:(cc + 1) * 512], writes=[R_aw[j]])
                    pt, rp = ps_next()
                    for c4 in range(4):
                        for kc in range(8):
                            k.op("pe", lambda e, pt=pt, j=j, c4=c4, kc=kc: e.matmul(
                                pt[:, c4 * 3:c4 * 3 + 3], aw[j][:, kc, c4 * 128:(c4 + 1) * 128], sT[:, kc, :],
                                start=(kc == 0), stop=(kc == 7)), reads=[R_aw[j], R_s], writes=[rp])
                    k.op("dve", lambda e, pt=pt, l=l, cc=cc: e.tensor_tensor(
                        out=MOD[:, l, cc * 4:(cc + 1) * 4, :], in0=pt[:, 0:12].rearrange("p (a b) -> p a b", b=3),
                        in1=abT[:, cc * 4:(cc + 1) * 4].unsqueeze(2).to_broadcast([128, 4, 3]), op=ALU.add),
                        reads=[rp, R_ab], writes=[R_mod])
                for which, (gt, mi) in enumerate(((gmix, 1), (gffn, 4))):
                    k.op("dve", lambda e, l=l, which=which, gt=gt, mi=mi: e.scalar_tensor_tensor(
                        out=GS[:, l, which, :, :], in0=MOD[:, l, mi * 8:(mi + 1) * 8, :], scalar=1.0,
                        in1=gt[:, l, :].unsqueeze(2).to_broadcast([128, 8, 3]), op0=ALU.add, op1=ALU.mult),
                        reads=[R_mod, R_g], writes=[R_gs])
        k.barrier()

        def which_of(b, t0):
            return 2 if t0 < LC else b

        def stage_norm(l, which, shift_idx, dst, R_dst, gsel=None, dst32=None, R_dst32=None, bsel=range(NB), tsel=None):
            with ExitStack() as s1:
                xb = [sb("nx%d" % i, [128, 8, 256], F32, s1) for i in range(2)]; R_xb = [Res(), Res()]
                sq = [sb("nsq%d" % i, [128, 8, 256], F32, s1) for i in range(2)]; R_sq = [Res(), Res()]
                rs = [sb("nrs%d" % i, [128, 256], F32, s1) for i in range(2)]; R_rs = [Res(), Res()]
                tm = [sb("ntm%d" % i, [128, 8, 256], F32, s1) for i in range(2)]; R_tm = [Res(), Res()]
                hb = [sb("nhb%d" % i, [128, 8, 256], BF16, s1) for i in range(2)]; R_hb = [Res(), Res()]
                n = 0
                for b in bsel:
                    for t0 in (tsel if tsel is not None else range(0, S, 256)):
                        j = n % 2; n += 1
                        w = which_of(b, t0)
                        k.dma("sp", xb[j][:], XT[b][:, :, t0:t0 + 256], reads=[R_XT[b]], writes=[R_xb[j]])
                        k.op("act", lambda e, j=j: e.activation(out=sq[j][:], in_=xb[j][:], func=AF.Square), reads=[R_xb[j]], writes=[R_sq[j]])
                        pt, rp = ps_next()
                        for c in range(8):
                            k.op("pe", lambda e, pt=pt, j=j, c=c: e.matmul(pt[:, 0:256], ones[:], sq[j][:, c, :], start=(c == 0), stop=(c == 7)),
                                 reads=[R_ones, R_sq[j]], writes=[rp])
                        k.op("act", lambda e, pt=pt, j=j: e.activation(out=rs[j][:], in_=pt[:, 0:256], func=AF.Sqrt, bias=eps_t[:, 0:1], scale=1.0 / D),
                             reads=[rp, R_eps], writes=[R_rs[j]])
                        k.op("dve", lambda e, j=j: e.reciprocal(out=rs[j][:], in_=rs[j][:]), reads=[R_rs[j]], writes=[R_rs[j]])
                        for c in range(8):
                            k.op("dve", lambda e, j=j, c=c, w=w: e.scalar_tensor_tensor(
                                out=tm[j][:, c, :], in0=xb[j][:, c, :], scalar=GS[:, l, which, c, w:w + 1], in1=rs[j][:],
                                op0=ALU.mult, op1=ALU.mult), reads=[R_xb[j], R_gs, R_rs[j]], writes=[R_tm[j]])
                            k.op("act", lambda e, j=j, c=c, w=w: e.activation(
                                out=hb[j][:, c, :], in_=tm[j][:, c, :], func=AF.Identity, bias=MOD[:, l, shift_idx * 8 + c, w:w + 1], scale=1.0),
                                reads=[R_tm[j], R_mod], writes=[R_hb[j]])
                            if dst32 is not None:
                                k.op("pool", lambda e, j=j, c=c, w=w: e.tensor_scalar(
                                    out=tm[j][:, c, :], in0=tm[j][:, c, :], scalar1=MOD[:, l, shift_idx * 8 + c, w:w + 1], scalar2=None, op0=ALU.add),
                                    reads=[R_tm[j], R_mod, R_hb[j]], writes=[R_tm[j]])
                        k.dma("sp", dst[b][:, :, t0:t0 + 256], hb[j][:], reads=[R_hb[j]], writes=[R_dst[b]])
                        if dst32 is not None:
                            k.dma("sp", dst32[b][:, :, t0:t0 + 256], tm[j][:], reads=[R_tm[j]], writes=[R_dst32[b]])
            k.barrier()

        def stage_proj(l):
            with ExitStack() as s1:
                w = sb("pw", [128, 8, D_IN], BF16, s1); R_w = Res()
                mu = sb("pmu", [128, 15], F32, s1); R_mu = Res()
                mu1 = sb("pmu1", [128, 15], F32, s1)
                muh = sb("pmuh", [128, 15], F32, s1)
                hb = [sb("ph%d" % i, [128, 8, 258], BF16, s1) for i in range(2)]; R_hb = [Res(), Res()]
                hs = [sb("phs%d" % i, [128, 8, 256], BF16, s1) for i in range(2)]; R_hs = [Res(), Res()]
                oq = [sb("poq%d" % i, [128, 8, 256], BF16, s1) for i in range(2)]; R_oq = [Res(), Res()]
                ov = [sb("pov%d" % i, [128, 2, 512], BF16, s1) for i in range(2)]; R_ov = [Res(), Res()]
                ou = [sb("pou%d" % i, [128, 15, 256], F32, s1) for i in range(2)]; R_ou = [Res(), Res()]
                t2 = [sb("pt2%d" % i, [128, 256], F32, s1) for i in range(2)]; R_t2 = [Res(), Res()]
                for kc in range(8):
                    k.dma("pool", w[:, kc, :], I["w_in"][l, :, kc, :], writes=[R_w])
                k.dma("sp", mu[:], I["mu"][l], writes=[R_mu])
                k.op("dve", lambda e: e.tensor_scalar(out=mu1[:], in0=mu[:], scalar1=-1.0, scalar2=1.0, op0=ALU.mult, op1=ALU.add), reads=[R_mu], writes=[R_mu])
                k.op("dve", lambda e: e.tensor_scalar(out=muh[:], in0=mu[:], scalar1=0.5, scalar2=None, op0=ALU.mult), reads=[R_mu], writes=[R_mu])
                n = 0
                for b in range(NB):
                    for t0 in range(0, S, 256):
                        j = n % 2; n += 1
                        seg0, seg1 = (0, LC) if t0 < LC else (LC, S)
                        lo, hi = max(t0 - 1, seg0), min(t0 + 257, seg1)
                        if lo == t0 or hi == t0 + 256:
                            k.op("dve", lambda e, j=j: e.memset(hb[j][:], 0.0), writes=[R_hb[j]])
                        k.dma("sp", hb[j][:, :, lo - (t0 - 1):hi - (t0 - 1)], HT[b][:, :, lo:hi], reads=[R_HT[b]], writes=[R_hb[j]])
                        k.op("dve", lambda e, j=j: e.tensor_tensor(out=hs[j][:], in0=hb[j][:, :, 0:256], in1=hb[j][:, :, 2:258], op=ALU.add),
                             reads=[R_hb[j]], writes=[R_hs[j]])
                        for cj in range(8):
                            pt, rp = ps_next()
                            for kc in range(8):
                                k.op("pe", lambda e, pt=pt, j=j, cj=cj, kc=kc: e.matmul(pt[:, 0:256], w[:, kc, cj * 128:(cj + 1) * 128], hb[j][:, kc, 1:257],
                                     start=(kc == 0), stop=(kc == 7)), reads=[R_w, R_hb[j]], writes=[rp])
                            k.op("act", lambda e, pt=pt, j=j, cj=cj: e.activation(out=oq[j][:, cj, :], in_=pt[:, 0:256], func=AF.Copy), reads=[rp], writes=[R_oq[j]])
                        k.dma("sp", QT[b][:, :, t0:t0 + 256], oq[j][:, 0:4, :], reads=[R_oq[j]], writes=[R_QK[b]])
                        k.dma("sp", KT[b][:, :, t0:t0 + 256], oq[j][:, 4:8, :], reads=[R_oq[j]], writes=[R_QK[b]])
                        for tt in range(2):
                            pt, rp = ps_next()
                            for kc in range(8):
                                k.op("pe", lambda e, pt=pt, j=j, tt=tt, kc=kc: e.matmul(pt[:, :], hb[j][:, kc, 1 + tt * 128:1 + (tt + 1) * 128], w[:, kc, 1024:1536],
                                     start=(kc == 0), stop=(kc == 7)), reads=[R_w, R_hb[j]], writes=[rp])
                            k.op("act", lambda e, pt=pt, j=j, tt=tt: e.activation(out=ov[j][:, tt, :], in_=pt[:, :], func=AF.Copy), reads=[rp], writes=[R_ov[j]])
                        k.dma("sp", VN[b][t0:t0 + 256, :].rearrange("(a p) c -> p a c", p=128), ov[j][:], reads=[R_ov[j]], writes=[R_VN[b]])
                        for cj in range(15):
                            c0 = 1536 + cj * 128
                            pa, ra = ps_next()
                            for kc in range(8):
                                k.op("pe", lambda e, pa=pa, j=j, c0=c0, kc=kc: e.matmul(pa[:, 0:256], w[:, kc, c0:c0 + 128], hb[j][:, kc, 1:257],
                                     start=(kc == 0), stop=(kc == 7)), reads=[R_w, R_hb[j]], writes=[ra])
                            pb, rb = ps_next()
                            for kc in range(8):
                                k.op("pe", lambda e, pb=pb, j=j, c0=c0, kc=kc: e.matmul(pb[:, 0:256], w[:, kc, c0:c0 + 128], hs[j][:, kc, :],
                                     start=(kc == 0), stop=(kc == 7)), reads=[R_w, R_hs[j]], writes=[rb])
                            k.op("act", lambda e, pb=pb, j=j, cj=cj: e.activation(out=t2[j][:], in_=pb[:, 0:256], func=AF.Copy, scale=muh[:, cj:cj + 1]),
                                 reads=[rb, R_mu], writes=[R_t2[j]])
                            k.op("dve", lambda e, pa=pa, j=j, cj=cj: e.scalar_tensor_tensor(out=ou[j][:, cj, :], in0=pa[:, 0:256], scalar=mu1[:, cj:cj + 1], in1=t2[j][:],
                                 op0=ALU.mult, op1=ALU.add), reads=[ra, R_mu, R_t2[j]], writes=[R_ou[j]])
                        k.dma("sp", UT[b][:, :, t0:t0 + 256], ou[j][:], reads=[R_ou[j]], writes=[R_UT[b]])
            k.barrier()

        def stage_na(l, need_ctx, hsel=range(8), bsel=range(NB)):
            NBUF = 3
            with ExitStack() as s1:
                bias = sb("na_bias", [128, 8, 960], F32, s1); R_bias = Res()
                k.dma("sp", bias[0:64], I["bfull"][l], writes=[R_bias])
                k.dma("sp", bias[64:128], I["bfull"][l], writes=[R_bias])
                qh = [sb("na_q%d" % i, [64, S], BF16, s1) for i in range(2)]
                kh = [sb("na_k%d" % i, [64, S], BF16, s1) for i in range(2)]
                VE = [sb("na_ve%d" % i, [128, 18, 64], BF16, s1) for i in range(2)]
                VO = [sb("na_vo%d" % i, [128, 18, 64], BF16, s1) for i in range(2)]
                R_in = [Res(), Res()]
                ytok = sb("na_ytok", [128, NT, 512], BF16, s1); R_ytok = Res()
                yfm = [sb("na_yfm%d" % i, [128, 4, 128], BF16, s1) for i in range(2)]; R_yfm = [Res(), Res()]
                ssb = [sb("na_s%d" % i, [128, 832], F32, s1) for i in range(NBUF)]; R_s = [Res() for _ in range(NBUF)]
                pbf = [sb("na_p%d" % i, [128, 832], BF16, s1) for i in range(NBUF)]; R_p = [Res() for _ in range(NBUF)]
                pT = [sb("na_pT%d" % i, [128, 896], BF16, s1) for i in range(NBUF)]; R_pT = [Res() for _ in range(NBUF)]
                st4 = [sb("na_st%d" % i, [128, 4], F32, s1) for i in range(NBUF)]; R_st = [Res() for _ in range(NBUF)]
                n = 0
                u = 0
                for b in bsel:
                    if not need_ctx:
                        pass
                    for h in hsel:
                        j = n % 2; n += 1
                        p0 = (h % 2) * 64
                        k.dma("sp", qh[j][:], QT[b][p0:p0 + 64, h // 2, :], reads=[R_QK[b]], writes=[R_in[j]])
                        k.dma("sp", kh[j][:], KT[b][p0:p0 + 64, h // 2, :], reads=[R_QK[b]], writes=[R_in[j]])
                        k.dma("sp", VE[j][:], VN[b][:, h * 64:(h + 1) * 64].rearrange("(a p) d -> p a d", p=128), reads=[R_VN[b]], writes=[R_in[j]])
                        k.dma("sp", VO[j][:, 0:17, :], VN[b][64:64 + 17 * 128, h * 64:(h + 1) * 64].rearrange("(a p) d -> p a d", p=128), reads=[R_VN[b]], writes=[R_in[j]])
                        k.dma("sp", VO[j][0:64, 17, :], VN[b][S - 64:S, h * 64:(h + 1) * 64], reads=[R_VN[b]], writes=[R_in[j]])
                        units = [("lat", r) for r in range(0, 32, 2)] + ([("ctx", 0), ("ctx", 1)] if need_ctx else [])
                        for kind, r in units:
                            i = u % NBUF; u += 1
                            b1, rb1 = ps_next()
                            if kind == "lat":
                                r0A = min(max(r - 4, 0), 24); r0B = min(max(r - 3, 0), 24)
                                R0 = min(r0A, 23)
                                tq = LC + r * 64; w0 = LC + R0 * 64
                                tile_i = tq // 128
                                b2, rb2 = ps_next()
                                k.op("pe", lambda e, b1=b1, j=j, tq=tq: e.matmul(b1[:, 0:256], qh[j][:, tq:tq + 128], kh[j][:, 0:256], start=True, stop=True), reads=[R_in[j]], writes=[rb1])
                                k.op("pe", lambda e, b1=b1, j=j, tq=tq, w0=w0: e.matmul(b1[:, 256:512], qh[j][:, tq:tq + 128], kh[j][:, w0:w0 + 256], start=True, stop=True), reads=[R_in[j]], writes=[rb1])
                                k.op("pe", lambda e, b2=b2, j=j, tq=tq, w0=w0: e.matmul(b2[:, 0:320], qh[j][:, tq:tq + 128], kh[j][:, w0 + 256:w0 + 576], start=True, stop=True), reads=[R_in[j]], writes=[rb2])
                                k.op("act", lambda e, b1=b1, i=i: e.activation(out=ssb[i][:, 0:256], in_=b1[:, 0:256], func=AF.Copy, scale=0.125), reads=[rb1], writes=[R_s[i]])
                                for half, (rq, r0X) in enumerate(((r, r0A), (r + 1, r0B))):
                                    sX = r0X - R0
                                    hp_ = slice(half * 64, (half + 1) * 64)
                                    bs0 = (r0X - rq + 7) * 64
                                    n1 = 256 - sX * 64
                                    k.op("dve", lambda e, b1=b1, i=i, h=h, hp_=hp_, sX=sX, bs0=bs0, n1=n1: e.scalar_tensor_tensor(
                                        out=ssb[i][hp_, 256 + sX * 64:512], in0=b1[hp_, 256 + sX * 64:512], scalar=0.125, in1=bias[hp_, h, bs0:bs0 + n1], op0=ALU.mult, op1=ALU.add),
                                        reads=[rb1, R_bias], writes=[R_s[i]])
                                    n2 = 512 - n1
                                    k.op("dve", lambda e, b2=b2, i=i, h=h, hp_=hp_, bs0=bs0, n1=n1, n2=n2: e.scalar_tensor_tensor(
                                        out=ssb[i][hp_, 512:512 + n2], in0=b2[hp_, 0:n2], scalar=0.125, in1=bias[hp_, h, bs0 + n1:bs0 + 512], op0=ALU.mult, op1=ALU.add),
                                        reads=[rb2, R_bias], writes=[R_s[i]])
                                    ng0 = 256 + (512 if sX == 0 else 0)
                                    k.op("pool", lambda e, i=i, hp_=hp_, ng0=ng0: e.memset(ssb[i][hp_, ng0:ng0 + 64], NEG), writes=[R_s[i]])
                                W = 832
                            else:
                                tile_i = r
                                k.op("pe", lambda e, b1=b1, j=j, r=r: e.matmul(b1[:, 0:256], qh[j][:, r * 128:(r + 1) * 128], kh[j][:, 0:256], start=True, stop=True), reads=[R_in[j]], writes=[rb1])
                                k.op("act", lambda e, b1=b1, i=i: e.activation(out=ssb[i][:, 0:256], in_=b1[:, 0:256], func=AF.Copy, scale=0.125), reads=[rb1], writes=[R_s[i]])
                                W = 256
                            k.op("dve", lambda e, i=i, W=W: e.tensor_reduce(out=st4[i][:, 0:1], in_=ssb[i][:, 0:W], axis=AX.X, op=ALU.max), reads=[R_s[i]], writes=[R_st[i]])
                            k.op("dve", lambda e, i=i: e.tensor_scalar(out=st4[i][:, 1:2], in0=st4[i][:, 0:1], scalar1=-1.0, scalar2=None, op0=ALU.mult), reads=[R_st[i]], writes=[R_st[i]])
                            k.op("act", lambda e, i=i, W=W: e.activation(out=pbf[i][:, 0:W], in_=ssb[i][:, 0:W], func=AF.Exp, bias=st4[i][:, 1:2], scale=1.0, accum_out=st4[i][:, 2:3]),
                                 reads=[R_s[i], R_st[i]], writes=[R_p[i], R_st[i]])
                            k.op("dve", lambda e, i=i: e.reciprocal(out=st4[i][:, 3:4], in_=st4[i][:, 2:3]), reads=[R_st[i]], writes=[R_st[i]])
                            pc, rc = ps_next()
                            pcb = pc[:].bitcast(BF16)
                            nch = (W + 127) // 128
                            for c in range(nch):
                                kw = min(128, W - c * 128)
                                k.op("pe", lambda e, pcb=pcb, i=i, c=c, kw=kw: e.transpose(pcb[0:kw, c * 128:(c + 1) * 128], pbf[i][:, c * 128:c * 128 + kw], ident_bf[:, :]),
                                     reads=[R_p[i], R_id], writes=[rc])
                            k.op("dve" if kind == "lat" else "act",
                                 (lambda e, pcb=pcb, i=i, nch=nch: e.tensor_copy(out=pT[i][:, 0:nch * 128], in_=pcb[:, 0:nch * 128])) if kind == "lat" else
                                 (lambda e, pcb=pcb, i=i, nch=nch: e.activation(out=pT[i][:, 0:nch * 128], in_=pcb[:, 0:nch * 128], func=AF.Copy)),
                                 reads=[rc], writes=[R_pT[i]])
                            pd, rd = ps_next()
                            for c in range(nch):
                                kw = min(128, W - c * 128)
                                if c < 2:
                                    vt = VE[j][:, c, :]
                                else:
                                    even = (R0 % 2 == 0)
                                    base_t = (w0 // 128) if even else ((w0 - 64) // 128)
                                    vt = (VE[j] if even else VO[j])[0:kw, base_t + (c - 2), :]
                                k.op("pe", lambda e, pd=pd, i=i, c=c, kw=kw, vt=vt, nch=nch: e.matmul(pd[:, 0:64], pT[i][0:kw, c * 128:(c + 1) * 128], vt, start=(c == 0), stop=(c == nch - 1)),
                                     reads=[R_in[j], R_pT[i]], writes=[rd])
                            k.op("act", lambda e, pd=pd, i=i, tile_i=tile_i, h=h: e.activation(out=ytok[:, tile_i, h * 64:(h + 1) * 64], in_=pd[:, 0:64], func=AF.Copy, scale=st4[i][:, 3:4]),
                                 reads=[rd, R_st[i]], writes=[R_ytok])
                    for tt in range(0 if need_ctx else 2, NT):
                        jj = tt % 2
                        pt, rp = ps_next()
                        ptb = pt[:].bitcast(BF16)
                        for q in range(4):
                            k.op("pe", lambda e, ptb=ptb, q=q, tt=tt: e.transpose(ptb[:, q * 128:(q + 1) * 128], ytok[:, tt, q * 128:(q + 1) * 128], ident_bf[:, :]), reads=[R_ytok, R_id], writes=[rp])
                        k.op("act", lambda e, ptb=ptb, jj=jj: e.activation(out=yfm[jj][:].rearrange("p q t -> p (q t)"), in_=ptb[:, 0:512], func=AF.Copy), reads=[rp], writes=[R_yfm[jj]])
                        k.dma("sp", YT[b][:, 4:8, tt * 128:(tt + 1) * 128], yfm[jj][:], reads=[R_yfm[jj]], writes=[R_YT[b]])
            k.barrier()

        def stage_wout(l, last):
            with ExitStack() as s1:
                w = sb("wo_w", [128, 8, D], BF16, s1); R_w = Res()
                for kc in range(8):
                    k.dma("pool", w[:, kc, :], I["w_out"][l, :, kc, :], writes=[R_w])
                yb = [sb("wo_y%d" % i, [128, 8, 256], BF16, s1) for i in range(2)]; R_yb = [Res(), Res()]
                xb = [sb("wo_x%d" % i, [128, 8, 256], F32, s1) for i in range(2)]; R_xb = [Res(), Res()]
                n = 0
                for b in range(NB):
                    for t0 in range(LC if last else 0, S, 256):
                        j = n % 2; n += 1
                        wq = which_of(b, t0)
                        k.dma("sp", yb[j][:], YT[b][:, :, t0:t0 + 256], reads=[R_YT[b]], writes=[R_yb[j]])
                        k.dma("sp", xb[j][:], XT[b][:, :, t0:t0 + 256], reads=[R_XT[b]], writes=[R_xb[j]])
                        for oc in range(8):
                            pt, rp = ps_next()
                            for kc in range(8):
                                k.op("pe", lambda e, pt=pt, j=j, oc=oc, kc=kc: e.matmul(pt[:, 0:256], w[:, kc, oc * 128:(oc + 1) * 128], yb[j][:, kc, :],
                                     start=(kc == 0), stop=(kc == 7)), reads=[R_w, R_yb[j]], writes=[rp])
                            k.op("dve", lambda e, pt=pt, j=j, oc=oc, wq=wq: e.scalar_tensor_tensor(out=xb[j][:, oc, :], in0=pt[:, 0:256],
                                 scalar=MOD[:, l, 16 + oc, wq:wq + 1], in1=xb[j][:, oc, :], op0=ALU.mult, op1=ALU.add), reads=[rp, R_mod, R_xb[j]], writes=[R_xb[j]])
                        k.dma("sp", XT[b][:, :, t0:t0 + 256], xb[j][:], reads=[R_xb[j]], writes=[R_XT[b]])
            k.barrier()

        def stage_router(l, last):
            i_moe = l // 2
            with ExitStack() as s1:
                rw = sb("rt_w", [128, 8, NE], F32, s1); R_rw = Res()
                k.dma("sp", rw[:], I["router"][i_moe], writes=[R_rw])
                hb = [sb("rt_h%d" % i, [128, 8, 128], F32, s1) for i in range(2)]; R_hb = [Res(), Res()]
                lg = [sb("rt_l%d" % i, [128, 40], F32, s1) for i in range(2)]; R_lg = [Res(), Res()]
                ge = [sb("rt_ge%d" % i, [128, 128], F32, s1) for i in range(2)]; R_ge = [Res(), Res()]
                gb = [sb("rt_gb%d" % i, [128, NE, 128], F32, s1) for i in range(2)]; R_gb = [Res(), Res()]
                n = 0; m = 0
                for b in range(NB):
                    for tt in range(2 if last else 0, NT):
                        j = n % 2; n += 1
                        t0 = tt * 128
                        k.dma("sp", hb[j][:], H2F[b][:, :, t0:t0 + 128], reads=[R_H2F[b]], writes=[R_hb[j]])
                        pt, rp = ps_next()
                        for kc in range(8):
                            k.op("pe", lambda e, pt=pt, j=j, kc=kc: e.matmul(pt[:, 0:NE], hb[j][:, kc, :], rw[:, kc, :], start=(kc == 0), stop=(kc == 7)),
                                 reads=[R_hb[j], R_rw], writes=[rp])
                        L = lg[j]
                        k.op("dve", lambda e, pt=pt, L=L: e.tensor_copy(out=L[:, 0:8], in_=pt[:, 0:8]), reads=[rp], writes=[R_lg[j]])
                        k.op("dve", lambda e, L=L: e.tensor_reduce(out=L[:, 8:9], in_=L[:, 0:8], axis=AX.X, op=ALU.max), reads=[R_lg[j]], writes=[R_lg[j]])
                        k.op("dve", lambda e, L=L: e.tensor_scalar(out=L[:, 9:17], in0=L[:, 0:8], scalar1=L[:, 8:9], scalar2=None, op0=ALU.is_ge), reads=[R_lg[j]], writes=[R_lg[j]])
                        k.op("dve", lambda e, L=L: e.scalar_tensor_tensor(out=L[:, 17:25], in0=L[:, 9:17], scalar=-1e30, in1=L[:, 0:8], op0=ALU.mult, op1=ALU.add), reads=[R_lg[j]], writes=[R_lg[j]])
                        k.op("dve", lambda e, L=L: e.tensor_reduce(out=L[:, 25:26], in_=L[:, 17:25], axis=AX.X, op=ALU.max), reads=[R_lg[j]], writes=[R_lg[j]])
                        k.op("dve", lambda e, L=L: e.tensor_scalar(out=L[:, 26:34], in0=L[:, 17:25], scalar1=L[:, 25:26], scalar2=None, op0=ALU.is_ge), reads=[R_lg[j]], writes=[R_lg[j]])
                        k.op("dve", lambda e, L=L: e.tensor_tensor(out=L[:, 34:35], in0=L[:, 8:9], in1=L[:, 25:26], op=ALU.subtract), reads=[R_lg[j]], writes=[R_lg[j]])
                        k.op("act", lambda e, L=L: e.activation(out=L[:, 35:36], in_=L[:, 34:35], func=AF.Sigmoid), reads=[R_lg[j]], writes=[R_lg[j]])
                        k.op("dve", lambda e, L=L: e.tensor_scalar(out=L[:, 36:37], in0=L[:, 35:36], scalar1=-1.0, scalar2=1.0, op0=ALU.mult, op1=ALU.add), reads=[R_lg[j]], writes=[R_lg[j]])
                        k.op("dve", lambda e, L=L: e.tensor_scalar(out=L[:, 9:17], in0=L[:, 9:17], scalar1=L[:, 35:36], scalar2=None, op0=ALU.mult), reads=[R_lg[j]], writes=[R_lg[j]])
                        k.op("dve", lambda e, L=L: e.scalar_tensor_tensor(out=L[:, 9:17], in0=L[:, 26:34], scalar=L[:, 36:37], in1=L[:, 9:17], op0=ALU.mult, op1=ALU.add), reads=[R_lg[j]], writes=[R_lg[j]])
                        for ex in range(NE):
                            i2 = m % 2; m += 1
                            k.op("pool", lambda e, L=L, i2=i2, ex=ex: e.tensor_copy(out=ge[i2][:], in_=L[:, 9 + ex:10 + ex].to_broadcast([128, 128])), reads=[R_lg[j]], writes=[R_ge[i2]])
                            pg, rg = ps_next()
                            k.op("pe", lambda e, pg=pg, i2=i2: e.matmul(pg[:, 0:128], ge[i2][:], ident[:], start=True, stop=True), reads=[R_ge[i2], R_id], writes=[rg])
                            k.op("act", lambda e, pg=pg, j=j, ex=ex: e.activation(out=gb[j][:, ex, :], in_=pg[:, 0:128], func=AF.Copy), reads=[rg], writes=[R_gb[j]])
                        k.dma("sp", GB[b][:, :, t0:t0 + 128], gb[j][:], reads=[R_gb[j]], writes=[R_GB[b]])
            k.barrier()

        def stage_ffn(l, last):
            moe = (l % 2 == 1)
            i_w = l // 2
            F = DFE if moe else DFF
            GW = 512 if moe else 256
            ng = F // GW
            nfc = GW // 128
            tstart = LC if last else 0
            with ExitStack() as s1:
                hT = sb("ff_h", [128, 8, S], BF16, s1); R_h = Res()
                yacc = sb("ff_y", [128, 8, S], F32, s1); R_ya = [Res() for _ in range(9)]
                w1 = [sb("ff_w1%d" % i, [128, 8, GW], BF16, s1) for i in range(2)]
                w3 = [sb("ff_w3%d" % i, [128, 8, GW], BF16, s1) for i in range(2)]
                w2 = [sb("ff_w2%d" % i, [128, nfc, D], BF16, s1) for i in range(2)]
                R_wg = [Res(), Res()]
                sg = [sb("ff_s%d" % i, [128, 256], F32, s1) for i in range(2)]; R_sg = [Res(), Res()]
                ac = [sb("ff_a%d" % i, [128, nfc, 256], BF16, s1) for i in range(2)]; R_ac = [Res(), Res()]
                gt = [sb("ff_g%d" % i, [128, 256], F32, s1) for i in range(2)]; R_gt = [Res(), Res()]
                xb = [sb("ff_x%d" % i, [128, 8, 256], F32, s1) for i in range(2)]; R_xb = [Res(), Res()]
                nw = 0; na_ = 0; ns = 0; ngt = 0
                for b in range(NB):
                    k.dma("sp", hT[:, :, tstart:S], H2T[b][:, :, tstart:S], reads=[R_H2T[b]], writes=[R_h])
                    first = True
                    for ex in range(NE if moe else 1):
                        for g in range(ng):
                            jw = nw % 2; nw += 1
                            if moe:
                                s_w1, s_w3, s_w2 = I["moe_w1"][i_w, ex], I["moe_w3"][i_w, ex], I["moe_w2"][i_w, ex]
                            else:
                                s_w1, s_w3, s_w2 = I["ffn_w1"][i_w], I["ffn_w3"][i_w], I["ffn_w2"][i_w]
                            k.dma("pool", w1[jw][:], s_w1[:, :, g * GW:(g + 1) * GW], writes=[R_wg[jw]])
                            k.dma("pool", w3[jw][:], s_w3[:, :, g * GW:(g + 1) * GW], writes=[R_wg[jw]])
                            k.dma("pool", w2[jw][:], s_w2[:, g * nfc:(g + 1) * nfc, :], writes=[R_wg[jw]])
                            for tb in range(tstart // 256, S // 256):
                                t0 = tb * 256
                                ja = na_ % 2; na_ += 1
                                if moe:
                                    jg = ngt % 2; ngt += 1
                                    k.dma("sp", gt[jg][:], GB[b][:, ex, t0:t0 + 256], reads=[R_GB[b]], writes=[R_gt[jg]])
                                for fc in range(nfc):
                                    p1, r1 = ps_next()
                                    for kc in range(8):
                                        k.op("pe", lambda e, p1=p1, jw=jw, fc=fc, kc=kc, t0=t0: e.matmul(p1[:, 0:256], w1[jw][:, kc, fc * 128:(fc + 1) * 128], hT[:, kc, t0:t0 + 256],
                                             start=(kc == 0), stop=(kc == 7)), reads=[R_wg[jw], R_h], writes=[r1])
                                    p3, r3 = ps_next()
                                    for kc in range(8):
                                        k.op("pe", lambda e, p3=p3, jw=jw, fc=fc, kc=kc, t0=t0: e.matmul(p3[:, 0:256], w3[jw][:, kc, fc * 128:(fc + 1) * 128], hT[:, kc, t0:t0 + 256],
                                             start=(kc == 0), stop=(kc == 7)), reads=[R_wg[jw], R_h], writes=[r3])
                                    js = ns % 2; ns += 1
                                    k.op("act", lambda e, p1=p1, js=js: e.activation(out=sg[js][:], in_=p1[:, 0:256], func=AF.Silu), reads=[r1], writes=[R_sg[js]])
                                    if moe:
                                        k.op("pool", lambda e, js=js, jg=jg: e.tensor_tensor(out=sg[js][:], in0=sg[js][:], in1=gt[jg][:], op=ALU.mult), reads=[R_sg[js], R_gt[jg]], writes=[R_sg[js]])
                                    k.op("dve", lambda e, p3=p3, js=js, ja=ja, fc=fc: e.tensor_tensor(out=ac[ja][:, fc, :], in0=sg[js][:], in1=p3[:, 0:256], op=ALU.mult),
                                         reads=[R_sg[js], r3], writes=[R_ac[ja]])
                                for oc in range(8):
                                    po, ro = ps_next()
                                    for fc in range(nfc):
                                        k.op("pe", lambda e, po=po, jw=jw, ja=ja, fc=fc, oc=oc: e.matmul(po[:, 0:256], w2[jw][:, fc, oc * 128:(oc + 1) * 128], ac[ja][:, fc, :],
                                             start=(fc == 0), stop=(fc == nfc - 1)), reads=[R_wg[jw], R_ac[ja]], writes=[ro])
                                    if first:
                                        k.op("act", lambda e, po=po, oc=oc, t0=t0: e.activation(out=yacc[:, oc, t0:t0 + 256], in_=po[:, 0:256], func=AF.Copy), reads=[ro], writes=[R_ya[tb]])
                                    else:
                                        k.op("dve", lambda e, po=po, oc=oc, t0=t0: e.tensor_tensor(out=yacc[:, oc, t0:t0 + 256], in0=yacc[:, oc, t0:t0 + 256], in1=po[:, 0:256], op=ALU.add),
                                             reads=[ro, R_ya[tb]], writes=[R_ya[tb]])
                            first = False
                    for tb in range(tstart // 256, S // 256):
                        t0 = tb * 256
                        j = tb % 2
                        wq = which_of(b, t0)
                        k.dma("sp", xb[j][:], XT[b][:, :, t0:t0 + 256], reads=[R_XT[b]], writes=[R_xb[j]])
                        for oc in range(8):
                            k.op("dve", lambda e, j=j, oc=oc, t0=t0, wq=wq: e.scalar_tensor_tensor(out=xb[j][:, oc, :], in0=yacc[:, oc, t0:t0 + 256],
                                 scalar=MOD[:, l, 40 + oc, wq:wq + 1], in1=xb[j][:, oc, :], op0=ALU.mult, op1=ALU.add), reads=[R_ya[tb], R_mod, R_xb[j]], writes=[R_xb[j]])
                        k.dma("sp", XT[b][:, :, t0:t0 + 256], xb[j][:], reads=[R_xb[j]], writes=[R_XT[b]])
            k.barrier()

        def stage_final():
            with ExitStack() as s1:
                fg = sb("fn_g", [128, 8], F32, s1); R_fg = Res()
                k.dma("sp", fg[:], I["final_g"][:, :], writes=[R_fg])
                xb = [sb("fx%d" % i, [128, 8, 256], F32, s1) for i in range(2)]; R_xb = [Res(), Res()]
                sq = [sb("fsq%d" % i, [128, 8, 256], F32, s1) for i in range(2)]; R_sq = [Res(), Res()]
                rs = [sb("frs%d" % i, [128, 256], F32, s1) for i in range(2)]; R_rs = [Res(), Res()]
                ob = [sb("fo%d" % i, [128, 8, 256], F32, s1) for i in range(2)]; R_ob = [Res(), Res()]
                n = 0
                outs = []
                for b in range(NB):
                    for t0 in range(LC, S, 256):
                        j = n % 2; n += 1
                        k.dma("sp", xb[j][:], XT[b][:, :, t0:t0 + 256], reads=[R_XT[b]], writes=[R_xb[j]])
                        k.op("act", lambda e, j=j: e.activation(out=sq[j][:], in_=xb[j][:], func=AF.Square), reads=[R_xb[j]], writes=[R_sq[j]])
                        pt, rp = ps_next()
                        for c in range(8):
                            k.op("pe", lambda e, pt=pt, j=j, c=c: e.matmul(pt[:, 0:256], ones[:], sq[j][:, c, :], start=(c == 0), stop=(c == 7)), reads=[R_ones, R_sq[j]], writes=[rp])
                        k.op("act", lambda e, pt=pt, j=j: e.activation(out=rs[j][:], in_=pt[:, 0:256], func=AF.Sqrt, bias=eps_t[:, 0:1], scale=1.0 / D), reads=[rp, R_eps], writes=[R_rs[j]])
                        k.op("dve", lambda e, j=j: e.reciprocal(out=rs[j][:], in_=rs[j][:]), reads=[R_rs[j]], writes=[R_rs[j]])
                        for c in range(8):
                            k.op("dve", lambda e, j=j, c=c: e.scalar_tensor_tensor(out=ob[j][:, c, :], in0=xb[j][:, c, :], scalar=fg[:, c:c + 1], in1=rs[j][:],
                                 op0=ALU.mult, op1=ALU.mult), reads=[R_xb[j], R_fg, R_rs[j]], writes=[R_ob[j]])
                        outs.append(k.dma("sp", out[b, :, :, t0 - LC:t0 - LC + 256], ob[j][:], reads=[R_ob[j]]))
                return outs

        def stage_rwkv(l, need_ctx):
            with ExitStack() as s1:
                def T_(name, shape, dt=F32, n=2):
                    return [sb(name + str(i), shape, dt, s1) for i in range(n)], [Res() for _ in range(n)]
                w2 = sb("rw_w2", [128, 512], F32, s1); a2 = sb("rw_a2", [128, 512], F32, s1); g2 = sb("rw_g2", [128, 512], F32, s1)
                w0b = sb("rw_w0b", [64, 2, 512], F32, s1); a0T = sb("rw_a0T", [128, 2, 4], F32, s1)
                kkp = sb("rw_kkp", [128, 4], F32, s1); kap = sb("rw_kap", [128, 4], F32, s1); okap = sb("rw_okap", [128, 4], F32, s1)
                rkp = sb("rw_rkp", [128, 4], F32, s1)
                lnw = sb("rw_lnw", [128, 512], F32, s1); lnb = sb("rw_lnb", [128, 512], F32, s1)
                m2 = sb("rw_m2", [64, 2, 128], F32, s1); m3 = sb("rw_m3", [64, 2, 128], F32, s1); mL = sb("rw_mL", [64, 2, 64], F32, s1)
                bones = sb("rw_bones", [128, 128], F32, s1); sel = sb("rw_sel", [128, 2], F32, s1)
                c1 = sb("rw_c1", [128, 4], F32, s1)
                R_par = Res()
                for dst, src in ((w2, I["rw_w2"][l]), (a2, I["rw_a2"][l]), (g2, I["rw_g2"][l]), (a0T, I["rw_a0T"][l]),
                                 (kkp, I["rw_kk"][l]), (kap, I["rw_ka"][l]), (rkp, I["rw_rk"][l]), (lnw, I["rw_lnw"][l]), (lnb, I["rw_lnb"][l]),
                                 (m2, I["rw_m2"]), (m3, I["rw_m3"]), (mL, I["rw_mL"]), (bones, I["rw_bones"]), (sel, I["rw_sel"]), (c1, I["rw_c1"]),
                                 (w0b, I["rw_w0b"][l])):
                    k.dma("sp", dst[:], src, writes=[R_par])
                k.op("dve", lambda e: e.tensor_scalar(out=okap[:], in0=kap[:], scalar1=-1.0, scalar2=1.0, op0=ALU.mult, op1=ALU.add), reads=[R_par], writes=[R_par])
                Hst = sb("rw_H", [128, 4, 64], F32, s1); R_H = Res()
                ub, R_ub = T_("rw_u", [128, 15, 64])
                twl, R_twl = T_("rw_twl", [128, 64])
                sgl, R_sgl = T_("rw_sgl", [128, 64])
                e2a, R_e2a = T_("rw_e2a", [64, 512])
                e2b, R_e2b = T_("rw_e2b", [64, 512])
                a_sb, R_a = T_("rw_a", [128, 4, 64])
                a1_sb, R_a1 = T_("rw_a1", [128, 4, 64])
                kr, R_kr = T_("rw_kr", [128, 4, 64])
                sq, R_sq = T_("rw_sq", [128, 4, 64])
                kk_t, R_kk = T_("rw_kkt", [128, 4, 64])
                ff, R_ff = T_("rw_ff", [128, 4, 64])
                kd, R_kd = T_("rw_kd", [128, 4, 64])
                bb, R_bb = T_("rw_bb", [128, 4, 64])
                EG, R_EG = T_("rw_EG", [128, 4, 2, 64])
                IEG, R_IEG = T_("rw_IEG", [128, 4, 64])
                fm, R_fm = T_("rw_fm", [128, 4, 4, 64])
                tm, R_tm = T_("rw_tm", [64, 4, 4, 128])
                LA, R_LA = T_("rw_LA", [64, 8, 128])
                NBt, R_NB = T_("rw_NB", [64, 8, 128])
                Lm, R_Lm = T_("rw_Lm", [64, 8, 64])
                Pm, R_Pm = T_("rw_Pm", [64, 8, 64])
                Nl, R_Nl = T_("rw_Nl", [64, 8, 64], n=4)
                Ll, R_Ll = T_("rw_Ll", [64, 8, 64], n=4)
                WT, R_WT = T_("rw_WT", [128, 4, 64])
                Xa, R_Xa = T_("rw_Xa", [64, 8, 64])
                Uta, R_Uta = T_("rw_Uta", [64, 8, 64])
                Ua, R_Ua = T_("rw_Ua", [64, 8, 64])
                ysb, R_ysb = T_("rw_ysb", [64, 512])
                htmp, R_htmp = T_("rw_htmp", [128, 4, 64])
                rk_t, R_rk = T_("rw_rkt", [128, 4, 64])
                rkh, R_rkh = T_("rw_rkh", [64, 8])
                bon, R_bon = T_("rw_bon", [64, 512])
                gsb, R_gsb = T_("rw_gsb", [64, 512])
                n = 0
                nl4 = [0]
                for b in range(NB if RW_DBG["nb"] is None else RW_DBG["nb"]):
                    for d in range(RW_DBG["nd"]):
                        k.op("pool", lambda e: e.memset(Hst[:], 0.0), writes=[R_H])
                        order = list(range(36)) if d == 0 else [3, 2, 1, 0] + list(range(35, 3, -1))
                        if RW_DBG["nch"] is not None:
                            order = order[:RW_DBG["nch"]]
                        last_col = 63 if d == 0 else 0
                        for ch in order:
                            if (not need_ctx) and False:
                                pass
                            j = n % 2; n += 1
                            t0 = ch * 64
                            U_ = ub[j]
                            k.dma("sp", U_[:], UT[b][:, :, t0:t0 + 64], reads=[R_UT[b]], writes=[R_ub[j]])
                            k.op("act", lambda e, j=j, U_=U_: e.activation(out=twl[j][:], in_=U_[:, 12, :], func=AF.Tanh), reads=[R_ub[j]], writes=[R_twl[j]])
                            pt, rp = ps_next()
                            k.op("pe", lambda e, pt=pt, j=j, d=d: e.matmul(pt[0:64, :], twl[j][d * 64:(d + 1) * 64, :], w2[d * 64:(d + 1) * 64, :], start=True, stop=True),
                                 reads=[R_twl[j], R_par], writes=[rp])
                            k.op("dve", lambda e, pt=pt, j=j, d=d: e.tensor_tensor(out=e2a[j][:], in0=pt[0:64, :], in1=w0b[:, d, :], op=ALU.add), reads=[rp, R_par], writes=[R_e2a[j]])
                            k.op("act", lambda e, j=j: e.activation(out=e2b[j][:], in_=e2a[j][:], func=AF.Exp, scale=-1.0), reads=[R_e2a[j]], writes=[R_e2b[j]])
                            k.op("act", lambda e, j=j: e.activation(out=e2a[j][:], in_=e2b[j][:], func=AF.Ln, bias=c1[0:64, 0:1], scale=1.0), reads=[R_e2b[j], R_par], writes=[R_e2a[j]])
                            k.op("act", lambda e, j=j: e.activation(out=e2b[j][:], in_=e2a[j][:], func=AF.Exp, bias=c1[0:64, 1:2], scale=-1.0), reads=[R_e2a[j], R_par], writes=[R_e2b[j]])
                            def a_path(dd, dst, R_dst):
                                pa, ra = ps_next()
                                for q in range(4):
                                    k.op("pe", lambda e, pa=pa, q=q, dd=dd, U_=U_: e.matmul(pa[:, q * 64:(q + 1) * 64], a2[dd * 64:(dd + 1) * 64, q * 128:(q + 1) * 128], U_[dd * 64:(dd + 1) * 64, 13, :], start=True, stop=True),
                                         reads=[R_par, R_ub[j]], writes=[ra])
                                for q in range(4):
                                    k.op("act", lambda e, pa=pa, q=q, dd=dd, dst=dst: e.activation(out=dst[:, q, :], in_=pa[:, q * 64:(q + 1) * 64], func=AF.Sigmoid, bias=a0T[:, dd, q:q + 1], scale=1.0),
                                         reads=[ra, R_par], writes=[R_dst])
                            a_path(d, a_sb[j], R_a[j])
                            k.op("dve", lambda e, j=j, U_=U_: e.tensor_tensor(out=kr[j][:], in0=U_[:, 4:8, :], in1=kkp[:, :].unsqueeze(2).to_broadcast([128, 4, 64]), op=ALU.mult), reads=[R_ub[j], R_par], writes=[R_kr[j]])
                            k.op("pool", lambda e, j=j: e.tensor_tensor(out=sq[j][:], in0=kr[j][:], in1=kr[j][:], op=ALU.mult), reads=[R_kr[j]], writes=[R_sq[j]])
                            pn_, rn = ps_next()
                            k.op("pe", lambda e, pn_=pn_, j=j: e.matmul(pn_[:, 0:256], bones[:], sq[j][:].rearrange("p q t -> p (q t)"), start=True, stop=True), reads=[R_par, R_sq[j]], writes=[rn])
                            k.op("act", lambda e, pn_=pn_, j=j: e.activation(out=sq[j][:].rearrange("p q t -> p (q t)"), in_=pn_[:, 0:256], func=AF.Sqrt), reads=[rn], writes=[R_sq[j]])
                            k.op("dve", lambda e, j=j: e.tensor_scalar(out=sq[j][:], in0=sq[j][:], scalar1=1e-12, scalar2=None, op0=ALU.max), reads=[R_sq[j]], writes=[R_sq[j]])
                            k.op("dve", lambda e, j=j: e.reciprocal(out=sq[j][:], in_=sq[j][:]), reads=[R_sq[j]], writes=[R_sq[j]])
                            k.op("dve", lambda e, j=j: e.tensor_tensor(out=kk_t[j][:], in0=kr[j][:], in1=sq[j][:], op=ALU.mult), reads=[R_kr[j], R_sq[j]], writes=[R_kk[j]])
                            k.op("pool", lambda e, j=j: e.tensor_tensor(out=ff[j][:], in0=a_sb[j][:], in1=kap[:, :].unsqueeze(2).to_broadcast([128, 4, 64]), op=ALU.mult), reads=[R_a[j], R_par], writes=[R_ff[j]])
                            k.op("pool", lambda e, j=j: e.tensor_tensor(out=ff[j][:], in0=ff[j][:], in1=okap[:, :].unsqueeze(2).to_broadcast([128, 4, 64]), op=ALU.add), reads=[R_ff[j], R_par], writes=[R_ff[j]])
                            k.op("pool", lambda e, j=j, U_=U_: e.tensor_tensor(out=kd[j][:], in0=U_[:, 4:8, :], in1=ff[j][:], op=ALU.mult), reads=[R_ub[j], R_ff[j]], writes=[R_kd[j]])
                            k.op("pool", lambda e, j=j: e.tensor_tensor(out=bb[j][:], in0=kk_t[j][:], in1=a_sb[j][:], op=ALU.mult), reads=[R_kk[j], R_a[j]], writes=[R_bb[j]])
                            pc, rc = ps_next()
                            for q in range(4):
                                k.op("pe", lambda e, pc=pc, q=q, j=j, d=d: e.matmul(pc[:, q * 128:(q + 1) * 128], e2b[j][:, q * 128:(q + 1) * 128], m2[:, d, :], start=True, stop=True),
                                     reads=[R_e2b[j], R_par], writes=[rc])
                            k.op("act", lambda e, pc=pc, j=j: e.activation(out=EG[j][:].rearrange("p q s t -> p (q s t)"), in_=pc[:, :], func=AF.Exp, scale=-1.0), reads=[rc], writes=[R_EG[j]])
                            k.op("act", lambda e, pc=pc, j=j: e.activation(out=IEG[j][:], in_=pc[:, :].rearrange("p (q s t) -> p q s t", q=4, s=2)[:, :, 1, :], func=AF.Exp, scale=1.0), reads=[rc], writes=[R_IEG[j]])
                            F_ = fm[j]
                            k.op("dve", lambda e, j=j, F_=F_: e.tensor_tensor(out=F_[:, :, 0, :], in0=kd[j][:], in1=IEG[j][:], op=ALU.mult), reads=[R_kd[j], R_IEG[j]], writes=[R_fm[j]])
                            k.op("dve", lambda e, j=j, F_=F_: e.tensor_tensor(out=F_[:, :, 1, :], in0=bb[j][:], in1=IEG[j][:], op=ALU.mult), reads=[R_bb[j], R_IEG[j]], writes=[R_fm[j]])
                            k.op("pool", lambda e, j=j, F_=F_: e.tensor_tensor(out=F_[:, :, 2, :], in0=kk_t[j][:], in1=EG[j][:, :, 0, :], op=ALU.mult), reads=[R_kk[j], R_EG[j]], writes=[R_fm[j]])
                            k.op("pool", lambda e, j=j, F_=F_, U_=U_: e.tensor_tensor(out=F_[:, :, 3, :], in0=U_[:, 0:4, :], in1=EG[j][:, :, 1, :], op=ALU.mult), reads=[R_ub[j], R_EG[j]], writes=[R_fm[j]])
                            T_m = tm[j]
                            for kind, (srcf, sc) in enumerate(((lambda q, F_=F_: F_[:, q, 2, :], 1.0), (lambda q, F_=F_: F_[:, q, 0, :], 1.0), (lambda q, F_=F_: F_[:, q, 1, :], -1.0), (lambda q, U_=U_: U_[:, 8 + q, :], 1.0))):
                                ptx, rtx = ps_next()
                                for q in range(4):
                                    k.op("pe", lambda e, ptx=ptx, q=q, srcf=srcf: e.transpose(ptx[0:64, q * 128:(q + 1) * 128], srcf(q), ident[:, :]),
                                         reads=[R_fm[j], R_ub[j], R_id], writes=[rtx])
                                eng = "act" if kind % 2 == 0 else "dve"
                                if eng == "act":
                                    k.op("act", lambda e, ptx=ptx, kind=kind, sc=sc, T_m=T_m: e.activation(out=T_m[:, :, kind, :], in_=ptx[0:64, :].rearrange("p (q c) -> p q c", q=4), func=AF.Copy, scale=sc),
                                         reads=[rtx], writes=[R_tm[j]])
                                else:
                                    k.op("dve", lambda e, ptx=ptx, kind=kind, sc=sc, T_m=T_m: e.tensor_scalar(out=T_m[:, :, kind, :], in0=ptx[0:64, :].rearrange("p (q c) -> p q c", q=4), scalar1=sc, scalar2=None, op0=ALU.mult),
                                         reads=[rtx], writes=[R_tm[j]])
                            if RW_DBG["upto"] < "A2":
                                continue
                            if d == 0 and (need_ctx or ch >= 4):
                                a_path(1, a1_sb[j], R_a1[j])
                                k.op("dve", lambda e, j=j: e.tensor_tensor(out=a1_sb[j][:], in0=a1_sb[j][:], in1=a_sb[j][:], op=ALU.add), reads=[R_a1[j], R_a[j]], writes=[R_a1[j]])
                                k.op("dve", lambda e, j=j: e.scalar_tensor_tensor(out=a1_sb[j][:], in0=a1_sb[j][:], scalar=0.5, in1=kap[:, :].unsqueeze(2).to_broadcast([128, 4, 64]), op0=ALU.mult, op1=ALU.mult), reads=[R_a1[j], R_par], writes=[R_a1[j]])
                                k.op("dve", lambda e, j=j: e.tensor_tensor(out=a1_sb[j][:], in0=a1_sb[j][:], in1=okap[:, :].unsqueeze(2).to_broadcast([128, 4, 64]), op=ALU.add), reads=[R_a1[j], R_par], writes=[R_a1[j]])
                                k.op("dve", lambda e, j=j, U_=U_: e.tensor_tensor(out=rk_t[j][:], in0=a1_sb[j][:], in1=U_[:, 4:8, :], op=ALU.mult), reads=[R_a1[j], R_ub[j]], writes=[R_rk[j]])
                                k.op("dve", lambda e, j=j, U_=U_: e.tensor_tensor(out=rk_t[j][:], in0=rk_t[j][:], in1=U_[:, 0:4, :], op=ALU.mult), reads=[R_rk[j], R_ub[j]], writes=[R_rk[j]])
                                k.op("dve", lambda e, j=j: e.tensor_tensor(out=rk_t[j][:], in0=rk_t[j][:], in1=rkp[:, :].unsqueeze(2).to_broadcast([128, 4, 64]), op=ALU.mult), reads=[R_rk[j], R_par], writes=[R_rk[j]])
                                pr, rr = ps_next()
                                for q in range(4):
                                    k.op("pe", lambda e, pr=pr, q=q, j=j: e.matmul(pr[0:64, q * 2:(q + 1) * 2], rk_t[j][:, q, :], sel[:, :], start=True, stop=True), reads=[R_rk[j], R_par], writes=[rr])
                                k.op("act", lambda e, pr=pr, j=j: e.activation(out=rkh[j][:], in_=pr[0:64, 0:8], func=AF.Copy), reads=[rr], writes=[R_rkh[j]])
                                k.op("dve", lambda e, j=j, T_m=T_m: e.tensor_tensor(out=bon[j][:].rearrange("p (q h v) -> p q h v", q=4, h=2), in0=T_m[:, :, 3, :].rearrange("p q (h v) -> p q h v", h=2),
                                     in1=rkh[j][:, :].rearrange("p (q h) -> p q h", q=4).unsqueeze(3).to_broadcast([64, 4, 2, 64]), op=ALU.mult), reads=[R_tm[j], R_rkh[j]], writes=[R_bon[j]])
                                k.dma("sp", BON[b][t0:t0 + 64, :], bon[j][:], reads=[R_bon[j]], writes=[R_BON[b]])
                                k.op("act", lambda e, j=j, U_=U_: e.activation(out=sgl[j][:], in_=U_[:, 14, :], func=AF.Sigmoid), reads=[R_ub[j]], writes=[R_sgl[j]])
                                pg, rg = ps_next()
                                k.op("pe", lambda e, pg=pg, j=j: e.matmul(pg[0:64, :], sgl[j][:], g2[:], start=True, stop=True), reads=[R_sgl[j], R_par], writes=[rg])
                                k.op("act", lambda e, pg=pg, j=j: e.activation(out=gsb[j][:], in_=pg[0:64, :], func=AF.Copy), reads=[rg], writes=[R_gsb[j]])
                                k.dma("sp", GG[b][t0:t0 + 64, :], gsb[j][:], reads=[R_gsb[j]], writes=[R_GG[b]])
                            if RW_DBG["upto"] < "B":
                                continue
                            def hp(h):
                                return h // 2, (h % 2) * 64
                            hv = lambda t, h2: t[:].rearrange("p (q h) c -> p q h c", q=4, h=2)[:, :, h2, :]
                            for h2 in range(2):
                                p0 = h2 * 64
                                p1, r1 = ps_next()
                                p2, r2 = ps_next()
                                p3, r3 = ps_next()
                                for q in range(4):
                                    k.op("pe", lambda e, p1=p1, q=q, p0=p0, F_=F_: e.matmul(p1[0:64, q * 128:(q + 1) * 128], F_[p0:p0 + 64, q, 0, :], F_[p0:p0 + 64, q, 2:4, :].rearrange("p s t -> p (s t)"), start=True, stop=True),
                                         reads=[R_fm[j]], writes=[r1])
                                    k.op("pe", lambda e, p2=p2, q=q, p0=p0, F_=F_: e.matmul(p2[0:64, q * 128:(q + 1) * 128], F_[p0:p0 + 64, q, 1, :], F_[p0:p0 + 64, q, 2:4, :].rearrange("p s t -> p (s t)"), start=True, stop=True),
                                         reads=[R_fm[j]], writes=[r2])
                                    k.op("pe", lambda e, p3=p3, q=q, p0=p0, F_=F_: e.matmul(p3[0:64, q * 64:(q + 1) * 64], F_[p0:p0 + 64, q, 2, :], F_[p0:p0 + 64, q, 1, :], start=True, stop=True), reads=[R_fm[j]], writes=[r3])
                                k.op("dve", lambda e, p1=p1, h2=h2, j=j, d=d: e.tensor_tensor(out=hv(LA[j], h2), in0=p1[0:64, :].rearrange("p (q c) -> p q c", q=4),
                                     in1=m2[:, d, :].unsqueeze(1).to_broadcast([64, 4, 128]), op=ALU.mult), reads=[r1, R_par], writes=[R_LA[j]])
                                k.op("dve", lambda e, p2=p2, h2=h2, j=j, d=d: e.tensor_tensor(out=hv(NBt[j], h2), in0=p2[0:64, :].rearrange("p (q c) -> p q c", q=4),
                                     in1=m3[:, d, :].unsqueeze(1).to_broadcast([64, 4, 128]), op=ALU.mult), reads=[r2, R_par], writes=[R_NB[j]])
                                k.op("dve", lambda e, p3=p3, h2=h2, j=j, d=d: e.tensor_tensor(out=hv(Lm[j], h2), in0=p3[0:64, 0:256].rearrange("p (q c) -> p q c", q=4),
                                     in1=mL[:, d, :].unsqueeze(1).to_broadcast([64, 4, 64]), op=ALU.mult), reads=[r3, R_par], writes=[R_Lm[j]])
                            if RW_DBG["upto"] < "B0":
                                continue
                            k.op("dve", lambda e, j=j: e.scalar_tensor_tensor(out=Pm[j][:], in0=NBt[j][:, :, 0:64], scalar=-1.0, in1=ident[0:64, 0:64].unsqueeze(1).to_broadcast([64, 8, 64]), op0=ALU.mult, op1=ALU.add),
                                 reads=[R_NB[j], R_id], writes=[R_Pm[j]])
                            Ncur = lambda h, j=j: NBt[j][:, h, 0:64]
                            Lcur = lambda h, j=j: Lm[j][:, h, :]
                            R_Nc, R_Lc = R_NB[j], R_Lm[j]
                            for lev in range(RW_DBG.get("nlev", 5)):
                                i4 = nl4[0] % 4; nl4[0] += 1
                                pL, rL = ps_next()
                                for h in range(8):
                                    k.op("pe", lambda e, pL=pL, h=h, Ncur=Ncur, Lcur=Lcur: e.matmul(pL[0:64, h * 64:(h + 1) * 64], Ncur(h), Lcur(h), start=True, stop=True), reads=[R_Nc, R_Lc], writes=[rL])
                                k.op("act", lambda e, pL=pL, i4=i4: e.activation(out=Ll[i4][:].rearrange("p h c -> p (h c)"), in_=pL[0:64, :], func=AF.Copy), reads=[rL], writes=[R_Ll[i4]])
                                if lev < 4:
                                    pN, rN = ps_next()
                                    for h in range(8):
                                        k.op("pe", lambda e, pN=pN, h=h, Ncur=Ncur, Lcur=Lcur: e.matmul(pN[0:64, h * 64:(h + 1) * 64], Lcur(h), Ncur(h), start=True, stop=True), reads=[R_Nc, R_Lc], writes=[rN])
                                    k.op("act", lambda e, pN=pN, i4=i4: e.activation(out=Nl[i4][:].rearrange("p h c -> p (h c)"), in_=pN[0:64, :], func=AF.Copy), reads=[rN], writes=[R_Nl[i4]])
                                pP, rP = ps_next()
                                for h in range(8):
                                    k.op("pe", lambda e, pP=pP, h=h, i4=i4, j=j: e.matmul(pP[0:64, h * 64:(h + 1) * 64], Ll[i4][:, h, :], Pm[j][:, h, :], start=True, stop=True), reads=[R_Ll[i4], R_Pm[j]], writes=[rP])
                                k.op("dve", lambda e, pP=pP, j=j: e.tensor_tensor(out=Pm[j][:].rearrange("p h c -> p (h c)"), in0=Pm[j][:].rearrange("p h c -> p (h c)"), in1=pP[0:64, :], op=ALU.add), reads=[rP, R_Pm[j]], writes=[R_Pm[j]])
                                Ncur = lambda h, i4=i4: Nl[i4][:, h, :]
                                Lcur = lambda h, i4=i4: Ll[i4][:, h, :]
                                R_Nc, R_Lc = R_Nl[i4], R_Ll[i4]
                            if RW_DBG["upto"] < "B2":
                                continue
                            pW, rW = ps_next()
                            for h in range(8):
                                q, p0 = hp(h)
                                k.op("pe", lambda e, pW=pW, h=h, q=q, j=j, T_m=T_m: e.matmul(pW[:, h * 64:(h + 1) * 64], T_m[:, q, 0, :], Pm[j][:, h, :], start=True, stop=True), reads=[R_tm[j], R_Pm[j]], writes=[rW])
                            for h2 in range(2):
                                k.op("act" if h2 == 0 else "dve",
                                     (lambda e, pW=pW, j=j, h2=h2: e.activation(out=WT[j][h2 * 64:(h2 + 1) * 64, :, :], in_=pW[h2 * 64:(h2 + 1) * 64, :].rearrange("p (q h i) -> p q h i", q=4, h=2)[:, :, h2, :], func=AF.Copy)) if h2 == 0 else
                                     (lambda e, pW=pW, j=j, h2=h2: e.tensor_copy(out=WT[j][h2 * 64:(h2 + 1) * 64, :, :], in_=pW[h2 * 64:(h2 + 1) * 64, :].rearrange("p (q h i) -> p q h i", q=4, h=2)[:, :, h2, :])),
                                     reads=[rW], writes=[R_WT[j]])
                            pX, rX = ps_next()
                            for h in range(8):
                                q, p0 = hp(h)
                                k.op("pe", lambda e, pX=pX, h=h, q=q, p0=p0, j=j, T_m=T_m: e.matmul(pX[0:64, h * 64:(h + 1) * 64], LA[j][:, h, 0:64], T_m[:, q, 3, p0:p0 + 64], start=True, stop=True), reads=[R_LA[j], R_tm[j]], writes=[rX])
                            k.op("act", lambda e, pX=pX, j=j: e.activation(out=Xa[j][:].rearrange("p h c -> p (h c)"), in_=pX[0:64, :], func=AF.Copy), reads=[rX], writes=[R_Xa[j]])
                            pU, rU = ps_next()
                            for h in range(8):
                                k.op("pe", lambda e, pU=pU, h=h, j=j: e.matmul(pU[0:64, h * 64:(h + 1) * 64], Pm[j][:, h, :], Xa[j][:, h, :], start=True, stop=True), reads=[R_Pm[j], R_Xa[j]], writes=[rU])
                            k.op("act", lambda e, pU=pU, j=j: e.activation(out=Uta[j][:].rearrange("p h c -> p (h c)"), in_=pU[0:64, :], func=AF.Copy), reads=[rU], writes=[R_Uta[j]])
                            if RW_DBG["upto"] < "C":
                                continue
                            for h2 in range(2):
                                p0 = h2 * 64
                                pS, rS = ps_next()
                                for q in range(4):
                                    k.op("pe", lambda e, pS=pS, q=q, p0=p0, j=j: e.matmul(pS[0:64, q * 64:(q + 1) * 64], WT[j][p0:p0 + 64, q, :], Hst[p0:p0 + 64, q, :], start=True, stop=True), reads=[R_WT[j], R_H], writes=[rS])
                                k.op("dve", lambda e, pS=pS, j=j, h2=h2: e.tensor_tensor(out=hv(Ua[j], h2), in0=hv(Uta[j], h2), in1=pS[0:64, 0:256].rearrange("p (q c) -> p q c", q=4), op=ALU.add),
                                     reads=[rS, R_Uta[j]], writes=[R_Ua[j]])
                            if RW_DBG["upto"] < "C1":
                                continue
                            pY, rY = ps_next()
                            for h in range(8):
                                q, p0 = hp(h)
                                k.op("pe", lambda e, pY=pY, h=h, q=q, p0=p0, j=j, T_m=T_m: e.matmul(pY[0:64, h * 64:(h + 1) * 64], LA[j][:, h, 64:128], T_m[:, q, 3, p0:p0 + 64], start=True, stop=False), reads=[R_LA[j], R_tm[j]], writes=[rY])
                                k.op("pe", lambda e, pY=pY, h=h, j=j: e.matmul(pY[0:64, h * 64:(h + 1) * 64], NBt[j][:, h, 64:128], Ua[j][:, h, :], start=False, stop=True), reads=[R_NB[j], R_Ua[j]], writes=[rY])
                            k.op("act", lambda e, pY=pY, j=j: e.activation(out=ysb[j][:], in_=pY[0:64, :], func=AF.Copy), reads=[rY], writes=[R_ysb[j]])
                            for h2 in range(2):
                                p0 = h2 * 64
                                pR, rR = ps_next()
                                for q in range(4):
                                    k.op("pe", lambda e, pR=pR, q=q, p0=p0, j=j, F_=F_: e.matmul(pR[0:64, q * 64:(q + 1) * 64], F_[p0:p0 + 64, q, 3, :], Hst[p0:p0 + 64, q, :], start=True, stop=True), reads=[R_fm[j], R_H], writes=[rR])
                                yv = lambda t, h2: t[:].rearrange("p (q h c) -> p q h c", q=4, h=2)[:, :, h2, :]
                                k.op("dve", lambda e, pR=pR, j=j, h2=h2, yv=yv: e.tensor_tensor(out=yv(ysb[j], h2), in0=yv(ysb[j], h2), in1=pR[0:64, 0:256].rearrange("p (q c) -> p q c", q=4), op=ALU.add),
                                     reads=[rR, R_ysb[j]], writes=[R_ysb[j]])
                            k.dma("sp", YD[d][b][t0:t0 + 64, :], ysb[j][:], reads=[R_ysb[j]], writes=[R_YD[d][b]])
                            if RW_DBG["upto"] < "C2":
                                continue
                            pH, rH = ps_next()
                            for q in range(4):
                                k.op("pe", lambda e, pH=pH, q=q, T_m=T_m: e.matmul(pH[:, q * 128:(q + 1) * 128], T_m[:, q, 1, :], T_m[:, q, 3, :], start=True, stop=False), reads=[R_tm[j]], writes=[rH])
                                k.op("pe", lambda e, pH=pH, q=q, j=j, T_m=T_m: e.matmul(pH[:, q * 128:(q + 1) * 128], T_m[:, q, 2, :], Ua[j][:, 2 * q:2 * q + 2, :].rearrange("p h c -> p (h c)"), start=False, stop=True), reads=[R_tm[j], R_Ua[j]], writes=[rH])
                            for h2 in range(2):
                                ps_ = slice(h2 * 64, (h2 + 1) * 64)
                                k.op("dve", lambda e, pH=pH, j=j, h2=h2, ps_=ps_: e.tensor_tensor(out=htmp[j][ps_, :, :], in0=pH[ps_, :].rearrange("p (q h v) -> p q h v", q=4, h=2)[:, :, h2, :], in1=Hst[ps_, :, :], op=ALU.add),
                                     reads=[rH, R_H], writes=[R_htmp[j]])
                                k.op("dve", lambda e, j=j, ps_=ps_, last_col=last_col: e.tensor_tensor(out=Hst[ps_, :, :], in0=htmp[j][ps_, :, :], in1=EG[j][ps_, :, 1, last_col:last_col + 1].to_broadcast([64, 4, 64]), op=ALU.mult),
                                     reads=[R_htmp[j], R_EG[j]], writes=[R_H])
            k.barrier()
            if not RW_DBG["readout"]:
                return
            with ExitStack() as s1:
                lnw_ro = sb("ro_lnw", [128, 512], F32, s1); lnb_ro = sb("ro_lnb", [128, 512], F32, s1); c1_ro = sb("ro_c1", [128, 4], F32, s1); R_par_ro = Res()
                k.dma("sp", lnw_ro[:], I["rw_lnw"][l], writes=[R_par_ro]); k.dma("sp", lnb_ro[:], I["rw_lnb"][l], writes=[R_par_ro]); k.dma("sp", c1_ro[:], I["rw_c1"], writes=[R_par_ro])
                def T2(name, shape, dt=F32):
                    return [sb(name + str(i), shape, dt, s1) for i in range(2)], [Res(), Res()]
                y0_ro, R_y0_ro = T2("ro_y0", [128, 512]); y1_ro, R_y1_ro = T2("ro_y1", [128, 512]); bo_ro, R_bo_ro = T2("ro_bo", [128, 512]); gg_ro, R_gg_ro = T2("ro_gg", [128, 512])
                stt__ro, R_stt_ro = T2("ro_st", [128, 32]); yc_ro, R_yc_ro = T2("ro_yc", [128, 512]); sq_ro, R_sq_ro = T2("ro_sq", [128, 512]); ot_ro, R_ot_ro = T2("ro_ot", [128, 4, 128], BF16)
                n = 0
                for b in range(NB):
                    for tt in range(0 if need_ctx else 2, NT):
                        j = n % 2; n += 1
                        t0 = tt * 128
                        k.dma("sp", y0_ro[j][:], YD[0][b][t0:t0 + 128, :], reads=[R_YD[0][b]], writes=[R_y0_ro[j]])
                        k.dma("sp", y1_ro[j][:], YD[1][b][t0:t0 + 128, :], reads=[R_YD[1][b]], writes=[R_y1_ro[j]])
                        k.dma("sp", bo_ro[j][:], BON[b][t0:t0 + 128, :], reads=[R_BON[b]], writes=[R_bo_ro[j]])
                        k.dma("sp", gg_ro[j][:], GG[b][t0:t0 + 128, :], reads=[R_GG[b]], writes=[R_gg_ro[j]])
                        S_ = stt__ro[j]
                        v3 = lambda t: t[:].rearrange("p (h v) -> p h v", h=8)
                        bc = lambda a: a.unsqueeze(2).to_broadcast([128, 8, 64])
                        k.op("dve", lambda e, j=j: e.tensor_tensor(out=y0_ro[j][:], in0=y0_ro[j][:], in1=y1_ro[j][:], op=ALU.add), reads=[R_y0_ro[j], R_y1_ro[j]], writes=[R_y0_ro[j]])
                        k.op("dve", lambda e, j=j, S_=S_: e.tensor_reduce(out=S_[:, 0:8], in_=v3(y0_ro[j]), axis=AX.X, op=ALU.add), reads=[R_y0_ro[j]], writes=[R_stt_ro[j]])
                        k.op("dve", lambda e, S_=S_: e.tensor_scalar(out=S_[:, 8:16], in0=S_[:, 0:8], scalar1=1.0 / 64, scalar2=None, op0=ALU.mult), reads=[R_stt_ro[j]], writes=[R_stt_ro[j]])
                        k.op("dve", lambda e, j=j, S_=S_: e.tensor_tensor(out=v3(yc_ro[j]), in0=v3(y0_ro[j]), in1=bc(S_[:, 8:16]), op=ALU.subtract), reads=[R_y0_ro[j], R_stt_ro[j]], writes=[R_yc_ro[j]])
                        k.op("pool", lambda e, j=j: e.tensor_tensor(out=sq_ro[j][:], in0=yc_ro[j][:], in1=yc_ro[j][:], op=ALU.mult), reads=[R_yc_ro[j]], writes=[R_sq_ro[j]])
                        k.op("dve", lambda e, j=j, S_=S_: e.tensor_reduce(out=S_[:, 16:24], in_=v3(sq_ro[j]), axis=AX.X, op=ALU.add), reads=[R_sq_ro[j]], writes=[R_stt_ro[j]])
                        k.op("act", lambda e, S_=S_: e.activation(out=S_[:, 24:32], in_=S_[:, 16:24], func=AF.Sqrt, bias=c1_ro[:, 2:3], scale=1.0 / 64), reads=[R_stt_ro[j], R_par_ro], writes=[R_stt_ro[j]])
                        k.op("dve", lambda e, S_=S_: e.reciprocal(out=S_[:, 24:32], in_=S_[:, 24:32]), reads=[R_stt_ro[j]], writes=[R_stt_ro[j]])
                        k.op("dve", lambda e, j=j, S_=S_: e.tensor_tensor(out=v3(yc_ro[j]), in0=v3(yc_ro[j]), in1=bc(S_[:, 24:32]), op=ALU.mult), reads=[R_yc_ro[j], R_stt_ro[j]], writes=[R_yc_ro[j]])
                        k.op("pool", lambda e, j=j: e.tensor_tensor(out=yc_ro[j][:], in0=yc_ro[j][:], in1=lnw_ro[:], op=ALU.mult), reads=[R_yc_ro[j], R_par_ro], writes=[R_yc_ro[j]])
                        k.op("pool", lambda e, j=j: e.tensor_tensor(out=yc_ro[j][:], in0=yc_ro[j][:], in1=lnb_ro[:], op=ALU.add), reads=[R_yc_ro[j], R_par_ro], writes=[R_yc_ro[j]])
                        k.op("dve", lambda e, j=j: e.tensor_tensor(out=yc_ro[j][:], in0=yc_ro[j][:], in1=bo_ro[j][:], op=ALU.add), reads=[R_yc_ro[j], R_bo_ro[j]], writes=[R_yc_ro[j]])
                        k.op("dve", lambda e, j=j: e.tensor_tensor(out=yc_ro[j][:], in0=yc_ro[j][:], in1=gg_ro[j][:], op=ALU.mult), reads=[R_yc_ro[j], R_gg_ro[j]], writes=[R_yc_ro[j]])
                        pt, rp = ps_next()
                        for q in range(4):
                            k.op("pe", lambda e, pt=pt, q=q, j=j: e.transpose(pt[:, q * 128:(q + 1) * 128], yc_ro[j][:, q * 128:(q + 1) * 128], ident[:, :]), reads=[R_yc_ro[j], R_id], writes=[rp])
                        k.op("act", lambda e, pt=pt, j=j: e.activation(out=ot_ro[j][:].rearrange("p q t -> p (q t)"), in_=pt[:, :], func=AF.Copy), reads=[rp], writes=[R_ot_ro[j]])
                        k.dma("sp", YT[b][:, 0:4, t0:t0 + 128], ot_ro[j][:], reads=[R_ot_ro[j]], writes=[R_YT[b]])
            k.barrier()

        toks = []
        for l in range(n_layers):
            if stages is None or "norm1" in stages:
                stage_norm(l, 0, 0, HT, R_HT)
            if stages is None or "proj" in stages:
                stage_proj(l)
            last = (l == DEPTH - 1)
            if stages is None or "na" in stages:
                stage_na(l, not last, **na_kw)
            if stages is None or "rwkv" in stages:
                stage_rwkv(l, not last)
            if stages is None or "wout" in stages:
                stage_wout(l, last)
            if stages is None or "ffn" in stages:
                moe = (l % 2 == 1)
                tsel = range(LC, S, 256) if last else None
                if moe:
                    stage_norm(l, 1, 3, H2T, R_H2T, dst32=H2F, R_dst32=R_H2F, tsel=tsel)
                    stage_router(l, last)
                else:
                    stage_norm(l, 1, 3, H2T, R_H2T, tsel=tsel)
                stage_ffn(l, last)
        if stages is None or "final" in stages:
            fin_out = stage_final()
        else:
            fin_out = []

        fin = list(fin_out)
        for r in R_XT + R_HT + R_QK + R_VN + R_UT + R_YT + R_YD[0] + R_YD[1] + R_BON + R_GG:
            if r.w is not None:
                fin.append(r.w)
        k.final_wait("sp", fin)
        k.emit()
        print("insts", k.n_inst, "epochs", k.n_epochs)
    return nc


def _fm(a, nchunk):
    return np.ascontiguousarray(a.reshape(nchunk, 128, -1).transpose(1, 0, 2))


def _build_bfull(rpb):
    L, H = rpb.shape[:2]
    q = np.arange(64)[:, None]; kc = np.arange(64)[None, :]
    lo = np.clip(q - 8, 0, 48)
    valid = (kc >= lo) & (kc < lo + 16)
    idx = np.clip(kc - q + 15, 0, 30)
    g = rpb[:, :, :, idx]
    g = np.where(valid[None, None, None], g, np.float32(NEG)).astype(np.float32)
    return np.ascontiguousarray(g.transpose(0, 3, 1, 2, 4).reshape(L, 64, H, 960))


def _prep_shared(inp):
    m = {}
    m["ada_w"] = np.stack([_fm(inp["ada_w"][l], 8) for l in range(4)])
    m["ada_bT"] = np.stack([inp["ada_b"][l].reshape(48, 128).T.copy() for l in range(4)])
    m["g_mix"] = np.stack([inp["norm_mix_g"][l].reshape(8, 128).T.copy() for l in range(4)])
    m["g_ffn"] = np.stack([inp["norm_ffn_g"][l].reshape(8, 128).T.copy() for l in range(4)])
    m["w_in"] = np.stack([_fm(inp["w_in"][l], 8) for l in range(4)])
    m["mu"] = np.stack([inp["shift_mu"][l].reshape(15, 128).T.copy() for l in range(4)])
    m["ident"] = np.eye(128, dtype=np.float32)
    m["final_g"] = inp["final_g"].reshape(8, 128).T.copy()
    m["bfull"] = _build_bfull(inp["na_rpb"])
    m["w_out"] = np.stack([_fm(inp["w_out"][l], 8) for l in range(4)])
    m["rw_w2"] = inp["w2"].reshape(4, 128, 512); m["rw_a2"] = inp["a2"].reshape(4, 128, 512); m["rw_g2"] = inp["g2"]
    m["rw_a0T"] = np.stack([inp["a0"][l].reshape(2, 4, 128).transpose(2, 0, 1) for l in range(4)])
    c4 = lambda a: np.stack([a[l].reshape(4, 128).T for l in range(4)])
    m["rw_kk"] = c4(inp["k_k"]); m["rw_ka"] = c4(inp["k_a"]); m["rw_rk"] = c4(inp["r_k"].reshape(4, 512))
    m["rw_lnw"] = np.broadcast_to(inp["ln_x_w"][:, None, :], (4, 128, 512)); m["rw_lnb"] = np.broadcast_to(inp["ln_x_b"][:, None, :], (4, 128, 512))
    m["rw_w0b"] = np.broadcast_to(inp["w0"][:, None, :, :], (4, 64, 2, 512))
    idx = np.arange(64)
    strict = [(idx[:, None] < idx[None, :]), (idx[:, None] > idx[None, :])]
    incl = [(idx[:, None] <= idx[None, :]), (idx[:, None] >= idx[None, :])]
    m["rw_m2"] = np.stack([np.concatenate([strict[d], incl[d]], 1) for d in range(2)], 1).astype(np.float32)
    m["rw_m3"] = np.stack([np.concatenate([strict[d].astype(np.float32), -incl[d].astype(np.float32)], 1) for d in range(2)], 1)
    m["rw_mL"] = np.stack([strict[d].T for d in range(2)], 1).astype(np.float32)
    bo = np.zeros((128, 128), np.float32); bo[:64, :64] = 1; bo[64:, 64:] = 1
    m["rw_bones"] = bo
    se = np.zeros((128, 2), np.float32); se[:64, 0] = 1; se[64:, 1] = 1
    m["rw_sel"] = se
    m["rw_c1"] = np.tile(np.array([1.0, -0.5, 64e-5, 1e-12], np.float32)[None], (128, 1))
    m["ffn_w1"] = np.stack([_fm(inp["ffn_w1"][i], 8) for i in range(2)])
    m["ffn_w3"] = np.stack([_fm(inp["ffn_w3"][i], 8) for i in range(2)])
    m["ffn_w2"] = np.stack([_fm(inp["ffn_w2"][i], DFF // 128) for i in range(2)])
    m["router"] = np.stack([_fm(inp["router"][i], 8) for i in range(2)])
    m["moe_w1"] = np.stack([np.stack([_fm(inp["moe_w1"][i, e], 8) for e in range(NE)]) for i in range(2)])
    m["moe_w3"] = np.stack([np.stack([_fm(inp["moe_w3"][i, e], 8) for e in range(NE)]) for i in range(2)])
    m["moe_w2"] = np.stack([np.stack([_fm(inp["moe_w2"][i, e], DFE // 128) for e in range(NE)]) for i in range(2)])
    return {k_: np.ascontiguousarray(v, dtype=np.float32) for k_, v in m.items()}


def _prep_core(inp, core):
    bs = [2 * core, 2 * core + 1]
    m = {}
    xcat = [np.concatenate([inp["ctx"][b], inp["x"][b]], 0) for b in bs]
    m["xT"] = np.stack([_fm(np.ascontiguousarray(xc.T), 8) for xc in xcat]).astype(np.float32)
    cc = np.stack([inp["c"][bs[0]], inp["c"][bs[1]], inp["c_ctx"]], 1)
    m["cT"] = _fm(cc, 8).astype(np.float32)
    return m


def kernel(**inputs):
    inp = {k_: np.asarray(v) for k_, v in inputs.items()}
    n = 8
    shared = _prep_shared(inp)
    in_maps = []
    for core in range(n):
        m = dict(shared)
        m.update(_prep_core(inp, core))
        in_maps.append(m)
    nc = build_program()
    res = run_bass_kernel_spmd(nc, in_maps, core_ids=list(range(n)))
    outs = []
    for core in range(n):
        o = np.asarray(res.results[core]["out"])
        for b in range(NB):
            outs.append(o[b].transpose(1, 0, 2).reshape(D, T).T)
    return np.ascontiguousarray(np.stack(outs, 0)).astype(np.float32)
```

```python
import numpy as np
import concourse.bass as bass
import concourse.mybir as mybir
from concourse.bass_utils import run_bass_kernel_spmd
from contextlib import ExitStack

F32 = mybir.dt.float32
BF16 = mybir.dt.bfloat16
AF = mybir.ActivationFunctionType
ALU = mybir.AluOpType
AX = mybir.AxisListType

SAME_ENGINE_SYNC = True
DMA_RING = {"sp": 16, "act": 4, "pool": 16}
SEM_LIMIT = 60000
MAX_EPOCHS = 7


class Res:
    __slots__ = ("name", "w", "rd")

    def __init__(self, name=""):
        self.name = name
        self.w = None
        self.rd = []


class K:
    ENG = ("pe", "act", "dve", "pool", "sp")
    CE = ("pe", "act", "dve", "pool")

    def __init__(self, nc, stack):
        self.nc = nc
        self.stack = stack
        self.recs = []
        self.cnt = {e: 0 for e in self.ENG}
        self.slots = []
        self.dq = {}
        for q in ("sp", "act", "pool"):
            idx = []
            for i in range(DMA_RING[q]):
                self.slots.append(0)
                idx.append(len(self.slots) - 1)
            self.dq[q] = {"idx": idx, "n": 0}
        self.waited_e = {e: {} for e in self.ENG}
        self.waited_d = {e: {} for e in self.ENG}
        self.n_inst = 0

    def _need(self, eng, tok, waits, force=False):
        if tok is None:
            return
        if tok[0] == "e":
            _, x, seq = tok
            if x == eng and (not SAME_ENGINE_SYNC or eng == "pe") and not force:
                return
            if self.waited_e[eng].get(x, 0) >= seq:
                return
            self.waited_e[eng][x] = seq
            waits.append(tok)
        else:
            _, si, use = tok
            if self.waited_d[eng].get(si, 0) >= use:
                return
            self.waited_d[eng][si] = use
            waits.append(tok)

    def _deps(self, eng, reads, writes):
        waits = []
        for r in reads:
            self._need(eng, r.w, waits)
        for w in writes:
            self._need(eng, w.w, waits)
            best = {}
            for t in w.rd:
                key = (t[0], t[1])
                if key not in best or best[key][2] < t[2]:
                    best[key] = t
            for t in best.values():
                self._need(eng, t, waits)
        return waits

    def _commit(self, tok, reads, writes):
        for r in reads:
            if r.rd and r.rd[-1][0] == tok[0] and r.rd[-1][1] == tok[1]:
                r.rd[-1] = tok
            else:
                r.rd.append(tok)
        for w in writes:
            w.w = tok
            w.rd = []

    def op(self, eng, fn, reads=(), writes=()):
        waits = self._deps(eng, reads, writes)
        self.cnt[eng] += 1
        tok = ("e", eng, self.cnt[eng])
        self.recs.append({"eng": eng, "waits": waits, "fn": fn, "tok": tok})
        self._commit(tok, reads, writes)
        self.n_inst += 1
        return tok

    def dma(self, q, out, in_, reads=(), writes=(), **kw):
        waits = self._deps(q, reads, writes)
        d = self.dq[q]
        si = d["idx"][d["n"] % len(d["idx"])]
        d["n"] += 1
        prev = self.slots[si]
        if prev > 0:
            self._need(q, ("d", si, prev), waits)
        self.slots[si] = prev + 1
        tok = ("d", si, prev + 1)
        fn = (lambda e, o=out, i=in_, kw=kw: e.dma_start(out=o, in_=i, **kw))
        self.recs.append({"eng": q, "waits": waits, "fn": fn, "tok": tok})
        self._commit(tok, reads, writes)
        self.n_inst += 1
        return tok

    def _all_waits(self, eng):
        waits = []
        for x in self.CE:
            if self.cnt[x] > 0:
                self._need(eng, ("e", x, self.cnt[x]), waits, force=True)
        for si, v in enumerate(self.slots):
            if v > 0:
                self._need(eng, ("d", si, v), waits)
        return waits

    def barrier(self):
        for eng in self.ENG:
            self.recs.append({"eng": eng, "waits": self._all_waits(eng), "fn": None, "tok": None})

    def final_wait(self, eng, toks):
        waits = []
        for t in toks:
            self._need(eng, t, waits)
        self.recs.append({"eng": eng, "waits": waits, "fn": None, "tok": None})

    def emit(self):
        nc, st = self.nc, self.stack
        recs = self.recs
        needed = set()
        for r in recs:
            for w in r["waits"]:
                if w[0] == "e":
                    needed.add((w[1], w[2]))
        out = []
        ep = 0
        ms = {e: 0 for e in self.CE}
        du = [0] * len(self.slots)
        last_e = {e: 0 for e in self.CE}
        last_d = [0] * len(self.slots)
        val = {}
        pend_last = {}

        def close_epoch():
            nonlocal ep, ms, du
            bw = {}
            for e in self.CE:
                if e in pend_last:
                    rr = out[pend_last[e]]
                    t = rr["tok"]
                    if t not in val:
                        ms[e] += 1
                        val[t] = (ep, ms[e])
                        rr["inc"] = True
            allw = []
            for e in self.CE:
                if e in pend_last:
                    allw.append(out[pend_last[e]]["tok"])
            for si in range(len(self.slots)):
                if du[si] > 0:
                    allw.append(("d", si, last_d[si]))
            for eng in self.ENG:
                out.append({"eng": eng, "waits": list(allw), "fn": None, "tok": None, "ep": ep})
            ep += 1
            ms = {e: 0 for e in self.CE}
            du = [0] * len(self.slots)
            pend_last.clear()

        for r in recs:
            t = r["tok"]
            if t is not None:
                if t[0] == "e":
                    if ms[t[1]] + 2 > SEM_LIMIT:
                        close_epoch()
                else:
                    if (du[t[1]] + 2) * 16 > SEM_LIMIT:
                        close_epoch()
            r = dict(r)
            r["ep"] = ep
            out.append(r)
            if t is not None:
                if t[0] == "e":
                    pend_last[t[1]] = len(out) - 1
                    if (t[1], t[2]) in needed:
                        ms[t[1]] += 1
                        val[t] = (ep, ms[t[1]])
                        r["inc"] = True
                    else:
                        r["inc"] = False
                else:
                    du[t[1]] += 1
                    last_d[t[1]] = t[2]
                    val[t] = (ep, du[t[1]] * 16)
        n_ep = ep + 1
        print("epochs needed", n_ep, "milestones", ms, "dma uses", max(du))
        assert n_ep <= MAX_EPOCHS, "too many epochs %d" % n_ep
        self.n_epochs = n_ep
        esem = [{e: st.enter_context(nc.semaphore("c%d_%s" % (i, e))) for e in self.CE} for i in range(n_ep)]
        dsem = [[st.enter_context(nc.semaphore("d%d_%d" % (i, j))) for j in range(len(self.slots))] for i in range(n_ep)]
        streams = {e: [] for e in self.ENG}
        for r in out:
            streams[r["eng"]].append(r)

        with nc.Block() as block:
            def run(engname, e):
                for ep_ in range(1, n_ep):
                    if engname in self.CE:
                        e.sem_clear(esem[ep_][engname])
                    if engname in self.dq:
                        for si in self.dq[engname]["idx"]:
                            e.sem_clear(dsem[ep_][si])
                for r in streams[engname]:
                    for w in r["waits"]:
                        v = val.get(w)
                        if v is None or v[0] != r["ep"]:
                            continue
                        if w[0] == "e":
                            e.wait_ge(esem[v[0]][w[1]], v[1])
                        else:
                            e.wait_ge(dsem[v[0]][w[1]], v[1])
                    if r["fn"] is None:
                        continue
                    ins = r["fn"](e)
                    t = r["tok"]
                    if t[0] == "e":
                        if r.get("inc"):
                            ins.then_inc(esem[r["ep"]][t[1]], 1)
                    else:
                        ins.then_inc(dsem[r["ep"]][t[1]], 16)

            @block.sync
            def _(e):
                run("sp", e)

            @block.tensor
            def _(e):
                run("pe", e)

            @block.vector
            def _(e):
                run("dve", e)

            @block.scalar
            def _(e):
                run("act", e)

            @block.gpsimd
            def _(e):
                run("pool", e)


D = 1024
DEPTH = 4
NB = 2
LC = 256
T = 2048
S = LC + T
NT = S // 128
D_IN = 3456
DFF = 2816
DFE = 3584
NE = 8
NEG = -30000.0

DEBUG_OUT = []
RW_DBG = {"nch": None, "upto": "Z", "nb": None, "nd": 2, "readout": True}
N_LAYERS_RUN = DEPTH


def build_program(n_layers=DEPTH, stages=None, debug=(), na_kw={}):
    nc = bass.Bass("TRN2", target_bir_lowering=False)
    st = ExitStack()
    with st:
        k = K(nc, st)

        def din(name, shape, dt=F32):
            return nc.dram_tensor(name, list(shape), dt, kind="ExternalInput").ap()

        def dscr(name, shape, dt=F32):
            kind = "ExternalOutput" if name in debug else "Internal"
            return nc.dram_tensor(name, list(shape), dt, kind=kind).ap()

        I = {}
        I["xT"] = din("xT", [NB, 128, 8, S])
        I["cT"] = din("cT", [128, 8, 3])
        I["ada_w"] = din("ada_w", [DEPTH, 128, 8, 6 * D])
        I["ada_bT"] = din("ada_bT", [DEPTH, 128, 48])
        I["g_mix"] = din("g_mix", [DEPTH, 128, 8])
        I["g_ffn"] = din("g_ffn", [DEPTH, 128, 8])
        I["w_in"] = din("w_in", [DEPTH, 128, 8, D_IN])
        I["mu"] = din("mu", [DEPTH, 128, 15])
        I["ident"] = din("ident", [128, 128])
        I["final_g"] = din("final_g", [128, 8])
        I["bfull"] = din("bfull", [DEPTH, 64, 8, 960])
        I["w_out"] = din("w_out", [DEPTH, 128, 8, D])
        I["rw_w2"] = din("rw_w2", [DEPTH, 128, 512]); I["rw_a2"] = din("rw_a2", [DEPTH, 128, 512]); I["rw_g2"] = din("rw_g2", [DEPTH, 128, 512])
        I["rw_a0T"] = din("rw_a0T", [DEPTH, 128, 2, 4]); I["rw_kk"] = din("rw_kk", [DEPTH, 128, 4]); I["rw_ka"] = din("rw_ka", [DEPTH, 128, 4]); I["rw_rk"] = din("rw_rk", [DEPTH, 128, 4])
        I["rw_lnw"] = din("rw_lnw", [DEPTH, 128, 512]); I["rw_lnb"] = din("rw_lnb", [DEPTH, 128, 512]); I["rw_w0b"] = din("rw_w0b", [DEPTH, 64, 2, 512])
        I["rw_m2"] = din("rw_m2", [64, 2, 128]); I["rw_m3"] = din("rw_m3", [64, 2, 128]); I["rw_mL"] = din("rw_mL", [64, 2, 64])
        I["rw_bones"] = din("rw_bones", [128, 128]); I["rw_sel"] = din("rw_sel", [128, 2]); I["rw_c1"] = din("rw_c1", [128, 4])
        I["ffn_w1"] = din("ffn_w1", [2, 128, 8, DFF]); I["ffn_w3"] = din("ffn_w3", [2, 128, 8, DFF]); I["ffn_w2"] = din("ffn_w2", [2, 128, DFF // 128, D])
        I["router"] = din("router", [2, 128, 8, NE])
        I["moe_w1"] = din("moe_w1", [2, NE, 128, 8, DFE]); I["moe_w3"] = din("moe_w3", [2, NE, 128, 8, DFE]); I["moe_w2"] = din("moe_w2", [2, NE, 128, DFE // 128, D])
        out = nc.dram_tensor("out", [NB, 128, 8, T], F32, kind="ExternalOutput").ap()

        XT = [dscr("xt%d" % b, [128, 8, S]) for b in range(NB)]
        HT = [dscr("ht%d" % b, [128, 8, S], BF16) for b in range(NB)]
        QT = [dscr("qt%d" % b, [128, 4, S], BF16) for b in range(NB)]
        KT = [dscr("kt%d" % b, [128, 4, S], BF16) for b in range(NB)]
        VN = [dscr("vn%d" % b, [S, 512], BF16) for b in range(NB)]
        UT = [dscr("ut%d" % b, [128, 15, S]) for b in range(NB)]
        R_XT = [Res() for _ in range(NB)]
        R_HT = [Res() for _ in range(NB)]
        R_QK = [Res() for _ in range(NB)]
        R_VN = [Res() for _ in range(NB)]
        R_UT = [Res() for _ in range(NB)]
        YT = [dscr("yt%d" % b, [128, 8, S], BF16) for b in range(NB)]
        H2T = [dscr("h2t%d" % b, [128, 8, S], BF16) for b in range(NB)]
        H2F = [dscr("h2f%d" % b, [128, 8, S]) for b in range(NB)]
        GB = [dscr("gb%d" % b, [128, NE, S]) for b in range(NB)]
        YD = [[dscr("yd%d_%d" % (d, b), [S, 512]) for b in range(NB)] for d in range(2)]
        R_YD = [[Res() for b in range(NB)] for d in range(2)]
        BON = [dscr("bon%d" % b, [S, 512]) for b in range(NB)]; R_BON = [Res() for _ in range(NB)]
        GG = [dscr("gg%d" % b, [S, 512]) for b in range(NB)]; R_GG = [Res() for _ in range(NB)]
        R_H2T = [Res() for _ in range(NB)]; R_H2F = [Res() for _ in range(NB)]; R_GB = [Res() for _ in range(NB)]
        R_YT = [Res() for _ in range(NB)]

        uid = [0]

        def sb(name, shape, dt=F32, stack=st):
            uid[0] += 1
            return stack.enter_context(nc.sbuf_tensor("%s_%d" % (name, uid[0]), list(shape), dt))

        ident = sb("ident_sb", [128, 128]); R_id = Res()
        ones = sb("ones_sb", [128, 128]); R_ones = Res()
        MOD = sb("mod_sb", [128, DEPTH, 48, 3]); R_mod = Res()
        GS = sb("gs_sb", [128, DEPTH, 2, 8, 3]); R_gs = Res()
        gmix = sb("gmix_sb", [128, DEPTH, 8]); gffn = sb("gffn_sb", [128, DEPTH, 8]); R_g = Res()
        eps_t = sb("eps_sb", [128, 1]); R_eps = Res()
        psum = [st.enter_context(nc.psum_tensor("ps%d" % i, [128, 512], F32)) for i in range(8)]
        R_ps = [Res() for _ in range(8)]
        pctr = [0]

        def ps_next():
            i = pctr[0] % 8
            pctr[0] += 1
            return psum[i], R_ps[i]

        k.dma("sp", ident[:], I["ident"][:, :], writes=[R_id])
        ident_bf = sb("identbf_sb", [128, 128], BF16)
        k.op("dve", lambda e: e.tensor_copy(out=ident_bf[:], in_=ident[:]), reads=[R_id], writes=[R_id])
        k.op("dve", lambda e: e.memset(ones[:], 1.0), writes=[R_ones])
        k.op("dve", lambda e: e.memset(eps_t[:], 1e-6), writes=[R_eps])
        for l in range(DEPTH):
            k.dma("sp", gmix[:, l, :], I["g_mix"][l], writes=[R_g])
            k.dma("sp", gffn[:, l, :], I["g_ffn"][l], writes=[R_g])

        with ExitStack() as s1:
            cT = sb("cT_sb", [128, 8, 3], F32, s1); R_c = Res()
            sT = sb("sT", [128, 8, 3], F32, s1); R_s = Res()
            abT = sb("abT", [128, 48], F32, s1); R_ab = Res()
            aw = [sb("aw%d" % i, [128, 8, 512], F32, s1) for i in range(2)]
            R_aw = [Res(), Res()]
            xcp = [sb("xcp%d" % i, [128, 8, 256], F32, s1) for i in range(2)]
            R_xcp = [Res(), Res()]
            k.dma("sp", cT[:], I["cT"][:, :, :], writes=[R_c])
            k.op("act", lambda e: e.activation(out=sT[:], in_=cT[:], func=AF.Silu), reads=[R_c], writes=[R_s])
            n = 0
            for b in range(NB):
                for t0 in range(0, S, 256):
                    j = n % 2; n += 1
                    k.dma("sp", xcp[j][:], I["xT"][b, :, :, t0:t0 + 256], writes=[R_xcp[j]])
                    k.dma("sp", XT[b][:, :, t0:t0 + 256], xcp[j][:], reads=[R_xcp[j]], writes=[R_XT[b]])
            n = 0
            for l in range(n_layers):
                k.dma("sp", abT[:], I["ada_bT"][l], writes=[R_ab])
                for cc in range(12):
                    j = n % 2; n += 1
                    k.dma("sp", aw[j][:], I["ada_w"][l, :, :, cc * 512:(cc + 1) * 512], writes=[R_aw[j]])
                    pt, rp = ps_next()
                    for c4 in range(4):
                        for kc in range(8):
                            k.op("pe", lambda e, pt=pt, j=j, c4=c4, kc=kc: e.matmul(
                                pt[:, c4 * 3:c4 * 3 + 3], aw[j][:, kc, c4 * 128:(c4 + 1) * 128], sT[:, kc, :],
                                start=(kc == 0), stop=(kc == 7)), reads=[R_aw[j], R_s], writes=[rp])
                    k.op("dve", lambda e, pt=pt, l=l, cc=cc: e.tensor_tensor(
                        out=MOD[:, l, cc * 4:(cc + 1) * 4, :], in0=pt[:, 0:12].rearrange("p (a b) -> p a b", b=3),
                        in1=abT[:, cc * 4:(cc + 1) * 4].unsqueeze(2).to_broadcast([128, 4, 3]), op=ALU.add),
                        reads=[rp, R_ab], writes=[R_mod])
                for which, (gt, mi) in enumerate(((gmix, 1), (gffn, 4))):
                    k.op("dve", lambda e, l=l, which=which, gt=gt, mi=mi: e.scalar_tensor_tensor(
                        out=GS[:, l, which, :, :], in0=MOD[:, l, mi * 8:(mi + 1) * 8, :], scalar=1.0,
                        in1=gt[:, l, :].unsqueeze(2).to_broadcast([128, 8, 3]), op0=ALU.add, op1=ALU.mult),
                        reads=[R_mod, R_g], writes=[R_gs])
        k.barrier()

        def which_of(b, t0):
            return 2 if t0 < LC else b

        def stage_norm(l, which, shift_idx, dst, R_dst, gsel=None, dst32=None, R_dst32=None, bsel=range(NB), tsel=None):
            with ExitStack() as s1:
                xb = [sb("nx%d" % i, [128, 8, 256], F32, s1) for i in range(2)]; R_xb = [Res(), Res()]
                sq = [sb("nsq%d" % i, [128, 8, 256], F32, s1) for i in range(2)]; R_sq = [Res(), Res()]
                rs = [sb("nrs%d" % i, [128, 256], F32, s1) for i in range(2)]; R_rs = [Res(), Res()]
                tm = [sb("ntm%d" % i, [128, 8, 256], F32, s1) for i in range(2)]; R_tm = [Res(), Res()]
                hb = [sb("nhb%d" % i, [128, 8, 256], BF16, s1) for i in range(2)]; R_hb = [Res(), Res()]
                n = 0
                for b in bsel:
                    for t0 in (tsel if tsel is not None else range(0, S, 256)):
                        j = n % 2; n += 1
                        w = which_of(b, t0)
                        k.dma("sp", xb[j][:], XT[b][:, :, t0:t0 + 256], reads=[R_XT[b]], writes=[R_xb[j]])
                        k.op("act", lambda e, j=j: e.activation(out=sq[j][:], in_=xb[j][:], func=AF.Square), reads=[R_xb[j]], writes=[R_sq[j]])
                        pt, rp = ps_next()
                        for c in range(8):
                            k.op("pe", lambda e, pt=pt, j=j, c=c: e.matmul(pt[:, 0:256], ones[:], sq[j][:, c, :], start=(c == 0), stop=(c == 7)),
                                 reads=[R_ones, R_sq[j]], writes=[rp])
                        k.op("act", lambda e, pt=pt, j=j: e.activation(out=rs[j][:], in_=pt[:, 0:256], func=AF.Sqrt, bias=eps_t[:, 0:1], scale=1.0 / D),
                             reads=[rp, R_eps], writes=[R_rs[j]])
                        k.op("dve", lambda e, j=j: e.reciprocal(out=rs[j][:], in_=rs[j][:]), reads=[R_rs[j]], writes=[R_rs[j]])
                        for c in range(8):
                            k.op("dve", lambda e, j=j, c=c, w=w: e.scalar_tensor_tensor(
                                out=tm[j][:, c, :], in0=xb[j][:, c, :], scalar=GS[:, l, which, c, w:w + 1], in1=rs[j][:],
                                op0=ALU.mult, op1=ALU.mult), reads=[R_xb[j], R_gs, R_rs[j]], writes=[R_tm[j]])
                            k.op("act", lambda e, j=j, c=c, w=w: e.activation(
                                out=hb[j][:, c, :], in_=tm[j][:, c, :], func=AF.Identity, bias=MOD[:, l, shift_idx * 8 + c, w:w + 1], scale=1.0),
                                reads=[R_tm[j], R_mod], writes=[R_hb[j]])
                            if dst32 is not None:
                                k.op("pool", lambda e, j=j, c=c, w=w: e.tensor_scalar(
                                    out=tm[j][:, c, :], in0=tm[j][:, c, :], scalar1=MOD[:, l, shift_idx * 8 + c, w:w + 1], scalar2=None, op0=ALU.add),
                                    reads=[R_tm[j], R_mod, R_hb[j]], writes=[R_tm[j]])
                        k.dma("sp", dst[b][:, :, t0:t0 + 256], hb[j][:], reads=[R_hb[j]], writes=[R_dst[b]])
                        if dst32 is not None:
                            k.dma("sp", dst32[b][:, :, t0:t0 + 256], tm[j][:], reads=[R_tm[j]], writes=[R_dst32[b]])
            k.barrier()

        def stage_proj(l):
            with ExitStack() as s1:
                w = sb("pw", [128, 8, D_IN], BF16, s1); R_w = Res()
                mu = sb("pmu", [128, 15], F32, s1); R_mu = Res()
                mu1 = sb("pmu1", [128, 15], F32, s1)
                muh = sb("pmuh", [128, 15], F32, s1)
                hb = [sb("ph%d" % i, [128, 8, 258], BF16, s1) for i in range(2)]; R_hb = [Res(), Res()]
                hs = [sb("phs%d" % i, [128, 8, 256], BF16, s1) for i in range(2)]; R_hs = [Res(), Res()]
                oq = [sb("poq%d" % i, [128, 8, 256], BF16, s1) for i in range(2)]; R_oq = [Res(), Res()]
                ov = [sb("pov%d" % i, [128, 2, 512], BF16, s1) for i in range(2)]; R_ov = [Res(), Res()]
                ou = [sb("pou%d" % i, [128, 15, 256], F32, s1) for i in range(2)]; R_ou = [Res(), Res()]
                t2 = [sb("pt2%d" % i, [128, 256], F32, s1) for i in range(2)]; R_t2 = [Res(), Res()]
                for kc in range(8):
                    k.dma("pool", w[:, kc, :], I["w_in"][l, :, kc, :], writes=[R_w])
                k.dma("sp", mu[:], I["mu"][l], writes=[R_mu])
                k.op("dve", lambda e: e.tensor_scalar(out=mu1[:], in0=mu[:], scalar1=-1.0, scalar2=1.0, op0=ALU.mult, op1=ALU.add), reads=[R_mu], writes=[R_mu])
                k.op("dve", lambda e: e.tensor_scalar(out=muh[:], in0=mu[:], scalar1=0.5, scalar2=None, op0=ALU.mult), reads=[R_mu], writes=[R_mu])
                n = 0
                for b in range(NB):
                    for t0 in range(0, S, 256):
                        j = n % 2; n += 1
                        seg0, seg1 = (0, LC) if t0 < LC else (LC, S)
                        lo, hi = max(t0 - 1, seg0), min(t0 + 257, seg1)
                        if lo == t0 or hi == t0 + 256:
                            k.op("dve", lambda e, j=j: e.memset(hb[j][:], 0.0), writes=[R_hb[j]])
                        k.dma("sp", hb[j][:, :, lo - (t0 - 1):hi - (t0 - 1)], HT[b][:, :, lo:hi], reads=[R_HT[b]], writes=[R_hb[j]])
                        k.op("dve", lambda e, j=j: e.tensor_tensor(out=hs[j][:], in0=hb[j][:, :, 0:256], in1=hb[j][:, :, 2:258], op=ALU.add),
                             reads=[R_hb[j]], writes=[R_hs[j]])
                        for cj in range(8):
                            pt, rp = ps_next()
                            for kc in range(8):
                                k.op("pe", lambda e, pt=pt, j=j, cj=cj, kc=kc: e.matmul(pt[:, 0:256], w[:, kc, cj * 128:(cj + 1) * 128], hb[j][:, kc, 1:257],
                                     start=(kc == 0), stop=(kc == 7)), reads=[R_w, R_hb[j]], writes=[rp])
                            k.op("act", lambda e, pt=pt, j=j, cj=cj: e.activation(out=oq[j][:, cj, :], in_=pt[:, 0:256], func=AF.Copy), reads=[rp], writes=[R_oq[j]])
                        k.dma("sp", QT[b][:, :, t0:t0 + 256], oq[j][:, 0:4, :], reads=[R_oq[j]], writes=[R_QK[b]])
                        k.dma("sp", KT[b][:, :, t0:t0 + 256], oq[j][:, 4:8, :], reads=[R_oq[j]], writes=[R_QK[b]])
                        for tt in range(2):
                            pt, rp = ps_next()
                            for kc in range(8):
                                k.op("pe", lambda e, pt=pt, j=j, tt=tt, kc=kc: e.matmul(pt[:, :], hb[j][:, kc, 1 + tt * 128:1 + (tt + 1) * 128], w[:, kc, 1024:1536],
                                     start=(kc == 0), stop=(kc == 7)), reads=[R_w, R_hb[j]], writes=[rp])
                            k.op("act", lambda e, pt=pt, j=j, tt=tt: e.activation(out=ov[j][:, tt, :], in_=pt[:, :], func=AF.Copy), reads=[rp], writes=[R_ov[j]])
                        k.dma("sp", VN[b][t0:t0 + 256, :].rearrange("(a p) c -> p a c", p=128), ov[j][:], reads=[R_ov[j]], writes=[R_VN[b]])
                        for cj in range(15):
                            c0 = 1536 + cj * 128
                            pa, ra = ps_next()
                            for kc in range(8):
                                k.op("pe", lambda e, pa=pa, j=j, c0=c0, kc=kc: e.matmul(pa[:, 0:256], w[:, kc, c0:c0 + 128], hb[j][:, kc, 1:257],
                                     start=(kc == 0), stop=(kc == 7)), reads=[R_w, R_hb[j]], writes=[ra])
                            pb, rb = ps_next()
                            for kc in range(8):
                                k.op("pe", lambda e, pb=pb, j=j, c0=c0, kc=kc: e.matmul(pb[:, 0:256], w[:, kc, c0:c0 + 128], hs[j][:, kc, :],
                                     start=(kc == 0), stop=(kc == 7)), reads=[R_w, R_hs[j]], writes=[rb])
                            k.op("act", lambda e, pb=pb, j=j, cj=cj: e.activation(out=t2[j][:], in_=pb[:, 0:256], func=AF.Copy, scale=muh[:, cj:cj + 1]),
                                 reads=[rb, R_mu], writes=[R_t2[j]])
                            k.op("dve", lambda e, pa=pa, j=j, cj=cj: e.scalar_tensor_tensor(out=ou[j][:, cj, :], in0=pa[:, 0:256], scalar=mu1[:, cj:cj + 1], in1=t2[j][:],
                                 op0=ALU.mult, op1=ALU.add), reads=[ra, R_mu, R_t2[j]], writes=[R_ou[j]])
                        k.dma("sp", UT[b][:, :, t0:t0 + 256], ou[j][:], reads=[R_ou[j]], writes=[R_UT[b]])
            k.barrier()

        def stage_na(l, need_ctx, hsel=range(8), bsel=range(NB)):
            NBUF = 3
            with ExitStack() as s1:
                bias = sb("na_bias", [128, 8, 960], F32, s1); R_bias = Res()
                k.dma("sp", bias[0:64], I["bfull"][l], writes=[R_bias])
                k.dma("sp", bias[64:128], I["bfull"][l], writes=[R_bias])
                qh = [sb("na_q%d" % i, [64, S], BF16, s1) for i in range(2)]
                kh = [sb("na_k%d" % i, [64, S], BF16, s1) for i in range(2)]
                VE = [sb("na_ve%d" % i, [128, 18, 64], BF16, s1) for i in range(2)]
                VO = [sb("na_vo%d" % i, [128, 18, 64], BF16, s1) for i in range(2)]
                R_in = [Res(), Res()]
                ytok = sb("na_ytok", [128, NT, 512], BF16, s1); R_ytok = Res()
                yfm = [sb("na_yfm%d" % i, [128, 4, 128], BF16, s1) for i in range(2)]; R_yfm = [Res(), Res()]
                ssb = [sb("na_s%d" % i, [128, 832], F32, s1) for i in range(NBUF)]; R_s = [Res() for _ in range(NBUF)]
                pbf = [sb("na_p%d" % i, [128, 832], BF16, s1) for i in range(NBUF)]; R_p = [Res() for _ in range(NBUF)]
                pT = [sb("na_pT%d" % i, [128, 896], BF16, s1) for i in range(NBUF)]; R_pT = [Res() for _ in range(NBUF)]
                st4 = [sb("na_st%d" % i, [128, 4], F32, s1) for i in range(NBUF)]; R_st = [Res() for _ in range(NBUF)]
                n = 0
                u = 0
                for b in bsel:
                    if not need_ctx:
                        pass
                    for h in hsel:
                        j = n % 2; n += 1
                        p0 = (h % 2) * 64
                        k.dma("sp", qh[j][:], QT[b][p0:p0 + 64, h // 2, :], reads=[R_QK[b]], writes=[R_in[j]])
                        k.dma("sp", kh[j][:], KT[b][p0:p0 + 64, h // 2, :], reads=[R_QK[b]], writes=[R_in[j]])
                        k.dma("sp", VE[j][:], VN[b][:, h * 64:(h + 1) * 64].rearrange("(a p) d -> p a d", p=128), reads=[R_VN[b]], writes=[R_in[j]])
                        k.dma("sp", VO[j][:, 0:17, :], VN[b][64:64 + 17 * 128, h * 64:(h + 1) * 64].rearrange("(a p) d -> p a d", p=128), reads=[R_VN[b]], writes=[R_in[j]])
                        k.dma("sp", VO[j][0:64, 17, :], VN[b][S - 64:S, h * 64:(h + 1) * 64], reads=[R_VN[b]], writes=[R_in[j]])
                        units = [("lat", r) for r in range(0, 32, 2)] + ([("ctx", 0), ("ctx", 1)] if need_ctx else [])
                        for kind, r in units:
                            i = u % NBUF; u += 1
                            b1, rb1 = ps_next()
                            if kind == "lat":
                                r0A = min(max(r - 4, 0), 24); r0B = min(max(r - 3, 0), 24)
                                R0 = min(r0A, 23)
                                tq = LC + r * 64; w0 = LC + R0 * 64
                                tile_i = tq // 128
                                b2, rb2 = ps_next()
                                k.op("pe", lambda e, b1=b1, j=j, tq=tq: e.matmul(b1[:, 0:256], qh[j][:, tq:tq + 128], kh[j][:, 0:256], start=True, stop=True), reads=[R_in[j]], writes=[rb1])
                                k.op("pe", lambda e, b1=b1, j=j, tq=tq, w0=w0: e.matmul(b1[:, 256:512], qh[j][:, tq:tq + 128], kh[j][:, w0:w0 + 256], start=True, stop=True), reads=[R_in[j]], writes=[rb1])
                                k.op("pe", lambda e, b2=b2, j=j, tq=tq, w0=w0: e.matmul(b2[:, 0:320], qh[j][:, tq:tq + 128], kh[j][:, w0 + 256:w0 + 576], start=True, stop=True), reads=[R_in[j]], writes=[rb2])
                                k.op("act", lambda e, b1=b1, i=i: e.activation(out=ssb[i][:, 0:256], in_=b1[:, 0:256], func=AF.Copy, scale=0.125), reads=[rb1], writes=[R_s[i]])
                                for half, (rq, r0X) in enumerate(((r, r0A), (r + 1, r0B))):
                                    sX = r0X - R0
                                    hp_ = slice(half * 64, (half + 1) * 64)
                                    bs0 = (r0X - rq + 7) * 64
                                    n1 = 256 - sX * 64
                                    k.op("dve", lambda e, b1=b1, i=i, h=h, hp_=hp_, sX=sX, bs0=bs0, n1=n1: e.scalar_tensor_tensor(
                                        out=ssb[i][hp_, 256 + sX * 64:512], in0=b1[hp_, 256 + sX * 64:512], scalar=0.125, in1=bias[hp_, h, bs0:bs0 + n1], op0=ALU.mult, op1=ALU.add),
                                        reads=[rb1, R_bias], writes=[R_s[i]])
                                    n2 = 512 - n1
                                    k.op("dve", lambda e, b2=b2, i=i, h=h, hp_=hp_, bs0=bs0, n1=n1, n2=n2: e.scalar_tensor_tensor(
                                        out=ssb[i][hp_, 512:512 + n2], in0=b2[hp_, 0:n2], scalar=0.125, in1=bias[hp_, h, bs0 + n1:bs0 + 512], op0=ALU.mult, op1=ALU.add),
                                        reads=[rb2, R_bias], writes=[R_s[i]])
                                    ng0 = 256 + (512 if sX == 0 else 0)
                                    k.op("pool", lambda e, i=i, hp_=hp_, ng0=ng0: e.memset(ssb[i][hp_, ng0:ng0 + 64], NEG), writes=[R_s[i]])
                                W = 832
                            else:
                                tile_i = r
                                k.op("pe", lambda e, b1=b1, j=j, r=r: e.matmul(b1[:, 0:256], qh[j][:, r * 128:(r + 1) * 128], kh[j][:, 0:256], start=True, stop=True), reads=[R_in[j]], writes=[rb1])
                                k.op("act", lambda e, b1=b1, i=i: e.activation(out=ssb[i][:, 0:256], in_=b1[:, 0:256], func=AF.Copy, scale=0.125), reads=[rb1], writes=[R_s[i]])
                                W = 256
                            k.op("dve", lambda e, i=i, W=W: e.tensor_reduce(out=st4[i][:, 0:1], in_=ssb[i][:, 0:W], axis=AX.X, op=ALU.max), reads=[R_s[i]], writes=[R_st[i]])
                            k.op("dve", lambda e, i=i: e.tensor_scalar(out=st4[i][:, 1:2], in0=st4[i][:, 0:1], scalar1=-1.0, scalar2=None, op0=ALU.mult), reads=[R_st[i]], writes=[R_st[i]])
                            k.op("act", lambda e, i=i, W=W: e.activation(out=pbf[i][:, 0:W], in_=ssb[i][:, 0:W], func=AF.Exp, bias=st4[i][:, 1:2], scale=1.0, accum_out=st4[i][:, 2:3]),
                                 reads=[R_s[i], R_st[i]], writes=[R_p[i], R_st[i]])
                            k.op("dve", lambda e, i=i: e.reciprocal(out=st4[i][:, 3:4], in_=st4[i][:, 2:3]), reads=[R_st[i]], writes=[R_st[i]])
                            pc, rc = ps_next()
                            pcb = pc[:].bitcast(BF16)
                            nch = (W + 127) // 128
                            for c in range(nch):
                                kw = min(128, W - c * 128)
                                k.op("pe", lambda e, pcb=pcb, i=i, c=c, kw=kw: e.transpose(pcb[0:kw, c * 128:(c + 1) * 128], pbf[i][:, c * 128:c * 128 + kw], ident_bf[:, :]),
                                     reads=[R_p[i], R_id], writes=[rc])
                            k.op("dve" if kind == "lat" else "act",
                                 (lambda e, pcb=pcb, i=i, nch=nch: e.tensor_copy(out=pT[i][:, 0:nch * 128], in_=pcb[:, 0:nch * 128])) if kind == "lat" else
                                 (lambda e, pcb=pcb, i=i, nch=nch: e.activation(out=pT[i][:, 0:nch * 128], in_=pcb[:, 0:nch * 128], func=AF.Copy)),
                                 reads=[rc], writes=[R_pT[i]])
                            pd, rd = ps_next()
                            for c in range(nch):
                                kw = min(128, W - c * 128)
                                if c < 2:
                                    vt = VE[j][:, c, :]
                                else:
                                    even = (R0 % 2 == 0)
                                    base_t = (w0 // 128) if even else ((w0 - 64) // 128)
                                    vt = (VE[j] if even else VO[j])[0:kw, base_t + (c - 2), :]
                                k.op("pe", lambda e, pd=pd, i=i, c=c, kw=kw, vt=vt, nch=nch: e.matmul(pd[:, 0:64], pT[i][0:kw, c * 128:(c + 1) * 128], vt, start=(c == 0), stop=(c == nch - 1)),
                                     reads=[R_in[j], R_pT[i]], writes=[rd])
                            k.op("act", lambda e, pd=pd, i=i, tile_i=tile_i, h=h: e.activation(out=ytok[:, tile_i, h * 64:(h + 1) * 64], in_=pd[:, 0:64], func=AF.Copy, scale=st4[i][:, 3:4]),
                                 reads=[rd, R_st[i]], writes=[R_ytok])
                    for tt in range(0 if need_ctx else 2, NT):
                        jj = tt % 2
                        pt, rp = ps_next()
                        ptb = pt[:].bitcast(BF16)
                        for q in range(4):
                            k.op("pe", lambda e, ptb=ptb, q=q, tt=tt: e.transpose(ptb[:, q * 128:(q + 1) * 128], ytok[:, tt, q * 128:(q + 1) * 128], ident_bf[:, :]), reads=[R_ytok, R_id], writes=[rp])
                        k.op("act", lambda e, ptb=ptb, jj=jj: e.activation(out=yfm[jj][:].rearrange("p q t -> p (q t)"), in_=ptb[:, 0:512], func=AF.Copy), reads=[rp], writes=[R_yfm[jj]])
                        k.dma("sp", YT[b][:, 4:8, tt * 128:(tt + 1) * 128], yfm[jj][:], reads=[R_yfm[jj]], writes=[R_YT[b]])
            k.barrier()

        def stage_wout(l, last):
            with ExitStack() as s1:
                w = sb("wo_w", [128, 8, D], BF16, s1); R_w = Res()
                for kc in range(8):
                    k.dma("pool", w[:, kc, :], I["w_out"][l, :, kc, :], writes=[R_w])
                yb = [sb("wo_y%d" % i, [128, 8, 256], BF16, s1) for i in range(2)]; R_yb = [Res(), Res()]
                xb = [sb("wo_x%d" % i, [128, 8, 256], F32, s1) for i in range(2)]; R_xb = [Res(), Res()]
                n = 0
                for b in range(NB):
                    for t0 in range(LC if last else 0, S, 256):
                        j = n % 2; n += 1
                        wq = which_of(b, t0)
                        k.dma("sp", yb[j][:], YT[b][:, :, t0:t0 + 256], reads=[R_YT[b]], writes=[R_yb[j]])
                        k.dma("sp", xb[j][:], XT[b][:, :, t0:t0 + 256], reads=[R_XT[b]], writes=[R_xb[j]])
                        for oc in range(8):
                            pt, rp = ps_next()
                            for kc in range(8):
                                k.op("pe", lambda e, pt=pt, j=j, oc=oc, kc=kc: e.matmul(pt[:, 0:256], w[:, kc, oc * 128:(oc + 1) * 128], yb[j][:, kc, :],
                                     start=(kc == 0), stop=(kc == 7)), reads=[R_w, R_yb[j]], writes=[rp])
                            k.op("dve", lambda e, pt=pt, j=j, oc=oc, wq=wq: e.scalar_tensor_tensor(out=xb[j][:, oc, :], in0=pt[:, 0:256],
                                 scalar=MOD[:, l, 16 + oc, wq:wq + 1], in1=xb[j][:, oc, :], op0=ALU.mult, op1=ALU.add), reads=[rp, R_mod, R_xb[j]], writes=[R_xb[j]])
                        k.dma("sp", XT[b][:, :, t0:t0 + 256], xb[j][:], reads=[R_xb[j]], writes=[R_XT[b]])
            k.barrier()

        def stage_router(l, last):
            i_moe = l // 2
            with ExitStack() as s1:
                rw = sb("rt_w", [128, 8, NE], F32, s1); R_rw = Res()
                k.dma("sp", rw[:], I["router"][i_moe], writes=[R_rw])
                hb = [sb("rt_h%d" % i, [128, 8, 128], F32, s1) for i in range(2)]; R_hb = [Res(), Res()]
                lg = [sb("rt_l%d" % i, [128, 40], F32, s1) for i in range(2)]; R_lg = [Res(), Res()]
                ge = [sb("rt_ge%d" % i, [128, 128], F32, s1) for i in range(2)]; R_ge = [Res(), Res()]
                gb = [sb("rt_gb%d" % i, [128, NE, 128], F32, s1) for i in range(2)]; R_gb = [Res(), Res()]
                n = 0; m = 0
                for b in range(NB):
                    for tt in range(2 if last else 0, NT):
                        j = n % 2; n += 1
                        t0 = tt * 128
                        k.dma("sp", hb[j][:], H2F[b][:, :, t0:t0 + 128], reads=[R_H2F[b]], writes=[R_hb[j]])
                        pt, rp = ps_next()
                        for kc in range(8):
                            k.op("pe", lambda e, pt=pt, j=j, kc=kc: e.matmul(pt[:, 0:NE], hb[j][:, kc, :], rw[:, kc, :], start=(kc == 0), stop=(kc == 7)),
                                 reads=[R_hb[j], R_rw], writes=[rp])
                        L = lg[j]
                        k.op("dve", lambda e, pt=pt, L=L: e.tensor_copy(out=L[:, 0:8], in_=pt[:, 0:8]), reads=[rp], writes=[R_lg[j]])
                        k.op("dve", lambda e, L=L: e.tensor_reduce(out=L[:, 8:9], in_=L[:, 0:8], axis=AX.X, op=ALU.max), reads=[R_lg[j]], writes=[R_lg[j]])
                        k.op("dve", lambda e, L=L: e.tensor_scalar(out=L[:, 9:17], in0=L[:, 0:8], scalar1=L[:, 8:9], scalar2=None, op0=ALU.is_ge), reads=[R_lg[j]], writes=[R_lg[j]])
                        k.op("dve", lambda e, L=L: e.scalar_tensor_tensor(out=L[:, 17:25], in0=L[:, 9:17], scalar=-1e30, in1=L[:, 0:8], op0=ALU.mult, op1=ALU.add), reads=[R_lg[j]], writes=[R_lg[j]])
                        k.op("dve", lambda e, L=L: e.tensor_reduce(out=L[:, 25:26], in_=L[:, 17:25], axis=AX.X, op=ALU.max), reads=[R_lg[j]], writes=[R_lg[j]])
                        k.op("dve", lambda e, L=L: e.tensor_scalar(out=L[:, 26:34], in0=L[:, 17:25], scalar1=L[:, 25:26], scalar2=None, op0=ALU.is_ge), reads=[R_lg[j]], writes=[R_lg[j]])
                        k.op("dve", lambda e, L=L: e.tensor_tensor(out=L[:, 34:35], in0=L[:, 8:9], in1=L[:, 25:26], op=ALU.subtract), reads=[R_lg[j]], writes=[R_lg[j]])
                        k.op("act", lambda e, L=L: e.activation(out=L[:, 35:36], in_=L[:, 34:35], func=AF.Sigmoid), reads=[R_lg[j]], writes=[R_lg[j]])
                        k.op("dve", lambda e, L=L: e.tensor_scalar(out=L[:, 36:37], in0=L[:, 35:36], scalar1=-1.0, scalar2=1.0, op0=ALU.mult, op1=ALU.add), reads=[R_lg[j]], writes=[R_lg[j]])
                        k.op("dve", lambda e, L=L: e.tensor_scalar(out=L[:, 9:17], in0=L[:, 9:17], scalar1=L[:, 35:36], scalar2=None, op0=ALU.mult), reads=[R_lg[j]], writes=[R_lg[j]])
                        k.op("dve", lambda e, L=L: e.scalar_tensor_tensor(out=L[:, 9:17], in0=L[:, 26:34], scalar=L[:, 36:37], in1=L[:, 9:17], op0=ALU.mult, op1=ALU.add), reads=[R_lg[j]], writes=[R_lg[j]])
                        for ex in range(NE):
                            i2 = m % 2; m += 1
                            k.op("pool", lambda e, L=L, i2=i2, ex=ex: e.tensor_copy(out=ge[i2][:], in_=L[:, 9 + ex:10 + ex].to_broadcast([128, 128])), reads=[R_lg[j]], writes=[R_ge[i2]])
                            pg, rg = ps_next()
                            k.op("pe", lambda e, pg=pg, i2=i2: e.matmul(pg[:, 0:128], ge[i2][:], ident[:], start=True, stop=True), reads=[R_ge[i2], R_id], writes=[rg])
                            k.op("act", lambda e, pg=pg, j=j, ex=ex: e.activation(out=gb[j][:, ex, :], in_=pg[:, 0:128], func=AF.Copy), reads=[rg], writes=[R_gb[j]])
                        k.dma("sp", GB[b][:, :, t0:t0 + 128], gb[j][:], reads=[R_gb[j]], writes=[R_GB[b]])
            k.barrier()

        def stage_ffn(l, last):
            moe = (l % 2 == 1)
            i_w = l // 2
            F = DFE if moe else DFF
            GW = 512 if moe else 256
            ng = F // GW
            nfc = GW // 128
            tstart = LC if last else 0
            with ExitStack() as s1:
                hT = sb("ff_h", [128, 8, S], BF16, s1); R_h = Res()
                yacc = sb("ff_y", [128, 8, S], F32, s1); R_ya = [Res() for _ in range(9)]
                w1 = [sb("ff_w1%d" % i, [128, 8, GW], BF16, s1) for i in range(2)]
                w3 = [sb("ff_w3%d" % i, [128, 8, GW], BF16, s1) for i in range(2)]
                w2 = [sb("ff_w2%d" % i, [128, nfc, D], BF16, s1) for i in range(2)]
                R_wg = [Res(), Res()]
                sg = [sb("ff_s%d" % i, [128, 256], F32, s1) for i in range(2)]; R_sg = [Res(), Res()]
                ac = [sb("ff_a%d" % i, [128, nfc, 256], BF16, s1) for i in range(2)]; R_ac = [Res(), Res()]
                gt = [sb("ff_g%d" % i, [128, 256], F32, s1) for i in range(2)]; R_gt = [Res(), Res()]
                xb = [sb("ff_x%d" % i, [128, 8, 256], F32, s1) for i in range(2)]; R_xb = [Res(), Res()]
                nw = 0; na_ = 0; ns = 0; ngt = 0
                for b in range(NB):
                    k.dma("sp", hT[:, :, tstart:S], H2T[b][:, :, tstart:S], reads=[R_H2T[b]], writes=[R_h])
                    first = True
                    for ex in range(NE if moe else 1):
                        for g in range(ng):
                            jw = nw % 2; nw += 1
                            if moe:
                                s_w1, s_w3, s_w2 = I["moe_w1"][i_w, ex], I["moe_w3"][i_w, ex], I["moe_w2"][i_w, ex]
                            else:
                                s_w1, s_w3, s_w2 = I["ffn_w1"][i_w], I["ffn_w3"][i_w], I["ffn_w2"][i_w]
                            k.dma("pool", w1[jw][:], s_w1[:, :, g * GW:(g + 1) * GW], writes=[R_wg[jw]])
                            k.dma("pool", w3[jw][:], s_w3[:, :, g * GW:(g + 1) * GW], writes=[R_wg[jw]])
                            k.dma("pool", w2[jw][:], s_w2[:, g * nfc:(g + 1) * nfc, :], writes=[R_wg[jw]])
                            for tb in range(tstart // 256, S // 256):
                                t0 = tb * 256
                                ja = na_ % 2; na_ += 1
                                if moe:
                                    jg = ngt % 2; ngt += 1
                                    k.dma("sp", gt[jg][:], GB[b][:, ex, t0:t0 + 256], reads=[R_GB[b]], writes=[R_gt[jg]])
                                for fc in range(nfc):
                                    p1, r1 = ps_next()
                                    for kc in range(8):
                                        k.op("pe", lambda e, p1=p1, jw=jw, fc=fc, kc=kc, t0=t0: e.matmul(p1[:, 0:256], w1[jw][:, kc, fc * 128:(fc + 1) * 128], hT[:, kc, t0:t0 + 256],
                                             start=(kc == 0), stop=(kc == 7)), reads=[R_wg[jw], R_h], writes=[r1])
                                    p3, r3 = ps_next()
                                    for kc in range(8):
                                        k.op("pe", lambda e, p3=p3, jw=jw, fc=fc, kc=kc, t0=t0: e.matmul(p3[:, 0:256], w3[jw][:, kc, fc * 128:(fc + 1) * 128], hT[:, kc, t0:t0 + 256],
                                             start=(kc == 0), stop=(kc == 7)), reads=[R_wg[jw], R_h], writes=[r3])
                                    js = ns % 2; ns += 1
                                    k.op("act", lambda e, p1=p1, js=js: e.activation(out=sg[js][:], in_=p1[:, 0:256], func=AF.Silu), reads=[r1], writes=[R_sg[js]])
                                    if moe:
                                        k.op("pool", lambda e, js=js, jg=jg: e.tensor_tensor(out=sg[js][:], in0=sg[js][:], in1=gt[jg][:], op=ALU.mult), reads=[R_sg[js], R_gt[jg]], writes=[R_sg[js]])
                                    k.op("dve", lambda e, p3=p3, js=js, ja=ja, fc=fc: e.tensor_tensor(out=ac[ja][:, fc, :], in0=sg[js][:], in1=p3[:, 0:256], op=ALU.mult),
                                         reads=[R_sg[js], r3], writes=[R_ac[ja]])
                                for oc in range(8):
                                    po, ro = ps_next()
                                    for fc in range(nfc):
                                        k.op("pe", lambda e, po=po, jw=jw, ja=ja, fc=fc, oc=oc: e.matmul(po[:, 0:256], w2[jw][:, fc, oc * 128:(oc + 1) * 128], ac[ja][:, fc, :],
                                             start=(fc == 0), stop=(fc == nfc - 1)), reads=[R_wg[jw], R_ac[ja]], writes=[ro])
                                    if first:
                                        k.op("act", lambda e, po=po, oc=oc, t0=t0: e.activation(out=yacc[:, oc, t0:t0 + 256], in_=po[:, 0:256], func=AF.Copy), reads=[ro], writes=[R_ya[tb]])
                                    else:
                                        k.op("dve", lambda e, po=po, oc=oc, t0=t0: e.tensor_tensor(out=yacc[:, oc, t0:t0 + 256], in0=yacc[:, oc, t0:t0 + 256], in1=po[:, 0:256], op=ALU.add),
                                             reads=[ro, R_ya[tb]], writes=[R_ya[tb]])
                            first = False
                    for tb in range(tstart // 256, S // 256):
                        t0 = tb * 256
                        j = tb % 2
                        wq = which_of(b, t0)
                        k.dma("sp", xb[j][:], XT[b][:, :, t0:t0 + 256], reads=[R_XT[b]], writes=[R_xb[j]])
                        for oc in range(8):
                            k.op("dve", lambda e, j=j, oc=oc, t0=t0, wq=wq: e.scalar_tensor_tensor(out=xb[j][:, oc, :], in0=yacc[:, oc, t0:t0 + 256],
                                 scalar=MOD[:, l, 40 + oc, wq:wq + 1], in1=xb[j][:, oc, :], op0=ALU.mult, op1=ALU.add), reads=[R_ya[tb], R_mod, R_xb[j]], writes=[R_xb[j]])
                        k.dma("sp", XT[b][:, :, t0:t0 + 256], xb[j][:], reads=[R_xb[j]], writes=[R_XT[b]])
            k.barrier()

        def stage_final():
            with ExitStack() as s1:
                fg = sb("fn_g", [128, 8], F32, s1); R_fg = Res()
                k.dma("sp", fg[:], I["final_g"][:, :], writes=[R_fg])
                xb = [sb("fx%d" % i, [128, 8, 256], F32, s1) for i in range(2)]; R_xb = [Res(), Res()]
                sq = [sb("fsq%d" % i, [128, 8, 256], F32, s1) for i in range(2)]; R_sq = [Res(), Res()]
                rs = [sb("frs%d" % i, [128, 256], F32, s1) for i in range(2)]; R_rs = [Res(), Res()]
                ob = [sb("fo%d" % i, [128, 8, 256], F32, s1) for i in range(2)]; R_ob = [Res(), Res()]
                n = 0
                outs = []
                for b in range(NB):
                    for t0 in range(LC, S, 256):
                        j = n % 2; n += 1
                        k.dma("sp", xb[j][:], XT[b][:, :, t0:t0 + 256], reads=[R_XT[b]], writes=[R_xb[j]])
                        k.op("act", lambda e, j=j: e.activation(out=sq[j][:], in_=xb[j][:], func=AF.Square), reads=[R_xb[j]], writes=[R_sq[j]])
                        pt, rp = ps_next()
                        for c in range(8):
                            k.op("pe", lambda e, pt=pt, j=j, c=c: e.matmul(pt[:, 0:256], ones[:], sq[j][:, c, :], start=(c == 0), stop=(c == 7)), reads=[R_ones, R_sq[j]], writes=[rp])
                        k.op("act", lambda e, pt=pt, j=j: e.activation(out=rs[j][:], in_=pt[:, 0:256], func=AF.Sqrt, bias=eps_t[:, 0:1], scale=1.0 / D), reads=[rp, R_eps], writes=[R_rs[j]])
                        k.op("dve", lambda e, j=j: e.reciprocal(out=rs[j][:], in_=rs[j][:]), reads=[R_rs[j]], writes=[R_rs[j]])
                        for c in range(8):
                            k.op("dve", lambda e, j=j, c=c: e.scalar_tensor_tensor(out=ob[j][:, c, :], in0=xb[j][:, c, :], scalar=fg[:, c:c + 1], in1=rs[j][:],
                                 op0=ALU.mult, op1=ALU.mult), reads=[R_xb[j], R_fg, R_rs[j]], writes=[R_ob[j]])
                        outs.append(k.dma("sp", out[b, :, :, t0 - LC:t0 - LC + 256], ob[j][:], reads=[R_ob[j]]))
                return outs

        def stage_rwkv(l, need_ctx):
            with ExitStack() as s1:
                NSET = 3
                def T_(name, shape, dt=F32, n=3):
                    return [sb(name + str(i), shape, dt, s1) for i in range(n)], [Res() for _ in range(n)]
                w2 = sb("rw_w2", [128, 512], F32, s1); a2 = sb("rw_a2", [128, 512], F32, s1); g2 = sb("rw_g2", [128, 512], F32, s1)
                w0b = sb("rw_w0b", [64, 2, 512], F32, s1); a0T = sb("rw_a0T", [128, 2, 4], F32, s1)
                kkp = sb("rw_kkp", [128, 4], F32, s1); kap = sb("rw_kap", [128, 4], F32, s1); okap = sb("rw_okap", [128, 4], F32, s1)
                rkp = sb("rw_rkp", [128, 4], F32, s1)
                m2 = sb("rw_m2", [64, 2, 128], F32, s1); m3 = sb("rw_m3", [64, 2, 128], F32, s1); mL = sb("rw_mL", [64, 2, 64], F32, s1)
                bones = sb("rw_bones", [128, 128], F32, s1); sel = sb("rw_sel", [128, 2], F32, s1)
                c1 = sb("rw_c1", [128, 4], F32, s1)
                R_par = Res()
                for dst, src in ((w2, I["rw_w2"][l]), (a2, I["rw_a2"][l]), (g2, I["rw_g2"][l]), (a0T, I["rw_a0T"][l]),
                                 (kkp, I["rw_kk"][l]), (kap, I["rw_ka"][l]), (rkp, I["rw_rk"][l]),
                                 (m2, I["rw_m2"]), (m3, I["rw_m3"]), (mL, I["rw_mL"]), (bones, I["rw_bones"]), (sel, I["rw_sel"]), (c1, I["rw_c1"]),
                                 (w0b, I["rw_w0b"][l])):
                    k.dma("sp", dst[:], src, writes=[R_par])
                k.op("dve", lambda e: e.tensor_scalar(out=okap[:], in0=kap[:], scalar1=-1.0, scalar2=1.0, op0=ALU.mult, op1=ALU.add), reads=[R_par], writes=[R_par])
                Hst_b = [sb("rw_H%d" % i, [128, 4, 64], F32, s1) for i in range(NB)]; R_H_b = [Res() for _ in range(NB)]
                ub, R_ub = T_("rw_u", [128, 15, 64])
                twl, R_twl = T_("rw_twl", [128, 64])
                sgl, R_sgl = T_("rw_sgl", [128, 64], n=2)
                e2a, R_e2a = T_("rw_e2a", [64, 512], n=2)
                e2b, R_e2b = T_("rw_e2b", [64, 512], n=2)
                a_sb, R_a = T_("rw_a", [128, 4, 64])
                a1_sb, R_a1 = T_("rw_a1", [128, 4, 64], n=2)
                kr, R_kr = T_("rw_kr", [128, 4, 64])
                sq, R_sq = T_("rw_sq", [128, 4, 64])
                kk_t, R_kk = T_("rw_kkt", [128, 4, 64])
                ff, R_ff = T_("rw_ff", [128, 4, 64])
                kd, R_kd = T_("rw_kd", [128, 4, 64])
                bb, R_bb = T_("rw_bb", [128, 4, 64])
                EG, R_EG = T_("rw_EG", [128, 4, 2, 64])
                IEG, R_IEG = T_("rw_IEG", [128, 4, 64])
                fm, R_fm = T_("rw_fm", [128, 4, 4, 64])
                tm, R_tm = T_("rw_tm", [64, 4, 4, 128])
                LA, R_LA = T_("rw_LA", [64, 8, 128])
                NBt, R_NB = T_("rw_NB", [64, 8, 128])
                Lm, R_Lm = T_("rw_Lm", [64, 8, 64])
                Ll32, R_Ll32 = T_("rw_Ll32", [64, 8, 64])
                Pb, R_Pb = T_("rw_Pb", [64, 8, 64], BF16)
                Pm, R_Pm = T_("rw_Pm", [64, 8, 64])
                Nl, R_Nl = T_("rw_Nl", [64, 8, 64], BF16, n=4)
                Ll, R_Ll = T_("rw_Ll", [64, 8, 64], BF16, n=4)
                WT, R_WT = T_("rw_WT", [128, 4, 64])
                Xa, R_Xa = T_("rw_Xa", [64, 8, 64])
                Uta, R_Uta = T_("rw_Uta", [64, 8, 64])
                Ua, R_Ua = T_("rw_Ua", [64, 8, 64])
                ysb, R_ysb = T_("rw_ysb", [64, 512])
                htmp, R_htmp = T_("rw_htmp", [128, 4, 64])
                rk_t, R_rk = T_("rw_rkt", [128, 4, 64], n=2)
                rkh, R_rkh = T_("rw_rkh", [64, 8], n=2)
                bon, R_bon = T_("rw_bon", [64, 512], n=2)
                gsb, R_gsb = T_("rw_gsb", [64, 512], n=2)
                n = 0
                nl4 = [0]
                nb_run = NB if RW_DBG["nb"] is None else RW_DBG["nb"]
                for d in range(RW_DBG["nd"]):
                    for b in range(nb_run):
                        k.op("pool", lambda e, b=b: e.memset(Hst_b[b][:], 0.0), writes=[R_H_b[b]])
                    order = list(range(36)) if d == 0 else [3, 2, 1, 0] + list(range(35, 3, -1))
                    if RW_DBG["nch"] is not None:
                        order = order[:RW_DBG["nch"]]
                    last_col = 63 if d == 0 else 0
                    for ch in order:
                      for b in range(nb_run):
                        j = n % NSET; j2 = n % 2; n += 1
                        def chunk_body(b=b, ch=ch, d=d, j=j, j2=j2, last_col=last_col, Hst=Hst_b[b], R_H=R_H_b[b]):
                            if (not need_ctx) and False:
                                pass
                            t0 = ch * 64
                            U_ = ub[j]
                            k.dma("sp", U_[:], UT[b][:, :, t0:t0 + 64], reads=[R_UT[b]], writes=[R_ub[j]])
                            k.op("act", lambda e, j=j, U_=U_: e.activation(out=twl[j][:], in_=U_[:, 12, :], func=AF.Tanh), reads=[R_ub[j]], writes=[R_twl[j]])
                            pt, rp = ps_next()
                            k.op("pe", lambda e, pt=pt, j=j, d=d: e.matmul(pt[0:64, :], twl[j][d * 64:(d + 1) * 64, :], w2[d * 64:(d + 1) * 64, :], start=True, stop=True),
                                 reads=[R_twl[j], R_par], writes=[rp])
                            k.op("dve", lambda e, j2=j2, pt=pt, j=j, d=d: e.tensor_tensor(out=e2a[j2][:], in0=pt[0:64, :], in1=w0b[:, d, :], op=ALU.add), reads=[rp, R_par], writes=[R_e2a[j2]])
                            k.op("act", lambda e, j2=j2, j=j: e.activation(out=e2b[j2][:], in_=e2a[j2][:], func=AF.Exp, scale=-1.0), reads=[R_e2a[j2]], writes=[R_e2b[j2]])
                            k.op("act", lambda e, j2=j2, j=j: e.activation(out=e2a[j2][:], in_=e2b[j2][:], func=AF.Ln, bias=c1[0:64, 0:1], scale=1.0), reads=[R_e2b[j2], R_par], writes=[R_e2a[j2]])
                            k.op("act", lambda e, j2=j2, j=j: e.activation(out=e2b[j2][:], in_=e2a[j2][:], func=AF.Exp, bias=c1[0:64, 1:2], scale=-1.0), reads=[R_e2a[j2], R_par], writes=[R_e2b[j2]])
                            def a_path(dd, dst, R_dst):
                                pa, ra = ps_next()
                                for q in range(4):
                                    k.op("pe", lambda e, pa=pa, q=q, dd=dd, U_=U_: e.matmul(pa[:, q * 64:(q + 1) * 64], a2[dd * 64:(dd + 1) * 64, q * 128:(q + 1) * 128], U_[dd * 64:(dd + 1) * 64, 13, :], start=True, stop=True),
                                         reads=[R_par, R_ub[j]], writes=[ra])
                                for q in range(4):
                                    k.op("act", lambda e, pa=pa, q=q, dd=dd, dst=dst: e.activation(out=dst[:, q, :], in_=pa[:, q * 64:(q + 1) * 64], func=AF.Sigmoid, bias=a0T[:, dd, q:q + 1], scale=1.0),
                                         reads=[ra, R_par], writes=[R_dst])
                            a_path(d, a_sb[j], R_a[j])
                            k.op("dve", lambda e, j=j, U_=U_: e.tensor_tensor(out=kr[j][:], in0=U_[:, 4:8, :], in1=kkp[:, :].unsqueeze(2).to_broadcast([128, 4, 64]), op=ALU.mult), reads=[R_ub[j], R_par], writes=[R_kr[j]])
                            k.op("pool", lambda e, j=j: e.tensor_tensor(out=sq[j][:], in0=kr[j][:], in1=kr[j][:], op=ALU.mult), reads=[R_kr[j]], writes=[R_sq[j]])
                            pn_, rn = ps_next()
                            k.op("pe", lambda e, pn_=pn_, j=j: e.matmul(pn_[:, 0:256], bones[:], sq[j][:].rearrange("p q t -> p (q t)"), start=True, stop=True), reads=[R_par, R_sq[j]], writes=[rn])
                            k.op("act", lambda e, pn_=pn_, j=j: e.activation(out=sq[j][:].rearrange("p q t -> p (q t)"), in_=pn_[:, 0:256], func=AF.Sqrt), reads=[rn], writes=[R_sq[j]])
                            k.op("dve", lambda e, j=j: e.tensor_scalar(out=sq[j][:], in0=sq[j][:], scalar1=1e-12, scalar2=None, op0=ALU.max), reads=[R_sq[j]], writes=[R_sq[j]])
                            k.op("dve", lambda e, j=j: e.reciprocal(out=sq[j][:], in_=sq[j][:]), reads=[R_sq[j]], writes=[R_sq[j]])
                            k.op("dve", lambda e, j=j: e.tensor_tensor(out=kk_t[j][:], in0=kr[j][:], in1=sq[j][:], op=ALU.mult), reads=[R_kr[j], R_sq[j]], writes=[R_kk[j]])
                            k.op("pool", lambda e, j=j: e.tensor_tensor(out=ff[j][:], in0=a_sb[j][:], in1=kap[:, :].unsqueeze(2).to_broadcast([128, 4, 64]), op=ALU.mult), reads=[R_a[j], R_par], writes=[R_ff[j]])
                            k.op("pool", lambda e, j=j: e.tensor_tensor(out=ff[j][:], in0=ff[j][:], in1=okap[:, :].unsqueeze(2).to_broadcast([128, 4, 64]), op=ALU.add), reads=[R_ff[j], R_par], writes=[R_ff[j]])
                            k.op("pool", lambda e, j=j, U_=U_: e.tensor_tensor(out=kd[j][:], in0=U_[:, 4:8, :], in1=ff[j][:], op=ALU.mult), reads=[R_ub[j], R_ff[j]], writes=[R_kd[j]])
                            k.op("pool", lambda e, j=j: e.tensor_tensor(out=bb[j][:], in0=kk_t[j][:], in1=a_sb[j][:], op=ALU.mult), reads=[R_kk[j], R_a[j]], writes=[R_bb[j]])
                            pc, rc = ps_next()
                            for q in range(4):
                                k.op("pe", lambda e, j2=j2, pc=pc, q=q, j=j, d=d: e.matmul(pc[:, q * 128:(q + 1) * 128], e2b[j2][:, q * 128:(q + 1) * 128], m2[:, d, :], start=True, stop=True),
                                     reads=[R_e2b[j2], R_par], writes=[rc])
                            k.op("act", lambda e, pc=pc, j=j: e.activation(out=EG[j][:].rearrange("p q s t -> p (q s t)"), in_=pc[:, :], func=AF.Exp, scale=-1.0), reads=[rc], writes=[R_EG[j]])
                            k.op("act", lambda e, pc=pc, j=j: e.activation(out=IEG[j][:], in_=pc[:, :].rearrange("p (q s t) -> p q s t", q=4, s=2)[:, :, 1, :], func=AF.Exp, scale=1.0), reads=[rc], writes=[R_IEG[j]])
                            F_ = fm[j]
                            k.op("dve", lambda e, j=j, F_=F_: e.tensor_tensor(out=F_[:, :, 0, :], in0=kd[j][:], in1=IEG[j][:], op=ALU.mult), reads=[R_kd[j], R_IEG[j]], writes=[R_fm[j]])
                            k.op("dve", lambda e, j=j, F_=F_: e.tensor_tensor(out=F_[:, :, 1, :], in0=bb[j][:], in1=IEG[j][:], op=ALU.mult), reads=[R_bb[j], R_IEG[j]], writes=[R_fm[j]])
                            k.op("pool", lambda e, j=j, F_=F_: e.tensor_tensor(out=F_[:, :, 2, :], in0=kk_t[j][:], in1=EG[j][:, :, 0, :], op=ALU.mult), reads=[R_kk[j], R_EG[j]], writes=[R_fm[j]])
                            k.op("pool", lambda e, j=j, F_=F_, U_=U_: e.tensor_tensor(out=F_[:, :, 3, :], in0=U_[:, 0:4, :], in1=EG[j][:, :, 1, :], op=ALU.mult), reads=[R_ub[j], R_EG[j]], writes=[R_fm[j]])
                            T_m = tm[j]
                            for kind, (srcf, sc) in enumerate(((lambda q, F_=F_: F_[:, q, 2, :], 1.0), (lambda q, F_=F_: F_[:, q, 0, :], 1.0), (lambda q, F_=F_: F_[:, q, 1, :], -1.0), (lambda q, U_=U_: U_[:, 8 + q, :], 1.0))):
                                ptx, rtx = ps_next()
                                for q in range(4):
                                    k.op("pe", lambda e, ptx=ptx, q=q, srcf=srcf: e.transpose(ptx[0:64, q * 128:(q + 1) * 128], srcf(q), ident[:, :]),
                                         reads=[R_fm[j], R_ub[j], R_id], writes=[rtx])
                                eng = "act" if kind % 2 == 0 else "dve"
                                if eng == "act":
                                    k.op("act", lambda e, ptx=ptx, kind=kind, sc=sc, T_m=T_m: e.activation(out=T_m[:, :, kind, :], in_=ptx[0:64, :].rearrange("p (q c) -> p q c", q=4), func=AF.Copy, scale=sc),
                                         reads=[rtx], writes=[R_tm[j]])
                                else:
                                    k.op("dve", lambda e, ptx=ptx, kind=kind, sc=sc, T_m=T_m: e.tensor_scalar(out=T_m[:, :, kind, :], in0=ptx[0:64, :].rearrange("p (q c) -> p q c", q=4), scalar1=sc, scalar2=None, op0=ALU.mult),
                                         reads=[rtx], writes=[R_tm[j]])
                            if RW_DBG["upto"] < "A2":
                                return
                            if d == 0 and (need_ctx or ch >= 4):
                                a_path(1, a1_sb[j2], R_a1[j2])
                                k.op("dve", lambda e, j2=j2, j=j: e.tensor_tensor(out=a1_sb[j2][:], in0=a1_sb[j2][:], in1=a_sb[j][:], op=ALU.add), reads=[R_a1[j2], R_a[j]], writes=[R_a1[j2]])
                                k.op("dve", lambda e, j2=j2, j=j: e.scalar_tensor_tensor(out=a1_sb[j2][:], in0=a1_sb[j2][:], scalar=0.5, in1=kap[:, :].unsqueeze(2).to_broadcast([128, 4, 64]), op0=ALU.mult, op1=ALU.mult), reads=[R_a1[j2], R_par], writes=[R_a1[j2]])
                                k.op("dve", lambda e, j2=j2, j=j: e.tensor_tensor(out=a1_sb[j2][:], in0=a1_sb[j2][:], in1=okap[:, :].unsqueeze(2).to_broadcast([128, 4, 64]), op=ALU.add), reads=[R_a1[j2], R_par], writes=[R_a1[j2]])
                                k.op("dve", lambda e, j2=j2, j=j, U_=U_: e.tensor_tensor(out=rk_t[j2][:], in0=a1_sb[j2][:], in1=U_[:, 4:8, :], op=ALU.mult), reads=[R_a1[j2], R_ub[j]], writes=[R_rk[j2]])
                                k.op("dve", lambda e, j2=j2, j=j, U_=U_: e.tensor_tensor(out=rk_t[j2][:], in0=rk_t[j2][:], in1=U_[:, 0:4, :], op=ALU.mult), reads=[R_rk[j2], R_ub[j]], writes=[R_rk[j2]])
                                k.op("dve", lambda e, j2=j2, j=j: e.tensor_tensor(out=rk_t[j2][:], in0=rk_t[j2][:], in1=rkp[:, :].unsqueeze(2).to_broadcast([128, 4, 64]), op=ALU.mult), reads=[R_rk[j2], R_par], writes=[R_rk[j2]])
                                pr, rr = ps_next()
                                for q in range(4):
                                    k.op("pe", lambda e, j2=j2, pr=pr, q=q, j=j: e.matmul(pr[0:64, q * 2:(q + 1) * 2], rk_t[j2][:, q, :], sel[:, :], start=True, stop=True), reads=[R_rk[j2], R_par], writes=[rr])
                                k.op("act", lambda e, j2=j2, pr=pr, j=j: e.activation(out=rkh[j2][:], in_=pr[0:64, 0:8], func=AF.Copy), reads=[rr], writes=[R_rkh[j2]])
                                k.op("dve", lambda e, j2=j2, j=j, T_m=T_m: e.tensor_tensor(out=bon[j2][:].rearrange("p (q h v) -> p q h v", q=4, h=2), in0=T_m[:, :, 3, :].rearrange("p q (h v) -> p q h v", h=2),
                                     in1=rkh[j2][:, :].rearrange("p (q h) -> p q h", q=4).unsqueeze(3).to_broadcast([64, 4, 2, 64]), op=ALU.mult), reads=[R_tm[j], R_rkh[j2]], writes=[R_bon[j2]])
                                k.dma("sp", BON[b][t0:t0 + 64, :], bon[j2][:], reads=[R_bon[j2]], writes=[R_BON[b]])
                                k.op("act", lambda e, j2=j2, j=j, U_=U_: e.activation(out=sgl[j2][:], in_=U_[:, 14, :], func=AF.Sigmoid), reads=[R_ub[j]], writes=[R_sgl[j2]])
                                pg, rg = ps_next()
                                k.op("pe", lambda e, j2=j2, pg=pg, j=j: e.matmul(pg[0:64, :], sgl[j2][:], g2[:], start=True, stop=True), reads=[R_sgl[j2], R_par], writes=[rg])
                                k.op("act", lambda e, j2=j2, pg=pg, j=j: e.activation(out=gsb[j2][:], in_=pg[0:64, :], func=AF.Copy), reads=[rg], writes=[R_gsb[j2]])
                                k.dma("sp", GG[b][t0:t0 + 64, :], gsb[j2][:], reads=[R_gsb[j2]], writes=[R_GG[b]])
                            if RW_DBG["upto"] < "B":
                                return
                            def hp(h):
                                return h // 2, (h % 2) * 64
                            hv = lambda t, h2: t[:].rearrange("p (q h) c -> p q h c", q=4, h=2)[:, :, h2, :]
                            for h2 in range(2):
                                p0 = h2 * 64
                                p1, r1 = ps_next()
                                p2, r2 = ps_next()
                                p3, r3 = ps_next()
                                for q in range(4):
                                    k.op("pe", lambda e, p1=p1, q=q, p0=p0, F_=F_: e.matmul(p1[0:64, q * 128:(q + 1) * 128], F_[p0:p0 + 64, q, 0, :], F_[p0:p0 + 64, q, 2:4, :].rearrange("p s t -> p (s t)"), start=True, stop=True),
                                         reads=[R_fm[j]], writes=[r1])
                                    k.op("pe", lambda e, p2=p2, q=q, p0=p0, F_=F_: e.matmul(p2[0:64, q * 128:(q + 1) * 128], F_[p0:p0 + 64, q, 1, :], F_[p0:p0 + 64, q, 2:4, :].rearrange("p s t -> p (s t)"), start=True, stop=True),
                                         reads=[R_fm[j]], writes=[r2])
                                    k.op("pe", lambda e, p3=p3, q=q, p0=p0, F_=F_: e.matmul(p3[0:64, q * 64:(q + 1) * 64], F_[p0:p0 + 64, q, 2, :], F_[p0:p0 + 64, q, 1, :], start=True, stop=True), reads=[R_fm[j]], writes=[r3])
                                k.op("dve", lambda e, p1=p1, h2=h2, j=j, d=d: e.tensor_tensor(out=hv(LA[j], h2), in0=p1[0:64, :].rearrange("p (q c) -> p q c", q=4),
                                     in1=m2[:, d, :].unsqueeze(1).to_broadcast([64, 4, 128]), op=ALU.mult), reads=[r1, R_par], writes=[R_LA[j]])
                                k.op("dve", lambda e, p2=p2, h2=h2, j=j, d=d: e.tensor_tensor(out=hv(NBt[j], h2), in0=p2[0:64, :].rearrange("p (q c) -> p q c", q=4),
                                     in1=m3[:, d, :].unsqueeze(1).to_broadcast([64, 4, 128]), op=ALU.mult), reads=[r2, R_par], writes=[R_NB[j]])
                                k.op("dve", lambda e, p3=p3, h2=h2, j=j, d=d: e.tensor_tensor(out=hv(Lm[j], h2), in0=p3[0:64, 0:256].rearrange("p (q c) -> p q c", q=4),
                                     in1=mL[:, d, :].unsqueeze(1).to_broadcast([64, 4, 64]), op=ALU.mult), reads=[r3, R_par], writes=[R_Lm[j]])
                            if RW_DBG["upto"] < "B0":
                                return
                            k.op("dve", lambda e, j=j: e.scalar_tensor_tensor(out=Pm[j][:], in0=NBt[j][:, :, 0:64], scalar=-1.0, in1=ident[0:64, 0:64].unsqueeze(1).to_broadcast([64, 8, 64]), op0=ALU.mult, op1=ALU.add),
                                 reads=[R_NB[j], R_id], writes=[R_Pm[j]])
                            Ncur = lambda h, j=j: NBt[j][:, h, 0:64]
                            Lcur = lambda h, j=j: Lm[j][:, h, :]
                            R_Nc, R_Lc = R_NB[j], R_Lm[j]
                            nlev = RW_DBG.get("nlev", 5)
                            for lev in range(nlev):
                                i4 = nl4[0] % 4; nl4[0] += 1
                                pL, rL = ps_next()
                                for h in range(8):
                                    k.op("pe", lambda e, pL=pL, h=h, Ncur=Ncur, Lcur=Lcur: e.matmul(pL[0:64, h * 64:(h + 1) * 64], Ncur(h), Lcur(h), start=True, stop=True), reads=[R_Nc, R_Lc], writes=[rL])
                                if lev == 0:
                                    k.op("act", lambda e, pL=pL, j=j: e.activation(out=Ll32[j][:].rearrange("p h c -> p (h c)"), in_=pL[0:64, :], func=AF.Copy), reads=[rL], writes=[R_Ll32[j]])
                                    k.op("pool", lambda e, i4=i4, j=j: e.tensor_copy(out=Ll[i4][:], in_=Ll32[j][:]), reads=[R_Ll32[j]], writes=[R_Ll[i4]])
                                else:
                                    k.op("act", lambda e, pL=pL, i4=i4: e.activation(out=Ll[i4][:].rearrange("p h c -> p (h c)"), in_=pL[0:64, :], func=AF.Copy), reads=[rL], writes=[R_Ll[i4]])
                                if lev < nlev - 1:
                                    pN, rN = ps_next()
                                    for h in range(8):
                                        k.op("pe", lambda e, pN=pN, h=h, Ncur=Ncur, Lcur=Lcur: e.matmul(pN[0:64, h * 64:(h + 1) * 64], Lcur(h), Ncur(h), start=True, stop=True), reads=[R_Nc, R_Lc], writes=[rN])
                                    k.op("dve", lambda e, pN=pN, i4=i4: e.tensor_copy(out=Nl[i4][:].rearrange("p h c -> p (h c)"), in_=pN[0:64, :]), reads=[rN], writes=[R_Nl[i4]])
                                pP, rP = ps_next()
                                for h in range(8):
                                    if lev == 0:
                                        k.op("pe", lambda e, pP=pP, h=h, j=j: e.matmul(pP[0:64, h * 64:(h + 1) * 64], Ll32[j][:, h, :], Pm[j][:, h, :], start=True, stop=True), reads=[R_Ll32[j], R_Pm[j]], writes=[rP])
                                    else:
                                        k.op("pe", lambda e, pP=pP, h=h, i4=i4, j=j: e.matmul(pP[0:64, h * 64:(h + 1) * 64], Ll[i4][:, h, :], Pb[j][:, h, :], start=True, stop=True), reads=[R_Ll[i4], R_Pb[j]], writes=[rP])
                                k.op("dve", lambda e, pP=pP, j=j: e.tensor_tensor(out=Pm[j][:].rearrange("p h c -> p (h c)"), in0=Pm[j][:].rearrange("p h c -> p (h c)"), in1=pP[0:64, :], op=ALU.add), reads=[rP, R_Pm[j]], writes=[R_Pm[j]])
                                if lev < nlev - 1:
                                    k.op("act", lambda e, j=j: e.activation(out=Pb[j][:], in_=Pm[j][:], func=AF.Copy), reads=[R_Pm[j]], writes=[R_Pb[j]])
                                Ncur = lambda h, i4=i4: Nl[i4][:, h, :]
                                Lcur = lambda h, i4=i4: Ll[i4][:, h, :]
                                R_Nc, R_Lc = R_Nl[i4], R_Ll[i4]
                            if RW_DBG["upto"] < "B2":
                                return
                            pW, rW = ps_next()
                            for h in range(8):
                                q, p0 = hp(h)
                                k.op("pe", lambda e, pW=pW, h=h, q=q, j=j, T_m=T_m: e.matmul(pW[:, h * 64:(h + 1) * 64], T_m[:, q, 0, :], Pm[j][:, h, :], start=True, stop=True), reads=[R_tm[j], R_Pm[j]], writes=[rW])
                            for h2 in range(2):
                                k.op("act" if h2 == 0 else "dve",
                                     (lambda e, pW=pW, j=j, h2=h2: e.activation(out=WT[j][h2 * 64:(h2 + 1) * 64, :, :], in_=pW[h2 * 64:(h2 + 1) * 64, :].rearrange("p (q h i) -> p q h i", q=4, h=2)[:, :, h2, :], func=AF.Copy)) if h2 == 0 else
                                     (lambda e, pW=pW, j=j, h2=h2: e.tensor_copy(out=WT[j][h2 * 64:(h2 + 1) * 64, :, :], in_=pW[h2 * 64:(h2 + 1) * 64, :].rearrange("p (q h i) -> p q h i", q=4, h=2)[:, :, h2, :])),
                                     reads=[rW], writes=[R_WT[j]])
                            pX, rX = ps_next()
                            for h in range(8):
                                q, p0 = hp(h)
                                k.op("pe", lambda e, pX=pX, h=h, q=q, p0=p0, j=j, T_m=T_m: e.matmul(pX[0:64, h * 64:(h + 1) * 64], LA[j][:, h, 0:64], T_m[:, q, 3, p0:p0 + 64], start=True, stop=True), reads=[R_LA[j], R_tm[j]], writes=[rX])
                            k.op("act", lambda e, pX=pX, j=j: e.activation(out=Xa[j][:].rearrange("p h c -> p (h c)"), in_=pX[0:64, :], func=AF.Copy), reads=[rX], writes=[R_Xa[j]])
                            pU, rU = ps_next()
                            for h in range(8):
                                k.op("pe", lambda e, pU=pU, h=h, j=j: e.matmul(pU[0:64, h * 64:(h + 1) * 64], Pm[j][:, h, :], Xa[j][:, h, :], start=True, stop=True), reads=[R_Pm[j], R_Xa[j]], writes=[rU])
                            k.op("act", lambda e, pU=pU, j=j: e.activation(out=Uta[j][:].rearrange("p h c -> p (h c)"), in_=pU[0:64, :], func=AF.Copy), reads=[rU], writes=[R_Uta[j]])
                            if RW_DBG["upto"] < "C":
                                return
                            for h2 in range(2):
                                p0 = h2 * 64
                                pS, rS = ps_next()
                                for q in range(4):
                                    k.op("pe", lambda e, Hst=Hst, pS=pS, q=q, p0=p0, j=j: e.matmul(pS[0:64, q * 64:(q + 1) * 64], WT[j][p0:p0 + 64, q, :], Hst[p0:p0 + 64, q, :], start=True, stop=True), reads=[R_WT[j], R_H], writes=[rS])
                                k.op("dve", lambda e, pS=pS, j=j, h2=h2: e.tensor_tensor(out=hv(Ua[j], h2), in0=hv(Uta[j], h2), in1=pS[0:64, 0:256].rearrange("p (q c) -> p q c", q=4), op=ALU.add),
                                     reads=[rS, R_Uta[j]], writes=[R_Ua[j]])
                            if RW_DBG["upto"] < "C1":
                                return
                            pY, rY = ps_next()
                            for h in range(8):
                                q, p0 = hp(h)
                                k.op("pe", lambda e, pY=pY, h=h, q=q, p0=p0, j=j, T_m=T_m: e.matmul(pY[0:64, h * 64:(h + 1) * 64], LA[j][:, h, 64:128], T_m[:, q, 3, p0:p0 + 64], start=True, stop=False), reads=[R_LA[j], R_tm[j]], writes=[rY])
                                k.op("pe", lambda e, pY=pY, h=h, j=j: e.matmul(pY[0:64, h * 64:(h + 1) * 64], NBt[j][:, h, 64:128], Ua[j][:, h, :], start=False, stop=True), reads=[R_NB[j], R_Ua[j]], writes=[rY])
                            k.op("act", lambda e, pY=pY, j=j: e.activation(out=ysb[j][:], in_=pY[0:64, :], func=AF.Copy), reads=[rY], writes=[R_ysb[j]])
                            for h2 in range(2):
                                p0 = h2 * 64
                                pR, rR = ps_next()
                                for q in range(4):
                                    k.op("pe", lambda e, Hst=Hst, pR=pR, q=q, p0=p0, j=j, F_=F_: e.matmul(pR[0:64, q * 64:(q + 1) * 64], F_[p0:p0 + 64, q, 3, :], Hst[p0:p0 + 64, q, :], start=True, stop=True), reads=[R_fm[j], R_H], writes=[rR])
                                yv = lambda t, h2: t[:].rearrange("p (q h c) -> p q h c", q=4, h=2)[:, :, h2, :]
                                k.op("dve", lambda e, pR=pR, j=j, h2=h2, yv=yv: e.tensor_tensor(out=yv(ysb[j], h2), in0=yv(ysb[j], h2), in1=pR[0:64, 0:256].rearrange("p (q c) -> p q c", q=4), op=ALU.add),
                                     reads=[rR, R_ysb[j]], writes=[R_ysb[j]])
                            k.dma("sp", YD[d][b][t0:t0 + 64, :], ysb[j][:], reads=[R_ysb[j]], writes=[R_YD[d][b]])
                            if RW_DBG["upto"] < "C2":
                                return
                            pH, rH = ps_next()
                            for q in range(4):
                                k.op("pe", lambda e, pH=pH, q=q, T_m=T_m: e.matmul(pH[:, q * 128:(q + 1) * 128], T_m[:, q, 1, :], T_m[:, q, 3, :], start=True, stop=False), reads=[R_tm[j]], writes=[rH])
                                k.op("pe", lambda e, pH=pH, q=q, j=j, T_m=T_m: e.matmul(pH[:, q * 128:(q + 1) * 128], T_m[:, q, 2, :], Ua[j][:, 2 * q:2 * q + 2, :].rearrange("p h c -> p (h c)"), start=False, stop=True), reads=[R_tm[j], R_Ua[j]], writes=[rH])
                            for h2 in range(2):
                                ps_ = slice(h2 * 64, (h2 + 1) * 64)
                                k.op("dve", lambda e, Hst=Hst, pH=pH, j=j, h2=h2, ps_=ps_: e.tensor_tensor(out=htmp[j][ps_, :, :], in0=pH[ps_, :].rearrange("p (q h v) -> p q h v", q=4, h=2)[:, :, h2, :], in1=Hst[ps_, :, :], op=ALU.add),
                                     reads=[rH, R_H], writes=[R_htmp[j]])
                                k.op("dve", lambda e, Hst=Hst, j=j, ps_=ps_, last_col=last_col: e.tensor_tensor(out=Hst[ps_, :, :], in0=htmp[j][ps_, :, :], in1=EG[j][ps_, :, 1, last_col:last_col + 1].to_broadcast([64, 4, 64]), op=ALU.mult),
                                     reads=[R_htmp[j], R_EG[j]], writes=[R_H])
                        chunk_body()
            k.barrier()
            if not RW_DBG["readout"]:
                return
            with ExitStack() as s1:
                lnw_ro = sb("ro_lnw", [128, 512], F32, s1); lnb_ro = sb("ro_lnb", [128, 512], F32, s1); c1_ro = sb("ro_c1", [128, 4], F32, s1); R_par_ro = Res()
                k.dma("sp", lnw_ro[:], I["rw_lnw"][l], writes=[R_par_ro]); k.dma("sp", lnb_ro[:], I["rw_lnb"][l], writes=[R_par_ro]); k.dma("sp", c1_ro[:], I["rw_c1"], writes=[R_par_ro])
                def T2(name, shape, dt=F32):
                    return [sb(name + str(i), shape, dt, s1) for i in range(2)], [Res(), Res()]
                y0_ro, R_y0_ro = T2("ro_y0", [128, 512]); y1_ro, R_y1_ro = T2("ro_y1", [128, 512]); bo_ro, R_bo_ro = T2("ro_bo", [128, 512]); gg_ro, R_gg_ro = T2("ro_gg", [128, 512])
                stt__ro, R_stt_ro = T2("ro_st", [128, 32]); yc_ro, R_yc_ro = T2("ro_yc", [128, 512]); sq_ro, R_sq_ro = T2("ro_sq", [128, 512]); ot_ro, R_ot_ro = T2("ro_ot", [128, 4, 128], BF16)
                n = 0
                for b in range(NB):
                    for tt in range(0 if need_ctx else 2, NT):
                        j = n % 2; n += 1
                        t0 = tt * 128
                        k.dma("sp", y0_ro[j][:], YD[0][b][t0:t0 + 128, :], reads=[R_YD[0][b]], writes=[R_y0_ro[j]])
                        k.dma("sp", y1_ro[j][:], YD[1][b][t0:t0 + 128, :], reads=[R_YD[1][b]], writes=[R_y1_ro[j]])
                        k.dma("sp", bo_ro[j][:], BON[b][t0:t0 + 128, :], reads=[R_BON[b]], writes=[R_bo_ro[j]])
                        k.dma("sp", gg_ro[j][:], GG[b][t0:t0 + 128, :], reads=[R_GG[b]], writes=[R_gg_ro[j]])
                        S_ = stt__ro[j]
                        v3 = lambda t: t[:].rearrange("p (h v) -> p h v", h=8)
                        bc = lambda a: a.unsqueeze(2).to_broadcast([128, 8, 64])
                        k.op("dve", lambda e, j=j: e.tensor_tensor(out=y0_ro[j][:], in0=y0_ro[j][:], in1=y1_ro[j][:], op=ALU.add), reads=[R_y0_ro[j], R_y1_ro[j]], writes=[R_y0_ro[j]])
                        k.op("dve", lambda e, j=j, S_=S_: e.tensor_reduce(out=S_[:, 0:8], in_=v3(y0_ro[j]), axis=AX.X, op=ALU.add), reads=[R_y0_ro[j]], writes=[R_stt_ro[j]])
                        k.op("dve", lambda e, S_=S_: e.tensor_scalar(out=S_[:, 8:16], in0=S_[:, 0:8], scalar1=1.0 / 64, scalar2=None, op0=ALU.mult), reads=[R_stt_ro[j]], writes=[R_stt_ro[j]])
                        k.op("dve", lambda e, j=j, S_=S_: e.tensor_tensor(out=v3(yc_ro[j]), in0=v3(y0_ro[j]), in1=bc(S_[:, 8:16]), op=ALU.subtract), reads=[R_y0_ro[j], R_stt_ro[j]], writes=[R_yc_ro[j]])
                        k.op("pool", lambda e, j=j: e.tensor_tensor(out=sq_ro[j][:], in0=yc_ro[j][:], in1=yc_ro[j][:], op=ALU.mult), reads=[R_yc_ro[j]], writes=[R_sq_ro[j]])
                        k.op("dve", lambda e, j=j, S_=S_: e.tensor_reduce(out=S_[:, 16:24], in_=v3(sq_ro[j]), axis=AX.X, op=ALU.add), reads=[R_sq_ro[j]], writes=[R_stt_ro[j]])
                        k.op("act", lambda e, S_=S_: e.activation(out=S_[:, 24:32], in_=S_[:, 16:24], func=AF.Sqrt, bias=c1_ro[:, 2:3], scale=1.0 / 64), reads=[R_stt_ro[j], R_par_ro], writes=[R_stt_ro[j]])
                        k.op("dve", lambda e, S_=S_: e.reciprocal(out=S_[:, 24:32], in_=S_[:, 24:32]), reads=[R_stt_ro[j]], writes=[R_stt_ro[j]])
                        k.op("dve", lambda e, j=j, S_=S_: e.tensor_tensor(out=v3(yc_ro[j]), in0=v3(yc_ro[j]), in1=bc(S_[:, 24:32]), op=ALU.mult), reads=[R_yc_ro[j], R_stt_ro[j]], writes=[R_yc_ro[j]])
                        k.op("pool", lambda e, j=j: e.tensor_tensor(out=yc_ro[j][:], in0=yc_ro[j][:], in1=lnw_ro[:], op=ALU.mult), reads=[R_yc_ro[j], R_par_ro], writes=[R_yc_ro[j]])
                        k.op("pool", lambda e, j=j: e.tensor_tensor(out=yc_ro[j][:], in0=yc_ro[j][:], in1=lnb_ro[:], op=ALU.add), reads=[R_yc_ro[j], R_par_ro], writes=[R_yc_ro[j]])
                        k.op("dve", lambda e, j=j: e.tensor_tensor(out=yc_ro[j][:], in0=yc_ro[j][:], in1=bo_ro[j][:], op=ALU.add), reads=[R_yc_ro[j], R_bo_ro[j]], writes=[R_yc_ro[j]])
                        k.op("dve", lambda e, j=j: e.tensor_tensor(out=yc_ro[j][:], in0=yc_ro[j][:], in1=gg_ro[j][:], op=ALU.mult), reads=[R_yc_ro[j], R_gg_ro[j]], writes=[R_yc_ro[j]])
                        pt, rp = ps_next()
                        for q in range(4):
                            k.op("pe", lambda e, pt=pt, q=q, j=j: e.transpose(pt[:, q * 128:(q + 1) * 128], yc_ro[j][:, q * 128:(q + 1) * 128], ident[:, :]), reads=[R_yc_ro[j], R_id], writes=[rp])
                        k.op("act", lambda e, pt=pt, j=j: e.activation(out=ot_ro[j][:].rearrange("p q t -> p (q t)"), in_=pt[:, :], func=AF.Copy), reads=[rp], writes=[R_ot_ro[j]])
                        k.dma("sp", YT[b][:, 0:4, t0:t0 + 128], ot_ro[j][:], reads=[R_ot_ro[j]], writes=[R_YT[b]])
            k.barrier()

        toks = []
        for l in range(n_layers):
            if stages is None or "norm1" in stages:
                stage_norm(l, 0, 0, HT, R_HT)
            if stages is None or "proj" in stages:
                stage_proj(l)
            last = (l == DEPTH - 1)
            if stages is None or "na" in stages:
                stage_na(l, not last, **na_kw)
            if stages is None or "rwkv" in stages:
                stage_rwkv(l, not last)
            if stages is None or "wout" in stages:
                stage_wout(l, last)
            if stages is None or "ffn" in stages:
                moe = (l % 2 == 1)
                tsel = range(LC, S, 256) if last else None
                if moe:
                    stage_norm(l, 1, 3, H2T, R_H2T, dst32=H2F, R_dst32=R_H2F, tsel=tsel)
                    stage_router(l, last)
                else:
                    stage_norm(l, 1, 3, H2T, R_H2T, tsel=tsel)
                stage_ffn(l, last)
        if stages is None or "final" in stages:
            fin_out = stage_final()
        else:
            fin_out = []

        fin = list(fin_out)
        for r in R_XT + R_HT + R_QK + R_VN + R_UT + R_YT + R_YD[0] + R_YD[1] + R_BON + R_GG:
            if r.w is not None:
                fin.append(r.w)
        k.final_wait("sp", fin)
        k.emit()
        print("insts", k.n_inst, "epochs", k.n_epochs)
    return nc


def _fm(a, nchunk):
    return np.ascontiguousarray(a.reshape(nchunk, 128, -1).transpose(1, 0, 2))


def _build_bfull(rpb):
    L, H = rpb.shape[:2]
    q = np.arange(64)[:, None]; kc = np.arange(64)[None, :]
    lo = np.clip(q - 8, 0, 48)
    valid = (kc >= lo) & (kc < lo + 16)
    idx = np.clip(kc - q + 15, 0, 30)
    g = rpb[:, :, :, idx]
    g = np.where(valid[None, None, None], g, np.float32(NEG)).astype(np.float32)
    return np.ascontiguousarray(g.transpose(0, 3, 1, 2, 4).reshape(L, 64, H, 960))


def _prep_shared(inp):
    m = {}
    m["ada_w"] = np.stack([_fm(inp["ada_w"][l], 8) for l in range(4)])
    m["ada_bT"] = np.stack([inp["ada_b"][l].reshape(48, 128).T.copy() for l in range(4)])
    m["g_mix"] = np.stack([inp["norm_mix_g"][l].reshape(8, 128).T.copy() for l in range(4)])
    m["g_ffn"] = np.stack([inp["norm_ffn_g"][l].reshape(8, 128).T.copy() for l in range(4)])
    m["w_in"] = np.stack([_fm(inp["w_in"][l], 8) for l in range(4)])
    m["mu"] = np.stack([inp["shift_mu"][l].reshape(15, 128).T.copy() for l in range(4)])
    m["ident"] = np.eye(128, dtype=np.float32)
    m["final_g"] = inp["final_g"].reshape(8, 128).T.copy()
    m["bfull"] = _build_bfull(inp["na_rpb"])
    m["w_out"] = np.stack([_fm(inp["w_out"][l], 8) for l in range(4)])
    m["rw_w2"] = inp["w2"].reshape(4, 128, 512); m["rw_a2"] = inp["a2"].reshape(4, 128, 512); m["rw_g2"] = inp["g2"]
    m["rw_a0T"] = np.stack([inp["a0"][l].reshape(2, 4, 128).transpose(2, 0, 1) for l in range(4)])
    c4 = lambda a: np.stack([a[l].reshape(4, 128).T for l in range(4)])
    m["rw_kk"] = c4(inp["k_k"]); m["rw_ka"] = c4(inp["k_a"]); m["rw_rk"] = c4(inp["r_k"].reshape(4, 512))
    m["rw_lnw"] = np.broadcast_to(inp["ln_x_w"][:, None, :], (4, 128, 512)); m["rw_lnb"] = np.broadcast_to(inp["ln_x_b"][:, None, :], (4, 128, 512))
    m["rw_w0b"] = np.broadcast_to(inp["w0"][:, None, :, :], (4, 64, 2, 512))
    idx = np.arange(64)
    strict = [(idx[:, None] < idx[None, :]), (idx[:, None] > idx[None, :])]
    incl = [(idx[:, None] <= idx[None, :]), (idx[:, None] >= idx[None, :])]
    m["rw_m2"] = np.stack([np.concatenate([strict[d], incl[d]], 1) for d in range(2)], 1).astype(np.float32)
    m["rw_m3"] = np.stack([np.concatenate([strict[d].astype(np.float32), -incl[d].astype(np.float32)], 1) for d in range(2)], 1)
    m["rw_mL"] = np.stack([strict[d].T for d in range(2)], 1).astype(np.float32)
    bo = np.zeros((128, 128), np.float32); bo[:64, :64] = 1; bo[64:, 64:] = 1
    m["rw_bones"] = bo
    se = np.zeros((128, 2), np.float32); se[:64, 0] = 1; se[64:, 1] = 1
    m["rw_sel"] = se
    m["rw_c1"] = np.tile(np.array([1.0, -0.5, 64e-5, 1e-12], np.float32)[None], (128, 1))
    m["ffn_w1"] = np.stack([_fm(inp["ffn_w1"][i], 8) for i in range(2)])
    m["ffn_w3"] = np.stack([_fm(inp["ffn_w3"][i], 8) for i in range(2)])
    m["ffn_w2"] = np.stack([_fm(inp["ffn_w2"][i], DFF // 128) for i in range(2)])
    m["router"] = np.stack([_fm(inp["router"][i], 8) for i in range(2)])
    m["moe_w1"] = np.stack([np.stack([_fm(inp["moe_w1"][i, e], 8) for e in range(NE)]) for i in range(2)])
    m["moe_w3"] = np.stack([np.stack([_fm(inp["moe_w3"][i, e], 8) for e in range(NE)]) for i in range(2)])
    m["moe_w2"] = np.stack([np.stack([_fm(inp["moe_w2"][i, e], DFE // 128) for e in range(NE)]) for i in range(2)])
    return {k_: np.ascontiguousarray(v, dtype=np.float32) for k_, v in m.items()}


def _prep_core(inp, core):
    bs = [2 * core, 2 * core + 1]
    m = {}
    xcat = [np.concatenate([inp["ctx"][b], inp["x"][b]], 0) for b in bs]
    m["xT"] = np.stack([_fm(np.ascontiguousarray(xc.T), 8) for xc in xcat]).astype(np.float32)
    cc = np.stack([inp["c"][bs[0]], inp["c"][bs[1]], inp["c_ctx"]], 1)
    m["cT"] = _fm(cc, 8).astype(np.float32)
    return m


def kernel(**inputs):
    inp = {k_: np.asarray(v) for k_, v in inputs.items()}
    n = 8
    shared = _prep_shared(inp)
    in_maps = []
    for core in range(n):
        m = dict(shared)
        m.update(_prep_core(inp, core))
        in_maps.append(m)
    nc = build_program()
    res = run_bass_kernel_spmd(nc, in_maps, core_ids=list(range(n)))
    outs = []
    for core in range(n):
        o = np.asarray(res.results[core]["out"])
        for b in range(NB):
            outs.append(o[b].transpose(1, 0, 2).reshape(D, T).T)
    return np.ascontiguousarray(np.stack(outs, 0)).astype(np.float32)
```

```python
import numpy as np
import concourse.bass as bass
import concourse.mybir as mybir
from concourse.bass_utils import run_bass_kernel_spmd
from contextlib import ExitStack

F32 = mybir.dt.float32
BF16 = mybir.dt.bfloat16
AF = mybir.ActivationFunctionType
ALU = mybir.AluOpType
AX = mybir.AxisListType

SAME_ENGINE_SYNC = True
DMA_RING = {"sp": 16, "act": 4, "pool": 16}
SEM_LIMIT = 60000
MAX_EPOCHS = 7


class Res:
    __slots__ = ("name", "w", "rd")

    def __init__(self, name=""):
        self.name = name
        self.w = None
        self.rd = []


class K:
    ENG = ("pe", "act", "dve", "pool", "sp")
    CE = ("pe", "act", "dve", "pool")

    def __init__(self, nc, stack):
        self.nc = nc
        self.stack = stack
        self.recs = []
        self.cnt = {e: 0 for e in self.ENG}
        self.slots = []
        self.dq = {}
        for q in ("sp", "act", "pool"):
            idx = []
            for i in range(DMA_RING[q]):
                self.slots.append(0)
                idx.append(len(self.slots) - 1)
            self.dq[q] = {"idx": idx, "n": 0}
        self.waited_e = {e: {} for e in self.ENG}
        self.waited_d = {e: {} for e in self.ENG}
        self.n_inst = 0

    def _need(self, eng, tok, waits, force=False):
        if tok is None:
            return
        if tok[0] == "e":
            _, x, seq = tok
            if x == eng and (not SAME_ENGINE_SYNC or eng == "pe") and not force:
                return
            if self.waited_e[eng].get(x, 0) >= seq:
                return
            self.waited_e[eng][x] = seq
            waits.append(tok)
        else:
            _, si, use = tok
            if self.waited_d[eng].get(si, 0) >= use:
                return
            self.waited_d[eng][si] = use
            waits.append(tok)

    def _deps(self, eng, reads, writes):
        waits = []
        for r in reads:
            self._need(eng, r.w, waits)
        for w in writes:
            self._need(eng, w.w, waits)
            best = {}
            for t in w.rd:
                key = (t[0], t[1])
                if key not in best or best[key][2] < t[2]:
                    best[key] = t
            for t in best.values():
                self._need(eng, t, waits)
        return waits

    def _commit(self, tok, reads, writes):
        for r in reads:
            if r.rd and r.rd[-1][0] == tok[0] and r.rd[-1][1] == tok[1]:
                r.rd[-1] = tok
            else:
                r.rd.append(tok)
        for w in writes:
            w.w = tok
            w.rd = []

    def op(self, eng, fn, reads=(), writes=()):
        waits = self._deps(eng, reads, writes)
        self.cnt[eng] += 1
        tok = ("e", eng, self.cnt[eng])
        self.recs.append({"eng": eng, "waits": waits, "fn": fn, "tok": tok})
        self._commit(tok, reads, writes)
        self.n_inst += 1
        return tok

    def dma(self, q, out, in_, reads=(), writes=(), **kw):
        waits = self._deps(q, reads, writes)
        d = self.dq[q]
        si = d["idx"][d["n"] % len(d["idx"])]
        d["n"] += 1
        prev = self.slots[si]
        if prev > 0:
            self._need(q, ("d", si, prev), waits)
        self.slots[si] = prev + 1
        tok = ("d", si, prev + 1)
        fn = (lambda e, o=out, i=in_, kw=kw: e.dma_start(out=o, in_=i, **kw))
        self.recs.append({"eng": q, "waits": waits, "fn": fn, "tok": tok})
        self._commit(tok, reads, writes)
        self.n_inst += 1
        return tok

    def _all_waits(self, eng):
        waits = []
        for x in self.CE:
            if self.cnt[x] > 0:
                self._need(eng, ("e", x, self.cnt[x]), waits, force=True)
        for si, v in enumerate(self.slots):
            if v > 0:
                self._need(eng, ("d", si, v), waits)
        return waits

    def barrier(self):
        for eng in self.ENG:
            self.recs.append({"eng": eng, "waits": self._all_waits(eng), "fn": None, "tok": None})

    def final_wait(self, eng, toks):
        waits = []
        for t in toks:
            self._need(eng, t, waits)
        self.recs.append({"eng": eng, "waits": waits, "fn": None, "tok": None})

    def emit(self):
        nc, st = self.nc, self.stack
        recs = self.recs
        needed = set()
        for r in recs:
            for w in r["waits"]:
                if w[0] == "e":
                    needed.add((w[1], w[2]))
        out = []
        ep = 0
        ms = {e: 0 for e in self.CE}
        du = [0] * len(self.slots)
        last_e = {e: 0 for e in self.CE}
        last_d = [0] * len(self.slots)
        val = {}
        pend_last = {}

        def close_epoch():
            nonlocal ep, ms, du
            bw = {}
            for e in self.CE:
                if e in pend_last:
                    rr = out[pend_last[e]]
                    t = rr["tok"]
                    if t not in val:
                        ms[e] += 1
                        val[t] = (ep, ms[e])
                        rr["inc"] = True
            allw = []
            for e in self.CE:
                if e in pend_last:
                    allw.append(out[pend_last[e]]["tok"])
            for si in range(len(self.slots)):
                if du[si] > 0:
                    allw.append(("d", si, last_d[si]))
            for eng in self.ENG:
                out.append({"eng": eng, "waits": list(allw), "fn": None, "tok": None, "ep": ep})
            ep += 1
            ms = {e: 0 for e in self.CE}
            du = [0] * len(self.slots)
            pend_last.clear()

        for r in recs:
            t = r["tok"]
            if t is not None:
                if t[0] == "e":
                    if ms[t[1]] + 2 > SEM_LIMIT:
                        close_epoch()
                else:
                    if (du[t[1]] + 2) * 16 > SEM_LIMIT:
                        close_epoch()
            r = dict(r)
            r["ep"] = ep
            out.append(r)
            if t is not None:
                if t[0] == "e":
                    pend_last[t[1]] = len(out) - 1
                    if (t[1], t[2]) in needed:
                        ms[t[1]] += 1
                        val[t] = (ep, ms[t[1]])
                        r["inc"] = True
                    else:
                        r["inc"] = False
                else:
                    du[t[1]] += 1
                    last_d[t[1]] = t[2]
                    val[t] = (ep, du[t[1]] * 16)
        n_ep = ep + 1
        print("epochs needed", n_ep, "milestones", ms, "dma uses", max(du))
        assert n_ep <= MAX_EPOCHS, "too many epochs %d" % n_ep
        self.n_epochs = n_ep
        esem = [{e: st.enter_context(nc.semaphore("c%d_%s" % (i, e))) for e in self.CE} for i in range(n_ep)]
        dsem = [[st.enter_context(nc.semaphore("d%d_%d" % (i, j))) for j in range(len(self.slots))] for i in range(n_ep)]
        streams = {e: [] for e in self.ENG}
        for r in out:
            streams[r["eng"]].append(r)

        with nc.Block() as block:
            def run(engname, e):
                for ep_ in range(1, n_ep):
                    if engname in self.CE:
                        e.sem_clear(esem[ep_][engname])
                    if engname in self.dq:
                        for si in self.dq[engname]["idx"]:
                            e.sem_clear(dsem[ep_][si])
                for r in streams[engname]:
                    for w in r["waits"]:
                        v = val.get(w)
                        if v is None or v[0] != r["ep"]:
                            continue
                        if w[0] == "e":
                            e.wait_ge(esem[v[0]][w[1]], v[1])
                        else:
                            e.wait_ge(dsem[v[0]][w[1]], v[1])
                    if r["fn"] is None:
                        continue
                    ins = r["fn"](e)
                    t = r["tok"]
                    if t[0] == "e":
                        if r.get("inc"):
                            ins.then_inc(esem[r["ep"]][t[1]], 1)
                    else:
                        ins.then_inc(dsem[r["ep"]][t[1]], 16)

            @block.sync
            def _(e):
                run("sp", e)

            @block.tensor
            def _(e):
                run("pe", e)

            @block.vector
            def _(e):
                run("dve", e)

            @block.scalar
            def _(e):
                run("act", e)

            @block.gpsimd
            def _(e):
                run("pool", e)


D = 1024
DEPTH = 4
NB = 2
LC = 256
T = 2048
S = LC + T
NT = S // 128
D_IN = 3456
DFF = 2816
DFE = 3584
NE = 8
NEG = -30000.0

DEBUG_OUT = []
RW_DBG = {"nch": None, "upto": "Z", "nb": None, "nd": 2, "readout": True}
N_LAYERS_RUN = DEPTH


def build_program(n_layers=DEPTH, stages=None, debug=(), na_kw={}):
    nc = bass.Bass("TRN2", target_bir_lowering=False)
    st = ExitStack()
    with st:
        k = K(nc, st)

        def din(name, shape, dt=F32):
            return nc.dram_tensor(name, list(shape), dt, kind="ExternalInput").ap()

        def dscr(name, shape, dt=F32):
            kind = "ExternalOutput" if name in debug else "Internal"
            return nc.dram_tensor(name, list(shape), dt, kind=kind).ap()

        I = {}
        I["xT"] = din("xT", [NB, 128, 8, S])
        I["cT"] = din("cT", [128, 8, 3])
        I["ada_w"] = din("ada_w", [DEPTH, 128, 8, 6 * D])
        I["ada_bT"] = din("ada_bT", [DEPTH, 128, 48])
        I["g_mix"] = din("g_mix", [DEPTH, 128, 8])
        I["g_ffn"] = din("g_ffn", [DEPTH, 128, 8])
        I["w_in"] = din("w_in", [DEPTH, 128, 8, D_IN])
        I["mu"] = din("mu", [DEPTH, 128, 15])
        I["ident"] = din("ident", [128, 128])
        I["final_g"] = din("final_g", [128, 8])
        I["bfull"] = din("bfull", [DEPTH, 64, 8, 960])
        I["w_out"] = din("w_out", [DEPTH, 128, 8, D])
        I["rw_w2"] = din("rw_w2", [DEPTH, 128, 512]); I["rw_a2"] = din("rw_a2", [DEPTH, 128, 512]); I["rw_g2"] = din("rw_g2", [DEPTH, 128, 512])
        I["rw_a0T"] = din("rw_a0T", [DEPTH, 128, 2, 4]); I["rw_kk"] = din("rw_kk", [DEPTH, 128, 4]); I["rw_ka"] = din("rw_ka", [DEPTH, 128, 4]); I["rw_rk"] = din("rw_rk", [DEPTH, 128, 4])
        I["rw_lnw"] = din("rw_lnw", [DEPTH, 128, 512]); I["rw_lnb"] = din("rw_lnb", [DEPTH, 128, 512]); I["rw_w0b"] = din("rw_w0b", [DEPTH, 64, 2, 512])
        I["rw_m2"] = din("rw_m2", [64, 2, 128]); I["rw_m3"] = din("rw_m3", [64, 2, 128]); I["rw_mL"] = din("rw_mL", [64, 2, 64])
        I["rw_bones"] = din("rw_bones", [128, 128]); I["rw_sel"] = din("rw_sel", [128, 2]); I["rw_c1"] = din("rw_c1", [128, 4])
        I["ffn_w1"] = din("ffn_w1", [2, 128, 8, DFF]); I["ffn_w3"] = din("ffn_w3", [2, 128, 8, DFF]); I["ffn_w2"] = din("ffn_w2", [2, 128, DFF // 128, D])
        I["router"] = din("router", [2, 128, 8, NE])
        I["moe_w1"] = din("moe_w1", [2, NE, 128, 8, DFE]); I["moe_w3"] = din("moe_w3", [2, NE, 128, 8, DFE]); I["moe_w2"] = din("moe_w2", [2, NE, 128, DFE // 128, D])
        out = nc.dram_tensor("out", [NB, 128, 8, T], F32, kind="ExternalOutput").ap()

        XT = [dscr("xt%d" % b, [128, 8, S]) for b in range(NB)]
        HT = [dscr("ht%d" % b, [128, 8, S], BF16) for b in range(NB)]
        QT = [dscr("qt%d" % b, [128, 4, S], BF16) for b in range(NB)]
        KT = [dscr("kt%d" % b, [128, 4, S], BF16) for b in range(NB)]
        VN = [dscr("vn%d" % b, [S, 512], BF16) for b in range(NB)]
        UT = [dscr("ut%d" % b, [128, 15, S]) for b in range(NB)]
        R_XT = [Res() for _ in range(NB)]
        R_HT = [Res() for _ in range(NB)]
        R_QK = [Res() for _ in range(NB)]
        R_VN = [Res() for _ in range(NB)]
        R_UT = [Res() for _ in range(NB)]
        YT = [dscr("yt%d" % b, [128, 8, S], BF16) for b in range(NB)]
        H2T = [dscr("h2t%d" % b, [128, 8, S], BF16) for b in range(NB)]
        H2F = [dscr("h2f%d" % b, [128, 8, S]) for b in range(NB)]
        GB = [dscr("gb%d" % b, [128, NE, S]) for b in range(NB)]
        YD = [[dscr("yd%d_%d" % (d, b), [S, 512]) for b in range(NB)] for d in range(2)]
        R_YD = [[Res() for b in range(NB)] for d in range(2)]
        BON = [dscr("bon%d" % b, [S, 512]) for b in range(NB)]; R_BON = [Res() for _ in range(NB)]
        GG = [dscr("gg%d" % b, [S, 512]) for b in range(NB)]; R_GG = [Res() for _ in range(NB)]
        R_H2T = [Res() for _ in range(NB)]; R_H2F = [Res() for _ in range(NB)]; R_GB = [Res() for _ in range(NB)]
        R_YT = [Res() for _ in range(NB)]

        uid = [0]

        def sb(name, shape, dt=F32, stack=st):
            uid[0] += 1
            return stack.enter_context(nc.sbuf_tensor("%s_%d" % (name, uid[0]), list(shape), dt))

        ident = sb("ident_sb", [128, 128]); R_id = Res()
        ones = sb("ones_sb", [128, 128]); R_ones = Res()
        MOD = sb("mod_sb", [128, DEPTH, 48, 3]); R_mod = Res()
        GS = sb("gs_sb", [128, DEPTH, 2, 8, 3]); R_gs = Res()
        gmix = sb("gmix_sb", [128, DEPTH, 8]); gffn = sb("gffn_sb", [128, DEPTH, 8]); R_g = Res()
        eps_t = sb("eps_sb", [128, 1]); R_eps = Res()
        psum = [st.enter_context(nc.psum_tensor("ps%d" % i, [128, 512], F32)) for i in range(8)]
        R_ps = [Res() for _ in range(8)]
        pctr = [0]

        def ps_next():
            i = pctr[0] % 8
            pctr[0] += 1
            return psum[i], R_ps[i]

        k.dma("sp", ident[:], I["ident"][:, :], writes=[R_id])
        ident_bf = sb("identbf_sb", [128, 128], BF16)
        k.op("dve", lambda e: e.tensor_copy(out=ident_bf[:], in_=ident[:]), reads=[R_id], writes=[R_id])
        k.op("dve", lambda e: e.memset(ones[:], 1.0), writes=[R_ones])
        k.op("dve", lambda e: e.memset(eps_t[:], 1e-6), writes=[R_eps])
        for l in range(DEPTH):
            k.dma("sp", gmix[:, l, :], I["g_mix"][l], writes=[R_g])
            k.dma("sp", gffn[:, l, :], I["g_ffn"][l], writes=[R_g])

        with ExitStack() as s1:
            cT = sb("cT_sb", [128, 8, 3], F32, s1); R_c = Res()
            sT = sb("sT", [128, 8, 3], F32, s1); R_s = Res()
            abT = sb("abT", [128, 48], F32, s1); R_ab = Res()
            aw = [sb("aw%d" % i, [128, 8, 512], F32, s1) for i in range(2)]
            R_aw = [Res(), Res()]
            xcp = [sb("xcp%d" % i, [128, 8, 256], F32, s1) for i in range(2)]
            R_xcp = [Res(), Res()]
            k.dma("sp", cT[:], I["cT"][:, :, :], writes=[R_c])
            k.op("act", lambda e: e.activation(out=sT[:], in_=cT[:], func=AF.Silu), reads=[R_c], writes=[R_s])
            n = 0
            for b in range(NB):
                for t0 in range(0, S, 256):
                    j = n % 2; n += 1
                    k.dma("sp", xcp[j][:], I["xT"][b, :, :, t0:t0 + 256], writes=[R_xcp[j]])
                    k.dma("sp", XT[b][:, :, t0:t0 + 256], xcp[j][:], reads=[R_xcp[j]], writes=[R_XT[b]])
            n = 0
            for l in range(n_layers):
                k.dma("sp", abT[:], I["ada_bT"][l], writes=[R_ab])
                for cc in range(12):
                    j = n % 2; n += 1
                    k.dma("sp", aw[j][:], I["ada_w"][l, :, :, cc * 512:(cc + 1) * 512], writes=[R_aw[j]])
                    pt, rp = ps_next()
                    for c4 in range(4):
                        for kc in range(8):
                            k.op("pe", lambda e, pt=pt, j=j, c4=c4, kc=kc: e.matmul(
                                pt[:, c4 * 3:c4 * 3 + 3], aw[j][:, kc, c4 * 128:(c4 + 1) * 128], sT[:, kc, :],
                                start=(kc == 0), stop=(kc == 7)), reads=[R_aw[j], R_s], writes=[rp])
                    k.op("dve", lambda e, pt=pt, l=l, cc=cc: e.tensor_tensor(
                        out=MOD[:, l, cc * 4:(cc + 1) * 4, :], in0=pt[:, 0:12].rearrange("p (a b) -> p a b", b=3),
                        in1=abT[:, cc * 4:(cc + 1) * 4].unsqueeze(2).to_broadcast([128, 4, 3]), op=ALU.add),
                        reads=[rp, R_ab], writes=[R_mod])
                for which, (gt, mi) in enumerate(((gmix, 1), (gffn, 4))):
                    k.op("dve", lambda e, l=l, which=which, gt=gt, mi=mi: e.scalar_tensor_tensor(
                        out=GS[:, l, which, :, :], in0=MOD[:, l, mi * 8:(mi + 1) * 8, :], scalar=1.0,
                        in1=gt[:, l, :].unsqueeze(2).to_broadcast([128, 8, 3]), op0=ALU.add, op1=ALU.mult),
                        reads=[R_mod, R_g], writes=[R_gs])
        k.barrier()

        def which_of(b, t0):
            return 2 if t0 < LC else b

        def stage_norm(l, which, shift_idx, dst, R_dst, gsel=None, dst32=None, R_dst32=None, bsel=range(NB), tsel=None):
            with ExitStack() as s1:
                xb = [sb("nx%d" % i, [128, 8, 256], F32, s1) for i in range(2)]; R_xb = [Res(), Res()]
                sq = [sb("nsq%d" % i, [128, 8, 256], F32, s1) for i in range(2)]; R_sq = [Res(), Res()]
                rs = [sb("nrs%d" % i, [128, 256], F32, s1) for i in range(2)]; R_rs = [Res(), Res()]
                tm = [sb("ntm%d" % i, [128, 8, 256], F32, s1) for i in range(2)]; R_tm = [Res(), Res()]
                hb = [sb("nhb%d" % i, [128, 8, 256], BF16, s1) for i in range(2)]; R_hb = [Res(), Res()]
                n = 0
                for b in bsel:
                    for t0 in (tsel if tsel is not None else range(0, S, 256)):
                        j = n % 2; n += 1
                        w = which_of(b, t0)
                        k.dma("sp", xb[j][:], XT[b][:, :, t0:t0 + 256], reads=[R_XT[b]], writes=[R_xb[j]])
                        k.op("act", lambda e, j=j: e.activation(out=sq[j][:], in_=xb[j][:], func=AF.Square), reads=[R_xb[j]], writes=[R_sq[j]])
                        pt, rp = ps_next()
                        for c in range(8):
                            k.op("pe", lambda e, pt=pt, j=j, c=c: e.matmul(pt[:, 0:256], ones[:], sq[j][:, c, :], start=(c == 0), stop=(c == 7)),
                                 reads=[R_ones, R_sq[j]], writes=[rp])
                        k.op("act", lambda e, pt=pt, j=j: e.activation(out=rs[j][:], in_=pt[:, 0:256], func=AF.Sqrt, bias=eps_t[:, 0:1], scale=1.0 / D),
                             reads=[rp, R_eps], writes=[R_rs[j]])
                        k.op("dve", lambda e, j=j: e.reciprocal(out=rs[j][:], in_=rs[j][:]), reads=[R_rs[j]], writes=[R_rs[j]])
                        for c in range(8):
                            k.op("dve", lambda e, j=j, c=c, w=w: e.scalar_tensor_tensor(
                                out=tm[j][:, c, :], in0=xb[j][:, c, :], scalar=GS[:, l, which, c, w:w + 1], in1=rs[j][:],
                                op0=ALU.mult, op1=ALU.mult), reads=[R_xb[j], R_gs, R_rs[j]], writes=[R_tm[j]])
                            k.op("act", lambda e, j=j, c=c, w=w: e.activation(
                                out=hb[j][:, c, :], in_=tm[j][:, c, :], func=AF.Identity, bias=MOD[:, l, shift_idx * 8 + c, w:w + 1], scale=1.0),
                                reads=[R_tm[j], R_mod], writes=[R_hb[j]])
                            if dst32 is not None:
                                k.op("pool", lambda e, j=j, c=c, w=w: e.tensor_scalar(
                                    out=tm[j][:, c, :], in0=tm[j][:, c, :], scalar1=MOD[:, l, shift_idx * 8 + c, w:w + 1], scalar2=None, op0=ALU.add),
                                    reads=[R_tm[j], R_mod, R_hb[j]], writes=[R_tm[j]])
                        k.dma("sp", dst[b][:, :, t0:t0 + 256], hb[j][:], reads=[R_hb[j]], writes=[R_dst[b]])
                        if dst32 is not None:
                            k.dma("sp", dst32[b][:, :, t0:t0 + 256], tm[j][:], reads=[R_tm[j]], writes=[R_dst32[b]])
            k.barrier()

        def stage_proj(l):
            with ExitStack() as s1:
                w = sb("pw", [128, 8, D_IN], BF16, s1); R_w = Res()
                mu = sb("pmu", [128, 15], F32, s1); R_mu = Res()
                mu1 = sb("pmu1", [128, 15], F32, s1)
                muh = sb("pmuh", [128, 15], F32, s1)
                hb = [sb("ph%d" % i, [128, 8, 258], BF16, s1) for i in range(2)]; R_hb = [Res(), Res()]
                hs = [sb("phs%d" % i, [128, 8, 256], BF16, s1) for i in range(2)]; R_hs = [Res(), Res()]
                oq = [sb("poq%d" % i, [128, 8, 256], BF16, s1) for i in range(2)]; R_oq = [Res(), Res()]
                ov = [sb("pov%d" % i, [128, 2, 512], BF16, s1) for i in range(2)]; R_ov = [Res(), Res()]
                ou = [sb("pou%d" % i, [128, 15, 256], F32, s1) for i in range(2)]; R_ou = [Res(), Res()]
                t2 = [sb("pt2%d" % i, [128, 256], F32, s1) for i in range(2)]; R_t2 = [Res(), Res()]
                for kc in range(8):
                    k.dma("pool", w[:, kc, :], I["w_in"][l, :, kc, :], writes=[R_w])
                k.dma("sp", mu[:], I["mu"][l], writes=[R_mu])
                k.op("dve", lambda e: e.tensor_scalar(out=mu1[:], in0=mu[:], scalar1=-1.0, scalar2=1.0, op0=ALU.mult, op1=ALU.add), reads=[R_mu], writes=[R_mu])
                k.op("dve", lambda e: e.tensor_scalar(out=muh[:], in0=mu[:], scalar1=0.5, scalar2=None, op0=ALU.mult), reads=[R_mu], writes=[R_mu])
                n = 0
                for b in range(NB):
                    for t0 in range(0, S, 256):
                        j = n % 2; n += 1
                        seg0, seg1 = (0, LC) if t0 < LC else (LC, S)
                        lo, hi = max(t0 - 1, seg0), min(t0 + 257, seg1)
                        if lo == t0 or hi == t0 + 256:
                            k.op("dve", lambda e, j=j: e.memset(hb[j][:], 0.0), writes=[R_hb[j]])
                        k.dma("sp", hb[j][:, :, lo - (t0 - 1):hi - (t0 - 1)], HT[b][:, :, lo:hi], reads=[R_HT[b]], writes=[R_hb[j]])
                        k.op("dve", lambda e, j=j: e.tensor_tensor(out=hs[j][:], in0=hb[j][:, :, 0:256], in1=hb[j][:, :, 2:258], op=ALU.add),
                             reads=[R_hb[j]], writes=[R_hs[j]])
                        for cj in range(8):
                            pt, rp = ps_next()
                            for kc in range(8):
                                k.op("pe", lambda e, pt=pt, j=j, cj=cj, kc=kc: e.matmul(pt[:, 0:256], w[:, kc, cj * 128:(cj + 1) * 128], hb[j][:, kc, 1:257],
                                     start=(kc == 0), stop=(kc == 7)), reads=[R_w, R_hb[j]], writes=[rp])
                            k.op("act", lambda e, pt=pt, j=j, cj=cj: e.activation(out=oq[j][:, cj, :], in_=pt[:, 0:256], func=AF.Copy), reads=[rp], writes=[R_oq[j]])
                        k.dma("sp", QT[b][:, :, t0:t0 + 256], oq[j][:, 0:4, :], reads=[R_oq[j]], writes=[R_QK[b]])
                        k.dma("sp", KT[b][:, :, t0:t0 + 256], oq[j][:, 4:8, :], reads=[R_oq[j]], writes=[R_QK[b]])
                        for tt in range(2):
                            pt, rp = ps_next()
                            for kc in range(8):
                                k.op("pe", lambda e, pt=pt, j=j, tt=tt, kc=kc: e.matmul(pt[:, :], hb[j][:, kc, 1 + tt * 128:1 + (tt + 1) * 128], w[:, kc, 1024:1536],
                                     start=(kc == 0), stop=(kc == 7)), reads=[R_w, R_hb[j]], writes=[rp])
                            k.op("act", lambda e, pt=pt, j=j, tt=tt: e.activation(out=ov[j][:, tt, :], in_=pt[:, :], func=AF.Copy), reads=[rp], writes=[R_ov[j]])
                        k.dma("sp", VN[b][t0:t0 + 256, :].rearrange("(a p) c -> p a c", p=128), ov[j][:], reads=[R_ov[j]], writes=[R_VN[b]])
                        for cj in range(15):
                            c0 = 1536 + cj * 128
                            pa, ra = ps_next()
                            for kc in range(8):
                                k.op("pe", lambda e, pa=pa, j=j, c0=c0, kc=kc: e.matmul(pa[:, 0:256], w[:, kc, c0:c0 + 128], hb[j][:, kc, 1:257],
                                     start=(kc == 0), stop=(kc == 7)), reads=[R_w, R_hb[j]], writes=[ra])
                            pb, rb = ps_next()
                            for kc in range(8):
                                k.op("pe", lambda e, pb=pb, j=j, c0=c0, kc=kc: e.matmul(pb[:, 0:256], w[:, kc, c0:c0 + 128], hs[j][:, kc, :],
                                     start=(kc == 0), stop=(kc == 7)), reads=[R_w, R_hs[j]], writes=[rb])
                            k.op("act", lambda e, pb=pb, j=j, cj=cj: e.activation(out=t2[j][:], in_=pb[:, 0:256], func=AF.Copy, scale=muh[:, cj:cj + 1]),
                                 reads=[rb, R_mu], writes=[R_t2[j]])
                            k.op("dve", lambda e, pa=pa, j=j, cj=cj: e.scalar_tensor_tensor(out=ou[j][:, cj, :], in0=pa[:, 0:256], scalar=mu1[:, cj:cj + 1], in1=t2[j][:],
                                 op0=ALU.mult, op1=ALU.add), reads=[ra, R_mu, R_t2[j]], writes=[R_ou[j]])
                        k.dma("sp", UT[b][:, :, t0:t0 + 256], ou[j][:], reads=[R_ou[j]], writes=[R_UT[b]])
            k.barrier()

        def stage_na(l, need_ctx, hsel=range(8), bsel=range(NB)):
            NBUF = 3
            with ExitStack() as s1:
                bias = sb("na_bias", [128, 8, 960], F32, s1); R_bias = Res()
                k.dma("sp", bias[0:64], I["bfull"][l], writes=[R_bias])
                k.dma("sp", bias[64:128], I["bfull"][l], writes=[R_bias])
                qh = [sb("na_q%d" % i, [64, S], BF16, s1) for i in range(2)]
                kh = [sb("na_k%d" % i, [64, S], BF16, s1) for i in range(2)]
                VE = [sb("na_ve%d" % i, [128, 18, 64], BF16, s1) for i in range(2)]
                VO = [sb("na_vo%d" % i, [128, 18, 64], BF16, s1) for i in range(2)]
                R_in = [Res(), Res()]
                ytok = sb("na_ytok", [128, NT, 512], BF16, s1); R_ytok = Res()
                yfm = [sb("na_yfm%d" % i, [128, 4, 128], BF16, s1) for i in range(2)]; R_yfm = [Res(), Res()]
                ssb = [sb("na_s%d" % i, [128, 832], F32, s1) for i in range(NBUF)]; R_s = [Res() for _ in range(NBUF)]
                pbf = [sb("na_p%d" % i, [128, 832], BF16, s1) for i in range(NBUF)]; R_p = [Res() for _ in range(NBUF)]
                pT = [sb("na_pT%d" % i, [128, 896], BF16, s1) for i in range(NBUF)]; R_pT = [Res() for _ in range(NBUF)]
                st4 = [sb("na_st%d" % i, [128, 4], F32, s1) for i in range(NBUF)]; R_st = [Res() for _ in range(NBUF)]
                n = 0
                u = 0
                for b in bsel:
                    if not need_ctx:
                        pass
                    for h in hsel:
                        j = n % 2; n += 1
                        p0 = (h % 2) * 64
                        k.dma("sp", qh[j][:], QT[b][p0:p0 + 64, h // 2, :], reads=[R_QK[b]], writes=[R_in[j]])
                        k.dma("sp", kh[j][:], KT[b][p0:p0 + 64, h // 2, :], reads=[R_QK[b]], writes=[R_in[j]])
                        k.dma("sp", VE[j][:], VN[b][:, h * 64:(h + 1) * 64].rearrange("(a p) d -> p a d", p=128), reads=[R_VN[b]], writes=[R_in[j]])
                        k.dma("sp", VO[j][:, 0:17, :], VN[b][64:64 + 17 * 128, h * 64:(h + 1) * 64].rearrange("(a p) d -> p a d", p=128), reads=[R_VN[b]], writes=[R_in[j]])
                        k.dma("sp", VO[j][0:64, 17, :], VN[b][S - 64:S, h * 64:(h + 1) * 64], reads=[R_VN[b]], writes=[R_in[j]])
                        units = [("lat", r) for r in range(0, 32, 2)] + ([("ctx", 0), ("ctx", 1)] if need_ctx else [])
                        for kind, r in units:
                            i = u % NBUF; u += 1
                            b1, rb1 = ps_next()
                            if kind == "lat":
                                r0A = min(max(r - 4, 0), 24); r0B = min(max(r - 3, 0), 24)
                                R0 = min(r0A, 23)
                                tq = LC + r * 64; w0 = LC + R0 * 64
                                tile_i = tq // 128
                                b2, rb2 = ps_next()
                                k.op("pe", lambda e, b1=b1, j=j, tq=tq: e.matmul(b1[:, 0:256], qh[j][:, tq:tq + 128], kh[j][:, 0:256], start=True, stop=True), reads=[R_in[j]], writes=[rb1])
                                k.op("pe", lambda e, b1=b1, j=j, tq=tq, w0=w0: e.matmul(b1[:, 256:512], qh[j][:, tq:tq + 128], kh[j][:, w0:w0 + 256], start=True, stop=True), reads=[R_in[j]], writes=[rb1])
                                k.op("pe", lambda e, b2=b2, j=j, tq=tq, w0=w0: e.matmul(b2[:, 0:320], qh[j][:, tq:tq + 128], kh[j][:, w0 + 256:w0 + 576], start=True, stop=True), reads=[R_in[j]], writes=[rb2])
                                k.op("act", lambda e, b1=b1, i=i: e.activation(out=ssb[i][:, 0:256], in_=b1[:, 0:256], func=AF.Copy, scale=0.125), reads=[rb1], writes=[R_s[i]])
                                for half, (rq, r0X) in enumerate(((r, r0A), (r + 1, r0B))):
                                    sX = r0X - R0
                                    hp_ = slice(half * 64, (half + 1) * 64)
                                    bs0 = (r0X - rq + 7) * 64
                                    n1 = 256 - sX * 64
                                    k.op("dve", lambda e, b1=b1, i=i, h=h, hp_=hp_, sX=sX, bs0=bs0, n1=n1: e.scalar_tensor_tensor(
                                        out=ssb[i][hp_, 256 + sX * 64:512], in0=b1[hp_, 256 + sX * 64:512], scalar=0.125, in1=bias[hp_, h, bs0:bs0 + n1], op0=ALU.mult, op1=ALU.add),
                                        reads=[rb1, R_bias], writes=[R_s[i]])
                                    n2 = 512 - n1
                                    k.op("dve", lambda e, b2=b2, i=i, h=h, hp_=hp_, bs0=bs0, n1=n1, n2=n2: e.scalar_tensor_tensor(
                                        out=ssb[i][hp_, 512:512 + n2], in0=b2[hp_, 0:n2], scalar=0.125, in1=bias[hp_, h, bs0 + n1:bs0 + 512], op0=ALU.mult, op1=ALU.add),
                                        reads=[rb2, R_bias], writes=[R_s[i]])
                                    ng0 = 256 + (512 if sX == 0 else 0)
                                    k.op("pool", lambda e, i=i, hp_=hp_, ng0=ng0: e.memset(ssb[i][hp_, ng0:ng0 + 64], NEG), writes=[R_s[i]])
                                W = 832
                            else:
                                tile_i = r
                                k.op("pe", lambda e, b1=b1, j=j, r=r: e.matmul(b1[:, 0:256], qh[j][:, r * 128:(r + 1) * 128], kh[j][:, 0:256], start=True, stop=True), reads=[R_in[j]], writes=[rb1])
                                k.op("act", lambda e, b1=b1, i=i: e.activation(out=ssb[i][:, 0:256], in_=b1[:, 0:256], func=AF.Copy, scale=0.125), reads=[rb1], writes=[R_s[i]])
                                W = 256
                            k.op("dve", lambda e, i=i, W=W: e.tensor_reduce(out=st4[i][:, 0:1], in_=ssb[i][:, 0:W], axis=AX.X, op=ALU.max), reads=[R_s[i]], writes=[R_st[i]])
                            k.op("dve", lambda e, i=i: e.tensor_scalar(out=st4[i][:, 1:2], in0=st4[i][:, 0:1], scalar1=-1.0, scalar2=None, op0=ALU.mult), reads=[R_st[i]], writes=[R_st[i]])
                            k.op("act", lambda e, i=i, W=W: e.activation(out=pbf[i][:, 0:W], in_=ssb[i][:, 0:W], func=AF.Exp, bias=st4[i][:, 1:2], scale=1.0, accum_out=st4[i][:, 2:3]),
                                 reads=[R_s[i], R_st[i]], writes=[R_p[i], R_st[i]])
                            k.op("dve", lambda e, i=i: e.reciprocal(out=st4[i][:, 3:4], in_=st4[i][:, 2:3]), reads=[R_st[i]], writes=[R_st[i]])
                            pc, rc = ps_next()
                            pcb = pc[:].bitcast(BF16)
                            nch = (W + 127) // 128
                            for c in range(nch):
                                kw = min(128, W - c * 128)
                                k.op("pe", lambda e, pcb=pcb, i=i, c=c, kw=kw: e.transpose(pcb[0:kw, c * 128:(c + 1) * 128], pbf[i][:, c * 128:c * 128 + kw], ident_bf[:, :]),
                                     reads=[R_p[i], R_id], writes=[rc])
                            k.op("dve" if kind == "lat" else "act",
                                 (lambda e, pcb=pcb, i=i, nch=nch: e.tensor_copy(out=pT[i][:, 0:nch * 128], in_=pcb[:, 0:nch * 128])) if kind == "lat" else
                                 (lambda e, pcb=pcb, i=i, nch=nch: e.activation(out=pT[i][:, 0:nch * 128], in_=pcb[:, 0:nch * 128], func=AF.Copy)),
                                 reads=[rc], writes=[R_pT[i]])
                            pd, rd = ps_next()
                            for c in range(nch):
                                kw = min(128, W - c * 128)
                                if c < 2:
                                    vt = VE[j][:, c, :]
                                else:
                                    even = (R0 % 2 == 0)
                                    base_t = (w0 // 128) if even else ((w0 - 64) // 128)
                                    vt = (VE[j] if even else VO[j])[0:kw, base_t + (c - 2), :]
                                k.op("pe", lambda e, pd=pd, i=i, c=c, kw=kw, vt=vt, nch=nch: e.matmul(pd[:, 0:64], pT[i][0:kw, c * 128:(c + 1) * 128], vt, start=(c == 0), stop=(c == nch - 1)),
                                     reads=[R_in[j], R_pT[i]], writes=[rd])
                            k.op("act", lambda e, pd=pd, i=i, tile_i=tile_i, h=h: e.activation(out=ytok[:, tile_i, h * 64:(h + 1) * 64], in_=pd[:, 0:64], func=AF.Copy, scale=st4[i][:, 3:4]),
                                 reads=[rd, R_st[i]], writes=[R_ytok])
                    for tt in range(0 if need_ctx else 2, NT):
                        jj = tt % 2
                        pt, rp = ps_next()
                        ptb = pt[:].bitcast(BF16)
                        for q in range(4):
                            k.op("pe", lambda e, ptb=ptb, q=q, tt=tt: e.transpose(ptb[:, q * 128:(q + 1) * 128], ytok[:, tt, q * 128:(q + 1) * 128], ident_bf[:, :]), reads=[R_ytok, R_id], writes=[rp])
                        k.op("act", lambda e, ptb=ptb, jj=jj: e.activation(out=yfm[jj][:].rearrange("p q t -> p (q t)"), in_=ptb[:, 0:512], func=AF.Copy), reads=[rp], writes=[R_yfm[jj]])
                        k.dma("sp", YT[b][:, 4:8, tt * 128:(tt + 1) * 128], yfm[jj][:], reads=[R_yfm[jj]], writes=[R_YT[b]])
            k.barrier()

        def stage_wout(l, last):
            with ExitStack() as s1:
                w = sb("wo_w", [128, 8, D], BF16, s1); R_w = Res()
                for kc in range(8):
                    k.dma("pool", w[:, kc, :], I["w_out"][l, :, kc, :], writes=[R_w])
                yb = [sb("wo_y%d" % i, [128, 8, 256], BF16, s1) for i in range(2)]; R_yb = [Res(), Res()]
                xb = [sb("wo_x%d" % i, [128, 8, 256], F32, s1) for i in range(2)]; R_xb = [Res(), Res()]
                n = 0
                for b in range(NB):
                    for t0 in range(LC if last else 0, S, 256):
                        j = n % 2; n += 1
                        wq = which_of(b, t0)
                        k.dma("sp", yb[j][:], YT[b][:, :, t0:t0 + 256], reads=[R_YT[b]], writes=[R_yb[j]])
                        k.dma("sp", xb[j][:], XT[b][:, :, t0:t0 + 256], reads=[R_XT[b]], writes=[R_xb[j]])
                        for oc in range(8):
                            pt, rp = ps_next()
                            for kc in range(8):
                                k.op("pe", lambda e, pt=pt, j=j, oc=oc, kc=kc: e.matmul(pt[:, 0:256], w[:, kc, oc * 128:(oc + 1) * 128], yb[j][:, kc, :],
                                     start=(kc == 0), stop=(kc == 7)), reads=[R_w, R_yb[j]], writes=[rp])
                            k.op("dve", lambda e, pt=pt, j=j, oc=oc, wq=wq: e.scalar_tensor_tensor(out=xb[j][:, oc, :], in0=pt[:, 0:256],
                                 scalar=MOD[:, l, 16 + oc, wq:wq + 1], in1=xb[j][:, oc, :], op0=ALU.mult, op1=ALU.add), reads=[rp, R_mod, R_xb[j]], writes=[R_xb[j]])
                        k.dma("sp", XT[b][:, :, t0:t0 + 256], xb[j][:], reads=[R_xb[j]], writes=[R_XT[b]])
            k.barrier()

        def stage_router(l, last):
            i_moe = l // 2
            with ExitStack() as s1:
                rw = sb("rt_w", [128, 8, NE], F32, s1); R_rw = Res()
                k.dma("sp", rw[:], I["router"][i_moe], writes=[R_rw])
                hb = [sb("rt_h%d" % i, [128, 8, 128], F32, s1) for i in range(2)]; R_hb = [Res(), Res()]
                lg = [sb("rt_l%d" % i, [128, 40], F32, s1) for i in range(2)]; R_lg = [Res(), Res()]
                ge = [sb("rt_ge%d" % i, [128, 128], F32, s1) for i in range(2)]; R_ge = [Res(), Res()]
                gb = [sb("rt_gb%d" % i, [128, NE, 128], F32, s1) for i in range(2)]; R_gb = [Res(), Res()]
                n = 0; m = 0
                for b in range(NB):
                    for tt in range(2 if last else 0, NT):
                        j = n % 2; n += 1
                        t0 = tt * 128
                        k.dma("sp", hb[j][:], H2F[b][:, :, t0:t0 + 128], reads=[R_H2F[b]], writes=[R_hb[j]])
                        pt, rp = ps_next()
                        for kc in range(8):
                            k.op("pe", lambda e, pt=pt, j=j, kc=kc: e.matmul(pt[:, 0:NE], hb[j][:, kc, :], rw[:, kc, :], start=(kc == 0), stop=(kc == 7)),
                                 reads=[R_hb[j], R_rw], writes=[rp])
                        L = lg[j]
                        k.op("dve", lambda e, pt=pt, L=L: e.tensor_copy(out=L[:, 0:8], in_=pt[:, 0:8]), reads=[rp], writes=[R_lg[j]])
                        k.op("dve", lambda e, L=L: e.tensor_reduce(out=L[:, 8:9], in_=L[:, 0:8], axis=AX.X, op=ALU.max), reads=[R_lg[j]], writes=[R_lg[j]])
                        k.op("dve", lambda e, L=L: e.tensor_scalar(out=L[:, 9:17], in0=L[:, 0:8], scalar1=L[:, 8:9], scalar2=None, op0=ALU.is_ge), reads=[R_lg[j]], writes=[R_lg[j]])
                        k.op("dve", lambda e, L=L: e.scalar_tensor_tensor(out=L[:, 17:25], in0=L[:, 9:17], scalar=-1e30, in1=L[:, 0:8], op0=ALU.mult, op1=ALU.add), reads=[R_lg[j]], writes=[R_lg[j]])
                        k.op("dve", lambda e, L=L: e.tensor_reduce(out=L[:, 25:26], in_=L[:, 17:25], axis=AX.X, op=ALU.max), reads=[R_lg[j]], writes=[R_lg[j]])
                        k.op("dve", lambda e, L=L: e.tensor_scalar(out=L[:, 26:34], in0=L[:, 17:25], scalar1=L[:, 25:26], scalar2=None, op0=ALU.is_ge), reads=[R_lg[j]], writes=[R_lg[j]])
                        k.op("dve", lambda e, L=L: e.tensor_tensor(out=L[:, 34:35], in0=L[:, 8:9], in1=L[:, 25:26], op=ALU.subtract), reads=[R_lg[j]], writes=[R_lg[j]])
                        k.op("act", lambda e, L=L: e.activation(out=L[:, 35:36], in_=L[:, 34:35], func=AF.Sigmoid), reads=[R_lg[j]], writes=[R_lg[j]])
                        k.op("dve", lambda e, L=L: e.tensor_scalar(out=L[:, 36:37], in0=L[:, 35:36], scalar1=-1.0, scalar2=1.0, op0=ALU.mult, op1=ALU.add), reads=[R_lg[j]], writes=[R_lg[j]])
                        k.op("dve", lambda e, L=L: e.tensor_scalar(out=L[:, 9:17], in0=L[:, 9:17], scalar1=L[:, 35:36], scalar2=None, op0=ALU.mult), reads=[R_lg[j]], writes=[R_lg[j]])
                        k.op("dve", lambda e, L=L: e.scalar_tensor_tensor(out=L[:, 9:17], in0=L[:, 26:34], scalar=L[:, 36:37], in1=L[:, 9:17], op0=ALU.mult, op1=ALU.add), reads=[R_lg[j]], writes=[R_lg[j]])
                        for ex in range(NE):
                            i2 = m % 2; m += 1
                            k.op("pool", lambda e, L=L, i2=i2, ex=ex: e.tensor_copy(out=ge[i2][:], in_=L[:, 9 + ex:10 + ex].to_broadcast([128, 128])), reads=[R_lg[j]], writes=[R_ge[i2]])
                            pg, rg = ps_next()
                            k.op("pe", lambda e, pg=pg, i2=i2: e.matmul(pg[:, 0:128], ge[i2][:], ident[:], start=True, stop=True), reads=[R_ge[i2], R_id], writes=[rg])
                            k.op("act", lambda e, pg=pg, j=j, ex=ex: e.activation(out=gb[j][:, ex, :], in_=pg[:, 0:128], func=AF.Copy), reads=[rg], writes=[R_gb[j]])
                        k.dma("sp", GB[b][:, :, t0:t0 + 128], gb[j][:], reads=[R_gb[j]], writes=[R_GB[b]])
            k.barrier()

        def stage_ffn(l, last):
            moe = (l % 2 == 1)
            i_w = l // 2
            F = DFE if moe else DFF
            GW = 512 if moe else 256
            ng = F // GW
            nfc = GW // 128
            tstart = LC if last else 0
            with ExitStack() as s1:
                hT = sb("ff_h", [128, 8, S], BF16, s1); R_h = Res()
                yacc = sb("ff_y", [128, 8, S], F32, s1); R_ya = [Res() for _ in range(9)]
                w1 = [sb("ff_w1%d" % i, [128, 8, GW], BF16, s1) for i in range(2)]
                w3 = [sb("ff_w3%d" % i, [128, 8, GW], BF16, s1) for i in range(2)]
                w2 = [sb("ff_w2%d" % i, [128, nfc, D], BF16, s1) for i in range(2)]
                R_wg = [Res(), Res()]
                sg = [sb("ff_s%d" % i, [128, 256], F32, s1) for i in range(2)]; R_sg = [Res(), Res()]
                ac = [sb("ff_a%d" % i, [128, nfc, 256], BF16, s1) for i in range(2)]; R_ac = [Res(), Res()]
                gt = [sb("ff_g%d" % i, [128, 256], F32, s1) for i in range(2)]; R_gt = [Res(), Res()]
                xb = [sb("ff_x%d" % i, [128, 8, 256], F32, s1) for i in range(2)]; R_xb = [Res(), Res()]
                nw = 0; na_ = 0; ns = 0; ngt = 0
                for b in range(NB):
                    k.dma("sp", hT[:, :, tstart:S], H2T[b][:, :, tstart:S], reads=[R_H2T[b]], writes=[R_h])
                    first = True
                    prev = None
                    for ex in range(NE if moe else 1):
                        for g in range(ng):
                            jw = nw % 2; nw += 1
                            if moe:
                                s_w1, s_w3, s_w2 = I["moe_w1"][i_w, ex], I["moe_w3"][i_w, ex], I["moe_w2"][i_w, ex]
                            else:
                                s_w1, s_w3, s_w2 = I["ffn_w1"][i_w], I["ffn_w3"][i_w], I["ffn_w2"][i_w]
                            k.dma("pool", w1[jw][:], s_w1[:, :, g * GW:(g + 1) * GW], writes=[R_wg[jw]])
                            k.dma("pool", w3[jw][:], s_w3[:, :, g * GW:(g + 1) * GW], writes=[R_wg[jw]])
                            k.dma("pool", w2[jw][:], s_w2[:, g * nfc:(g + 1) * nfc, :], writes=[R_wg[jw]])
                            def phase_H(tb, ja, jw=jw, ex=ex, g=g):
                                nonlocal ns, ngt
                                t0 = tb * 256
                                if moe:
                                    jg = ngt % 2; ngt += 1
                                    k.dma("sp", gt[jg][:], GB[b][:, ex, t0:t0 + 256], reads=[R_GB[b]], writes=[R_gt[jg]])
                                for fc in range(nfc):
                                    ph, rh = ps_next()
                                    for kc in range(8):
                                        k.op("pe", lambda e, ph=ph, fc=fc, kc=kc, t0=t0: e.matmul(ph[:, 0:256], w1[jw][:, kc, fc * 128:(fc + 1) * 128], hT[:, kc, t0:t0 + 256],
                                             start=(kc == 0), stop=(kc == 7)), reads=[R_wg[jw], R_h], writes=[rh])
                                    for kc in range(8):
                                        k.op("pe", lambda e, ph=ph, fc=fc, kc=kc, t0=t0: e.matmul(ph[:, 256:512], w3[jw][:, kc, fc * 128:(fc + 1) * 128], hT[:, kc, t0:t0 + 256],
                                             start=(kc == 0), stop=(kc == 7)), reads=[R_wg[jw], R_h], writes=[rh])
                                    js = ns % 2; ns += 1
                                    k.op("act", lambda e, ph=ph, js=js: e.activation(out=sg[js][:], in_=ph[:, 0:256], func=AF.Silu), reads=[rh], writes=[R_sg[js]])
                                    if moe:
                                        k.op("pool", lambda e, js=js, jg=jg: e.tensor_tensor(out=sg[js][:], in0=sg[js][:], in1=gt[jg][:], op=ALU.mult), reads=[R_sg[js], R_gt[jg]], writes=[R_sg[js]])
                                    k.op("dve", lambda e, ph=ph, js=js, ja=ja, fc=fc: e.tensor_tensor(out=ac[ja][:, fc, :], in0=sg[js][:], in1=ph[:, 256:512], op=ALU.mult),
                                         reads=[R_sg[js], rh], writes=[R_ac[ja]])

                            def phase_O(tb, ja, jw=jw, first=first):
                                t0 = tb * 256
                                for ocp in range(4):
                                    po, ro = ps_next()
                                    for half in range(2):
                                        oc = 2 * ocp + half
                                        for fc in range(nfc):
                                            k.op("pe", lambda e, po=po, fc=fc, oc=oc, half=half: e.matmul(po[:, half * 256:(half + 1) * 256], w2[jw][:, fc, oc * 128:(oc + 1) * 128], ac[ja][:, fc, :],
                                                 start=(fc == 0), stop=(fc == nfc - 1)), reads=[R_wg[jw], R_ac[ja]], writes=[ro])
                                    yv_ = yacc[:, 2 * ocp:2 * ocp + 2, t0:t0 + 256]
                                    pv_ = po[:, :].rearrange("p (a c) -> p a c", a=2)
                                    if first:
                                        k.op("act", lambda e, yv_=yv_, pv_=pv_: e.activation(out=yv_, in_=pv_, func=AF.Copy), reads=[ro], writes=[R_ya[tb]])
                                    else:
                                        k.op("dve", lambda e, yv_=yv_, pv_=pv_: e.tensor_tensor(out=yv_, in0=yv_, in1=pv_, op=ALU.add), reads=[ro, R_ya[tb]], writes=[R_ya[tb]])

                            for tb in range(tstart // 256, S // 256):
                                ja = na_ % 2; na_ += 1
                                phase_H(tb, ja)
                                if prev is not None:
                                    prev[0](prev[1], prev[2])
                                prev = (phase_O, tb, ja)
                            first = False
                    prev[0](prev[1], prev[2])
                    for tb in range(tstart // 256, S // 256):
                        t0 = tb * 256
                        j = tb % 2
                        wq = which_of(b, t0)
                        k.dma("sp", xb[j][:], XT[b][:, :, t0:t0 + 256], reads=[R_XT[b]], writes=[R_xb[j]])
                        for oc in range(8):
                            k.op("dve", lambda e, j=j, oc=oc, t0=t0, wq=wq: e.scalar_tensor_tensor(out=xb[j][:, oc, :], in0=yacc[:, oc, t0:t0 + 256],
                                 scalar=MOD[:, l, 40 + oc, wq:wq + 1], in1=xb[j][:, oc, :], op0=ALU.mult, op1=ALU.add), reads=[R_ya[tb], R_mod, R_xb[j]], writes=[R_xb[j]])
                        k.dma("sp", XT[b][:, :, t0:t0 + 256], xb[j][:], reads=[R_xb[j]], writes=[R_XT[b]])
            k.barrier()

        def stage_final():
            with ExitStack() as s1:
                fg = sb("fn_g", [128, 8], F32, s1); R_fg = Res()
                k.dma("sp", fg[:], I["final_g"][:, :], writes=[R_fg])
                xb = [sb("fx%d" % i, [128, 8, 256], F32, s1) for i in range(2)]; R_xb = [Res(), Res()]
                sq = [sb("fsq%d" % i, [128, 8, 256], F32, s1) for i in range(2)]; R_sq = [Res(), Res()]
                rs = [sb("frs%d" % i, [128, 256], F32, s1) for i in range(2)]; R_rs = [Res(), Res()]
                ob = [sb("fo%d" % i, [128, 8, 256], F32, s1) for i in range(2)]; R_ob = [Res(), Res()]
                n = 0
                outs = []
                for b in range(NB):
                    for t0 in range(LC, S, 256):
                        j = n % 2; n += 1
                        k.dma("sp", xb[j][:], XT[b][:, :, t0:t0 + 256], reads=[R_XT[b]], writes=[R_xb[j]])
                        k.op("act", lambda e, j=j: e.activation(out=sq[j][:], in_=xb[j][:], func=AF.Square), reads=[R_xb[j]], writes=[R_sq[j]])
                        pt, rp = ps_next()
                        for c in range(8):
                            k.op("pe", lambda e, pt=pt, j=j, c=c: e.matmul(pt[:, 0:256], ones[:], sq[j][:, c, :], start=(c == 0), stop=(c == 7)), reads=[R_ones, R_sq[j]], writes=[rp])
                        k.op("act", lambda e, pt=pt, j=j: e.activation(out=rs[j][:], in_=pt[:, 0:256], func=AF.Sqrt, bias=eps_t[:, 0:1], scale=1.0 / D), reads=[rp, R_eps], writes=[R_rs[j]])
                        k.op("dve", lambda e, j=j: e.reciprocal(out=rs[j][:], in_=rs[j][:]), reads=[R_rs[j]], writes=[R_rs[j]])
                        for c in range(8):
                            k.op("dve", lambda e, j=j, c=c: e.scalar_tensor_tensor(out=ob[j][:, c, :], in0=xb[j][:, c, :], scalar=fg[:, c:c + 1], in1=rs[j][:],
                                 op0=ALU.mult, op1=ALU.mult), reads=[R_xb[j], R_fg, R_rs[j]], writes=[R_ob[j]])
                        outs.append(k.dma("sp", out[b, :, :, t0 - LC:t0 - LC + 256], ob[j][:], reads=[R_ob[j]]))
                return outs

        def stage_rwkv(l, need_ctx):
            with ExitStack() as s1:
                NSET = 3
                def T_(name, shape, dt=F32, n=3):
                    return [sb(name + str(i), shape, dt, s1) for i in range(n)], [Res() for _ in range(n)]
                w2 = sb("rw_w2", [128, 512], F32, s1); a2 = sb("rw_a2", [128, 512], F32, s1); g2 = sb("rw_g2", [128, 512], F32, s1)
                w0b = sb("rw_w0b", [64, 2, 512], F32, s1); a0T = sb("rw_a0T", [128, 2, 4], F32, s1)
                kkp = sb("rw_kkp", [128, 4], F32, s1); kap = sb("rw_kap", [128, 4], F32, s1); okap = sb("rw_okap", [128, 4], F32, s1)
                rkp = sb("rw_rkp", [128, 4], F32, s1)
                m2 = sb("rw_m2", [64, 2, 128], F32, s1); m3 = sb("rw_m3", [64, 2, 128], F32, s1); mL = sb("rw_mL", [64, 2, 64], F32, s1)
                bones = sb("rw_bones", [128, 128], F32, s1); sel = sb("rw_sel", [128, 2], F32, s1)
                c1 = sb("rw_c1", [128, 4], F32, s1)
                R_par = Res()
                for dst, src in ((w2, I["rw_w2"][l]), (a2, I["rw_a2"][l]), (g2, I["rw_g2"][l]), (a0T, I["rw_a0T"][l]),
                                 (kkp, I["rw_kk"][l]), (kap, I["rw_ka"][l]), (rkp, I["rw_rk"][l]),
                                 (m2, I["rw_m2"]), (m3, I["rw_m3"]), (mL, I["rw_mL"]), (bones, I["rw_bones"]), (sel, I["rw_sel"]), (c1, I["rw_c1"]),
                                 (w0b, I["rw_w0b"][l])):
                    k.dma("sp", dst[:], src, writes=[R_par])
                k.op("dve", lambda e: e.tensor_scalar(out=okap[:], in0=kap[:], scalar1=-1.0, scalar2=1.0, op0=ALU.mult, op1=ALU.add), reads=[R_par], writes=[R_par])
                Hst_b = [sb("rw_H%d" % i, [128, 4, 64], F32, s1) for i in range(NB)]; R_H_b = [Res() for _ in range(NB)]
                ub, R_ub = T_("rw_u", [128, 15, 64])
                twl, R_twl = T_("rw_twl", [128, 64])
                sgl, R_sgl = T_("rw_sgl", [128, 64], n=2)
                e2a, R_e2a = T_("rw_e2a", [64, 512], n=2)
                e2b, R_e2b = T_("rw_e2b", [64, 512], n=2)
                a_sb, R_a = T_("rw_a", [128, 4, 64])
                a1_sb, R_a1 = T_("rw_a1", [128, 4, 64], n=2)
                kr, R_kr = T_("rw_kr", [128, 4, 64])
                sq, R_sq = T_("rw_sq", [128, 4, 64])
                kk_t, R_kk = T_("rw_kkt", [128, 4, 64])
                ff, R_ff = T_("rw_ff", [128, 4, 64])
                kd, R_kd = T_("rw_kd", [128, 4, 64])
                bb, R_bb = T_("rw_bb", [128, 4, 64])
                EG, R_EG = T_("rw_EG", [128, 4, 2, 64])
                IEG, R_IEG = T_("rw_IEG", [128, 4, 64])
                fm, R_fm = T_("rw_fm", [128, 4, 4, 64])
                tm, R_tm = T_("rw_tm", [64, 4, 4, 128])
                LA, R_LA = T_("rw_LA", [64, 8, 128])
                NBt, R_NB = T_("rw_NB", [64, 8, 128])
                Lm, R_Lm = T_("rw_Lm", [64, 8, 64])
                Ll32, R_Ll32 = T_("rw_Ll32", [64, 8, 64])
                Pb, R_Pb = T_("rw_Pb", [64, 8, 64], BF16)
                Pm, R_Pm = T_("rw_Pm", [64, 8, 64])
                Nl, R_Nl = T_("rw_Nl", [64, 8, 64], BF16, n=4)
                Ll, R_Ll = T_("rw_Ll", [64, 8, 64], BF16, n=4)
                WT, R_WT = T_("rw_WT", [128, 4, 64])
                Xa, R_Xa = T_("rw_Xa", [64, 8, 64])
                Uta, R_Uta = T_("rw_Uta", [64, 8, 64])
                Ua, R_Ua = T_("rw_Ua", [64, 8, 64])
                ysb, R_ysb = T_("rw_ysb", [64, 512])
                htmp, R_htmp = T_("rw_htmp", [128, 4, 64])
                rk_t, R_rk = T_("rw_rkt", [128, 4, 64], n=2)
                rkh, R_rkh = T_("rw_rkh", [64, 8], n=2)
                bon, R_bon = T_("rw_bon", [64, 512], n=2)
                gsb, R_gsb = T_("rw_gsb", [64, 512], n=2)
                n = 0
                nl4 = [0]
                nb_run = NB if RW_DBG["nb"] is None else RW_DBG["nb"]
                for d in range(RW_DBG["nd"]):
                    for b in range(nb_run):
                        k.op("pool", lambda e, b=b: e.memset(Hst_b[b][:], 0.0), writes=[R_H_b[b]])
                    order = list(range(36)) if d == 0 else [3, 2, 1, 0] + list(range(35, 3, -1))
                    if RW_DBG["nch"] is not None:
                        order = order[:RW_DBG["nch"]]
                    last_col = 63 if d == 0 else 0
                    for ch in order:
                      for b in range(nb_run):
                        j = n % NSET; j2 = n % 2; n += 1
                        def chunk_body(b=b, ch=ch, d=d, j=j, j2=j2, last_col=last_col, Hst=Hst_b[b], R_H=R_H_b[b]):
                            if (not need_ctx) and False:
                                pass
                            t0 = ch * 64
                            U_ = ub[j]
                            k.dma("sp", U_[:], UT[b][:, :, t0:t0 + 64], reads=[R_UT[b]], writes=[R_ub[j]])
                            k.op("act", lambda e, j=j, U_=U_: e.activation(out=twl[j][:], in_=U_[:, 12, :], func=AF.Tanh), reads=[R_ub[j]], writes=[R_twl[j]])
                            pt, rp = ps_next()
                            k.op("pe", lambda e, pt=pt, j=j, d=d: e.matmul(pt[0:64, :], twl[j][d * 64:(d + 1) * 64, :], w2[d * 64:(d + 1) * 64, :], start=True, stop=True),
                                 reads=[R_twl[j], R_par], writes=[rp])
                            k.op("dve", lambda e, j2=j2, pt=pt, j=j, d=d: e.tensor_tensor(out=e2a[j2][:], in0=pt[0:64, :], in1=w0b[:, d, :], op=ALU.add), reads=[rp, R_par], writes=[R_e2a[j2]])
                            k.op("act", lambda e, j2=j2, j=j: e.activation(out=e2b[j2][:], in_=e2a[j2][:], func=AF.Exp, scale=-1.0), reads=[R_e2a[j2]], writes=[R_e2b[j2]])
                            k.op("act", lambda e, j2=j2, j=j: e.activation(out=e2a[j2][:], in_=e2b[j2][:], func=AF.Ln, bias=c1[0:64, 0:1], scale=1.0), reads=[R_e2b[j2], R_par], writes=[R_e2a[j2]])
                            k.op("act", lambda e, j2=j2, j=j: e.activation(out=e2b[j2][:], in_=e2a[j2][:], func=AF.Exp, bias=c1[0:64, 1:2], scale=-1.0), reads=[R_e2a[j2], R_par], writes=[R_e2b[j2]])
                            def a_path(dd, dst, R_dst):
                                pa, ra = ps_next()
                                for q in range(4):
                                    k.op("pe", lambda e, pa=pa, q=q, dd=dd, U_=U_: e.matmul(pa[:, q * 64:(q + 1) * 64], a2[dd * 64:(dd + 1) * 64, q * 128:(q + 1) * 128], U_[dd * 64:(dd + 1) * 64, 13, :], start=True, stop=True),
                                         reads=[R_par, R_ub[j]], writes=[ra])
                                for q in range(4):
                                    k.op("act", lambda e, pa=pa, q=q, dd=dd, dst=dst: e.activation(out=dst[:, q, :], in_=pa[:, q * 64:(q + 1) * 64], func=AF.Sigmoid, bias=a0T[:, dd, q:q + 1], scale=1.0),
                                         reads=[ra, R_par], writes=[R_dst])
                            a_path(d, a_sb[j], R_a[j])
                            k.op("dve", lambda e, j=j, U_=U_: e.tensor_tensor(out=kr[j][:], in0=U_[:, 4:8, :], in1=kkp[:, :].unsqueeze(2).to_broadcast([128, 4, 64]), op=ALU.mult), reads=[R_ub[j], R_par], writes=[R_kr[j]])
                            k.op("pool", lambda e, j=j: e.tensor_tensor(out=sq[j][:], in0=kr[j][:], in1=kr[j][:], op=ALU.mult), reads=[R_kr[j]], writes=[R_sq[j]])
                            pn_, rn = ps_next()
                            k.op("pe", lambda e, pn_=pn_, j=j: e.matmul(pn_[:, 0:256], bones[:], sq[j][:].rearrange("p q t -> p (q t)"), start=True, stop=True), reads=[R_par, R_sq[j]], writes=[rn])
                            k.op("act", lambda e, pn_=pn_, j=j: e.activation(out=sq[j][:].rearrange("p q t -> p (q t)"), in_=pn_[:, 0:256], func=AF.Sqrt), reads=[rn], writes=[R_sq[j]])
                            k.op("dve", lambda e, j=j: e.tensor_scalar(out=sq[j][:], in0=sq[j][:], scalar1=1e-12, scalar2=None, op0=ALU.max), reads=[R_sq[j]], writes=[R_sq[j]])
                            k.op("dve", lambda e, j=j: e.reciprocal(out=sq[j][:], in_=sq[j][:]), reads=[R_sq[j]], writes=[R_sq[j]])
                            k.op("dve", lambda e, j=j: e.tensor_tensor(out=kk_t[j][:], in0=kr[j][:], in1=sq[j][:], op=ALU.mult), reads=[R_kr[j], R_sq[j]], writes=[R_kk[j]])
                            k.op("pool", lambda e, j=j: e.tensor_tensor(out=ff[j][:], in0=a_sb[j][:], in1=kap[:, :].unsqueeze(2).to_broadcast([128, 4, 64]), op=ALU.mult), reads=[R_a[j], R_par], writes=[R_ff[j]])
                            k.op("pool", lambda e, j=j: e.tensor_tensor(out=ff[j][:], in0=ff[j][:], in1=okap[:, :].unsqueeze(2).to_broadcast([128, 4, 64]), op=ALU.add), reads=[R_ff[j], R_par], writes=[R_ff[j]])
                            k.op("pool", lambda e, j=j, U_=U_: e.tensor_tensor(out=kd[j][:], in0=U_[:, 4:8, :], in1=ff[j][:], op=ALU.mult), reads=[R_ub[j], R_ff[j]], writes=[R_kd[j]])
                            k.op("pool", lambda e, j=j: e.tensor_tensor(out=bb[j][:], in0=kk_t[j][:], in1=a_sb[j][:], op=ALU.mult), reads=[R_kk[j], R_a[j]], writes=[R_bb[j]])
                            pc, rc = ps_next()
                            for q in range(4):
                                k.op("pe", lambda e, j2=j2, pc=pc, q=q, j=j, d=d: e.matmul(pc[:, q * 128:(q + 1) * 128], e2b[j2][:, q * 128:(q + 1) * 128], m2[:, d, :], start=True, stop=True),
                                     reads=[R_e2b[j2], R_par], writes=[rc])
                            k.op("act", lambda e, pc=pc, j=j: e.activation(out=EG[j][:].rearrange("p q s t -> p (q s t)"), in_=pc[:, :], func=AF.Exp, scale=-1.0), reads=[rc], writes=[R_EG[j]])
                            k.op("act", lambda e, pc=pc, j=j: e.activation(out=IEG[j][:], in_=pc[:, :].rearrange("p (q s t) -> p q s t", q=4, s=2)[:, :, 1, :], func=AF.Exp, scale=1.0), reads=[rc], writes=[R_IEG[j]])
                            F_ = fm[j]
                            k.op("dve", lambda e, j=j, F_=F_: e.tensor_tensor(out=F_[:, :, 0, :], in0=kd[j][:], in1=IEG[j][:], op=ALU.mult), reads=[R_kd[j], R_IEG[j]], writes=[R_fm[j]])
                            k.op("dve", lambda e, j=j, F_=F_: e.tensor_tensor(out=F_[:, :, 1, :], in0=bb[j][:], in1=IEG[j][:], op=ALU.mult), reads=[R_bb[j], R_IEG[j]], writes=[R_fm[j]])
                            k.op("pool", lambda e, j=j, F_=F_: e.tensor_tensor(out=F_[:, :, 2, :], in0=kk_t[j][:], in1=EG[j][:, :, 0, :], op=ALU.mult), reads=[R_kk[j], R_EG[j]], writes=[R_fm[j]])
                            k.op("pool", lambda e, j=j, F_=F_, U_=U_: e.tensor_tensor(out=F_[:, :, 3, :], in0=U_[:, 0:4, :], in1=EG[j][:, :, 1, :], op=ALU.mult), reads=[R_ub[j], R_EG[j]], writes=[R_fm[j]])
                            T_m = tm[j]
                            for kind, (srcf, sc) in enumerate(((lambda q, F_=F_: F_[:, q, 2, :], 1.0), (lambda q, F_=F_: F_[:, q, 0, :], 1.0), (lambda q, F_=F_: F_[:, q, 1, :], -1.0), (lambda q, U_=U_: U_[:, 8 + q, :], 1.0))):
                                ptx, rtx = ps_next()
                                for q in range(4):
                                    k.op("pe", lambda e, ptx=ptx, q=q, srcf=srcf: e.transpose(ptx[0:64, q * 128:(q + 1) * 128], srcf(q), ident[:, :]),
                                         reads=[R_fm[j], R_ub[j], R_id], writes=[rtx])
                                eng = "act" if kind % 2 == 0 else "dve"
                                if eng == "act":
                                    k.op("act", lambda e, ptx=ptx, kind=kind, sc=sc, T_m=T_m: e.activation(out=T_m[:, :, kind, :], in_=ptx[0:64, :].rearrange("p (q c) -> p q c", q=4), func=AF.Copy, scale=sc),
                                         reads=[rtx], writes=[R_tm[j]])
                                else:
                                    k.op("dve", lambda e, ptx=ptx, kind=kind, sc=sc, T_m=T_m: e.tensor_scalar(out=T_m[:, :, kind, :], in0=ptx[0:64, :].rearrange("p (q c) -> p q c", q=4), scalar1=sc, scalar2=None, op0=ALU.mult),
                                         reads=[rtx], writes=[R_tm[j]])
                            if RW_DBG["upto"] < "A2":
                                return
                            if d == 0 and (need_ctx or ch >= 4):
                                a_path(1, a1_sb[j2], R_a1[j2])
                                k.op("dve", lambda e, j2=j2, j=j: e.tensor_tensor(out=a1_sb[j2][:], in0=a1_sb[j2][:], in1=a_sb[j][:], op=ALU.add), reads=[R_a1[j2], R_a[j]], writes=[R_a1[j2]])
                                k.op("dve", lambda e, j2=j2, j=j: e.scalar_tensor_tensor(out=a1_sb[j2][:], in0=a1_sb[j2][:], scalar=0.5, in1=kap[:, :].unsqueeze(2).to_broadcast([128, 4, 64]), op0=ALU.mult, op1=ALU.mult), reads=[R_a1[j2], R_par], writes=[R_a1[j2]])
                                k.op("dve", lambda e, j2=j2, j=j: e.tensor_tensor(out=a1_sb[j2][:], in0=a1_sb[j2][:], in1=okap[:, :].unsqueeze(2).to_broadcast([128, 4, 64]), op=ALU.add), reads=[R_a1[j2], R_par], writes=[R_a1[j2]])
                                k.op("dve", lambda e, j2=j2, j=j, U_=U_: e.tensor_tensor(out=rk_t[j2][:], in0=a1_sb[j2][:], in1=U_[:, 4:8, :], op=ALU.mult), reads=[R_a1[j2], R_ub[j]], writes=[R_rk[j2]])
                                k.op("dve", lambda e, j2=j2, j=j, U_=U_: e.tensor_tensor(out=rk_t[j2][:], in0=rk_t[j2][:], in1=U_[:, 0:4, :], op=ALU.mult), reads=[R_rk[j2], R_ub[j]], writes=[R_rk[j2]])
                                k.op("dve", lambda e, j2=j2, j=j: e.tensor_tensor(out=rk_t[j2][:], in0=rk_t[j2][:], in1=rkp[:, :].unsqueeze(2).to_broadcast([128, 4, 64]), op=ALU.mult), reads=[R_rk[j2], R_par], writes=[R_rk[j2]])
                                pr, rr = ps_next()
                                for q in range(4):
                                    k.op("pe", lambda e, j2=j2, pr=pr, q=q, j=j: e.matmul(pr[0:64, q * 2:(q + 1) * 2], rk_t[j2][:, q, :], sel[:, :], start=True, stop=True), reads=[R_rk[j2], R_par], writes=[rr])
                                k.op("act", lambda e, j2=j2, pr=pr, j=j: e.activation(out=rkh[j2][:], in_=pr[0:64, 0:8], func=AF.Copy), reads=[rr], writes=[R_rkh[j2]])
                                k.op("dve", lambda e, j2=j2, j=j, T_m=T_m: e.tensor_tensor(out=bon[j2][:].rearrange("p (q h v) -> p q h v", q=4, h=2), in0=T_m[:, :, 3, :].rearrange("p q (h v) -> p q h v", h=2),
                                     in1=rkh[j2][:, :].rearrange("p (q h) -> p q h", q=4).unsqueeze(3).to_broadcast([64, 4, 2, 64]), op=ALU.mult), reads=[R_tm[j], R_rkh[j2]], writes=[R_bon[j2]])
                                k.dma("sp", BON[b][t0:t0 + 64, :], bon[j2][:], reads=[R_bon[j2]], writes=[R_BON[b]])
                                k.op("act", lambda e, j2=j2, j=j, U_=U_: e.activation(out=sgl[j2][:], in_=U_[:, 14, :], func=AF.Sigmoid), reads=[R_ub[j]], writes=[R_sgl[j2]])
                                pg, rg = ps_next()
                                k.op("pe", lambda e, j2=j2, pg=pg, j=j: e.matmul(pg[0:64, :], sgl[j2][:], g2[:], start=True, stop=True), reads=[R_sgl[j2], R_par], writes=[rg])
                                k.op("act", lambda e, j2=j2, pg=pg, j=j: e.activation(out=gsb[j2][:], in_=pg[0:64, :], func=AF.Copy), reads=[rg], writes=[R_gsb[j2]])
                                k.dma("sp", GG[b][t0:t0 + 64, :], gsb[j2][:], reads=[R_gsb[j2]], writes=[R_GG[b]])
                            if RW_DBG["upto"] < "B":
                                return
                            def hp(h):
                                return h // 2, (h % 2) * 64
                            hv = lambda t, h2: t[:].rearrange("p (q h) c -> p q h c", q=4, h=2)[:, :, h2, :]
                            for h2 in range(2):
                                p0 = h2 * 64
                                p1, r1 = ps_next()
                                p2, r2 = ps_next()
                                p3, r3 = ps_next()
                                for q in range(4):
                                    k.op("pe", lambda e, p1=p1, q=q, p0=p0, F_=F_: e.matmul(p1[0:64, q * 128:(q + 1) * 128], F_[p0:p0 + 64, q, 0, :], F_[p0:p0 + 64, q, 2:4, :].rearrange("p s t -> p (s t)"), start=True, stop=True),
                                         reads=[R_fm[j]], writes=[r1])
                                    k.op("pe", lambda e, p2=p2, q=q, p0=p0, F_=F_: e.matmul(p2[0:64, q * 128:(q + 1) * 128], F_[p0:p0 + 64, q, 1, :], F_[p0:p0 + 64, q, 2:4, :].rearrange("p s t -> p (s t)"), start=True, stop=True),
                                         reads=[R_fm[j]], writes=[r2])
                                    k.op("pe", lambda e, p3=p3, q=q, p0=p0, F_=F_: e.matmul(p3[0:64, q * 64:(q + 1) * 64], F_[p0:p0 + 64, q, 2, :], F_[p0:p0 + 64, q, 1, :], start=True, stop=True), reads=[R_fm[j]], writes=[r3])
                                k.op("dve", lambda e, p1=p1, h2=h2, j=j, d=d: e.tensor_tensor(out=hv(LA[j], h2), in0=p1[0:64, :].rearrange("p (q c) -> p q c", q=4),
                                     in1=m2[:, d, :].unsqueeze(1).to_broadcast([64, 4, 128]), op=ALU.mult), reads=[r1, R_par], writes=[R_LA[j]])
                                k.op("dve", lambda e, p2=p2, h2=h2, j=j, d=d: e.tensor_tensor(out=hv(NBt[j], h2), in0=p2[0:64, :].rearrange("p (q c) -> p q c", q=4),
                                     in1=m3[:, d, :].unsqueeze(1).to_broadcast([64, 4, 128]), op=ALU.mult), reads=[r2, R_par], writes=[R_NB[j]])
                                k.op("dve", lambda e, p3=p3, h2=h2, j=j, d=d: e.tensor_tensor(out=hv(Lm[j], h2), in0=p3[0:64, 0:256].rearrange("p (q c) -> p q c", q=4),
                                     in1=mL[:, d, :].unsqueeze(1).to_broadcast([64, 4, 64]), op=ALU.mult), reads=[r3, R_par], writes=[R_Lm[j]])
                            if RW_DBG["upto"] < "B0":
                                return
                            k.op("dve", lambda e, j=j: e.scalar_tensor_tensor(out=Pm[j][:], in0=NBt[j][:, :, 0:64], scalar=-1.0, in1=ident[0:64, 0:64].unsqueeze(1).to_broadcast([64, 8, 64]), op0=ALU.mult, op1=ALU.add),
                                 reads=[R_NB[j], R_id], writes=[R_Pm[j]])
                            Ncur = lambda h, j=j: NBt[j][:, h, 0:64]
                            Lcur = lambda h, j=j: Lm[j][:, h, :]
                            R_Nc, R_Lc = R_NB[j], R_Lm[j]
                            nlev = RW_DBG.get("nlev", 5)
                            for lev in range(nlev):
                                i4 = nl4[0] % 4; nl4[0] += 1
                                pL, rL = ps_next()
                                for h in range(8):
                                    k.op("pe", lambda e, pL=pL, h=h, Ncur=Ncur, Lcur=Lcur: e.matmul(pL[0:64, h * 64:(h + 1) * 64], Ncur(h), Lcur(h), start=True, stop=True), reads=[R_Nc, R_Lc], writes=[rL])
                                if lev == 0:
                                    k.op("act", lambda e, pL=pL, j=j: e.activation(out=Ll32[j][:].rearrange("p h c -> p (h c)"), in_=pL[0:64, :], func=AF.Copy), reads=[rL], writes=[R_Ll32[j]])
                                    k.op("pool", lambda e, i4=i4, j=j: e.tensor_copy(out=Ll[i4][:], in_=Ll32[j][:]), reads=[R_Ll32[j]], writes=[R_Ll[i4]])
                                else:
                                    k.op("act", lambda e, pL=pL, i4=i4: e.activation(out=Ll[i4][:].rearrange("p h c -> p (h c)"), in_=pL[0:64, :], func=AF.Copy), reads=[rL], writes=[R_Ll[i4]])
                                if lev < nlev - 1:
                                    pN, rN = ps_next()
                                    for h in range(8):
                                        k.op("pe", lambda e, pN=pN, h=h, Ncur=Ncur, Lcur=Lcur: e.matmul(pN[0:64, h * 64:(h + 1) * 64], Lcur(h), Ncur(h), start=True, stop=True), reads=[R_Nc, R_Lc], writes=[rN])
                                    k.op("dve", lambda e, pN=pN, i4=i4: e.tensor_copy(out=Nl[i4][:].rearrange("p h c -> p (h c)"), in_=pN[0:64, :]), reads=[rN], writes=[R_Nl[i4]])
                                pP, rP = ps_next()
                                for h in range(8):
                                    if lev == 0:
                                        k.op("pe", lambda e, pP=pP, h=h, j=j: e.matmul(pP[0:64, h * 64:(h + 1) * 64], Ll32[j][:, h, :], Pm[j][:, h, :], start=True, stop=True), reads=[R_Ll32[j], R_Pm[j]], writes=[rP])
                                    else:
                                        k.op("pe", lambda e, pP=pP, h=h, i4=i4, j=j: e.matmul(pP[0:64, h * 64:(h + 1) * 64], Ll[i4][:, h, :], Pb[j][:, h, :], start=True, stop=True), reads=[R_Ll[i4], R_Pb[j]], writes=[rP])
                                k.op("dve", lambda e, pP=pP, j=j: e.tensor_tensor(out=Pm[j][:].rearrange("p h c -> p (h c)"), in0=Pm[j][:].rearrange("p h c -> p (h c)"), in1=pP[0:64, :], op=ALU.add), reads=[rP, R_Pm[j]], writes=[R_Pm[j]])
                                if lev < nlev - 1:
                                    k.op("act", lambda e, j=j: e.activation(out=Pb[j][:], in_=Pm[j][:], func=AF.Copy), reads=[R_Pm[j]], writes=[R_Pb[j]])
                                Ncur = lambda h, i4=i4: Nl[i4][:, h, :]
                                Lcur = lambda h, i4=i4: Ll[i4][:, h, :]
                                R_Nc, R_Lc = R_Nl[i4], R_Ll[i4]
                            if RW_DBG["upto"] < "B2":
                                return
                            pW, rW = ps_next()
                            for h in range(8):
                                q, p0 = hp(h)
                                k.op("pe", lambda e, pW=pW, h=h, q=q, j=j, T_m=T_m: e.matmul(pW[:, h * 64:(h + 1) * 64], T_m[:, q, 0, :], Pm[j][:, h, :], start=True, stop=True), reads=[R_tm[j], R_Pm[j]], writes=[rW])
                            for h2 in range(2):
                                k.op("act" if h2 == 0 else "dve",
                                     (lambda e, pW=pW, j=j, h2=h2: e.activation(out=WT[j][h2 * 64:(h2 + 1) * 64, :, :], in_=pW[h2 * 64:(h2 + 1) * 64, :].rearrange("p (q h i) -> p q h i", q=4, h=2)[:, :, h2, :], func=AF.Copy)) if h2 == 0 else
                                     (lambda e, pW=pW, j=j, h2=h2: e.tensor_copy(out=WT[j][h2 * 64:(h2 + 1) * 64, :, :], in_=pW[h2 * 64:(h2 + 1) * 64, :].rearrange("p (q h i) -> p q h i", q=4, h=2)[:, :, h2, :])),
                                     reads=[rW], writes=[R_WT[j]])
                            pX, rX = ps_next()
                            for h in range(8):
                                q, p0 = hp(h)
                                k.op("pe", lambda e, pX=pX, h=h, q=q, p0=p0, j=j, T_m=T_m: e.matmul(pX[0:64, h * 64:(h + 1) * 64], LA[j][:, h, 0:64], T_m[:, q, 3, p0:p0 + 64], start=True, stop=True), reads=[R_LA[j], R_tm[j]], writes=[rX])
                            k.op("act", lambda e, pX=pX, j=j: e.activation(out=Xa[j][:].rearrange("p h c -> p (h c)"), in_=pX[0:64, :], func=AF.Copy), reads=[rX], writes=[R_Xa[j]])
                            pU, rU = ps_next()
                            for h in range(8):
                                k.op("pe", lambda e, pU=pU, h=h, j=j: e.matmul(pU[0:64, h * 64:(h + 1) * 64], Pm[j][:, h, :], Xa[j][:, h, :], start=True, stop=True), reads=[R_Pm[j], R_Xa[j]], writes=[rU])
                            k.op("act", lambda e, pU=pU, j=j: e.activation(out=Uta[j][:].rearrange("p h c -> p (h c)"), in_=pU[0:64, :], func=AF.Copy), reads=[rU], writes=[R_Uta[j]])
                            if RW_DBG["upto"] < "C":
                                return
                            for h2 in range(2):
                                p0 = h2 * 64
                                pS, rS = ps_next()
                                for q in range(4):
                                    k.op("pe", lambda e, Hst=Hst, pS=pS, q=q, p0=p0, j=j: e.matmul(pS[0:64, q * 64:(q + 1) * 64], WT[j][p0:p0 + 64, q, :], Hst[p0:p0 + 64, q, :], start=True, stop=True), reads=[R_WT[j], R_H], writes=[rS])
                                k.op("dve", lambda e, pS=pS, j=j, h2=h2: e.tensor_tensor(out=hv(Ua[j], h2), in0=hv(Uta[j], h2), in1=pS[0:64, 0:256].rearrange("p (q c) -> p q c", q=4), op=ALU.add),
                                     reads=[rS, R_Uta[j]], writes=[R_Ua[j]])
                            if RW_DBG["upto"] < "C1":
                                return
                            pY, rY = ps_next()
                            for h in range(8):
                                q, p0 = hp(h)
                                k.op("pe", lambda e, pY=pY, h=h, q=q, p0=p0, j=j, T_m=T_m: e.matmul(pY[0:64, h * 64:(h + 1) * 64], LA[j][:, h, 64:128], T_m[:, q, 3, p0:p0 + 64], start=True, stop=False), reads=[R_LA[j], R_tm[j]], writes=[rY])
                                k.op("pe", lambda e, pY=pY, h=h, j=j: e.matmul(pY[0:64, h * 64:(h + 1) * 64], NBt[j][:, h, 64:128], Ua[j][:, h, :], start=False, stop=True), reads=[R_NB[j], R_Ua[j]], writes=[rY])
                            k.op("act", lambda e, pY=pY, j=j: e.activation(out=ysb[j][:], in_=pY[0:64, :], func=AF.Copy), reads=[rY], writes=[R_ysb[j]])
                            for h2 in range(2):
                                p0 = h2 * 64
                                pR, rR = ps_next()
                                for q in range(4):
                                    k.op("pe", lambda e, Hst=Hst, pR=pR, q=q, p0=p0, j=j, F_=F_: e.matmul(pR[0:64, q * 64:(q + 1) * 64], F_[p0:p0 + 64, q, 3, :], Hst[p0:p0 + 64, q, :], start=True, stop=True), reads=[R_fm[j], R_H], writes=[rR])
                                yv = lambda t, h2: t[:].rearrange("p (q h c) -> p q h c", q=4, h=2)[:, :, h2, :]
                                k.op("dve", lambda e, pR=pR, j=j, h2=h2, yv=yv: e.tensor_tensor(out=yv(ysb[j], h2), in0=yv(ysb[j], h2), in1=pR[0:64, 0:256].rearrange("p (q c) -> p q c", q=4), op=ALU.add),
                                     reads=[rR, R_ysb[j]], writes=[R_ysb[j]])
                            k.dma("sp", YD[d][b][t0:t0 + 64, :], ysb[j][:], reads=[R_ysb[j]], writes=[R_YD[d][b]])
                            if RW_DBG["upto"] < "C2":
                                return
                            pH, rH = ps_next()
                            for q in range(4):
                                k.op("pe", lambda e, pH=pH, q=q, T_m=T_m: e.matmul(pH[:, q * 128:(q + 1) * 128], T_m[:, q, 1, :], T_m[:, q, 3, :], start=True, stop=False), reads=[R_tm[j]], writes=[rH])
                                k.op("pe", lambda e, pH=pH, q=q, j=j, T_m=T_m: e.matmul(pH[:, q * 128:(q + 1) * 128], T_m[:, q, 2, :], Ua[j][:, 2 * q:2 * q + 2, :].rearrange("p h c -> p (h c)"), start=False, stop=True), reads=[R_tm[j], R_Ua[j]], writes=[rH])
                            for h2 in range(2):
                                ps_ = slice(h2 * 64, (h2 + 1) * 64)
                                k.op("dve", lambda e, Hst=Hst, pH=pH, j=j, h2=h2, ps_=ps_: e.tensor_tensor(out=htmp[j][ps_, :, :], in0=pH[ps_, :].rearrange("p (q h v) -> p q h v", q=4, h=2)[:, :, h2, :], in1=Hst[ps_, :, :], op=ALU.add),
                                     reads=[rH, R_H], writes=[R_htmp[j]])
                                k.op("dve", lambda e, Hst=Hst, j=j, ps_=ps_, last_col=last_col: e.tensor_tensor(out=Hst[ps_, :, :], in0=htmp[j][ps_, :, :], in1=EG[j][ps_, :, 1, last_col:last_col + 1].to_broadcast([64, 4, 64]), op=ALU.mult),
                                     reads=[R_htmp[j], R_EG[j]], writes=[R_H])
                        chunk_body()
            k.barrier()
            if not RW_DBG["readout"]:
                return
            with ExitStack() as s1:
                lnw_ro = sb("ro_lnw", [128, 512], F32, s1); lnb_ro = sb("ro_lnb", [128, 512], F32, s1); c1_ro = sb("ro_c1", [128, 4], F32, s1); R_par_ro = Res()
                k.dma("sp", lnw_ro[:], I["rw_lnw"][l], writes=[R_par_ro]); k.dma("sp", lnb_ro[:], I["rw_lnb"][l], writes=[R_par_ro]); k.dma("sp", c1_ro[:], I["rw_c1"], writes=[R_par_ro])
                def T2(name, shape, dt=F32):
                    return [sb(name + str(i), shape, dt, s1) for i in range(2)], [Res(), Res()]
                y0_ro, R_y0_ro = T2("ro_y0", [128, 512]); y1_ro, R_y1_ro = T2("ro_y1", [128, 512]); bo_ro, R_bo_ro = T2("ro_bo", [128, 512]); gg_ro, R_gg_ro = T2("ro_gg", [128, 512])
                stt__ro, R_stt_ro = T2("ro_st", [128, 32]); yc_ro, R_yc_ro = T2("ro_yc", [128, 512]); sq_ro, R_sq_ro = T2("ro_sq", [128, 512]); ot_ro, R_ot_ro = T2("ro_ot", [128, 4, 128], BF16)
                n = 0
                for b in range(NB):
                    for tt in range(0 if need_ctx else 2, NT):
                        j = n % 2; n += 1
                        t0 = tt * 128
                        k.dma("sp", y0_ro[j][:], YD[0][b][t0:t0 + 128, :], reads=[R_YD[0][b]], writes=[R_y0_ro[j]])
                        k.dma("sp", y1_ro[j][:], YD[1][b][t0:t0 + 128, :], reads=[R_YD[1][b]], writes=[R_y1_ro[j]])
                        k.dma("sp", bo_ro[j][:], BON[b][t0:t0 + 128, :], reads=[R_BON[b]], writes=[R_bo_ro[j]])
                        k.dma("sp", gg_ro[j][:], GG[b][t0:t0 + 128, :], reads=[R_GG[b]], writes=[R_gg_ro[j]])
                        S_ = stt__ro[j]
                        v3 = lambda t: t[:].rearrange("p (h v) -> p h v", h=8)
                        bc = lambda a: a.unsqueeze(2).to_broadcast([128, 8, 64])
                        k.op("dve", lambda e, j=j: e.tensor_tensor(out=y0_ro[j][:], in0=y0_ro[j][:], in1=y1_ro[j][:], op=ALU.add), reads=[R_y0_ro[j], R_y1_ro[j]], writes=[R_y0_ro[j]])
                        k.op("dve", lambda e, j=j, S_=S_: e.tensor_reduce(out=S_[:, 0:8], in_=v3(y0_ro[j]), axis=AX.X, op=ALU.add), reads=[R_y0_ro[j]], writes=[R_stt_ro[j]])
                        k.op("dve", lambda e, S_=S_: e.tensor_scalar(out=S_[:, 8:16], in0=S_[:, 0:8], scalar1=1.0 / 64, scalar2=None, op0=ALU.mult), reads=[R_stt_ro[j]], writes=[R_stt_ro[j]])
                        k.op("dve", lambda e, j=j, S_=S_: e.tensor_tensor(out=v3(yc_ro[j]), in0=v3(y0_ro[j]), in1=bc(S_[:, 8:16]), op=ALU.subtract), reads=[R_y0_ro[j], R_stt_ro[j]], writes=[R_yc_ro[j]])
                        k.op("pool", lambda e, j=j: e.tensor_tensor(out=sq_ro[j][:], in0=yc_ro[j][:], in1=yc_ro[j][:], op=ALU.mult), reads=[R_yc_ro[j]], writes=[R_sq_ro[j]])
                        k.op("dve", lambda e, j=j, S_=S_: e.tensor_reduce(out=S_[:, 16:24], in_=v3(sq_ro[j]), axis=AX.X, op=ALU.add), reads=[R_sq_ro[j]], writes=[R_stt_ro[j]])
                        k.op("act", lambda e, S_=S_: e.activation(out=S_[:, 24:32], in_=S_[:, 16:24], func=AF.Sqrt, bias=c1_ro[:, 2:3], scale=1.0 / 64), reads=[R_stt_ro[j], R_par_ro], writes=[R_stt_ro[j]])
                        k.op("dve", lambda e, S_=S_: e.reciprocal(out=S_[:, 24:32], in_=S_[:, 24:32]), reads=[R_stt_ro[j]], writes=[R_stt_ro[j]])
                        k.op("dve", lambda e, j=j, S_=S_: e.tensor_tensor(out=v3(yc_ro[j]), in0=v3(yc_ro[j]), in1=bc(S_[:, 24:32]), op=ALU.mult), reads=[R_yc_ro[j], R_stt_ro[j]], writes=[R_yc_ro[j]])
                        k.op("pool", lambda e, j=j: e.tensor_tensor(out=yc_ro[j][:], in0=yc_ro[j][:], in1=lnw_ro[:], op=ALU.mult), reads=[R_yc_ro[j], R_par_ro], writes=[R_yc_ro[j]])
                        k.op("pool", lambda e, j=j: e.tensor_tensor(out=yc_ro[j][:], in0=yc_ro[j][:], in1=lnb_ro[:], op=ALU.add), reads=[R_yc_ro[j], R_par_ro], writes=[R_yc_ro[j]])
                        k.op("dve", lambda e, j=j: e.tensor_tensor(out=yc_ro[j][:], in0=yc_ro[j][:], in1=bo_ro[j][:], op=ALU.add), reads=[R_yc_ro[j], R_bo_ro[j]], writes=[R_yc_ro[j]])
                        k.op("dve", lambda e, j=j: e.tensor_tensor(out=yc_ro[j][:], in0=yc_ro[j][:], in1=gg_ro[j][:], op=ALU.mult), reads=[R_yc_ro[j], R_gg_ro[j]], writes=[R_yc_ro[j]])
                        pt, rp = ps_next()
                        for q in range(4):
                            k.op("pe", lambda e, pt=pt, q=q, j=j: e.transpose(pt[:, q * 128:(q + 1) * 128], yc_ro[j][:, q * 128:(q + 1) * 128], ident[:, :]), reads=[R_yc_ro[j], R_id], writes=[rp])
                        k.op("act", lambda e, pt=pt, j=j: e.activation(out=ot_ro[j][:].rearrange("p q t -> p (q t)"), in_=pt[:, :], func=AF.Copy), reads=[rp], writes=[R_ot_ro[j]])
                        k.dma("sp", YT[b][:, 0:4, t0:t0 + 128], ot_ro[j][:], reads=[R_ot_ro[j]], writes=[R_YT[b]])
            k.barrier()

        toks = []
        for l in range(n_layers):
            if stages is None or "norm1" in stages:
                stage_norm(l, 0, 0, HT, R_HT)
            if stages is None or "proj" in stages:
                stage_proj(l)
            last = (l == DEPTH - 1)
            if stages is None or "na" in stages:
                stage_na(l, not last, **na_kw)
            if stages is None or "rwkv" in stages:
                stage_rwkv(l, not last)
            if stages is None or "wout" in stages:
                stage_wout(l, last)
            if stages is None or "ffn" in stages:
                moe = (l % 2 == 1)
                tsel = range(LC, S, 256) if last else None
                if moe:
                    stage_norm(l, 1, 3, H2T, R_H2T, dst32=H2F, R_dst32=R_H2F, tsel=tsel)
                    stage_router(l, last)
                else:
                    stage_norm(l, 1, 3, H2T, R_H2T, tsel=tsel)
                stage_ffn(l, last)
        if stages is None or "final" in stages:
            fin_out = stage_final()
        else:
            fin_out = []

        fin = list(fin_out)
        for r in R_XT + R_HT + R_QK + R_VN + R_UT + R_YT + R_YD[0] + R_YD[1] + R_BON + R_GG:
            if r.w is not None:
                fin.append(r.w)
        k.final_wait("sp", fin)
        k.emit()
        print("insts", k.n_inst, "epochs", k.n_epochs)
    return nc


def _fm(a, nchunk):
    return np.ascontiguousarray(a.reshape(nchunk, 128, -1).transpose(1, 0, 2))


def _build_bfull(rpb):
    L, H = rpb.shape[:2]
    q = np.arange(64)[:, None]; kc = np.arange(64)[None, :]
    lo = np.clip(q - 8, 0, 48)
    valid = (kc >= lo) & (kc < lo + 16)
    idx = np.clip(kc - q + 15, 0, 30)
    g = rpb[:, :, :, idx]
    g = np.where(valid[None, None, None], g, np.float32(NEG)).astype(np.float32)
    return np.ascontiguousarray(g.transpose(0, 3, 1, 2, 4).reshape(L, 64, H, 960))


def _prep_shared(inp):
    m = {}
    m["ada_w"] = np.stack([_fm(inp["ada_w"][l], 8) for l in range(4)])
    m["ada_bT"] = np.stack([inp["ada_b"][l].reshape(48, 128).T.copy() for l in range(4)])
    m["g_mix"] = np.stack([inp["norm_mix_g"][l].reshape(8, 128).T.copy() for l in range(4)])
    m["g_ffn"] = np.stack([inp["norm_ffn_g"][l].reshape(8, 128).T.copy() for l in range(4)])
    m["w_in"] = np.stack([_fm(inp["w_in"][l], 8) for l in range(4)])
    m["mu"] = np.stack([inp["shift_mu"][l].reshape(15, 128).T.copy() for l in range(4)])
    m["ident"] = np.eye(128, dtype=np.float32)
    m["final_g"] = inp["final_g"].reshape(8, 128).T.copy()
    m["bfull"] = _build_bfull(inp["na_rpb"])
    m["w_out"] = np.stack([_fm(inp["w_out"][l], 8) for l in range(4)])
    m["rw_w2"] = inp["w2"].reshape(4, 128, 512); m["rw_a2"] = inp["a2"].reshape(4, 128, 512); m["rw_g2"] = inp["g2"]
    m["rw_a0T"] = np.stack([inp["a0"][l].reshape(2, 4, 128).transpose(2, 0, 1) for l in range(4)])
    c4 = lambda a: np.stack([a[l].reshape(4, 128).T for l in range(4)])
    m["rw_kk"] = c4(inp["k_k"]); m["rw_ka"] = c4(inp["k_a"]); m["rw_rk"] = c4(inp["r_k"].reshape(4, 512))
    m["rw_lnw"] = np.broadcast_to(inp["ln_x_w"][:, None, :], (4, 128, 512)); m["rw_lnb"] = np.broadcast_to(inp["ln_x_b"][:, None, :], (4, 128, 512))
    m["rw_w0b"] = np.broadcast_to(inp["w0"][:, None, :, :], (4, 64, 2, 512))
    idx = np.arange(64)
    strict = [(idx[:, None] < idx[None, :]), (idx[:, None] > idx[None, :])]
    incl = [(idx[:, None] <= idx[None, :]), (idx[:, None] >= idx[None, :])]
    m["rw_m2"] = np.stack([np.concatenate([strict[d], incl[d]], 1) for d in range(2)], 1).astype(np.float32)
    m["rw_m3"] = np.stack([np.concatenate([strict[d].astype(np.float32), -incl[d].astype(np.float32)], 1) for d in range(2)], 1)
    m["rw_mL"] = np.stack([strict[d].T for d in range(2)], 1).astype(np.float32)
    bo = np.zeros((128, 128), np.float32); bo[:64, :64] = 1; bo[64:, 64:] = 1
    m["rw_bones"] = bo
    se = np.zeros((128, 2), np.float32); se[:64, 0] = 1; se[64:, 1] = 1
    m["rw_sel"] = se
    m["rw_c1"] = np.tile(np.array([1.0, -0.5, 64e-5, 1e-12], np.float32)[None], (128, 1))
    m["ffn_w1"] = np.stack([_fm(inp["ffn_w1"][i], 8) for i in range(2)])
    m["ffn_w3"] = np.stack([_fm(inp["ffn_w3"][i], 8) for i in range(2)])
    m["ffn_w2"] = np.stack([_fm(inp["ffn_w2"][i], DFF // 128) for i in range(2)])
    m["router"] = np.stack([_fm(inp["router"][i], 8) for i in range(2)])
    m["moe_w1"] = np.stack([np.stack([_fm(inp["moe_w1"][i, e], 8) for e in range(NE)]) for i in range(2)])
    m["moe_w3"] = np.stack([np.stack([_fm(inp["moe_w3"][i, e], 8) for e in range(NE)]) for i in range(2)])
    m["moe_w2"] = np.stack([np.stack([_fm(inp["moe_w2"][i, e], DFE // 128) for e in range(NE)]) for i in range(2)])
    return {k_: np.ascontiguousarray(v, dtype=np.float32) for k_, v in m.items()}


def _prep_core(inp, core):
    bs = [2 * core, 2 * core + 1]
    m = {}
    xcat = [np.concatenate([inp["ctx"][b], inp["x"][b]], 0) for b in bs]
    m["xT"] = np.stack([_fm(np.ascontiguousarray(xc.T), 8) for xc in xcat]).astype(np.float32)
    cc = np.stack([inp["c"][bs[0]], inp["c"][bs[1]], inp["c_ctx"]], 1)
    m["cT"] = _fm(cc, 8).astype(np.float32)
    return m


def kernel(**inputs):
    inp = {k_: np.asarray(v) for k_, v in inputs.items()}
    n = 8
    shared = _prep_shared(inp)
    in_maps = []
    for core in range(n):
        m = dict(shared)
        m.update(_prep_core(inp, core))
        in_maps.append(m)
    nc = build_program()
    res = run_bass_kernel_spmd(nc, in_maps, core_ids=list(range(n)))
    outs = []
    for core in range(n):
        o = np.asarray(res.results[core]["out"])
        for b in range(NB):
            outs.append(o[b].transpose(1, 0, 2).reshape(D, T).T)
    return np.ascontiguousarray(np.stack(outs, 0)).astype(np.float32)
```

```python
import numpy as np
import concourse.bass as bass
import concourse.mybir as mybir
from concourse.bass_utils import run_bass_kernel_spmd
from contextlib import ExitStack

F32 = mybir.dt.float32
BF16 = mybir.dt.bfloat16
AF = mybir.ActivationFunctionType
ALU = mybir.AluOpType
AX = mybir.AxisListType

SAME_ENGINE_SYNC = True
DMA_RING = {"sp": 16, "act": 4, "pool": 16}
SEM_LIMIT = 60000
LOAD_Q = "pool"
MAX_EPOCHS = 7


class Res:
    __slots__ = ("name", "w", "rd")

    def __init__(self, name=""):
        self.name = name
        self.w = None
        self.rd = []


class K:
    ENG = ("pe", "act", "dve", "pool", "sp")
    CE = ("pe", "act", "dve", "pool")

    def __init__(self, nc, stack):
        self.nc = nc
        self.stack = stack
        self.recs = []
        self.cnt = {e: 0 for e in self.ENG}
        self.slots = []
        self.dq = {}
        for q in ("sp", "act", "pool"):
            idx = []
            for i in range(DMA_RING[q]):
                self.slots.append(0)
                idx.append(len(self.slots) - 1)
            self.dq[q] = {"idx": idx, "n": 0}
        self.waited_e = {e: {} for e in self.ENG}
        self.waited_d = {e: {} for e in self.ENG}
        self.n_inst = 0

    def _need(self, eng, tok, waits, force=False):
        if tok is None:
            return
        if tok[0] == "e":
            _, x, seq = tok
            if x == eng and (not SAME_ENGINE_SYNC or eng == "pe") and not force:
                return
            if self.waited_e[eng].get(x, 0) >= seq:
                return
            self.waited_e[eng][x] = seq
            waits.append(tok)
        else:
            _, si, use = tok
            if self.waited_d[eng].get(si, 0) >= use:
                return
            self.waited_d[eng][si] = use
            waits.append(tok)

    def _deps(self, eng, reads, writes):
        waits = []
        for r in reads:
            self._need(eng, r.w, waits)
        for w in writes:
            self._need(eng, w.w, waits)
            best = {}
            for t in w.rd:
                key = (t[0], t[1])
                if key not in best or best[key][2] < t[2]:
                    best[key] = t
            for t in best.values():
                self._need(eng, t, waits)
        return waits

    def _commit(self, tok, reads, writes):
        for r in reads:
            if r.rd and r.rd[-1][0] == tok[0] and r.rd[-1][1] == tok[1]:
                r.rd[-1] = tok
            else:
                r.rd.append(tok)
        for w in writes:
            w.w = tok
            w.rd = []

    def op(self, eng, fn, reads=(), writes=()):
        waits = self._deps(eng, reads, writes)
        self.cnt[eng] += 1
        tok = ("e", eng, self.cnt[eng])
        self.recs.append({"eng": eng, "waits": waits, "fn": fn, "tok": tok})
        self._commit(tok, reads, writes)
        self.n_inst += 1
        return tok

    def dma(self, q, out, in_, reads=(), writes=(), **kw):
        if q == "sp" and LOAD_Q != "sp" and "DRAM" not in str(out.space):
            q = LOAD_Q
        waits = self._deps(q, reads, writes)
        d = self.dq[q]
        si = d["idx"][d["n"] % len(d["idx"])]
        d["n"] += 1
        prev = self.slots[si]
        if prev > 0:
            self._need(q, ("d", si, prev), waits)
        self.slots[si] = prev + 1
        tok = ("d", si, prev + 1)
        fn = (lambda e, o=out, i=in_, kw=kw: e.dma_start(out=o, in_=i, **kw))
        self.recs.append({"eng": q, "waits": waits, "fn": fn, "tok": tok})
        self._commit(tok, reads, writes)
        self.n_inst += 1
        return tok

    def _all_waits(self, eng):
        waits = []
        for x in self.CE:
            if self.cnt[x] > 0:
                self._need(eng, ("e", x, self.cnt[x]), waits, force=True)
        for si, v in enumerate(self.slots):
            if v > 0:
                self._need(eng, ("d", si, v), waits)
        return waits

    def barrier(self):
        for eng in self.ENG:
            self.recs.append({"eng": eng, "waits": self._all_waits(eng), "fn": None, "tok": None})

    def final_wait(self, eng, toks):
        waits = []
        for t in toks:
            self._need(eng, t, waits)
        self.recs.append({"eng": eng, "waits": waits, "fn": None, "tok": None})

    def emit(self):
        nc, st = self.nc, self.stack
        recs = self.recs
        needed = set()
        for r in recs:
            for w in r["waits"]:
                if w[0] == "e":
                    needed.add((w[1], w[2]))
        out = []
        ep = 0
        ms = {e: 0 for e in self.CE}
        du = [0] * len(self.slots)
        last_e = {e: 0 for e in self.CE}
        last_d = [0] * len(self.slots)
        val = {}
        pend_last = {}

        def close_epoch():
            nonlocal ep, ms, du
            bw = {}
            for e in self.CE:
                if e in pend_last:
                    rr = out[pend_last[e]]
                    t = rr["tok"]
                    if t not in val:
                        ms[e] += 1
                        val[t] = (ep, ms[e])
                        rr["inc"] = True
            allw = []
            for e in self.CE:
                if e in pend_last:
                    allw.append(out[pend_last[e]]["tok"])
            for si in range(len(self.slots)):
                if du[si] > 0:
                    allw.append(("d", si, last_d[si]))
            for eng in self.ENG:
                out.append({"eng": eng, "waits": list(allw), "fn": None, "tok": None, "ep": ep})
            ep += 1
            ms = {e: 0 for e in self.CE}
            du = [0] * len(self.slots)
            pend_last.clear()

        for r in recs:
            t = r["tok"]
            if t is not None:
                if t[0] == "e":
                    if ms[t[1]] + 2 > SEM_LIMIT:
                        close_epoch()
                else:
                    if (du[t[1]] + 2) * 16 > SEM_LIMIT:
                        close_epoch()
            r = dict(r)
            r["ep"] = ep
            out.append(r)
            if t is not None:
                if t[0] == "e":
                    pend_last[t[1]] = len(out) - 1
                    if (t[1], t[2]) in needed:
                        ms[t[1]] += 1
                        val[t] = (ep, ms[t[1]])
                        r["inc"] = True
                    else:
                        r["inc"] = False
                else:
                    du[t[1]] += 1
                    last_d[t[1]] = t[2]
                    val[t] = (ep, du[t[1]] * 16)
        n_ep = ep + 1
        print("epochs needed", n_ep, "milestones", ms, "dma uses", max(du))
        assert n_ep <= MAX_EPOCHS, "too many epochs %d" % n_ep
        self.n_epochs = n_ep
        esem = [{e: st.enter_context(nc.semaphore("c%d_%s" % (i, e))) for e in self.CE} for i in range(n_ep)]
        dsem = [[st.enter_context(nc.semaphore("d%d_%d" % (i, j))) for j in range(len(self.slots))] for i in range(n_ep)]
        streams = {e: [] for e in self.ENG}
        for r in out:
            streams[r["eng"]].append(r)

        with nc.Block() as block:
            def run(engname, e):
                for ep_ in range(1, n_ep):
                    if engname in self.CE:
                        e.sem_clear(esem[ep_][engname])
                    if engname in self.dq:
                        for si in self.dq[engname]["idx"]:
                            e.sem_clear(dsem[ep_][si])
                for r in streams[engname]:
                    for w in r["waits"]:
                        v = val.get(w)
                        if v is None or v[0] != r["ep"]:
                            continue
                        if w[0] == "e":
                            e.wait_ge(esem[v[0]][w[1]], v[1])
                        else:
                            e.wait_ge(dsem[v[0]][w[1]], v[1])
                    if r["fn"] is None:
                        continue
                    ins = r["fn"](e)
                    t = r["tok"]
                    if t[0] == "e":
                        if r.get("inc"):
                            ins.then_inc(esem[r["ep"]][t[1]], 1)
                    else:
                        ins.then_inc(dsem[r["ep"]][t[1]], 16)

            @block.sync
            def _(e):
                run("sp", e)

            @block.tensor
            def _(e):
                run("pe", e)

            @block.vector
            def _(e):
                run("dve", e)

            @block.scalar
            def _(e):
                run("act", e)

            @block.gpsimd
            def _(e):
                run("pool", e)


D = 1024
DEPTH = 4
NB = 2
LC = 256
T = 2048
S = LC + T
NT = S // 128
D_IN = 3456
DFF = 2816
DFE = 3584
NE = 8
NEG = -30000.0

DEBUG_OUT = []
RW_DBG = {"nch": None, "upto": "Z", "nb": None, "nd": 2, "readout": True}
N_LAYERS_RUN = DEPTH


def build_program(n_layers=DEPTH, stages=None, debug=(), na_kw={}):
    nc = bass.Bass("TRN2", target_bir_lowering=False)
    st = ExitStack()
    with st:
        k = K(nc, st)

        def din(name, shape, dt=F32):
            return nc.dram_tensor(name, list(shape), dt, kind="ExternalInput").ap()

        def dscr(name, shape, dt=F32):
            kind = "ExternalOutput" if name in debug else "Internal"
            return nc.dram_tensor(name, list(shape), dt, kind=kind).ap()

        I = {}
        I["xT"] = din("xT", [NB, 128, 8, S])
        I["cT"] = din("cT", [128, 8, 3])
        I["ada_w"] = din("ada_w", [DEPTH, 128, 8, 6 * D])
        I["ada_bT"] = din("ada_bT", [DEPTH, 128, 48])
        I["g_mix"] = din("g_mix", [DEPTH, 128, 8])
        I["g_ffn"] = din("g_ffn", [DEPTH, 128, 8])
        I["w_in"] = din("w_in", [DEPTH, 128, 8, D_IN])
        I["mu"] = din("mu", [DEPTH, 128, 15])
        I["ident"] = din("ident", [128, 128])
        I["final_g"] = din("final_g", [128, 8])
        I["bfull"] = din("bfull", [DEPTH, 64, 8, 960])
        I["w_out"] = din("w_out", [DEPTH, 128, 8, D])
        I["rw_w2"] = din("rw_w2", [DEPTH, 128, 512]); I["rw_a2"] = din("rw_a2", [DEPTH, 128, 512]); I["rw_g2"] = din("rw_g2", [DEPTH, 128, 512])
        I["rw_a0T"] = din("rw_a0T", [DEPTH, 128, 2, 4]); I["rw_kk"] = din("rw_kk", [DEPTH, 128, 4]); I["rw_ka"] = din("rw_ka", [DEPTH, 128, 4]); I["rw_rk"] = din("rw_rk", [DEPTH, 128, 4])
        I["rw_lnw"] = din("rw_lnw", [DEPTH, 128, 512]); I["rw_lnb"] = din("rw_lnb", [DEPTH, 128, 512]); I["rw_w0b"] = din("rw_w0b", [DEPTH, 64, 2, 512])
        I["rw_m2"] = din("rw_m2", [64, 2, 128]); I["rw_m3"] = din("rw_m3", [64, 2, 128]); I["rw_mL"] = din("rw_mL", [64, 2, 64])
        I["rw_bones"] = din("rw_bones", [128, 128]); I["rw_sel"] = din("rw_sel", [128, 2]); I["rw_c1"] = din("rw_c1", [128, 4])
        I["ffn_w1"] = din("ffn_w1", [2, 128, 8, DFF]); I["ffn_w3"] = din("ffn_w3", [2, 128, 8, DFF]); I["ffn_w2"] = din("ffn_w2", [2, 128, DFF // 128, D])
        I["router"] = din("router", [2, 128, 8, NE])
        I["moe_w1"] = din("moe_w1", [2, NE, 128, 8, DFE]); I["moe_w3"] = din("moe_w3", [2, NE, 128, 8, DFE]); I["moe_w2"] = din("moe_w2", [2, NE, 128, DFE // 128, D])
        out = nc.dram_tensor("out", [NB, 128, 8, T], F32, kind="ExternalOutput").ap()

        XT = [dscr("xt%d" % b, [128, 8, S]) for b in range(NB)]
        HT = [dscr("ht%d" % b, [128, 8, S], BF16) for b in range(NB)]
        QT = [dscr("qt%d" % b, [128, 4, S], BF16) for b in range(NB)]
        KT = [dscr("kt%d" % b, [128, 4, S], BF16) for b in range(NB)]
        VN = [dscr("vn%d" % b, [S, 512], BF16) for b in range(NB)]
        UT = [dscr("ut%d" % b, [128, 15, S]) for b in range(NB)]
        R_XT = [Res() for _ in range(NB)]
        R_HT = [Res() for _ in range(NB)]
        R_QK = [Res() for _ in range(NB)]
        R_VN = [Res() for _ in range(NB)]
        R_UT = [Res() for _ in range(NB)]
        YT = [dscr("yt%d" % b, [128, 8, S], BF16) for b in range(NB)]
        H2T = [dscr("h2t%d" % b, [128, 8, S], BF16) for b in range(NB)]
        H2F = [dscr("h2f%d" % b, [128, 8, S]) for b in range(NB)]
        GB = [dscr("gb%d" % b, [128, NE, S]) for b in range(NB)]
        YD = [[dscr("yd%d_%d" % (d, b), [S, 512]) for b in range(NB)] for d in range(2)]
        R_YD = [[Res() for b in range(NB)] for d in range(2)]
        BON = [dscr("bon%d" % b, [S, 512]) for b in range(NB)]; R_BON = [Res() for _ in range(NB)]
        GG = [dscr("gg%d" % b, [S, 512]) for b in range(NB)]; R_GG = [Res() for _ in range(NB)]
        R_H2T = [Res() for _ in range(NB)]; R_H2F = [Res() for _ in range(NB)]; R_GB = [Res() for _ in range(NB)]
        R_YT = [Res() for _ in range(NB)]

        uid = [0]

        def sb(name, shape, dt=F32, stack=st):
            uid[0] += 1
            return stack.enter_context(nc.sbuf_tensor("%s_%d" % (name, uid[0]), list(shape), dt))

        ident = sb("ident_sb", [128, 128]); R_id = Res()
        ones = sb("ones_sb", [128, 128]); R_ones = Res()
        MOD = sb("mod_sb", [128, DEPTH, 48, 3]); R_mod = Res()
        GS = sb("gs_sb", [128, DEPTH, 2, 8, 3]); R_gs = Res()
        gmix = sb("gmix_sb", [128, DEPTH, 8]); gffn = sb("gffn_sb", [128, DEPTH, 8]); R_g = Res()
        eps_t = sb("eps_sb", [128, 1]); R_eps = Res()
        psum = [st.enter_context(nc.psum_tensor("ps%d" % i, [128, 512], F32)) for i in range(8)]
        R_ps = [Res() for _ in range(8)]
        pctr = [0]

        def ps_next():
            i = pctr[0] % 8
            pctr[0] += 1
            return psum[i], R_ps[i]

        k.dma("sp", ident[:], I["ident"][:, :], writes=[R_id])
        ident_bf = sb("identbf_sb", [128, 128], BF16)
        k.op("dve", lambda e: e.tensor_copy(out=ident_bf[:], in_=ident[:]), reads=[R_id], writes=[R_id])
        k.op("dve", lambda e: e.memset(ones[:], 1.0), writes=[R_ones])
        k.op("dve", lambda e: e.memset(eps_t[:], 1e-6), writes=[R_eps])
        for l in range(DEPTH):
            k.dma("sp", gmix[:, l, :], I["g_mix"][l], writes=[R_g])
            k.dma("sp", gffn[:, l, :], I["g_ffn"][l], writes=[R_g])

        with ExitStack() as s1:
            cT = sb("cT_sb", [128, 8, 3], F32, s1); R_c = Res()
            sT = sb("sT", [128, 8, 3], F32, s1); R_s = Res()
            abT = sb("abT", [128, 48], F32, s1); R_ab = Res()
            aw = [sb("aw%d" % i, [128, 8, 512], F32, s1) for i in range(2)]
            R_aw = [Res(), Res()]
            xcp = [sb("xcp%d" % i, [128, 8, 256], F32, s1) for i in range(2)]
            R_xcp = [Res(), Res()]
            k.dma("sp", cT[:], I["cT"][:, :, :], writes=[R_c])
            k.op("act", lambda e: e.activation(out=sT[:], in_=cT[:], func=AF.Silu), reads=[R_c], writes=[R_s])
            n = 0
            for b in range(NB):
                for t0 in range(0, S, 256):
                    j = n % 2; n += 1
                    k.dma("sp", xcp[j][:], I["xT"][b, :, :, t0:t0 + 256], writes=[R_xcp[j]])
                    k.dma("sp", XT[b][:, :, t0:t0 + 256], xcp[j][:], reads=[R_xcp[j]], writes=[R_XT[b]])
            n = 0
            for l in range(n_layers):
                k.dma("sp", abT[:], I["ada_bT"][l], writes=[R_ab])
                for cc in range(12):
                    j = n % 2; n += 1
                    k.dma("sp", aw[j][:], I["ada_w"][l, :, :, cc * 512:(cc + 1) * 512], writes=[R_aw[j]])
                    pt, rp = ps_next()
                    for c4 in range(4):
                        for kc in range(8):
                            k.op("pe", lambda e, pt=pt, j=j, c4=c4, kc=kc: e.matmul(
                                pt[:, c4 * 3:c4 * 3 + 3], aw[j][:, kc, c4 * 128:(c4 + 1) * 128], sT[:, kc, :],
                                start=(kc == 0), stop=(kc == 7)), reads=[R_aw[j], R_s], writes=[rp])
                    k.op("dve", lambda e, pt=pt, l=l, cc=cc: e.tensor_tensor(
                        out=MOD[:, l, cc * 4:(cc + 1) * 4, :], in0=pt[:, 0:12].rearrange("p (a b) -> p a b", b=3),
                        in1=abT[:, cc * 4:(cc + 1) * 4].unsqueeze(2).to_broadcast([128, 4, 3]), op=ALU.add),
                        reads=[rp, R_ab], writes=[R_mod])
                for which, (gt, mi) in enumerate(((gmix, 1), (gffn, 4))):
                    k.op("dve", lambda e, l=l, which=which, gt=gt, mi=mi: e.scalar_tensor_tensor(
                        out=GS[:, l, which, :, :], in0=MOD[:, l, mi * 8:(mi + 1) * 8, :], scalar=1.0,
                        in1=gt[:, l, :].unsqueeze(2).to_broadcast([128, 8, 3]), op0=ALU.add, op1=ALU.mult),
                        reads=[R_mod, R_g], writes=[R_gs])
        k.barrier()

        def which_of(b, t0):
            return 2 if t0 < LC else b

        def stage_norm(l, which, shift_idx, dst, R_dst, gsel=None, dst32=None, R_dst32=None, bsel=range(NB), tsel=None):
            with ExitStack() as s1:
                xb = [sb("nx%d" % i, [128, 8, 256], F32, s1) for i in range(2)]; R_xb = [Res(), Res()]
                sq = [sb("nsq%d" % i, [128, 8, 256], F32, s1) for i in range(2)]; R_sq = [Res(), Res()]
                rs = [sb("nrs%d" % i, [128, 256], F32, s1) for i in range(2)]; R_rs = [Res(), Res()]
                tm = [sb("ntm%d" % i, [128, 8, 256], F32, s1) for i in range(2)]; R_tm = [Res(), Res()]
                hb = [sb("nhb%d" % i, [128, 8, 256], BF16, s1) for i in range(2)]; R_hb = [Res(), Res()]
                n = 0
                for b in bsel:
                    for t0 in (tsel if tsel is not None else range(0, S, 256)):
                        j = n % 2; n += 1
                        w = which_of(b, t0)
                        k.dma("sp", xb[j][:], XT[b][:, :, t0:t0 + 256], reads=[R_XT[b]], writes=[R_xb[j]])
                        k.op("act", lambda e, j=j: e.activation(out=sq[j][:], in_=xb[j][:], func=AF.Square), reads=[R_xb[j]], writes=[R_sq[j]])
                        pt, rp = ps_next()
                        for c in range(8):
                            k.op("pe", lambda e, pt=pt, j=j, c=c: e.matmul(pt[:, 0:256], ones[:], sq[j][:, c, :], start=(c == 0), stop=(c == 7)),
                                 reads=[R_ones, R_sq[j]], writes=[rp])
                        k.op("act", lambda e, pt=pt, j=j: e.activation(out=rs[j][:], in_=pt[:, 0:256], func=AF.Sqrt, bias=eps_t[:, 0:1], scale=1.0 / D),
                             reads=[rp, R_eps], writes=[R_rs[j]])
                        k.op("dve", lambda e, j=j: e.reciprocal(out=rs[j][:], in_=rs[j][:]), reads=[R_rs[j]], writes=[R_rs[j]])
                        for c in range(8):
                            k.op("dve", lambda e, j=j, c=c, w=w: e.scalar_tensor_tensor(
                                out=tm[j][:, c, :], in0=xb[j][:, c, :], scalar=GS[:, l, which, c, w:w + 1], in1=rs[j][:],
                                op0=ALU.mult, op1=ALU.mult), reads=[R_xb[j], R_gs, R_rs[j]], writes=[R_tm[j]])
                            k.op("act", lambda e, j=j, c=c, w=w: e.activation(
                                out=hb[j][:, c, :], in_=tm[j][:, c, :], func=AF.Identity, bias=MOD[:, l, shift_idx * 8 + c, w:w + 1], scale=1.0),
                                reads=[R_tm[j], R_mod], writes=[R_hb[j]])
                            if dst32 is not None:
                                k.op("pool", lambda e, j=j, c=c, w=w: e.tensor_scalar(
                                    out=tm[j][:, c, :], in0=tm[j][:, c, :], scalar1=MOD[:, l, shift_idx * 8 + c, w:w + 1], scalar2=None, op0=ALU.add),
                                    reads=[R_tm[j], R_mod, R_hb[j]], writes=[R_tm[j]])
                        k.dma("sp", dst[b][:, :, t0:t0 + 256], hb[j][:], reads=[R_hb[j]], writes=[R_dst[b]])
                        if dst32 is not None:
                            k.dma("sp", dst32[b][:, :, t0:t0 + 256], tm[j][:], reads=[R_tm[j]], writes=[R_dst32[b]])
            k.barrier()

        def stage_proj(l):
            with ExitStack() as s1:
                w = sb("pw", [128, 8, D_IN], BF16, s1); R_w = Res()
                mu = sb("pmu", [128, 15], F32, s1); R_mu = Res()
                mu1 = sb("pmu1", [128, 15], F32, s1)
                muh = sb("pmuh", [128, 15], F32, s1)
                hb = [sb("ph%d" % i, [128, 8, 258], BF16, s1) for i in range(2)]; R_hb = [Res(), Res()]
                hs = [sb("phs%d" % i, [128, 8, 256], BF16, s1) for i in range(2)]; R_hs = [Res(), Res()]
                oq = [sb("poq%d" % i, [128, 8, 256], BF16, s1) for i in range(2)]; R_oq = [Res(), Res()]
                ov = [sb("pov%d" % i, [128, 2, 512], BF16, s1) for i in range(2)]; R_ov = [Res(), Res()]
                ou = [sb("pou%d" % i, [128, 15, 256], F32, s1) for i in range(2)]; R_ou = [Res(), Res()]
                t2 = [sb("pt2%d" % i, [128, 256], F32, s1) for i in range(2)]; R_t2 = [Res(), Res()]
                for kc in range(8):
                    k.dma("pool", w[:, kc, :], I["w_in"][l, :, kc, :], writes=[R_w])
                k.dma("sp", mu[:], I["mu"][l], writes=[R_mu])
                k.op("dve", lambda e: e.tensor_scalar(out=mu1[:], in0=mu[:], scalar1=-1.0, scalar2=1.0, op0=ALU.mult, op1=ALU.add), reads=[R_mu], writes=[R_mu])
                k.op("dve", lambda e: e.tensor_scalar(out=muh[:], in0=mu[:], scalar1=0.5, scalar2=None, op0=ALU.mult), reads=[R_mu], writes=[R_mu])
                n = 0
                for b in range(NB):
                    for t0 in range(0, S, 256):
                        j = n % 2; n += 1
                        seg0, seg1 = (0, LC) if t0 < LC else (LC, S)
                        lo, hi = max(t0 - 1, seg0), min(t0 + 257, seg1)
                        if lo == t0 or hi == t0 + 256:
                            k.op("dve", lambda e, j=j: e.memset(hb[j][:], 0.0), writes=[R_hb[j]])
                        k.dma("sp", hb[j][:, :, lo - (t0 - 1):hi - (t0 - 1)], HT[b][:, :, lo:hi], reads=[R_HT[b]], writes=[R_hb[j]])
                        k.op("dve", lambda e, j=j: e.tensor_tensor(out=hs[j][:], in0=hb[j][:, :, 0:256], in1=hb[j][:, :, 2:258], op=ALU.add),
                             reads=[R_hb[j]], writes=[R_hs[j]])
                        for cj in range(8):
                            pt, rp = ps_next()
                            for kc in range(8):
                                k.op("pe", lambda e, pt=pt, j=j, cj=cj, kc=kc: e.matmul(pt[:, 0:256], w[:, kc, cj * 128:(cj + 1) * 128], hb[j][:, kc, 1:257],
                                     start=(kc == 0), stop=(kc == 7)), reads=[R_w, R_hb[j]], writes=[rp])
                            k.op("act", lambda e, pt=pt, j=j, cj=cj: e.activation(out=oq[j][:, cj, :], in_=pt[:, 0:256], func=AF.Copy), reads=[rp], writes=[R_oq[j]])
                        k.dma("sp", QT[b][:, :, t0:t0 + 256], oq[j][:, 0:4, :], reads=[R_oq[j]], writes=[R_QK[b]])
                        k.dma("sp", KT[b][:, :, t0:t0 + 256], oq[j][:, 4:8, :], reads=[R_oq[j]], writes=[R_QK[b]])
                        for tt in range(2):
                            pt, rp = ps_next()
                            for kc in range(8):
                                k.op("pe", lambda e, pt=pt, j=j, tt=tt, kc=kc: e.matmul(pt[:, :], hb[j][:, kc, 1 + tt * 128:1 + (tt + 1) * 128], w[:, kc, 1024:1536],
                                     start=(kc == 0), stop=(kc == 7)), reads=[R_w, R_hb[j]], writes=[rp])
                            k.op("act", lambda e, pt=pt, j=j, tt=tt: e.activation(out=ov[j][:, tt, :], in_=pt[:, :], func=AF.Copy), reads=[rp], writes=[R_ov[j]])
                        k.dma("sp", VN[b][t0:t0 + 256, :].rearrange("(a p) c -> p a c", p=128), ov[j][:], reads=[R_ov[j]], writes=[R_VN[b]])
                        for cj in range(15):
                            c0 = 1536 + cj * 128
                            pa, ra = ps_next()
                            for kc in range(8):
                                k.op("pe", lambda e, pa=pa, j=j, c0=c0, kc=kc: e.matmul(pa[:, 0:256], w[:, kc, c0:c0 + 128], hb[j][:, kc, 1:257],
                                     start=(kc == 0), stop=(kc == 7)), reads=[R_w, R_hb[j]], writes=[ra])
                            pb, rb = ps_next()
                            for kc in range(8):
                                k.op("pe", lambda e, pb=pb, j=j, c0=c0, kc=kc: e.matmul(pb[:, 0:256], w[:, kc, c0:c0 + 128], hs[j][:, kc, :],
                                     start=(kc == 0), stop=(kc == 7)), reads=[R_w, R_hs[j]], writes=[rb])
                            k.op("act", lambda e, pb=pb, j=j, cj=cj: e.activation(out=t2[j][:], in_=pb[:, 0:256], func=AF.Copy, scale=muh[:, cj:cj + 1]),
                                 reads=[rb, R_mu], writes=[R_t2[j]])
                            k.op("dve", lambda e, pa=pa, j=j, cj=cj: e.scalar_tensor_tensor(out=ou[j][:, cj, :], in0=pa[:, 0:256], scalar=mu1[:, cj:cj + 1], in1=t2[j][:],
                                 op0=ALU.mult, op1=ALU.add), reads=[ra, R_mu, R_t2[j]], writes=[R_ou[j]])
                        k.dma("sp", UT[b][:, :, t0:t0 + 256], ou[j][:], reads=[R_ou[j]], writes=[R_UT[b]])
            k.barrier()

        def stage_na(l, need_ctx, hsel=range(8), bsel=range(NB)):
            NBUF = 3
            with ExitStack() as s1:
                bias = sb("na_bias", [128, 8, 960], F32, s1); R_bias = Res()
                k.dma("sp", bias[0:64], I["bfull"][l], writes=[R_bias])
                k.dma("sp", bias[64:128], I["bfull"][l], writes=[R_bias])
                qh = [sb("na_q%d" % i, [64, S], BF16, s1) for i in range(2)]
                kh = [sb("na_k%d" % i, [64, S], BF16, s1) for i in range(2)]
                VE = [sb("na_ve%d" % i, [128, 18, 64], BF16, s1) for i in range(2)]
                VO = [sb("na_vo%d" % i, [128, 18, 64], BF16, s1) for i in range(2)]
                R_in = [Res(), Res()]
                ytok = sb("na_ytok", [128, NT, 512], BF16, s1); R_ytok = Res()
                yfm = [sb("na_yfm%d" % i, [128, 4, 128], BF16, s1) for i in range(2)]; R_yfm = [Res(), Res()]
                ssb = [sb("na_s%d" % i, [128, 832], F32, s1) for i in range(NBUF)]; R_s = [Res() for _ in range(NBUF)]
                pbf = [sb("na_p%d" % i, [128, 832], BF16, s1) for i in range(NBUF)]; R_p = [Res() for _ in range(NBUF)]
                pT = [sb("na_pT%d" % i, [128, 896], BF16, s1) for i in range(NBUF)]; R_pT = [Res() for _ in range(NBUF)]
                st4 = [sb("na_st%d" % i, [128, 4], F32, s1) for i in range(NBUF)]; R_st = [Res() for _ in range(NBUF)]
                n = 0
                u = 0
                for b in bsel:
                    if not need_ctx:
                        pass
                    for h in hsel:
                        j = n % 2; n += 1
                        p0 = (h % 2) * 64
                        k.dma("sp", qh[j][:], QT[b][p0:p0 + 64, h // 2, :], reads=[R_QK[b]], writes=[R_in[j]])
                        k.dma("sp", kh[j][:], KT[b][p0:p0 + 64, h // 2, :], reads=[R_QK[b]], writes=[R_in[j]])
                        k.dma("sp", VE[j][:], VN[b][:, h * 64:(h + 1) * 64].rearrange("(a p) d -> p a d", p=128), reads=[R_VN[b]], writes=[R_in[j]])
                        k.dma("sp", VO[j][:, 0:17, :], VN[b][64:64 + 17 * 128, h * 64:(h + 1) * 64].rearrange("(a p) d -> p a d", p=128), reads=[R_VN[b]], writes=[R_in[j]])
                        k.dma("sp", VO[j][0:64, 17, :], VN[b][S - 64:S, h * 64:(h + 1) * 64], reads=[R_VN[b]], writes=[R_in[j]])
                        units = [("lat", r) for r in range(0, 32, 2)] + ([("ctx", 0), ("ctx", 1)] if need_ctx else [])
                        for kind, r in units:
                            i = u % NBUF; u += 1
                            b1, rb1 = ps_next()
                            if kind == "lat":
                                r0A = min(max(r - 4, 0), 24); r0B = min(max(r - 3, 0), 24)
                                R0 = min(r0A, 23)
                                tq = LC + r * 64; w0 = LC + R0 * 64
                                tile_i = tq // 128
                                b2, rb2 = ps_next()
                                k.op("pe", lambda e, b1=b1, j=j, tq=tq: e.matmul(b1[:, 0:256], qh[j][:, tq:tq + 128], kh[j][:, 0:256], start=True, stop=True), reads=[R_in[j]], writes=[rb1])
                                k.op("pe", lambda e, b1=b1, j=j, tq=tq, w0=w0: e.matmul(b1[:, 256:512], qh[j][:, tq:tq + 128], kh[j][:, w0:w0 + 256], start=True, stop=True), reads=[R_in[j]], writes=[rb1])
                                k.op("pe", lambda e, b2=b2, j=j, tq=tq, w0=w0: e.matmul(b2[:, 0:320], qh[j][:, tq:tq + 128], kh[j][:, w0 + 256:w0 + 576], start=True, stop=True), reads=[R_in[j]], writes=[rb2])
                                k.op("act", lambda e, b1=b1, i=i: e.activation(out=ssb[i][:, 0:256], in_=b1[:, 0:256], func=AF.Copy, scale=0.125), reads=[rb1], writes=[R_s[i]])
                                for half, (rq, r0X) in enumerate(((r, r0A), (r + 1, r0B))):
                                    sX = r0X - R0
                                    hp_ = slice(half * 64, (half + 1) * 64)
                                    bs0 = (r0X - rq + 7) * 64
                                    n1 = 256 - sX * 64
                                    k.op("dve", lambda e, b1=b1, i=i, h=h, hp_=hp_, sX=sX, bs0=bs0, n1=n1: e.scalar_tensor_tensor(
                                        out=ssb[i][hp_, 256 + sX * 64:512], in0=b1[hp_, 256 + sX * 64:512], scalar=0.125, in1=bias[hp_, h, bs0:bs0 + n1], op0=ALU.mult, op1=ALU.add),
                                        reads=[rb1, R_bias], writes=[R_s[i]])
                                    n2 = 512 - n1
                                    k.op("dve", lambda e, b2=b2, i=i, h=h, hp_=hp_, bs0=bs0, n1=n1, n2=n2: e.scalar_tensor_tensor(
                                        out=ssb[i][hp_, 512:512 + n2], in0=b2[hp_, 0:n2], scalar=0.125, in1=bias[hp_, h, bs0 + n1:bs0 + 512], op0=ALU.mult, op1=ALU.add),
                                        reads=[rb2, R_bias], writes=[R_s[i]])
                                    ng0 = 256 + (512 if sX == 0 else 0)
                                    k.op("pool", lambda e, i=i, hp_=hp_, ng0=ng0: e.memset(ssb[i][hp_, ng0:ng0 + 64], NEG), writes=[R_s[i]])
                                W = 832
                            else:
                                tile_i = r
                                k.op("pe", lambda e, b1=b1, j=j, r=r: e.matmul(b1[:, 0:256], qh[j][:, r * 128:(r + 1) * 128], kh[j][:, 0:256], start=True, stop=True), reads=[R_in[j]], writes=[rb1])
                                k.op("act", lambda e, b1=b1, i=i: e.activation(out=ssb[i][:, 0:256], in_=b1[:, 0:256], func=AF.Copy, scale=0.125), reads=[rb1], writes=[R_s[i]])
                                W = 256
                            k.op("dve", lambda e, i=i, W=W: e.tensor_reduce(out=st4[i][:, 0:1], in_=ssb[i][:, 0:W], axis=AX.X, op=ALU.max), reads=[R_s[i]], writes=[R_st[i]])
                            k.op("dve", lambda e, i=i: e.tensor_scalar(out=st4[i][:, 1:2], in0=st4[i][:, 0:1], scalar1=-1.0, scalar2=None, op0=ALU.mult), reads=[R_st[i]], writes=[R_st[i]])
                            k.op("act", lambda e, i=i, W=W: e.activation(out=pbf[i][:, 0:W], in_=ssb[i][:, 0:W], func=AF.Exp, bias=st4[i][:, 1:2], scale=1.0, accum_out=st4[i][:, 2:3]),
                                 reads=[R_s[i], R_st[i]], writes=[R_p[i], R_st[i]])
                            k.op("dve", lambda e, i=i: e.reciprocal(out=st4[i][:, 3:4], in_=st4[i][:, 2:3]), reads=[R_st[i]], writes=[R_st[i]])
                            pc, rc = ps_next()
                            pcb = pc[:].bitcast(BF16)
                            nch = (W + 127) // 128
                            for c in range(nch):
                                kw = min(128, W - c * 128)
                                k.op("pe", lambda e, pcb=pcb, i=i, c=c, kw=kw: e.transpose(pcb[0:kw, c * 128:(c + 1) * 128], pbf[i][:, c * 128:c * 128 + kw], ident_bf[:, :]),
                                     reads=[R_p[i], R_id], writes=[rc])
                            k.op("dve" if kind == "lat" else "act",
                                 (lambda e, pcb=pcb, i=i, nch=nch: e.tensor_copy(out=pT[i][:, 0:nch * 128], in_=pcb[:, 0:nch * 128])) if kind == "lat" else
                                 (lambda e, pcb=pcb, i=i, nch=nch: e.activation(out=pT[i][:, 0:nch * 128], in_=pcb[:, 0:nch * 128], func=AF.Copy)),
                                 reads=[rc], writes=[R_pT[i]])
                            pd, rd = ps_next()
                            for c in range(nch):
                                kw = min(128, W - c * 128)
                                if c < 2:
                                    vt = VE[j][:, c, :]
                                else:
                                    even = (R0 % 2 == 0)
                                    base_t = (w0 // 128) if even else ((w0 - 64) // 128)
                                    vt = (VE[j] if even else VO[j])[0:kw, base_t + (c - 2), :]
                                k.op("pe", lambda e, pd=pd, i=i, c=c, kw=kw, vt=vt, nch=nch: e.matmul(pd[:, 0:64], pT[i][0:kw, c * 128:(c + 1) * 128], vt, start=(c == 0), stop=(c == nch - 1)),
                                     reads=[R_in[j], R_pT[i]], writes=[rd])
                            k.op("act", lambda e, pd=pd, i=i, tile_i=tile_i, h=h: e.activation(out=ytok[:, tile_i, h * 64:(h + 1) * 64], in_=pd[:, 0:64], func=AF.Copy, scale=st4[i][:, 3:4]),
                                 reads=[rd, R_st[i]], writes=[R_ytok])
                    for tt in range(0 if need_ctx else 2, NT):
                        jj = tt % 2
                        pt, rp = ps_next()
                        ptb = pt[:].bitcast(BF16)
                        for q in range(4):
                            k.op("pe", lambda e, ptb=ptb, q=q, tt=tt: e.transpose(ptb[:, q * 128:(q + 1) * 128], ytok[:, tt, q * 128:(q + 1) * 128], ident_bf[:, :]), reads=[R_ytok, R_id], writes=[rp])
                        k.op("act", lambda e, ptb=ptb, jj=jj: e.activation(out=yfm[jj][:].rearrange("p q t -> p (q t)"), in_=ptb[:, 0:512], func=AF.Copy), reads=[rp], writes=[R_yfm[jj]])
                        k.dma("sp", YT[b][:, 4:8, tt * 128:(tt + 1) * 128], yfm[jj][:], reads=[R_yfm[jj]], writes=[R_YT[b]])
            k.barrier()

        def stage_wout(l, last):
            with ExitStack() as s1:
                w = sb("wo_w", [128, 8, D], BF16, s1); R_w = Res()
                for kc in range(8):
                    k.dma("pool", w[:, kc, :], I["w_out"][l, :, kc, :], writes=[R_w])
                yb = [sb("wo_y%d" % i, [128, 8, 256], BF16, s1) for i in range(2)]; R_yb = [Res(), Res()]
                xb = [sb("wo_x%d" % i, [128, 8, 256], F32, s1) for i in range(2)]; R_xb = [Res(), Res()]
                n = 0
                for b in range(NB):
                    for t0 in range(LC if last else 0, S, 256):
                        j = n % 2; n += 1
                        wq = which_of(b, t0)
                        k.dma("sp", yb[j][:], YT[b][:, :, t0:t0 + 256], reads=[R_YT[b]], writes=[R_yb[j]])
                        k.dma("sp", xb[j][:], XT[b][:, :, t0:t0 + 256], reads=[R_XT[b]], writes=[R_xb[j]])
                        for oc in range(8):
                            pt, rp = ps_next()
                            for kc in range(8):
                                k.op("pe", lambda e, pt=pt, j=j, oc=oc, kc=kc: e.matmul(pt[:, 0:256], w[:, kc, oc * 128:(oc + 1) * 128], yb[j][:, kc, :],
                                     start=(kc == 0), stop=(kc == 7)), reads=[R_w, R_yb[j]], writes=[rp])
                            k.op("dve", lambda e, pt=pt, j=j, oc=oc, wq=wq: e.scalar_tensor_tensor(out=xb[j][:, oc, :], in0=pt[:, 0:256],
                                 scalar=MOD[:, l, 16 + oc, wq:wq + 1], in1=xb[j][:, oc, :], op0=ALU.mult, op1=ALU.add), reads=[rp, R_mod, R_xb[j]], writes=[R_xb[j]])
                        k.dma("sp", XT[b][:, :, t0:t0 + 256], xb[j][:], reads=[R_xb[j]], writes=[R_XT[b]])
            k.barrier()

        def stage_router(l, last):
            i_moe = l // 2
            with ExitStack() as s1:
                rw = sb("rt_w", [128, 8, NE], F32, s1); R_rw = Res()
                k.dma("sp", rw[:], I["router"][i_moe], writes=[R_rw])
                hb = [sb("rt_h%d" % i, [128, 8, 128], F32, s1) for i in range(2)]; R_hb = [Res(), Res()]
                lg = [sb("rt_l%d" % i, [128, 40], F32, s1) for i in range(2)]; R_lg = [Res(), Res()]
                ge = [sb("rt_ge%d" % i, [128, 128], F32, s1) for i in range(2)]; R_ge = [Res(), Res()]
                gb = [sb("rt_gb%d" % i, [128, NE, 128], F32, s1) for i in range(2)]; R_gb = [Res(), Res()]
                n = 0; m = 0
                for b in range(NB):
                    for tt in range(2 if last else 0, NT):
                        j = n % 2; n += 1
                        t0 = tt * 128
                        k.dma("sp", hb[j][:], H2F[b][:, :, t0:t0 + 128], reads=[R_H2F[b]], writes=[R_hb[j]])
                        pt, rp = ps_next()
                        for kc in range(8):
                            k.op("pe", lambda e, pt=pt, j=j, kc=kc: e.matmul(pt[:, 0:NE], hb[j][:, kc, :], rw[:, kc, :], start=(kc == 0), stop=(kc == 7)),
                                 reads=[R_hb[j], R_rw], writes=[rp])
                        L = lg[j]
                        k.op("dve", lambda e, pt=pt, L=L: e.tensor_copy(out=L[:, 0:8], in_=pt[:, 0:8]), reads=[rp], writes=[R_lg[j]])
                        k.op("dve", lambda e, L=L: e.tensor_reduce(out=L[:, 8:9], in_=L[:, 0:8], axis=AX.X, op=ALU.max), reads=[R_lg[j]], writes=[R_lg[j]])
                        k.op("dve", lambda e, L=L: e.tensor_scalar(out=L[:, 9:17], in0=L[:, 0:8], scalar1=L[:, 8:9], scalar2=None, op0=ALU.is_ge), reads=[R_lg[j]], writes=[R_lg[j]])
                        k.op("dve", lambda e, L=L: e.scalar_tensor_tensor(out=L[:, 17:25], in0=L[:, 9:17], scalar=-1e30, in1=L[:, 0:8], op0=ALU.mult, op1=ALU.add), reads=[R_lg[j]], writes=[R_lg[j]])
                        k.op("dve", lambda e, L=L: e.tensor_reduce(out=L[:, 25:26], in_=L[:, 17:25], axis=AX.X, op=ALU.max), reads=[R_lg[j]], writes=[R_lg[j]])
                        k.op("dve", lambda e, L=L: e.tensor_scalar(out=L[:, 26:34], in0=L[:, 17:25], scalar1=L[:, 25:26], scalar2=None, op0=ALU.is_ge), reads=[R_lg[j]], writes=[R_lg[j]])
                        k.op("dve", lambda e, L=L: e.tensor_tensor(out=L[:, 34:35], in0=L[:, 8:9], in1=L[:, 25:26], op=ALU.subtract), reads=[R_lg[j]], writes=[R_lg[j]])
                        k.op("act", lambda e, L=L: e.activation(out=L[:, 35:36], in_=L[:, 34:35], func=AF.Sigmoid), reads=[R_lg[j]], writes=[R_lg[j]])
                        k.op("dve", lambda e, L=L: e.tensor_scalar(out=L[:, 36:37], in0=L[:, 35:36], scalar1=-1.0, scalar2=1.0, op0=ALU.mult, op1=ALU.add), reads=[R_lg[j]], writes=[R_lg[j]])
                        k.op("dve", lambda e, L=L: e.tensor_scalar(out=L[:, 9:17], in0=L[:, 9:17], scalar1=L[:, 35:36], scalar2=None, op0=ALU.mult), reads=[R_lg[j]], writes=[R_lg[j]])
                        k.op("dve", lambda e, L=L: e.scalar_tensor_tensor(out=L[:, 9:17], in0=L[:, 26:34], scalar=L[:, 36:37], in1=L[:, 9:17], op0=ALU.mult, op1=ALU.add), reads=[R_lg[j]], writes=[R_lg[j]])
                        for ex in range(NE):
                            i2 = m % 2; m += 1
                            k.op("pool", lambda e, L=L, i2=i2, ex=ex: e.tensor_copy(out=ge[i2][:], in_=L[:, 9 + ex:10 + ex].to_broadcast([128, 128])), reads=[R_lg[j]], writes=[R_ge[i2]])
                            pg, rg = ps_next()
                            k.op("pe", lambda e, pg=pg, i2=i2: e.matmul(pg[:, 0:128], ge[i2][:], ident[:], start=True, stop=True), reads=[R_ge[i2], R_id], writes=[rg])
                            k.op("act", lambda e, pg=pg, j=j, ex=ex: e.activation(out=gb[j][:, ex, :], in_=pg[:, 0:128], func=AF.Copy), reads=[rg], writes=[R_gb[j]])
                        k.dma("sp", GB[b][:, :, t0:t0 + 128], gb[j][:], reads=[R_gb[j]], writes=[R_GB[b]])
            k.barrier()

        def stage_ffn(l, last):
            moe = (l % 2 == 1)
            i_w = l // 2
            F = DFE if moe else DFF
            GW = 512 if moe else 256
            ng = F // GW
            nfc = GW // 128
            tstart = LC if last else 0
            with ExitStack() as s1:
                hT = sb("ff_h", [128, 8, S], BF16, s1); R_h = Res()
                yacc = sb("ff_y", [128, 8, S], F32, s1); R_ya = [Res() for _ in range(9)]
                w1 = [sb("ff_w1%d" % i, [128, 8, GW], BF16, s1) for i in range(2)]
                w3 = [sb("ff_w3%d" % i, [128, 8, GW], BF16, s1) for i in range(2)]
                w2 = [sb("ff_w2%d" % i, [128, nfc, D], BF16, s1) for i in range(2)]
                R_wg = [Res(), Res()]
                sg = [sb("ff_s%d" % i, [128, 256], F32, s1) for i in range(2)]; R_sg = [Res(), Res()]
                ac = [sb("ff_a%d" % i, [128, nfc, 256], BF16, s1) for i in range(2)]; R_ac = [Res(), Res()]
                gt = [sb("ff_g%d" % i, [128, 256], F32, s1) for i in range(2)]; R_gt = [Res(), Res()]
                xb = [sb("ff_x%d" % i, [128, 8, 256], F32, s1) for i in range(2)]; R_xb = [Res(), Res()]
                nw = 0; na_ = 0; ns = 0; ngt = 0
                for b in range(NB):
                    k.dma("sp", hT[:, :, tstart:S], H2T[b][:, :, tstart:S], reads=[R_H2T[b]], writes=[R_h])
                    first = True
                    for ex in range(NE if moe else 1):
                        for g in range(ng):
                            jw = nw % 2; nw += 1
                            if moe:
                                s_w1, s_w3, s_w2 = I["moe_w1"][i_w, ex], I["moe_w3"][i_w, ex], I["moe_w2"][i_w, ex]
                            else:
                                s_w1, s_w3, s_w2 = I["ffn_w1"][i_w], I["ffn_w3"][i_w], I["ffn_w2"][i_w]
                            k.dma("pool", w1[jw][:], s_w1[:, :, g * GW:(g + 1) * GW], writes=[R_wg[jw]])
                            k.dma("pool", w3[jw][:], s_w3[:, :, g * GW:(g + 1) * GW], writes=[R_wg[jw]])
                            k.dma("pool", w2[jw][:], s_w2[:, g * nfc:(g + 1) * nfc, :], writes=[R_wg[jw]])
                            def phase_H(tb, ja, jw=jw, ex=ex, g=g):
                                nonlocal ns, ngt
                                t0 = tb * 256
                                if moe:
                                    jg = ngt % 2; ngt += 1
                                    k.dma("sp", gt[jg][:], GB[b][:, ex, t0:t0 + 256], reads=[R_GB[b]], writes=[R_gt[jg]])
                                for fc in range(nfc):
                                    ph, rh = ps_next()
                                    for kc in range(8):
                                        k.op("pe", lambda e, ph=ph, fc=fc, kc=kc, t0=t0: e.matmul(ph[:, 0:256], w1[jw][:, kc, fc * 128:(fc + 1) * 128], hT[:, kc, t0:t0 + 256],
                                             start=(kc == 0), stop=(kc == 7)), reads=[R_wg[jw], R_h], writes=[rh])
                                    for kc in range(8):
                                        k.op("pe", lambda e, ph=ph, fc=fc, kc=kc, t0=t0: e.matmul(ph[:, 256:512], w3[jw][:, kc, fc * 128:(fc + 1) * 128], hT[:, kc, t0:t0 + 256],
                                             start=(kc == 0), stop=(kc == 7)), reads=[R_wg[jw], R_h], writes=[rh])
                                    js = ns % 2; ns += 1
                                    k.op("act", lambda e, ph=ph, js=js: e.activation(out=sg[js][:], in_=ph[:, 0:256], func=AF.Silu), reads=[rh], writes=[R_sg[js]])
                                    if moe:
                                        k.op("pool", lambda e, js=js, jg=jg: e.tensor_tensor(out=sg[js][:], in0=sg[js][:], in1=gt[jg][:], op=ALU.mult), reads=[R_sg[js], R_gt[jg]], writes=[R_sg[js]])
                                    k.op("dve", lambda e, ph=ph, js=js, ja=ja, fc=fc: e.tensor_tensor(out=ac[ja][:, fc, :], in0=sg[js][:], in1=ph[:, 256:512], op=ALU.mult),
                                         reads=[R_sg[js], rh], writes=[R_ac[ja]])

                            def phase_O(tb, ja, jw=jw, first=first):
                                t0 = tb * 256
                                for ocp in range(4):
                                    po, ro = ps_next()
                                    for half in range(2):
                                        oc = 2 * ocp + half
                                        for fc in range(nfc):
                                            k.op("pe", lambda e, po=po, fc=fc, oc=oc, half=half: e.matmul(po[:, half * 256:(half + 1) * 256], w2[jw][:, fc, oc * 128:(oc + 1) * 128], ac[ja][:, fc, :],
                                                 start=(fc == 0), stop=(fc == nfc - 1)), reads=[R_wg[jw], R_ac[ja]], writes=[ro])
                                    yv_ = yacc[:, 2 * ocp:2 * ocp + 2, t0:t0 + 256]
                                    pv_ = po[:, :].rearrange("p (a c) -> p a c", a=2)
                                    if first:
                                        k.op("act", lambda e, yv_=yv_, pv_=pv_: e.activation(out=yv_, in_=pv_, func=AF.Copy), reads=[ro], writes=[R_ya[tb]])
                                    else:
                                        k.op("dve", lambda e, yv_=yv_, pv_=pv_: e.tensor_tensor(out=yv_, in0=yv_, in1=pv_, op=ALU.add), reads=[ro, R_ya[tb]], writes=[R_ya[tb]])

                            prev = None
                            for tb in range(tstart // 256, S // 256):
                                ja = na_ % 2; na_ += 1
                                phase_H(tb, ja)
                                if prev is not None:
                                    phase_O(*prev)
                                prev = (tb, ja)
                            phase_O(*prev)
                            first = False
                    for tb in range(tstart // 256, S // 256):
                        t0 = tb * 256
                        j = tb % 2
                        wq = which_of(b, t0)
                        k.dma("sp", xb[j][:], XT[b][:, :, t0:t0 + 256], reads=[R_XT[b]], writes=[R_xb[j]])
                        for oc in range(8):
                            k.op("dve", lambda e, j=j, oc=oc, t0=t0, wq=wq: e.scalar_tensor_tensor(out=xb[j][:, oc, :], in0=yacc[:, oc, t0:t0 + 256],
                                 scalar=MOD[:, l, 40 + oc, wq:wq + 1], in1=xb[j][:, oc, :], op0=ALU.mult, op1=ALU.add), reads=[R_ya[tb], R_mod, R_xb[j]], writes=[R_xb[j]])
                        k.dma("sp", XT[b][:, :, t0:t0 + 256], xb[j][:], reads=[R_xb[j]], writes=[R_XT[b]])
            k.barrier()

        def stage_final():
            with ExitStack() as s1:
                fg = sb("fn_g", [128, 8], F32, s1); R_fg = Res()
                k.dma("sp", fg[:], I["final_g"][:, :], writes=[R_fg])
                xb = [sb("fx%d" % i, [128, 8, 256], F32, s1) for i in range(2)]; R_xb = [Res(), Res()]
                sq = [sb("fsq%d" % i, [128, 8, 256], F32, s1) for i in range(2)]; R_sq = [Res(), Res()]
                rs = [sb("frs%d" % i, [128, 256], F32, s1) for i in range(2)]; R_rs = [Res(), Res()]
                ob = [sb("fo%d" % i, [128, 8, 256], F32, s1) for i in range(2)]; R_ob = [Res(), Res()]
                n = 0
                outs = []
                for b in range(NB):
                    for t0 in range(LC, S, 256):
                        j = n % 2; n += 1
                        k.dma("sp", xb[j][:], XT[b][:, :, t0:t0 + 256], reads=[R_XT[b]], writes=[R_xb[j]])
                        k.op("act", lambda e, j=j: e.activation(out=sq[j][:], in_=xb[j][:], func=AF.Square), reads=[R_xb[j]], writes=[R_sq[j]])
                        pt, rp = ps_next()
                        for c in range(8):
                            k.op("pe", lambda e, pt=pt, j=j, c=c: e.matmul(pt[:, 0:256], ones[:], sq[j][:, c, :], start=(c == 0), stop=(c == 7)), reads=[R_ones, R_sq[j]], writes=[rp])
                        k.op("act", lambda e, pt=pt, j=j: e.activation(out=rs[j][:], in_=pt[:, 0:256], func=AF.Sqrt, bias=eps_t[:, 0:1], scale=1.0 / D), reads=[rp, R_eps], writes=[R_rs[j]])
                        k.op("dve", lambda e, j=j: e.reciprocal(out=rs[j][:], in_=rs[j][:]), reads=[R_rs[j]], writes=[R_rs[j]])
                        for c in range(8):
                            k.op("dve", lambda e, j=j, c=c: e.scalar_tensor_tensor(out=ob[j][:, c, :], in0=xb[j][:, c, :], scalar=fg[:, c:c + 1], in1=rs[j][:],
                                 op0=ALU.mult, op1=ALU.mult), reads=[R_xb[j], R_fg, R_rs[j]], writes=[R_ob[j]])
                        outs.append(k.dma("sp", out[b, :, :, t0 - LC:t0 - LC + 256], ob[j][:], reads=[R_ob[j]]))
                return outs

        def stage_rwkv(l, need_ctx):
            with ExitStack() as s1:
                NSET = 3
                def T_(name, shape, dt=F32, n=3):
                    return [sb(name + str(i), shape, dt, s1) for i in range(n)], [Res() for _ in range(n)]
                w2 = sb("rw_w2", [128, 512], F32, s1); a2 = sb("rw_a2", [128, 512], F32, s1); g2 = sb("rw_g2", [128, 512], F32, s1)
                w0b = sb("rw_w0b", [64, 2, 512], F32, s1); a0T = sb("rw_a0T", [128, 2, 4], F32, s1)
                kkp = sb("rw_kkp", [128, 4], F32, s1); kap = sb("rw_kap", [128, 4], F32, s1); okap = sb("rw_okap", [128, 4], F32, s1)
                rkp = sb("rw_rkp", [128, 4], F32, s1)
                m2 = sb("rw_m2", [64, 2, 128], F32, s1); m3 = sb("rw_m3", [64, 2, 128], F32, s1); mL = sb("rw_mL", [64, 2, 64], F32, s1)
                bones = sb("rw_bones", [128, 128], F32, s1); sel = sb("rw_sel", [128, 2], F32, s1)
                c1 = sb("rw_c1", [128, 4], F32, s1)
                R_par = Res()
                for dst, src in ((w2, I["rw_w2"][l]), (a2, I["rw_a2"][l]), (g2, I["rw_g2"][l]), (a0T, I["rw_a0T"][l]),
                                 (kkp, I["rw_kk"][l]), (kap, I["rw_ka"][l]), (rkp, I["rw_rk"][l]),
                                 (m2, I["rw_m2"]), (m3, I["rw_m3"]), (mL, I["rw_mL"]), (bones, I["rw_bones"]), (sel, I["rw_sel"]), (c1, I["rw_c1"]),
                                 (w0b, I["rw_w0b"][l])):
                    k.dma("sp", dst[:], src, writes=[R_par])
                k.op("dve", lambda e: e.tensor_scalar(out=okap[:], in0=kap[:], scalar1=-1.0, scalar2=1.0, op0=ALU.mult, op1=ALU.add), reads=[R_par], writes=[R_par])
                Hst_b = [sb("rw_H%d" % i, [128, 4, 64], F32, s1) for i in range(NB)]; R_H_b = [Res() for _ in range(NB)]
                ub, R_ub = T_("rw_u", [128, 15, 64])
                twl, R_twl = T_("rw_twl", [128, 64])
                sgl, R_sgl = T_("rw_sgl", [128, 64], n=2)
                e2a, R_e2a = T_("rw_e2a", [64, 512], n=2)
                e2b, R_e2b = T_("rw_e2b", [64, 512], n=2)
                a_sb, R_a = T_("rw_a", [128, 4, 64])
                a1_sb, R_a1 = T_("rw_a1", [128, 4, 64], n=2)
                kr, R_kr = T_("rw_kr", [128, 4, 64])
                sq, R_sq = T_("rw_sq", [128, 4, 64])
                kk_t, R_kk = T_("rw_kkt", [128, 4, 64])
                ff, R_ff = T_("rw_ff", [128, 4, 64])
                kd, R_kd = T_("rw_kd", [128, 4, 64])
                bb, R_bb = T_("rw_bb", [128, 4, 64])
                EG, R_EG = T_("rw_EG", [128, 4, 2, 64])
                IEG, R_IEG = T_("rw_IEG", [128, 4, 64])
                fm, R_fm = T_("rw_fm", [128, 4, 4, 64])
                tm, R_tm = T_("rw_tm", [64, 4, 4, 128])
                LA, R_LA = T_("rw_LA", [64, 8, 128])
                NBt, R_NB = T_("rw_NB", [64, 8, 128])
                Lm, R_Lm = T_("rw_Lm", [64, 8, 64])
                Ll32, R_Ll32 = T_("rw_Ll32", [64, 8, 64])
                Pb, R_Pb = T_("rw_Pb", [64, 8, 64], BF16)
                Pm, R_Pm = T_("rw_Pm", [64, 8, 64])
                Nl, R_Nl = T_("rw_Nl", [64, 8, 64], BF16, n=4)
                Ll, R_Ll = T_("rw_Ll", [64, 8, 64], BF16, n=4)
                WT, R_WT = T_("rw_WT", [128, 4, 64])
                Xa, R_Xa = T_("rw_Xa", [64, 8, 64])
                Uta, R_Uta = T_("rw_Uta", [64, 8, 64])
                Ua, R_Ua = T_("rw_Ua", [64, 8, 64])
                ysb, R_ysb = T_("rw_ysb", [64, 512])
                htmp, R_htmp = T_("rw_htmp", [128, 4, 64])
                rk_t, R_rk = T_("rw_rkt", [128, 4, 64], n=2)
                rkh, R_rkh = T_("rw_rkh", [64, 8], n=2)
                bon, R_bon = T_("rw_bon", [64, 512], n=2)
                gsb, R_gsb = T_("rw_gsb", [64, 512], n=2)
                n = 0
                nl4 = [0]
                nb_run = NB if RW_DBG["nb"] is None else RW_DBG["nb"]
                for d in range(RW_DBG["nd"]):
                    for b in range(nb_run):
                        k.op("pool", lambda e, b=b: e.memset(Hst_b[b][:], 0.0), writes=[R_H_b[b]])
                    order = list(range(36)) if d == 0 else [3, 2, 1, 0] + list(range(35, 3, -1))
                    if RW_DBG["nch"] is not None:
                        order = order[:RW_DBG["nch"]]
                    last_col = 63 if d == 0 else 0
                    for ch in order:
                      for b in range(nb_run):
                        j = n % NSET; j2 = n % 2; n += 1
                        def chunk_body(b=b, ch=ch, d=d, j=j, j2=j2, last_col=last_col, Hst=Hst_b[b], R_H=R_H_b[b]):
                            if (not need_ctx) and False:
                                pass
                            t0 = ch * 64
                            U_ = ub[j]
                            k.dma("sp", U_[:], UT[b][:, :, t0:t0 + 64], reads=[R_UT[b]], writes=[R_ub[j]])
                            k.op("act", lambda e, j=j, U_=U_: e.activation(out=twl[j][:], in_=U_[:, 12, :], func=AF.Tanh), reads=[R_ub[j]], writes=[R_twl[j]])
                            pt, rp = ps_next()
                            k.op("pe", lambda e, pt=pt, j=j, d=d: e.matmul(pt[0:64, :], twl[j][d * 64:(d + 1) * 64, :], w2[d * 64:(d + 1) * 64, :], start=True, stop=True),
                                 reads=[R_twl[j], R_par], writes=[rp])
                            k.op("dve", lambda e, j2=j2, pt=pt, j=j, d=d: e.tensor_tensor(out=e2a[j2][:], in0=pt[0:64, :], in1=w0b[:, d, :], op=ALU.add), reads=[rp, R_par], writes=[R_e2a[j2]])
                            k.op("act", lambda e, j2=j2, j=j: e.activation(out=e2b[j2][:], in_=e2a[j2][:], func=AF.Exp, scale=-1.0), reads=[R_e2a[j2]], writes=[R_e2b[j2]])
                            k.op("act", lambda e, j2=j2, j=j: e.activation(out=e2a[j2][:], in_=e2b[j2][:], func=AF.Ln, bias=c1[0:64, 0:1], scale=1.0), reads=[R_e2b[j2], R_par], writes=[R_e2a[j2]])
                            k.op("act", lambda e, j2=j2, j=j: e.activation(out=e2b[j2][:], in_=e2a[j2][:], func=AF.Exp, bias=c1[0:64, 1:2], scale=-1.0), reads=[R_e2a[j2], R_par], writes=[R_e2b[j2]])
                            def a_path(dd, dst, R_dst):
                                pa, ra = ps_next()
                                for q in range(4):
                                    k.op("pe", lambda e, pa=pa, q=q, dd=dd, U_=U_: e.matmul(pa[:, q * 64:(q + 1) * 64], a2[dd * 64:(dd + 1) * 64, q * 128:(q + 1) * 128], U_[dd * 64:(dd + 1) * 64, 13, :], start=True, stop=True),
                                         reads=[R_par, R_ub[j]], writes=[ra])
                                for q in range(4):
                                    k.op("act", lambda e, pa=pa, q=q, dd=dd, dst=dst: e.activation(out=dst[:, q, :], in_=pa[:, q * 64:(q + 1) * 64], func=AF.Sigmoid, bias=a0T[:, dd, q:q + 1], scale=1.0),
                                         reads=[ra, R_par], writes=[R_dst])
                            a_path(d, a_sb[j], R_a[j])
                            k.op("dve", lambda e, j=j, U_=U_: e.tensor_tensor(out=kr[j][:], in0=U_[:, 4:8, :], in1=kkp[:, :].unsqueeze(2).to_broadcast([128, 4, 64]), op=ALU.mult), reads=[R_ub[j], R_par], writes=[R_kr[j]])
                            k.op("pool", lambda e, j=j: e.tensor_tensor(out=sq[j][:], in0=kr[j][:], in1=kr[j][:], op=ALU.mult), reads=[R_kr[j]], writes=[R_sq[j]])
                            pn_, rn = ps_next()
                            k.op("pe", lambda e, pn_=pn_, j=j: e.matmul(pn_[:, 0:256], bones[:], sq[j][:].rearrange("p q t -> p (q t)"), start=True, stop=True), reads=[R_par, R_sq[j]], writes=[rn])
                            k.op("act", lambda e, pn_=pn_, j=j: e.activation(out=sq[j][:].rearrange("p q t -> p (q t)"), in_=pn_[:, 0:256], func=AF.Sqrt), reads=[rn], writes=[R_sq[j]])
                            k.op("dve", lambda e, j=j: e.tensor_scalar(out=sq[j][:], in0=sq[j][:], scalar1=1e-12, scalar2=None, op0=ALU.max), reads=[R_sq[j]], writes=[R_sq[j]])
                            k.op("dve", lambda e, j=j: e.reciprocal(out=sq[j][:], in_=sq[j][:]), reads=[R_sq[j]], writes=[R_sq[j]])
                            k.op("dve", lambda e, j=j: e.tensor_tensor(out=kk_t[j][:], in0=kr[j][:], in1=sq[j][:], op=ALU.mult), reads=[R_kr[j], R_sq[j]], writes=[R_kk[j]])
                            k.op("pool", lambda e, j=j: e.tensor_tensor(out=ff[j][:], in0=a_sb[j][:], in1=kap[:, :].unsqueeze(2).to_broadcast([128, 4, 64]), op=ALU.mult), reads=[R_a[j], R_par], writes=[R_ff[j]])
                            k.op("pool", lambda e, j=j: e.tensor_tensor(out=ff[j][:], in0=ff[j][:], in1=okap[:, :].unsqueeze(2).to_broadcast([128, 4, 64]), op=ALU.add), reads=[R_ff[j], R_par], writes=[R_ff[j]])
                            k.op("pool", lambda e, j=j, U_=U_: e.tensor_tensor(out=kd[j][:], in0=U_[:, 4:8, :], in1=ff[j][:], op=ALU.mult), reads=[R_ub[j], R_ff[j]], writes=[R_kd[j]])
                            k.op("pool", lambda e, j=j: e.tensor_tensor(out=bb[j][:], in0=kk_t[j][:], in1=a_sb[j][:], op=ALU.mult), reads=[R_kk[j], R_a[j]], writes=[R_bb[j]])
                            pc, rc = ps_next()
                            for q in range(4):
                                k.op("pe", lambda e, j2=j2, pc=pc, q=q, j=j, d=d: e.matmul(pc[:, q * 128:(q + 1) * 128], e2b[j2][:, q * 128:(q + 1) * 128], m2[:, d, :], start=True, stop=True),
                                     reads=[R_e2b[j2], R_par], writes=[rc])
                            k.op("act", lambda e, pc=pc, j=j: e.activation(out=EG[j][:].rearrange("p q s t -> p (q s t)"), in_=pc[:, :], func=AF.Exp, scale=-1.0), reads=[rc], writes=[R_EG[j]])
                            k.op("act", lambda e, pc=pc, j=j: e.activation(out=IEG[j][:], in_=pc[:, :].rearrange("p (q s t) -> p q s t", q=4, s=2)[:, :, 1, :], func=AF.Exp, scale=1.0), reads=[rc], writes=[R_IEG[j]])
                            F_ = fm[j]
                            k.op("dve", lambda e, j=j, F_=F_: e.tensor_tensor(out=F_[:, :, 0, :], in0=kd[j][:], in1=IEG[j][:], op=ALU.mult), reads=[R_kd[j], R_IEG[j]], writes=[R_fm[j]])
                            k.op("dve", lambda e, j=j, F_=F_: e.tensor_tensor(out=F_[:, :, 1, :], in0=bb[j][:], in1=IEG[j][:], op=ALU.mult), reads=[R_bb[j], R_IEG[j]], writes=[R_fm[j]])
                            k.op("pool", lambda e, j=j, F_=F_: e.tensor_tensor(out=F_[:, :, 2, :], in0=kk_t[j][:], in1=EG[j][:, :, 0, :], op=ALU.mult), reads=[R_kk[j], R_EG[j]], writes=[R_fm[j]])
                            k.op("pool", lambda e, j=j, F_=F_, U_=U_: e.tensor_tensor(out=F_[:, :, 3, :], in0=U_[:, 0:4, :], in1=EG[j][:, :, 1, :], op=ALU.mult), reads=[R_ub[j], R_EG[j]], writes=[R_fm[j]])
                            T_m = tm[j]
                            for kind, (srcf, sc) in enumerate(((lambda q, F_=F_: F_[:, q, 2, :], 1.0), (lambda q, F_=F_: F_[:, q, 0, :], 1.0), (lambda q, F_=F_: F_[:, q, 1, :], -1.0), (lambda q, U_=U_: U_[:, 8 + q, :], 1.0))):
                                ptx, rtx = ps_next()
                                for q in range(4):
                                    k.op("pe", lambda e, ptx=ptx, q=q, srcf=srcf: e.transpose(ptx[0:64, q * 128:(q + 1) * 128], srcf(q), ident[:, :]),
                                         reads=[R_fm[j], R_ub[j], R_id], writes=[rtx])
                                eng = "act" if kind % 2 == 0 else "dve"
                                if eng == "act":
                                    k.op("act", lambda e, ptx=ptx, kind=kind, sc=sc, T_m=T_m: e.activation(out=T_m[:, :, kind, :], in_=ptx[0:64, :].rearrange("p (q c) -> p q c", q=4), func=AF.Copy, scale=sc),
                                         reads=[rtx], writes=[R_tm[j]])
                                else:
                                    k.op("dve", lambda e, ptx=ptx, kind=kind, sc=sc, T_m=T_m: e.tensor_scalar(out=T_m[:, :, kind, :], in0=ptx[0:64, :].rearrange("p (q c) -> p q c", q=4), scalar1=sc, scalar2=None, op0=ALU.mult),
                                         reads=[rtx], writes=[R_tm[j]])
                            if RW_DBG["upto"] < "A2":
                                return
                            if d == 0 and (need_ctx or ch >= 4):
                                a_path(1, a1_sb[j2], R_a1[j2])
                                k.op("dve", lambda e, j2=j2, j=j: e.tensor_tensor(out=a1_sb[j2][:], in0=a1_sb[j2][:], in1=a_sb[j][:], op=ALU.add), reads=[R_a1[j2], R_a[j]], writes=[R_a1[j2]])
                                k.op("dve", lambda e, j2=j2, j=j: e.scalar_tensor_tensor(out=a1_sb[j2][:], in0=a1_sb[j2][:], scalar=0.5, in1=kap[:, :].unsqueeze(2).to_broadcast([128, 4, 64]), op0=ALU.mult, op1=ALU.mult), reads=[R_a1[j2], R_par], writes=[R_a1[j2]])
                                k.op("dve", lambda e, j2=j2, j=j: e.tensor_tensor(out=a1_sb[j2][:], in0=a1_sb[j2][:], in1=okap[:, :].unsqueeze(2).to_broadcast([128, 4, 64]), op=ALU.add), reads=[R_a1[j2], R_par], writes=[R_a1[j2]])
                                k.op("dve", lambda e, j2=j2, j=j, U_=U_: e.tensor_tensor(out=rk_t[j2][:], in0=a1_sb[j2][:], in1=U_[:, 4:8, :], op=ALU.mult), reads=[R_a1[j2], R_ub[j]], writes=[R_rk[j2]])
                                k.op("dve", lambda e, j2=j2, j=j, U_=U_: e.tensor_tensor(out=rk_t[j2][:], in0=rk_t[j2][:], in1=U_[:, 0:4, :], op=ALU.mult), reads=[R_rk[j2], R_ub[j]], writes=[R_rk[j2]])
                                k.op("dve", lambda e, j2=j2, j=j: e.tensor_tensor(out=rk_t[j2][:], in0=rk_t[j2][:], in1=rkp[:, :].unsqueeze(2).to_broadcast([128, 4, 64]), op=ALU.mult), reads=[R_rk[j2], R_par], writes=[R_rk[j2]])
                                pr, rr = ps_next()
                                for q in range(4):
                                    k.op("pe", lambda e, j2=j2, pr=pr, q=q, j=j: e.matmul(pr[0:64, q * 2:(q + 1) * 2], rk_t[j2][:, q, :], sel[:, :], start=True, stop=True), reads=[R_rk[j2], R_par], writes=[rr])
                                k.op("act", lambda e, j2=j2, pr=pr, j=j: e.activation(out=rkh[j2][:], in_=pr[0:64, 0:8], func=AF.Copy), reads=[rr], writes=[R_rkh[j2]])
                                k.op("dve", lambda e, j2=j2, j=j, T_m=T_m: e.tensor_tensor(out=bon[j2][:].rearrange("p (q h v) -> p q h v", q=4, h=2), in0=T_m[:, :, 3, :].rearrange("p q (h v) -> p q h v", h=2),
                                     in1=rkh[j2][:, :].rearrange("p (q h) -> p q h", q=4).unsqueeze(3).to_broadcast([64, 4, 2, 64]), op=ALU.mult), reads=[R_tm[j], R_rkh[j2]], writes=[R_bon[j2]])
                                k.dma("sp", BON[b][t0:t0 + 64, :], bon[j2][:], reads=[R_bon[j2]], writes=[R_BON[b]])
                                k.op("act", lambda e, j2=j2, j=j, U_=U_: e.activation(out=sgl[j2][:], in_=U_[:, 14, :], func=AF.Sigmoid), reads=[R_ub[j]], writes=[R_sgl[j2]])
                                pg, rg = ps_next()
                                k.op("pe", lambda e, j2=j2, pg=pg, j=j: e.matmul(pg[0:64, :], sgl[j2][:], g2[:], start=True, stop=True), reads=[R_sgl[j2], R_par], writes=[rg])
                                k.op("act", lambda e, j2=j2, pg=pg, j=j: e.activation(out=gsb[j2][:], in_=pg[0:64, :], func=AF.Copy), reads=[rg], writes=[R_gsb[j2]])
                                k.dma("sp", GG[b][t0:t0 + 64, :], gsb[j2][:], reads=[R_gsb[j2]], writes=[R_GG[b]])
                            if RW_DBG["upto"] < "B":
                                return
                            def hp(h):
                                return h // 2, (h % 2) * 64
                            hv = lambda t, h2: t[:].rearrange("p (q h) c -> p q h c", q=4, h=2)[:, :, h2, :]
                            for h2 in range(2):
                                p0 = h2 * 64
                                p1, r1 = ps_next()
                                p2, r2 = ps_next()
                                p3, r3 = ps_next()
                                for q in range(4):
                                    k.op("pe", lambda e, p1=p1, q=q, p0=p0, F_=F_: e.matmul(p1[0:64, q * 128:(q + 1) * 128], F_[p0:p0 + 64, q, 0, :], F_[p0:p0 + 64, q, 2:4, :].rearrange("p s t -> p (s t)"), start=True, stop=True),
                                         reads=[R_fm[j]], writes=[r1])
                                    k.op("pe", lambda e, p2=p2, q=q, p0=p0, F_=F_: e.matmul(p2[0:64, q * 128:(q + 1) * 128], F_[p0:p0 + 64, q, 1, :], F_[p0:p0 + 64, q, 2:4, :].rearrange("p s t -> p (s t)"), start=True, stop=True),
                                         reads=[R_fm[j]], writes=[r2])
                                    k.op("pe", lambda e, p3=p3, q=q, p0=p0, F_=F_: e.matmul(p3[0:64, q * 64:(q + 1) * 64], F_[p0:p0 + 64, q, 2, :], F_[p0:p0 + 64, q, 1, :], start=True, stop=True), reads=[R_fm[j]], writes=[r3])
                                k.op("dve", lambda e, p1=p1, h2=h2, j=j, d=d: e.tensor_tensor(out=hv(LA[j], h2), in0=p1[0:64, :].rearrange("p (q c) -> p q c", q=4),
                                     in1=m2[:, d, :].unsqueeze(1).to_broadcast([64, 4, 128]), op=ALU.mult), reads=[r1, R_par], writes=[R_LA[j]])
                                k.op("dve", lambda e, p2=p2, h2=h2, j=j, d=d: e.tensor_tensor(out=hv(NBt[j], h2), in0=p2[0:64, :].rearrange("p (q c) -> p q c", q=4),
                                     in1=m3[:, d, :].unsqueeze(1).to_broadcast([64, 4, 128]), op=ALU.mult), reads=[r2, R_par], writes=[R_NB[j]])
                                k.op("dve", lambda e, p3=p3, h2=h2, j=j, d=d: e.tensor_tensor(out=hv(Lm[j], h2), in0=p3[0:64, 0:256].rearrange("p (q c) -> p q c", q=4),
                                     in1=mL[:, d, :].unsqueeze(1).to_broadcast([64, 4, 64]), op=ALU.mult), reads=[r3, R_par], writes=[R_Lm[j]])
                            if RW_DBG["upto"] < "B0":
                                return
                            k.op("dve", lambda e, j=j: e.scalar_tensor_tensor(out=Pm[j][:], in0=NBt[j][:, :, 0:64], scalar=-1.0, in1=ident[0:64, 0:64].unsqueeze(1).to_broadcast([64, 8, 64]), op0=ALU.mult, op1=ALU.add),
                                 reads=[R_NB[j], R_id], writes=[R_Pm[j]])
                            Ncur = lambda h, j=j: NBt[j][:, h, 0:64]
                            Lcur = lambda h, j=j: Lm[j][:, h, :]
                            R_Nc, R_Lc = R_NB[j], R_Lm[j]
                            nlev = RW_DBG.get("nlev", 5)
                            for lev in range(nlev):
                                i4 = nl4[0] % 4; nl4[0] += 1
                                pL, rL = ps_next()
                                for h in range(8):
                                    k.op("pe", lambda e, pL=pL, h=h, Ncur=Ncur, Lcur=Lcur: e.matmul(pL[0:64, h * 64:(h + 1) * 64], Ncur(h), Lcur(h), start=True, stop=True), reads=[R_Nc, R_Lc], writes=[rL])
                                if lev == 0:
                                    k.op("act", lambda e, pL=pL, j=j: e.activation(out=Ll32[j][:].rearrange("p h c -> p (h c)"), in_=pL[0:64, :], func=AF.Copy), reads=[rL], writes=[R_Ll32[j]])
                                    k.op("pool", lambda e, i4=i4, j=j: e.tensor_copy(out=Ll[i4][:], in_=Ll32[j][:]), reads=[R_Ll32[j]], writes=[R_Ll[i4]])
                                else:
                                    k.op("act", lambda e, pL=pL, i4=i4: e.activation(out=Ll[i4][:].rearrange("p h c -> p (h c)"), in_=pL[0:64, :], func=AF.Copy), reads=[rL], writes=[R_Ll[i4]])
                                if lev < nlev - 1:
                                    pN, rN = ps_next()
                                    for h in range(8):
                                        k.op("pe", lambda e, pN=pN, h=h, Ncur=Ncur, Lcur=Lcur: e.matmul(pN[0:64, h * 64:(h + 1) * 64], Lcur(h), Ncur(h), start=True, stop=True), reads=[R_Nc, R_Lc], writes=[rN])
                                    k.op("dve", lambda e, pN=pN, i4=i4: e.tensor_copy(out=Nl[i4][:].rearrange("p h c -> p (h c)"), in_=pN[0:64, :]), reads=[rN], writes=[R_Nl[i4]])
                                pP, rP = ps_next()
                                for h in range(8):
                                    if lev == 0:
                                        k.op("pe", lambda e, pP=pP, h=h, j=j: e.matmul(pP[0:64, h * 64:(h + 1) * 64], Ll32[j][:, h, :], Pm[j][:, h, :], start=True, stop=True), reads=[R_Ll32[j], R_Pm[j]], writes=[rP])
                                    else:
                                        k.op("pe", lambda e, pP=pP, h=h, i4=i4, j=j: e.matmul(pP[0:64, h * 64:(h + 1) * 64], Ll[i4][:, h, :], Pb[j][:, h, :], start=True, stop=True), reads=[R_Ll[i4], R_Pb[j]], writes=[rP])
                                k.op("dve", lambda e, pP=pP, j=j: e.tensor_tensor(out=Pm[j][:].rearrange("p h c -> p (h c)"), in0=Pm[j][:].rearrange("p h c -> p (h c)"), in1=pP[0:64, :], op=ALU.add), reads=[rP, R_Pm[j]], writes=[R_Pm[j]])
                                if lev < nlev - 1:
                                    k.op("act", lambda e, j=j: e.activation(out=Pb[j][:], in_=Pm[j][:], func=AF.Copy), reads=[R_Pm[j]], writes=[R_Pb[j]])
                                Ncur = lambda h, i4=i4: Nl[i4][:, h, :]
                                Lcur = lambda h, i4=i4: Ll[i4][:, h, :]
                                R_Nc, R_Lc = R_Nl[i4], R_Ll[i4]
                            if RW_DBG["upto"] < "B2":
                                return
                            pW, rW = ps_next()
                            for h in range(8):
                                q, p0 = hp(h)
                                k.op("pe", lambda e, pW=pW, h=h, q=q, j=j, T_m=T_m: e.matmul(pW[:, h * 64:(h + 1) * 64], T_m[:, q, 0, :], Pm[j][:, h, :], start=True, stop=True), reads=[R_tm[j], R_Pm[j]], writes=[rW])
                            for h2 in range(2):
                                k.op("act" if h2 == 0 else "dve",
                                     (lambda e, pW=pW, j=j, h2=h2: e.activation(out=WT[j][h2 * 64:(h2 + 1) * 64, :, :], in_=pW[h2 * 64:(h2 + 1) * 64, :].rearrange("p (q h i) -> p q h i", q=4, h=2)[:, :, h2, :], func=AF.Copy)) if h2 == 0 else
                                     (lambda e, pW=pW, j=j, h2=h2: e.tensor_copy(out=WT[j][h2 * 64:(h2 + 1) * 64, :, :], in_=pW[h2 * 64:(h2 + 1) * 64, :].rearrange("p (q h i) -> p q h i", q=4, h=2)[:, :, h2, :])),
                                     reads=[rW], writes=[R_WT[j]])
                            pX, rX = ps_next()
                            for h in range(8):
                                q, p0 = hp(h)
                                k.op("pe", lambda e, pX=pX, h=h, q=q, p0=p0, j=j, T_m=T_m: e.matmul(pX[0:64, h * 64:(h + 1) * 64], LA[j][:, h, 0:64], T_m[:, q, 3, p0:p0 + 64], start=True, stop=True), reads=[R_LA[j], R_tm[j]], writes=[rX])
                            k.op("act", lambda e, pX=pX, j=j: e.activation(out=Xa[j][:].rearrange("p h c -> p (h c)"), in_=pX[0:64, :], func=AF.Copy), reads=[rX], writes=[R_Xa[j]])
                            pU, rU = ps_next()
                            for h in range(8):
                                k.op("pe", lambda e, pU=pU, h=h, j=j: e.matmul(pU[0:64, h * 64:(h + 1) * 64], Pm[j][:, h, :], Xa[j][:, h, :], start=True, stop=True), reads=[R_Pm[j], R_Xa[j]], writes=[rU])
                            k.op("act", lambda e, pU=pU, j=j: e.activation(out=Uta[j][:].rearrange("p h c -> p (h c)"), in_=pU[0:64, :], func=AF.Copy), reads=[rU], writes=[R_Uta[j]])
                            if RW_DBG["upto"] < "C":
                                return
                            for h2 in range(2):
                                p0 = h2 * 64
                                pS, rS = ps_next()
                                for q in range(4):
                                    k.op("pe", lambda e, Hst=Hst, pS=pS, q=q, p0=p0, j=j: e.matmul(pS[0:64, q * 64:(q + 1) * 64], WT[j][p0:p0 + 64, q, :], Hst[p0:p0 + 64, q, :], start=True, stop=True), reads=[R_WT[j], R_H], writes=[rS])
                                k.op("dve", lambda e, pS=pS, j=j, h2=h2: e.tensor_tensor(out=hv(Ua[j], h2), in0=hv(Uta[j], h2), in1=pS[0:64, 0:256].rearrange("p (q c) -> p q c", q=4), op=ALU.add),
                                     reads=[rS, R_Uta[j]], writes=[R_Ua[j]])
                            if RW_DBG["upto"] < "C1":
                                return
                            pY, rY = ps_next()
                            for h in range(8):
                                q, p0 = hp(h)
                                k.op("pe", lambda e, pY=pY, h=h, q=q, p0=p0, j=j, T_m=T_m: e.matmul(pY[0:64, h * 64:(h + 1) * 64], LA[j][:, h, 64:128], T_m[:, q, 3, p0:p0 + 64], start=True, stop=False), reads=[R_LA[j], R_tm[j]], writes=[rY])
                                k.op("pe", lambda e, pY=pY, h=h, j=j: e.matmul(pY[0:64, h * 64:(h + 1) * 64], NBt[j][:, h, 64:128], Ua[j][:, h, :], start=False, stop=True), reads=[R_NB[j], R_Ua[j]], writes=[rY])
                            k.op("act", lambda e, pY=pY, j=j: e.activation(out=ysb[j][:], in_=pY[0:64, :], func=AF.Copy), reads=[rY], writes=[R_ysb[j]])
                            for h2 in range(2):
                                p0 = h2 * 64
                                pR, rR = ps_next()
                                for q in range(4):
                                    k.op("pe", lambda e, Hst=Hst, pR=pR, q=q, p0=p0, j=j, F_=F_: e.matmul(pR[0:64, q * 64:(q + 1) * 64], F_[p0:p0 + 64, q, 3, :], Hst[p0:p0 + 64, q, :], start=True, stop=True), reads=[R_fm[j], R_H], writes=[rR])
                                yv = lambda t, h2: t[:].rearrange("p (q h c) -> p q h c", q=4, h=2)[:, :, h2, :]
                                k.op("dve", lambda e, pR=pR, j=j, h2=h2, yv=yv: e.tensor_tensor(out=yv(ysb[j], h2), in0=yv(ysb[j], h2), in1=pR[0:64, 0:256].rearrange("p (q c) -> p q c", q=4), op=ALU.add),
                                     reads=[rR, R_ysb[j]], writes=[R_ysb[j]])
                            k.dma("sp", YD[d][b][t0:t0 + 64, :], ysb[j][:], reads=[R_ysb[j]], writes=[R_YD[d][b]])
                            if RW_DBG["upto"] < "C2":
                                return
                            pH, rH = ps_next()
                            for q in range(4):
                                k.op("pe", lambda e, pH=pH, q=q, T_m=T_m: e.matmul(pH[:, q * 128:(q + 1) * 128], T_m[:, q, 1, :], T_m[:, q, 3, :], start=True, stop=False), reads=[R_tm[j]], writes=[rH])
                                k.op("pe", lambda e, pH=pH, q=q, j=j, T_m=T_m: e.matmul(pH[:, q * 128:(q + 1) * 128], T_m[:, q, 2, :], Ua[j][:, 2 * q:2 * q + 2, :].rearrange("p h c -> p (h c)"), start=False, stop=True), reads=[R_tm[j], R_Ua[j]], writes=[rH])
                            for h2 in range(2):
                                ps_ = slice(h2 * 64, (h2 + 1) * 64)
                                k.op("dve", lambda e, Hst=Hst, pH=pH, j=j, h2=h2, ps_=ps_: e.tensor_tensor(out=htmp[j][ps_, :, :], in0=pH[ps_, :].rearrange("p (q h v) -> p q h v", q=4, h=2)[:, :, h2, :], in1=Hst[ps_, :, :], op=ALU.add),
                                     reads=[rH, R_H], writes=[R_htmp[j]])
                                k.op("dve", lambda e, Hst=Hst, j=j, ps_=ps_, last_col=last_col: e.tensor_tensor(out=Hst[ps_, :, :], in0=htmp[j][ps_, :, :], in1=EG[j][ps_, :, 1, last_col:last_col + 1].to_broadcast([64, 4, 64]), op=ALU.mult),
                                     reads=[R_htmp[j], R_EG[j]], writes=[R_H])
                        chunk_body()
            k.barrier()
            if not RW_DBG["readout"]:
                return
            with ExitStack() as s1:
                lnw_ro = sb("ro_lnw", [128, 512], F32, s1); lnb_ro = sb("ro_lnb", [128, 512], F32, s1); c1_ro = sb("ro_c1", [128, 4], F32, s1); R_par_ro = Res()
                k.dma("sp", lnw_ro[:], I["rw_lnw"][l], writes=[R_par_ro]); k.dma("sp", lnb_ro[:], I["rw_lnb"][l], writes=[R_par_ro]); k.dma("sp", c1_ro[:], I["rw_c1"], writes=[R_par_ro])
                def T2(name, shape, dt=F32):
                    return [sb(name + str(i), shape, dt, s1) for i in range(2)], [Res(), Res()]
                y0_ro, R_y0_ro = T2("ro_y0", [128, 512]); y1_ro, R_y1_ro = T2("ro_y1", [128, 512]); bo_ro, R_bo_ro = T2("ro_bo", [128, 512]); gg_ro, R_gg_ro = T2("ro_gg", [128, 512])
                stt__ro, R_stt_ro = T2("ro_st", [128, 32]); yc_ro, R_yc_ro = T2("ro_yc", [128, 512]); sq_ro, R_sq_ro = T2("ro_sq", [128, 512]); ot_ro, R_ot_ro = T2("ro_ot", [128, 4, 128], BF16)
                n = 0
                for b in range(NB):
                    for tt in range(0 if need_ctx else 2, NT):
                        j = n % 2; n += 1
                        t0 = tt * 128
                        k.dma("sp", y0_ro[j][:], YD[0][b][t0:t0 + 128, :], reads=[R_YD[0][b]], writes=[R_y0_ro[j]])
                        k.dma("sp", y1_ro[j][:], YD[1][b][t0:t0 + 128, :], reads=[R_YD[1][b]], writes=[R_y1_ro[j]])
                        k.dma("sp", bo_ro[j][:], BON[b][t0:t0 + 128, :], reads=[R_BON[b]], writes=[R_bo_ro[j]])
                        k.dma("sp", gg_ro[j][:], GG[b][t0:t0 + 128, :], reads=[R_GG[b]], writes=[R_gg_ro[j]])
                        S_ = stt__ro[j]
                        v3 = lambda t: t[:].rearrange("p (h v) -> p h v", h=8)
                        bc = lambda a: a.unsqueeze(2).to_broadcast([128, 8, 64])
                        k.op("dve", lambda e, j=j: e.tensor_tensor(out=y0_ro[j][:], in0=y0_ro[j][:], in1=y1_ro[j][:], op=ALU.add), reads=[R_y0_ro[j], R_y1_ro[j]], writes=[R_y0_ro[j]])
                        k.op("dve", lambda e, j=j, S_=S_: e.tensor_reduce(out=S_[:, 0:8], in_=v3(y0_ro[j]), axis=AX.X, op=ALU.add), reads=[R_y0_ro[j]], writes=[R_stt_ro[j]])
                        k.op("dve", lambda e, S_=S_: e.tensor_scalar(out=S_[:, 8:16], in0=S_[:, 0:8], scalar1=1.0 / 64, scalar2=None, op0=ALU.mult), reads=[R_stt_ro[j]], writes=[R_stt_ro[j]])
                        k.op("dve", lambda e, j=j, S_=S_: e.tensor_tensor(out=v3(yc_ro[j]), in0=v3(y0_ro[j]), in1=bc(S_[:, 8:16]), op=ALU.subtract), reads=[R_y0_ro[j], R_stt_ro[j]], writes=[R_yc_ro[j]])
                        k.op("pool", lambda e, j=j: e.tensor_tensor(out=sq_ro[j][:], in0=yc_ro[j][:], in1=yc_ro[j][:], op=ALU.mult), reads=[R_yc_ro[j]], writes=[R_sq_ro[j]])
                        k.op("dve", lambda e, j=j, S_=S_: e.tensor_reduce(out=S_[:, 16:24], in_=v3(sq_ro[j]), axis=AX.X, op=ALU.add), reads=[R_sq_ro[j]], writes=[R_stt_ro[j]])
                        k.op("act", lambda e, S_=S_: e.activation(out=S_[:, 24:32], in_=S_[:, 16:24], func=AF.Sqrt, bias=c1_ro[:, 2:3], scale=1.0 / 64), reads=[R_stt_ro[j], R_par_ro], writes=[R_stt_ro[j]])
                        k.op("dve", lambda e, S_=S_: e.reciprocal(out=S_[:, 24:32], in_=S_[:, 24:32]), reads=[R_stt_ro[j]], writes=[R_stt_ro[j]])
                        k.op("dve", lambda e, j=j, S_=S_: e.tensor_tensor(out=v3(yc_ro[j]), in0=v3(yc_ro[j]), in1=bc(S_[:, 24:32]), op=ALU.mult), reads=[R_yc_ro[j], R_stt_ro[j]], writes=[R_yc_ro[j]])
                        k.op("pool", lambda e, j=j: e.tensor_tensor(out=yc_ro[j][:], in0=yc_ro[j][:], in1=lnw_ro[:], op=ALU.mult), reads=[R_yc_ro[j], R_par_ro], writes=[R_yc_ro[j]])
                        k.op("pool", lambda e, j=j: e.tensor_tensor(out=yc_ro[j][:], in0=yc_ro[j][:], in1=lnb_ro[:], op=ALU.add), reads=[R_yc_ro[j], R_par_ro], writes=[R_yc_ro[j]])
                        k.op("dve", lambda e, j=j: e.tensor_tensor(out=yc_ro[j][:], in0=yc_ro[j][:], in1=bo_ro[j][:], op=ALU.add), reads=[R_yc_ro[j], R_bo_ro[j]], writes=[R_yc_ro[j]])
                        k.op("dve", lambda e, j=j: e.tensor_tensor(out=yc_ro[j][:], in0=yc_ro[j][:], in1=gg_ro[j][:], op=ALU.mult), reads=[R_yc_ro[j], R_gg_ro[j]], writes=[R_yc_ro[j]])
                        pt, rp = ps_next()
                        for q in range(4):
                            k.op("pe", lambda e, pt=pt, q=q, j=j: e.transpose(pt[:, q * 128:(q + 1) * 128], yc_ro[j][:, q * 128:(q + 1) * 128], ident[:, :]), reads=[R_yc_ro[j], R_id], writes=[rp])
                        k.op("act", lambda e, pt=pt, j=j: e.activation(out=ot_ro[j][:].rearrange("p q t -> p (q t)"), in_=pt[:, :], func=AF.Copy), reads=[rp], writes=[R_ot_ro[j]])
                        k.dma("sp", YT[b][:, 0:4, t0:t0 + 128], ot_ro[j][:], reads=[R_ot_ro[j]], writes=[R_YT[b]])
            k.barrier()

        toks = []
        for l in range(n_layers):
            if stages is None or "norm1" in stages:
                stage_norm(l, 0, 0, HT, R_HT)
            if stages is None or "proj" in stages:
                stage_proj(l)
            last = (l == DEPTH - 1)
            if stages is None or "na" in stages:
                stage_na(l, not last, **na_kw)
            if stages is None or "rwkv" in stages:
                stage_rwkv(l, not last)
            if stages is None or "wout" in stages:
                stage_wout(l, last)
            if stages is None or "ffn" in stages:
                moe = (l % 2 == 1)
                tsel = range(LC, S, 256) if last else None
                if moe:
                    stage_norm(l, 1, 3, H2T, R_H2T, dst32=H2F, R_dst32=R_H2F, tsel=tsel)
                    stage_router(l, last)
                else:
                    stage_norm(l, 1, 3, H2T, R_H2T, tsel=tsel)
                stage_ffn(l, last)
        if stages is None or "final" in stages:
            fin_out = stage_final()
        else:
            fin_out = []

        fin = list(fin_out)
        for r in R_XT + R_HT + R_QK + R_VN + R_UT + R_YT + R_YD[0] + R_YD[1] + R_BON + R_GG:
            if r.w is not None:
                fin.append(r.w)
        k.final_wait("sp", fin)
        k.emit()
        print("insts", k.n_inst, "epochs", k.n_epochs)
    return nc


def _fm(a, nchunk):
    return np.ascontiguousarray(a.reshape(nchunk, 128, -1).transpose(1, 0, 2))


def _build_bfull(rpb):
    L, H = rpb.shape[:2]
    q = np.arange(64)[:, None]; kc = np.arange(64)[None, :]
    lo = np.clip(q - 8, 0, 48)
    valid = (kc >= lo) & (kc < lo + 16)
    idx = np.clip(kc - q + 15, 0, 30)
    g = rpb[:, :, :, idx]
    g = np.where(valid[None, None, None], g, np.float32(NEG)).astype(np.float32)
    return np.ascontiguousarray(g.transpose(0, 3, 1, 2, 4).reshape(L, 64, H, 960))


def _prep_shared(inp):
    m = {}
    m["ada_w"] = np.stack([_fm(inp["ada_w"][l], 8) for l in range(4)])
    m["ada_bT"] = np.stack([inp["ada_b"][l].reshape(48, 128).T.copy() for l in range(4)])
    m["g_mix"] = np.stack([inp["norm_mix_g"][l].reshape(8, 128).T.copy() for l in range(4)])
    m["g_ffn"] = np.stack([inp["norm_ffn_g"][l].reshape(8, 128).T.copy() for l in range(4)])
    m["w_in"] = np.stack([_fm(inp["w_in"][l], 8) for l in range(4)])
    m["mu"] = np.stack([inp["shift_mu"][l].reshape(15, 128).T.copy() for l in range(4)])
    m["ident"] = np.eye(128, dtype=np.float32)
    m["final_g"] = inp["final_g"].reshape(8, 128).T.copy()
    m["bfull"] = _build_bfull(inp["na_rpb"])
    m["w_out"] = np.stack([_fm(inp["w_out"][l], 8) for l in range(4)])
    m["rw_w2"] = inp["w2"].reshape(4, 128, 512); m["rw_a2"] = inp["a2"].reshape(4, 128, 512); m["rw_g2"] = inp["g2"]
    m["rw_a0T"] = np.stack([inp["a0"][l].reshape(2, 4, 128).transpose(2, 0, 1) for l in range(4)])
    c4 = lambda a: np.stack([a[l].reshape(4, 128).T for l in range(4)])
    m["rw_kk"] = c4(inp["k_k"]); m["rw_ka"] = c4(inp["k_a"]); m["rw_rk"] = c4(inp["r_k"].reshape(4, 512))
    m["rw_lnw"] = np.broadcast_to(inp["ln_x_w"][:, None, :], (4, 128, 512)); m["rw_lnb"] = np.broadcast_to(inp["ln_x_b"][:, None, :], (4, 128, 512))
    m["rw_w0b"] = np.broadcast_to(inp["w0"][:, None, :, :], (4, 64, 2, 512))
    idx = np.arange(64)
    strict = [(idx[:, None] < idx[None, :]), (idx[:, None] > idx[None, :])]
    incl = [(idx[:, None] <= idx[None, :]), (idx[:, None] >= idx[None, :])]
    m["rw_m2"] = np.stack([np.concatenate([strict[d], incl[d]], 1) for d in range(2)], 1).astype(np.float32)
    m["rw_m3"] = np.stack([np.concatenate([strict[d].astype(np.float32), -incl[d].astype(np.float32)], 1) for d in range(2)], 1)
    m["rw_mL"] = np.stack([strict[d].T for d in range(2)], 1).astype(np.float32)
    bo = np.zeros((128, 128), np.float32); bo[:64, :64] = 1; bo[64:, 64:] = 1
    m["rw_bones"] = bo
    se = np.zeros((128, 2), np.float32); se[:64, 0] = 1; se[64:, 1] = 1
    m["rw_sel"] = se
    m["rw_c1"] = np.tile(np.array([1.0, -0.5, 64e-5, 1e-12], np.float32)[None], (128, 1))
    m["ffn_w1"] = np.stack([_fm(inp["ffn_w1"][i], 8) for i in range(2)])
    m["ffn_w3"] = np.stack([_fm(inp["ffn_w3"][i], 8) for i in range(2)])
    m["ffn_w2"] = np.stack([_fm(inp["ffn_w2"][i], DFF // 128) for i in range(2)])
    m["router"] = np.stack([_fm(inp["router"][i], 8) for i in range(2)])
    m["moe_w1"] = np.stack([np.stack([_fm(inp["moe_w1"][i, e], 8) for e in range(NE)]) for i in range(2)])
    m["moe_w3"] = np.stack([np.stack([_fm(inp["moe_w3"][i, e], 8) for e in range(NE)]) for i in range(2)])
    m["moe_w2"] = np.stack([np.stack([_fm(inp["moe_w2"][i, e], DFE // 128) for e in range(NE)]) for i in range(2)])
    return {k_: np.ascontiguousarray(v, dtype=np.float32) for k_, v in m.items()}


def _prep_core(inp, core):
    bs = [2 * core, 2 * core + 1]
    m = {}
    xcat = [np.concatenate([inp["ctx"][b], inp["x"][b]], 0) for b in bs]
    m["xT"] = np.stack([_fm(np.ascontiguousarray(xc.T), 8) for xc in xcat]).astype(np.float32)
    cc = np.stack([inp["c"][bs[0]], inp["c"][bs[1]], inp["c_ctx"]], 1)
    m["cT"] = _fm(cc, 8).astype(np.float32)
    return m


def kernel(**inputs):
    inp = {k_: np.asarray(v) for k_, v in inputs.items()}
    n = 8
    shared = _prep_shared(inp)
    in_maps = []
    for core in range(n):
        m = dict(shared)
        m.update(_prep_core(inp, core))
        in_maps.append(m)
    nc = build_program()
    res = run_bass_kernel_spmd(nc, in_maps, core_ids=list(range(n)))
    outs = []
    for core in range(n):
        o = np.asarray(res.results[core]["out"])
        for b in range(NB):
            outs.append(o[b].transpose(1, 0, 2).reshape(D, T).T)
    return np.ascontiguousarray(np.stack(outs, 0)).astype(np.float32)
```

```python
import numpy as np
import concourse.bass as bass
import concourse.mybir as mybir
from concourse.bass_utils import run_bass_kernel_spmd
from contextlib import ExitStack

F32 = mybir.dt.float32
BF16 = mybir.dt.bfloat16
AF = mybir.ActivationFunctionType
ALU = mybir.AluOpType
AX = mybir.AxisListType

SAME_ENGINE_SYNC = True
DMA_RING = {"sp": 16, "act": 4, "pool": 16}
SEM_LIMIT = 60000
LOAD_Q = "pool"
MAX_EPOCHS = 7


class Res:
    __slots__ = ("name", "w", "rd")

    def __init__(self, name=""):
        self.name = name
        self.w = None
        self.rd = []


class K:
    ENG = ("pe", "act", "dve", "pool", "sp")
    CE = ("pe", "act", "dve", "pool")

    def __init__(self, nc, stack):
        self.nc = nc
        self.stack = stack
        self.recs = []
        self.cnt = {e: 0 for e in self.ENG}
        self.slots = []
        self.dq = {}
        for q in ("sp", "act", "pool"):
            idx = []
            for i in range(DMA_RING[q]):
                self.slots.append(0)
                idx.append(len(self.slots) - 1)
            self.dq[q] = {"idx": idx, "n": 0}
        self.waited_e = {e: {} for e in self.ENG}
        self.waited_d = {e: {} for e in self.ENG}
        self.n_inst = 0

    def _need(self, eng, tok, waits, force=False):
        if tok is None:
            return
        if tok[0] == "e":
            _, x, seq = tok
            if x == eng and (not SAME_ENGINE_SYNC or eng == "pe") and not force:
                return
            if self.waited_e[eng].get(x, 0) >= seq:
                return
            self.waited_e[eng][x] = seq
            waits.append(tok)
        else:
            _, si, use = tok
            if self.waited_d[eng].get(si, 0) >= use:
                return
            self.waited_d[eng][si] = use
            waits.append(tok)

    def _deps(self, eng, reads, writes):
        waits = []
        for r in reads:
            self._need(eng, r.w, waits)
        for w in writes:
            self._need(eng, w.w, waits)
            best = {}
            for t in w.rd:
                if t[0] == "e" and t[1] == eng:
                    continue
                key = (t[0], t[1])
                if key not in best or best[key][2] < t[2]:
                    best[key] = t
            for t in best.values():
                self._need(eng, t, waits)
        return waits

    def _commit(self, tok, reads, writes):
        for r in reads:
            if r.rd and r.rd[-1][0] == tok[0] and r.rd[-1][1] == tok[1]:
                r.rd[-1] = tok
            else:
                r.rd.append(tok)
        for w in writes:
            w.w = tok
            w.rd = []

    def op(self, eng, fn, reads=(), writes=()):
        waits = self._deps(eng, reads, writes)
        self.cnt[eng] += 1
        tok = ("e", eng, self.cnt[eng])
        self.recs.append({"eng": eng, "waits": waits, "fn": fn, "tok": tok})
        self._commit(tok, reads, writes)
        self.n_inst += 1
        return tok

    def dma(self, q, out, in_, reads=(), writes=(), **kw):
        if q == "sp" and LOAD_Q != "sp" and "DRAM" not in str(out.space):
            q = LOAD_Q
        waits = self._deps(q, reads, writes)
        d = self.dq[q]
        si = d["idx"][d["n"] % len(d["idx"])]
        d["n"] += 1
        prev = self.slots[si]
        if prev > 0:
            self._need(q, ("d", si, prev), waits)
        self.slots[si] = prev + 1
        tok = ("d", si, prev + 1)
        fn = (lambda e, o=out, i=in_, kw=kw: e.dma_start(out=o, in_=i, **kw))
        self.recs.append({"eng": q, "waits": waits, "fn": fn, "tok": tok})
        self._commit(tok, reads, writes)
        self.n_inst += 1
        return tok

    def _all_waits(self, eng):
        waits = []
        for x in self.CE:
            if self.cnt[x] > 0:
                self._need(eng, ("e", x, self.cnt[x]), waits, force=True)
        for si, v in enumerate(self.slots):
            if v > 0:
                self._need(eng, ("d", si, v), waits)
        return waits

    def barrier(self):
        for eng in self.ENG:
            self.recs.append({"eng": eng, "waits": self._all_waits(eng), "fn": None, "tok": None})

    def final_wait(self, eng, toks):
        waits = []
        for t in toks:
            self._need(eng, t, waits)
        self.recs.append({"eng": eng, "waits": waits, "fn": None, "tok": None})

    def emit(self):
        nc, st = self.nc, self.stack
        recs = self.recs
        needed = set()
        for r in recs:
            for w in r["waits"]:
                if w[0] == "e":
                    needed.add((w[1], w[2]))
        out = []
        ep = 0
        ms = {e: 0 for e in self.CE}
        du = [0] * len(self.slots)
        last_e = {e: 0 for e in self.CE}
        last_d = [0] * len(self.slots)
        val = {}
        pend_last = {}

        def close_epoch():
            nonlocal ep, ms, du
            bw = {}
            for e in self.CE:
                if e in pend_last:
                    rr = out[pend_last[e]]
                    t = rr["tok"]
                    if t not in val:
                        ms[e] += 1
                        val[t] = (ep, ms[e])
                        rr["inc"] = True
            allw = []
            for e in self.CE:
                if e in pend_last:
                    allw.append(out[pend_last[e]]["tok"])
            for si in range(len(self.slots)):
                if du[si] > 0:
                    allw.append(("d", si, last_d[si]))
            for eng in self.ENG:
                out.append({"eng": eng, "waits": list(allw), "fn": None, "tok": None, "ep": ep})
            ep += 1
            ms = {e: 0 for e in self.CE}
            du = [0] * len(self.slots)
            pend_last.clear()

        for r in recs:
            t = r["tok"]
            if t is not None:
                if t[0] == "e":
                    if ms[t[1]] + 2 > SEM_LIMIT:
                        close_epoch()
                else:
                    if (du[t[1]] + 2) * 16 > SEM_LIMIT:
                        close_epoch()
            r = dict(r)
            r["ep"] = ep
            out.append(r)
            if t is not None:
                if t[0] == "e":
                    pend_last[t[1]] = len(out) - 1
                    if (t[1], t[2]) in needed:
                        ms[t[1]] += 1
                        val[t] = (ep, ms[t[1]])
                        r["inc"] = True
                    else:
                        r["inc"] = False
                else:
                    du[t[1]] += 1
                    last_d[t[1]] = t[2]
                    val[t] = (ep, du[t[1]] * 16)
        n_ep = ep + 1
        print("epochs needed", n_ep, "milestones", ms, "dma uses", max(du))
        assert n_ep <= MAX_EPOCHS, "too many epochs %d" % n_ep
        self.n_epochs = n_ep
        esem = [{e: st.enter_context(nc.semaphore("c%d_%s" % (i, e))) for e in self.CE} for i in range(n_ep)]
        dsem = [[st.enter_context(nc.semaphore("d%d_%d" % (i, j))) for j in range(len(self.slots))] for i in range(n_ep)]
        streams = {e: [] for e in self.ENG}
        for r in out:
            streams[r["eng"]].append(r)

        with nc.Block() as block:
            def run(engname, e):
                for ep_ in range(1, n_ep):
                    if engname in self.CE:
                        e.sem_clear(esem[ep_][engname])
                    if engname in self.dq:
                        for si in self.dq[engname]["idx"]:
                            e.sem_clear(dsem[ep_][si])
                for r in streams[engname]:
                    for w in r["waits"]:
                        v = val.get(w)
                        if v is None or v[0] != r["ep"]:
                            continue
                        if w[0] == "e":
                            e.wait_ge(esem[v[0]][w[1]], v[1])
                        else:
                            e.wait_ge(dsem[v[0]][w[1]], v[1])
                    if r["fn"] is None:
                        continue
                    ins = r["fn"](e)
                    t = r["tok"]
                    if t[0] == "e":
                        if r.get("inc"):
                            ins.then_inc(esem[r["ep"]][t[1]], 1)
                    else:
                        ins.then_inc(dsem[r["ep"]][t[1]], 16)

            @block.sync
            def _(e):
                run("sp", e)

            @block.tensor
            def _(e):
                run("pe", e)

            @block.vector
            def _(e):
                run("dve", e)

            @block.scalar
            def _(e):
                run("act", e)

            @block.gpsimd
            def _(e):
                run("pool", e)


D = 1024
DEPTH = 4
NB = 2
LC = 256
T = 2048
S = LC + T
NT = S // 128
D_IN = 3456
DFF = 2816
DFE = 3584
NE = 8
NEG = -30000.0

DEBUG_OUT = []
RW_DBG = {"nch": None, "upto": "Z", "nb": None, "nd": 2, "readout": True}
N_LAYERS_RUN = DEPTH


def build_program(n_layers=DEPTH, stages=None, debug=(), na_kw={}):
    nc = bass.Bass("TRN2", target_bir_lowering=False)
    st = ExitStack()
    with st:
        k = K(nc, st)

        def din(name, shape, dt=F32):
            return nc.dram_tensor(name, list(shape), dt, kind="ExternalInput").ap()

        def dscr(name, shape, dt=F32):
            kind = "ExternalOutput" if name in debug else "Internal"
            return nc.dram_tensor(name, list(shape), dt, kind=kind).ap()

        I = {}
        I["xT"] = din("xT", [NB, 128, 8, S])
        I["cT"] = din("cT", [128, 8, 3])
        I["ada_w"] = din("ada_w", [DEPTH, 128, 8, 6 * D])
        I["ada_bT"] = din("ada_bT", [DEPTH, 128, 48])
        I["g_mix"] = din("g_mix", [DEPTH, 128, 8])
        I["g_ffn"] = din("g_ffn", [DEPTH, 128, 8])
        I["w_in"] = din("w_in", [DEPTH, 128, 8, D_IN])
        I["mu"] = din("mu", [DEPTH, 128, 15])
        I["ident"] = din("ident", [128, 128])
        I["final_g"] = din("final_g", [128, 8])
        I["bfull"] = din("bfull", [DEPTH, 64, 8, 960])
        I["w_out"] = din("w_out", [DEPTH, 128, 8, D])
        I["rw_w2"] = din("rw_w2", [DEPTH, 128, 512]); I["rw_a2"] = din("rw_a2", [DEPTH, 128, 512]); I["rw_g2"] = din("rw_g2", [DEPTH, 128, 512])
        I["rw_a0T"] = din("rw_a0T", [DEPTH, 128, 2, 4]); I["rw_kk"] = din("rw_kk", [DEPTH, 128, 4]); I["rw_ka"] = din("rw_ka", [DEPTH, 128, 4]); I["rw_rk"] = din("rw_rk", [DEPTH, 128, 4])
        I["rw_lnw"] = din("rw_lnw", [DEPTH, 128, 512]); I["rw_lnb"] = din("rw_lnb", [DEPTH, 128, 512]); I["rw_w0b"] = din("rw_w0b", [DEPTH, 64, 2, 512])
        I["rw_m2"] = din("rw_m2", [64, 2, 128]); I["rw_m3"] = din("rw_m3", [64, 2, 128]); I["rw_mL"] = din("rw_mL", [64, 2, 64])
        I["rw_bones"] = din("rw_bones", [128, 128]); I["rw_sel"] = din("rw_sel", [128, 2]); I["rw_c1"] = din("rw_c1", [128, 4])
        I["ffn_w1"] = din("ffn_w1", [2, 128, 8, DFF]); I["ffn_w3"] = din("ffn_w3", [2, 128, 8, DFF]); I["ffn_w2"] = din("ffn_w2", [2, 128, DFF // 128, D])
        I["router"] = din("router", [2, 128, 8, NE])
        I["moe_w1"] = din("moe_w1", [2, NE, 128, 8, DFE]); I["moe_w3"] = din("moe_w3", [2, NE, 128, 8, DFE]); I["moe_w2"] = din("moe_w2", [2, NE, 128, DFE // 128, D])
        out = nc.dram_tensor("out", [NB, 128, 8, T], F32, kind="ExternalOutput").ap()

        XT = [dscr("xt%d" % b, [128, 8, S]) for b in range(NB)]
        HT = [dscr("ht%d" % b, [128, 8, S], BF16) for b in range(NB)]
        QT = [dscr("qt%d" % b, [128, 4, S], BF16) for b in range(NB)]
        KT = [dscr("kt%d" % b, [128, 4, S], BF16) for b in range(NB)]
        VN = [dscr("vn%d" % b, [S, 512], BF16) for b in range(NB)]
        UT = [dscr("ut%d" % b, [128, 15, S]) for b in range(NB)]
        R_XT = [Res() for _ in range(NB)]
        R_HT = [Res() for _ in range(NB)]
        R_QK = [Res() for _ in range(NB)]
        R_VN = [Res() for _ in range(NB)]
        R_UT = [Res() for _ in range(NB)]
        YT = [dscr("yt%d" % b, [128, 8, S], BF16) for b in range(NB)]
        H2T = [dscr("h2t%d" % b, [128, 8, S], BF16) for b in range(NB)]
        H2F = [dscr("h2f%d" % b, [128, 8, S]) for b in range(NB)]
        GB = [dscr("gb%d" % b, [128, NE, S]) for b in range(NB)]
        YD = [[dscr("yd%d_%d" % (d, b), [S, 512]) for b in range(NB)] for d in range(2)]
        R_YD = [[Res() for b in range(NB)] for d in range(2)]
        BON = [dscr("bon%d" % b, [S, 512]) for b in range(NB)]; R_BON = [Res() for _ in range(NB)]
        GG = [dscr("gg%d" % b, [S, 512]) for b in range(NB)]; R_GG = [Res() for _ in range(NB)]
        R_H2T = [Res() for _ in range(NB)]; R_H2F = [Res() for _ in range(NB)]; R_GB = [Res() for _ in range(NB)]
        R_YT = [Res() for _ in range(NB)]

        uid = [0]

        def sb(name, shape, dt=F32, stack=st):
            uid[0] += 1
            return stack.enter_context(nc.sbuf_tensor("%s_%d" % (name, uid[0]), list(shape), dt))

        ident = sb("ident_sb", [128, 128]); R_id = Res()
        ones = sb("ones_sb", [128, 128]); R_ones = Res()
        MOD = sb("mod_sb", [128, DEPTH, 48, 3]); R_mod = Res()
        GS = sb("gs_sb", [128, DEPTH, 2, 8, 3]); R_gs = Res()
        gmix = sb("gmix_sb", [128, DEPTH, 8]); gffn = sb("gffn_sb", [128, DEPTH, 8]); R_g = Res()
        eps_t = sb("eps_sb", [128, 1]); R_eps = Res()
        psum = [st.enter_context(nc.psum_tensor("ps%d" % i, [128, 512], F32)) for i in range(8)]
        R_ps = [Res() for _ in range(8)]
        pctr = [0]

        def ps_next():
            i = pctr[0] % 8
            pctr[0] += 1
            return psum[i], R_ps[i]

        k.dma("sp", ident[:], I["ident"][:, :], writes=[R_id])
        ident_bf = sb("identbf_sb", [128, 128], BF16)
        k.op("dve", lambda e: e.tensor_copy(out=ident_bf[:], in_=ident[:]), reads=[R_id], writes=[R_id])
        k.op("dve", lambda e: e.memset(ones[:], 1.0), writes=[R_ones])
        k.op("dve", lambda e: e.memset(eps_t[:], 1e-6), writes=[R_eps])
        for l in range(DEPTH):
            k.dma("sp", gmix[:, l, :], I["g_mix"][l], writes=[R_g])
            k.dma("sp", gffn[:, l, :], I["g_ffn"][l], writes=[R_g])

        with ExitStack() as s1:
            cT = sb("cT_sb", [128, 8, 3], F32, s1); R_c = Res()
            sT = sb("sT", [128, 8, 3], F32, s1); R_s = Res()
            abT = sb("abT", [128, 48], F32, s1); R_ab = Res()
            aw = [sb("aw%d" % i, [128, 8, 512], F32, s1) for i in range(2)]
            R_aw = [Res(), Res()]
            xcp = [sb("xcp%d" % i, [128, 8, 256], F32, s1) for i in range(2)]
            R_xcp = [Res(), Res()]
            k.dma("sp", cT[:], I["cT"][:, :, :], writes=[R_c])
            k.op("act", lambda e: e.activation(out=sT[:], in_=cT[:], func=AF.Silu), reads=[R_c], writes=[R_s])
            n = 0
            for b in range(NB):
                for t0 in range(0, S, 256):
                    j = n % 2; n += 1
                    k.dma("sp", xcp[j][:], I["xT"][b, :, :, t0:t0 + 256], writes=[R_xcp[j]])
                    k.dma("sp", XT[b][:, :, t0:t0 + 256], xcp[j][:], reads=[R_xcp[j]], writes=[R_XT[b]])
            n = 0
            for l in range(n_layers):
                k.dma("sp", abT[:], I["ada_bT"][l], writes=[R_ab])
                for cc in range(12):
                    j = n % 2; n += 1
                    k.dma("sp", aw[j][:], I["ada_w"][l, :, :, cc * 512:(cc + 1) * 512], writes=[R_aw[j]])
                    pt, rp = ps_next()
                    for c4 in range(4):
                        for kc in range(8):
                            k.op("pe", lambda e, pt=pt, j=j, c4=c4, kc=kc: e.matmul(
                                pt[:, c4 * 3:c4 * 3 + 3], aw[j][:, kc, c4 * 128:(c4 + 1) * 128], sT[:, kc, :],
                                start=(kc == 0), stop=(kc == 7)), reads=[R_aw[j], R_s], writes=[rp])
                    k.op("dve", lambda e, pt=pt, l=l, cc=cc: e.tensor_tensor(
                        out=MOD[:, l, cc * 4:(cc + 1) * 4, :], in0=pt[:, 0:12].rearrange("p (a b) -> p a b", b=3),
                        in1=abT[:, cc * 4:(cc + 1) * 4].unsqueeze(2).to_broadcast([128, 4, 3]), op=ALU.add),
                        reads=[rp, R_ab], writes=[R_mod])
                for which, (gt, mi) in enumerate(((gmix, 1), (gffn, 4))):
                    k.op("dve", lambda e, l=l, which=which, gt=gt, mi=mi: e.scalar_tensor_tensor(
                        out=GS[:, l, which, :, :], in0=MOD[:, l, mi * 8:(mi + 1) * 8, :], scalar=1.0,
                        in1=gt[:, l, :].unsqueeze(2).to_broadcast([128, 8, 3]), op0=ALU.add, op1=ALU.mult),
                        reads=[R_mod, R_g], writes=[R_gs])
        k.barrier()

        def which_of(b, t0):
            return 2 if t0 < LC else b

        def stage_norm(l, which, shift_idx, dst, R_dst, gsel=None, dst32=None, R_dst32=None, bsel=range(NB), tsel=None):
            with ExitStack() as s1:
                xb = [sb("nx%d" % i, [128, 8, 256], F32, s1) for i in range(2)]; R_xb = [Res(), Res()]
                sq = [sb("nsq%d" % i, [128, 8, 256], F32, s1) for i in range(2)]; R_sq = [Res(), Res()]
                rs = [sb("nrs%d" % i, [128, 256], F32, s1) for i in range(2)]; R_rs = [Res(), Res()]
                tm = [sb("ntm%d" % i, [128, 8, 256], F32, s1) for i in range(2)]; R_tm = [Res(), Res()]
                hb = [sb("nhb%d" % i, [128, 8, 256], BF16, s1) for i in range(2)]; R_hb = [Res(), Res()]
                n = 0
                for b in bsel:
                    for t0 in (tsel if tsel is not None else range(0, S, 256)):
                        j = n % 2; n += 1
                        w = which_of(b, t0)
                        k.dma("sp", xb[j][:], XT[b][:, :, t0:t0 + 256], reads=[R_XT[b]], writes=[R_xb[j]])
                        k.op("act", lambda e, j=j: e.activation(out=sq[j][:], in_=xb[j][:], func=AF.Square), reads=[R_xb[j]], writes=[R_sq[j]])
                        pt, rp = ps_next()
                        for c in range(8):
                            k.op("pe", lambda e, pt=pt, j=j, c=c: e.matmul(pt[:, 0:256], ones[:], sq[j][:, c, :], start=(c == 0), stop=(c == 7)),
                                 reads=[R_ones, R_sq[j]], writes=[rp])
                        k.op("act", lambda e, pt=pt, j=j: e.activation(out=rs[j][:], in_=pt[:, 0:256], func=AF.Sqrt, bias=eps_t[:, 0:1], scale=1.0 / D),
                             reads=[rp, R_eps], writes=[R_rs[j]])
                        k.op("dve", lambda e, j=j: e.reciprocal(out=rs[j][:], in_=rs[j][:]), reads=[R_rs[j]], writes=[R_rs[j]])
                        for c in range(8):
                            k.op("dve", lambda e, j=j, c=c, w=w: e.scalar_tensor_tensor(
                                out=tm[j][:, c, :], in0=xb[j][:, c, :], scalar=GS[:, l, which, c, w:w + 1], in1=rs[j][:],
                                op0=ALU.mult, op1=ALU.mult), reads=[R_xb[j], R_gs, R_rs[j]], writes=[R_tm[j]])
                            k.op("act", lambda e, j=j, c=c, w=w: e.activation(
                                out=hb[j][:, c, :], in_=tm[j][:, c, :], func=AF.Identity, bias=MOD[:, l, shift_idx * 8 + c, w:w + 1], scale=1.0),
                                reads=[R_tm[j], R_mod], writes=[R_hb[j]])
                            if dst32 is not None:
                                k.op("pool", lambda e, j=j, c=c, w=w: e.tensor_scalar(
                                    out=tm[j][:, c, :], in0=tm[j][:, c, :], scalar1=MOD[:, l, shift_idx * 8 + c, w:w + 1], scalar2=None, op0=ALU.add),
                                    reads=[R_tm[j], R_mod, R_hb[j]], writes=[R_tm[j]])
                        k.dma("sp", dst[b][:, :, t0:t0 + 256], hb[j][:], reads=[R_hb[j]], writes=[R_dst[b]])
                        if dst32 is not None:
                            k.dma("sp", dst32[b][:, :, t0:t0 + 256], tm[j][:], reads=[R_tm[j]], writes=[R_dst32[b]])
            k.barrier()

        def stage_proj(l):
            with ExitStack() as s1:
                w = sb("pw", [128, 8, D_IN], BF16, s1); R_w = Res()
                mu = sb("pmu", [128, 15], F32, s1); R_mu = Res()
                mu1 = sb("pmu1", [128, 15], F32, s1)
                muh = sb("pmuh", [128, 15], F32, s1)
                hb = [sb("ph%d" % i, [128, 8, 258], BF16, s1) for i in range(2)]; R_hb = [Res(), Res()]
                hs = [sb("phs%d" % i, [128, 8, 256], BF16, s1) for i in range(2)]; R_hs = [Res(), Res()]
                oq = [sb("poq%d" % i, [128, 8, 256], BF16, s1) for i in range(2)]; R_oq = [Res(), Res()]
                ov = [sb("pov%d" % i, [128, 2, 512], BF16, s1) for i in range(2)]; R_ov = [Res(), Res()]
                ou = [sb("pou%d" % i, [128, 15, 256], F32, s1) for i in range(2)]; R_ou = [Res(), Res()]
                t2 = [sb("pt2%d" % i, [128, 256], F32, s1) for i in range(2)]; R_t2 = [Res(), Res()]
                for kc in range(8):
                    k.dma("pool", w[:, kc, :], I["w_in"][l, :, kc, :], writes=[R_w])
                k.dma("sp", mu[:], I["mu"][l], writes=[R_mu])
                k.op("dve", lambda e: e.tensor_scalar(out=mu1[:], in0=mu[:], scalar1=-1.0, scalar2=1.0, op0=ALU.mult, op1=ALU.add), reads=[R_mu], writes=[R_mu])
                k.op("dve", lambda e: e.tensor_scalar(out=muh[:], in0=mu[:], scalar1=0.5, scalar2=None, op0=ALU.mult), reads=[R_mu], writes=[R_mu])
                n = 0
                for b in range(NB):
                    for t0 in range(0, S, 256):
                        j = n % 2; n += 1
                        seg0, seg1 = (0, LC) if t0 < LC else (LC, S)
                        lo, hi = max(t0 - 1, seg0), min(t0 + 257, seg1)
                        if lo == t0 or hi == t0 + 256:
                            k.op("dve", lambda e, j=j: e.memset(hb[j][:], 0.0), writes=[R_hb[j]])
                        k.dma("sp", hb[j][:, :, lo - (t0 - 1):hi - (t0 - 1)], HT[b][:, :, lo:hi], reads=[R_HT[b]], writes=[R_hb[j]])
                        k.op("dve", lambda e, j=j: e.tensor_tensor(out=hs[j][:], in0=hb[j][:, :, 0:256], in1=hb[j][:, :, 2:258], op=ALU.add),
                             reads=[R_hb[j]], writes=[R_hs[j]])
                        for cj in range(8):
                            pt, rp = ps_next()
                            for kc in range(8):
                                k.op("pe", lambda e, pt=pt, j=j, cj=cj, kc=kc: e.matmul(pt[:, 0:256], w[:, kc, cj * 128:(cj + 1) * 128], hb[j][:, kc, 1:257],
                                     start=(kc == 0), stop=(kc == 7)), reads=[R_w, R_hb[j]], writes=[rp])
                            k.op("act", lambda e, pt=pt, j=j, cj=cj: e.activation(out=oq[j][:, cj, :], in_=pt[:, 0:256], func=AF.Copy), reads=[rp], writes=[R_oq[j]])
                        k.dma("sp", QT[b][:, :, t0:t0 + 256], oq[j][:, 0:4, :], reads=[R_oq[j]], writes=[R_QK[b]])
                        k.dma("sp", KT[b][:, :, t0:t0 + 256], oq[j][:, 4:8, :], reads=[R_oq[j]], writes=[R_QK[b]])
                        for tt in range(2):
                            pt, rp = ps_next()
                            for kc in range(8):
                                k.op("pe", lambda e, pt=pt, j=j, tt=tt, kc=kc: e.matmul(pt[:, :], hb[j][:, kc, 1 + tt * 128:1 + (tt + 1) * 128], w[:, kc, 1024:1536],
                                     start=(kc == 0), stop=(kc == 7)), reads=[R_w, R_hb[j]], writes=[rp])
                            k.op("act", lambda e, pt=pt, j=j, tt=tt: e.activation(out=ov[j][:, tt, :], in_=pt[:, :], func=AF.Copy), reads=[rp], writes=[R_ov[j]])
                        k.dma("sp", VN[b][t0:t0 + 256, :].rearrange("(a p) c -> p a c", p=128), ov[j][:], reads=[R_ov[j]], writes=[R_VN[b]])
                        for cj in range(15):
                            c0 = 1536 + cj * 128
                            pa, ra = ps_next()
                            for kc in range(8):
                                k.op("pe", lambda e, pa=pa, j=j, c0=c0, kc=kc: e.matmul(pa[:, 0:256], w[:, kc, c0:c0 + 128], hb[j][:, kc, 1:257],
                                     start=(kc == 0), stop=(kc == 7)), reads=[R_w, R_hb[j]], writes=[ra])
                            pb, rb = ps_next()
                            for kc in range(8):
                                k.op("pe", lambda e, pb=pb, j=j, c0=c0, kc=kc: e.matmul(pb[:, 0:256], w[:, kc, c0:c0 + 128], hs[j][:, kc, :],
                                     start=(kc == 0), stop=(kc == 7)), reads=[R_w, R_hs[j]], writes=[rb])
                            k.op("act", lambda e, pb=pb, j=j, cj=cj: e.activation(out=t2[j][:], in_=pb[:, 0:256], func=AF.Copy, scale=muh[:, cj:cj + 1]),
                                 reads=[rb, R_mu], writes=[R_t2[j]])
                            k.op("dve", lambda e, pa=pa, j=j, cj=cj: e.scalar_tensor_tensor(out=ou[j][:, cj, :], in0=pa[:, 0:256], scalar=mu1[:, cj:cj + 1], in1=t2[j][:],
                                 op0=ALU.mult, op1=ALU.add), reads=[ra, R_mu, R_t2[j]], writes=[R_ou[j]])
                        k.dma("sp", UT[b][:, :, t0:t0 + 256], ou[j][:], reads=[R_ou[j]], writes=[R_UT[b]])
            k.barrier()

        def stage_na(l, need_ctx, hsel=range(8), bsel=range(NB)):
            NBUF = 3
            with ExitStack() as s1:
                bias = sb("na_bias", [128, 8, 960], F32, s1); R_bias = Res()
                k.dma("sp", bias[0:64], I["bfull"][l], writes=[R_bias])
                k.dma("sp", bias[64:128], I["bfull"][l], writes=[R_bias])
                qh = [sb("na_q%d" % i, [64, S], BF16, s1) for i in range(2)]
                kh = [sb("na_k%d" % i, [64, S], BF16, s1) for i in range(2)]
                VE = [sb("na_ve%d" % i, [128, 18, 64], BF16, s1) for i in range(2)]
                VO = [sb("na_vo%d" % i, [128, 18, 64], BF16, s1) for i in range(2)]
                R_in = [Res(), Res()]
                ytok = sb("na_ytok", [128, NT, 512], BF16, s1); R_ytok = Res()
                yfm = [sb("na_yfm%d" % i, [128, 4, 128], BF16, s1) for i in range(2)]; R_yfm = [Res(), Res()]
                ssb = [sb("na_s%d" % i, [128, 832], F32, s1) for i in range(NBUF)]; R_s = [Res() for _ in range(NBUF)]
                pbf = [sb("na_p%d" % i, [128, 832], BF16, s1) for i in range(NBUF)]; R_p = [Res() for _ in range(NBUF)]
                pT = [sb("na_pT%d" % i, [128, 896], BF16, s1) for i in range(NBUF)]; R_pT = [Res() for _ in range(NBUF)]
                st4 = [sb("na_st%d" % i, [128, 4], F32, s1) for i in range(NBUF)]; R_st = [Res() for _ in range(NBUF)]
                n = 0
                u = 0
                for b in bsel:
                    if not need_ctx:
                        pass
                    for h in hsel:
                        j = n % 2; n += 1
                        p0 = (h % 2) * 64
                        k.dma("sp", qh[j][:], QT[b][p0:p0 + 64, h // 2, :], reads=[R_QK[b]], writes=[R_in[j]])
                        k.dma("sp", kh[j][:], KT[b][p0:p0 + 64, h // 2, :], reads=[R_QK[b]], writes=[R_in[j]])
                        k.dma("sp", VE[j][:], VN[b][:, h * 64:(h + 1) * 64].rearrange("(a p) d -> p a d", p=128), reads=[R_VN[b]], writes=[R_in[j]])
                        k.dma("sp", VO[j][:, 0:17, :], VN[b][64:64 + 17 * 128, h * 64:(h + 1) * 64].rearrange("(a p) d -> p a d", p=128), reads=[R_VN[b]], writes=[R_in[j]])
                        k.dma("sp", VO[j][0:64, 17, :], VN[b][S - 64:S, h * 64:(h + 1) * 64], reads=[R_VN[b]], writes=[R_in[j]])
                        units = [("lat", r) for r in range(0, 32, 2)] + ([("ctx", 0), ("ctx", 1)] if need_ctx else [])
                        for kind, r in units:
                            i = u % NBUF; u += 1
                            b1, rb1 = ps_next()
                            if kind == "lat":
                                r0A = min(max(r - 4, 0), 24); r0B = min(max(r - 3, 0), 24)
                                R0 = min(r0A, 23)
                                tq = LC + r * 64; w0 = LC + R0 * 64
                                tile_i = tq // 128
                                b2, rb2 = ps_next()
                                k.op("pe", lambda e, b1=b1, j=j, tq=tq: e.matmul(b1[:, 0:256], qh[j][:, tq:tq + 128], kh[j][:, 0:256], start=True, stop=True), reads=[R_in[j]], writes=[rb1])
                                k.op("pe", lambda e, b1=b1, j=j, tq=tq, w0=w0: e.matmul(b1[:, 256:512], qh[j][:, tq:tq + 128], kh[j][:, w0:w0 + 256], start=True, stop=True), reads=[R_in[j]], writes=[rb1])
                                k.op("pe", lambda e, b2=b2, j=j, tq=tq, w0=w0: e.matmul(b2[:, 0:320], qh[j][:, tq:tq + 128], kh[j][:, w0 + 256:w0 + 576], start=True, stop=True), reads=[R_in[j]], writes=[rb2])
                                k.op("act", lambda e, b1=b1, i=i: e.activation(out=ssb[i][:, 0:256], in_=b1[:, 0:256], func=AF.Copy, scale=0.125), reads=[rb1], writes=[R_s[i]])
                                for half, (rq, r0X) in enumerate(((r, r0A), (r + 1, r0B))):
                                    sX = r0X - R0
                                    hp_ = slice(half * 64, (half + 1) * 64)
                                    bs0 = (r0X - rq + 7) * 64
                                    n1 = 256 - sX * 64
                                    k.op("dve", lambda e, b1=b1, i=i, h=h, hp_=hp_, sX=sX, bs0=bs0, n1=n1: e.scalar_tensor_tensor(
                                        out=ssb[i][hp_, 256 + sX * 64:512], in0=b1[hp_, 256 + sX * 64:512], scalar=0.125, in1=bias[hp_, h, bs0:bs0 + n1], op0=ALU.mult, op1=ALU.add),
                                        reads=[rb1, R_bias], writes=[R_s[i]])
                                    n2 = 512 - n1
                                    k.op("dve", lambda e, b2=b2, i=i, h=h, hp_=hp_, bs0=bs0, n1=n1, n2=n2: e.scalar_tensor_tensor(
                                        out=ssb[i][hp_, 512:512 + n2], in0=b2[hp_, 0:n2], scalar=0.125, in1=bias[hp_, h, bs0 + n1:bs0 + 512], op0=ALU.mult, op1=ALU.add),
                                        reads=[rb2, R_bias], writes=[R_s[i]])
                                    ng0 = 256 + (512 if sX == 0 else 0)
                                    k.op("pool", lambda e, i=i, hp_=hp_, ng0=ng0: e.memset(ssb[i][hp_, ng0:ng0 + 64], NEG), writes=[R_s[i]])
                                W = 832
                            else:
                                tile_i = r
                                k.op("pe", lambda e, b1=b1, j=j, r=r: e.matmul(b1[:, 0:256], qh[j][:, r * 128:(r + 1) * 128], kh[j][:, 0:256], start=True, stop=True), reads=[R_in[j]], writes=[rb1])
                                k.op("act", lambda e, b1=b1, i=i: e.activation(out=ssb[i][:, 0:256], in_=b1[:, 0:256], func=AF.Copy, scale=0.125), reads=[rb1], writes=[R_s[i]])
                                W = 256
                            k.op("dve", lambda e, i=i, W=W: e.tensor_reduce(out=st4[i][:, 0:1], in_=ssb[i][:, 0:W], axis=AX.X, op=ALU.max), reads=[R_s[i]], writes=[R_st[i]])
                            k.op("dve", lambda e, i=i: e.tensor_scalar(out=st4[i][:, 1:2], in0=st4[i][:, 0:1], scalar1=-1.0, scalar2=None, op0=ALU.mult), reads=[R_st[i]], writes=[R_st[i]])
                            k.op("act", lambda e, i=i, W=W: e.activation(out=pbf[i][:, 0:W], in_=ssb[i][:, 0:W], func=AF.Exp, bias=st4[i][:, 1:2], scale=1.0, accum_out=st4[i][:, 2:3]),
                                 reads=[R_s[i], R_st[i]], writes=[R_p[i], R_st[i]])
                            k.op("dve", lambda e, i=i: e.reciprocal(out=st4[i][:, 3:4], in_=st4[i][:, 2:3]), reads=[R_st[i]], writes=[R_st[i]])
                            pc, rc = ps_next()
                            pcb = pc[:].bitcast(BF16)
                            nch = (W + 127) // 128
                            for c in range(nch):
                                kw = min(128, W - c * 128)
                                k.op("pe", lambda e, pcb=pcb, i=i, c=c, kw=kw: e.transpose(pcb[0:kw, c * 128:(c + 1) * 128], pbf[i][:, c * 128:c * 128 + kw], ident_bf[:, :]),
                                     reads=[R_p[i], R_id], writes=[rc])
                            k.op("dve" if kind == "lat" else "act",
                                 (lambda e, pcb=pcb, i=i, nch=nch: e.tensor_copy(out=pT[i][:, 0:nch * 128], in_=pcb[:, 0:nch * 128])) if kind == "lat" else
                                 (lambda e, pcb=pcb, i=i, nch=nch: e.activation(out=pT[i][:, 0:nch * 128], in_=pcb[:, 0:nch * 128], func=AF.Copy)),
                                 reads=[rc], writes=[R_pT[i]])
                            pd, rd = ps_next()
                            for c in range(nch):
                                kw = min(128, W - c * 128)
                                if c < 2:
                                    vt = VE[j][:, c, :]
                                else:
                                    even = (R0 % 2 == 0)
                                    base_t = (w0 // 128) if even else ((w0 - 64) // 128)
                                    vt = (VE[j] if even else VO[j])[0:kw, base_t + (c - 2), :]
                                k.op("pe", lambda e, pd=pd, i=i, c=c, kw=kw, vt=vt, nch=nch: e.matmul(pd[:, 0:64], pT[i][0:kw, c * 128:(c + 1) * 128], vt, start=(c == 0), stop=(c == nch - 1)),
                                     reads=[R_in[j], R_pT[i]], writes=[rd])
                            k.op("act", lambda e, pd=pd, i=i, tile_i=tile_i, h=h: e.activation(out=ytok[:, tile_i, h * 64:(h + 1) * 64], in_=pd[:, 0:64], func=AF.Copy, scale=st4[i][:, 3:4]),
                                 reads=[rd, R_st[i]], writes=[R_ytok])
                    for tt in range(0 if need_ctx else 2, NT):
                        jj = tt % 2
                        pt, rp = ps_next()
                        ptb = pt[:].bitcast(BF16)
                        for q in range(4):
                            k.op("pe", lambda e, ptb=ptb, q=q, tt=tt: e.transpose(ptb[:, q * 128:(q + 1) * 128], ytok[:, tt, q * 128:(q + 1) * 128], ident_bf[:, :]), reads=[R_ytok, R_id], writes=[rp])
                        k.op("act", lambda e, ptb=ptb, jj=jj: e.activation(out=yfm[jj][:].rearrange("p q t -> p (q t)"), in_=ptb[:, 0:512], func=AF.Copy), reads=[rp], writes=[R_yfm[jj]])
                        k.dma("sp", YT[b][:, 4:8, tt * 128:(tt + 1) * 128], yfm[jj][:], reads=[R_yfm[jj]], writes=[R_YT[b]])
            k.barrier()

        def stage_wout(l, last):
            with ExitStack() as s1:
                w = sb("wo_w", [128, 8, D], BF16, s1); R_w = Res()
                for kc in range(8):
                    k.dma("pool", w[:, kc, :], I["w_out"][l, :, kc, :], writes=[R_w])
                yb = [sb("wo_y%d" % i, [128, 8, 256], BF16, s1) for i in range(2)]; R_yb = [Res(), Res()]
                xb = [sb("wo_x%d" % i, [128, 8, 256], F32, s1) for i in range(2)]; R_xb = [Res(), Res()]
                n = 0
                for b in range(NB):
                    for t0 in range(LC if last else 0, S, 256):
                        j = n % 2; n += 1
                        wq = which_of(b, t0)
                        k.dma("sp", yb[j][:], YT[b][:, :, t0:t0 + 256], reads=[R_YT[b]], writes=[R_yb[j]])
                        k.dma("sp", xb[j][:], XT[b][:, :, t0:t0 + 256], reads=[R_XT[b]], writes=[R_xb[j]])
                        for oc in range(8):
                            pt, rp = ps_next()
                            for kc in range(8):
                                k.op("pe", lambda e, pt=pt, j=j, oc=oc, kc=kc: e.matmul(pt[:, 0:256], w[:, kc, oc * 128:(oc + 1) * 128], yb[j][:, kc, :],
                                     start=(kc == 0), stop=(kc == 7)), reads=[R_w, R_yb[j]], writes=[rp])
                            k.op("dve", lambda e, pt=pt, j=j, oc=oc, wq=wq: e.scalar_tensor_tensor(out=xb[j][:, oc, :], in0=pt[:, 0:256],
                                 scalar=MOD[:, l, 16 + oc, wq:wq + 1], in1=xb[j][:, oc, :], op0=ALU.mult, op1=ALU.add), reads=[rp, R_mod, R_xb[j]], writes=[R_xb[j]])
                        k.dma("sp", XT[b][:, :, t0:t0 + 256], xb[j][:], reads=[R_xb[j]], writes=[R_XT[b]])
            k.barrier()

        def stage_router(l, last):
            i_moe = l // 2
            with ExitStack() as s1:
                rw = sb("rt_w", [128, 8, NE], F32, s1); R_rw = Res()
                k.dma("sp", rw[:], I["router"][i_moe], writes=[R_rw])
                hb = [sb("rt_h%d" % i, [128, 8, 128], F32, s1) for i in range(2)]; R_hb = [Res(), Res()]
                lg = [sb("rt_l%d" % i, [128, 40], F32, s1) for i in range(2)]; R_lg = [Res(), Res()]
                ge = [sb("rt_ge%d" % i, [128, 128], F32, s1) for i in range(2)]; R_ge = [Res(), Res()]
                gb = [sb("rt_gb%d" % i, [128, NE, 128], F32, s1) for i in range(2)]; R_gb = [Res(), Res()]
                n = 0; m = 0
                for b in range(NB):
                    for tt in range(2 if last else 0, NT):
                        j = n % 2; n += 1
                        t0 = tt * 128
                        k.dma("sp", hb[j][:], H2F[b][:, :, t0:t0 + 128], reads=[R_H2F[b]], writes=[R_hb[j]])
                        pt, rp = ps_next()
                        for kc in range(8):
                            k.op("pe", lambda e, pt=pt, j=j, kc=kc: e.matmul(pt[:, 0:NE], hb[j][:, kc, :], rw[:, kc, :], start=(kc == 0), stop=(kc == 7)),
                                 reads=[R_hb[j], R_rw], writes=[rp])
                        L = lg[j]
                        k.op("dve", lambda e, pt=pt, L=L: e.tensor_copy(out=L[:, 0:8], in_=pt[:, 0:8]), reads=[rp], writes=[R_lg[j]])
                        k.op("dve", lambda e, L=L: e.tensor_reduce(out=L[:, 8:9], in_=L[:, 0:8], axis=AX.X, op=ALU.max), reads=[R_lg[j]], writes=[R_lg[j]])
                        k.op("dve", lambda e, L=L: e.tensor_scalar(out=L[:, 9:17], in0=L[:, 0:8], scalar1=L[:, 8:9], scalar2=None, op0=ALU.is_ge), reads=[R_lg[j]], writes=[R_lg[j]])
                        k.op("dve", lambda e, L=L: e.scalar_tensor_tensor(out=L[:, 17:25], in0=L[:, 9:17], scalar=-1e30, in1=L[:, 0:8], op0=ALU.mult, op1=ALU.add), reads=[R_lg[j]], writes=[R_lg[j]])
                        k.op("dve", lambda e, L=L: e.tensor_reduce(out=L[:, 25:26], in_=L[:, 17:25], axis=AX.X, op=ALU.max), reads=[R_lg[j]], writes=[R_lg[j]])
                        k.op("dve", lambda e, L=L: e.tensor_scalar(out=L[:, 26:34], in0=L[:, 17:25], scalar1=L[:, 25:26], scalar2=None, op0=ALU.is_ge), reads=[R_lg[j]], writes=[R_lg[j]])
                        k.op("dve", lambda e, L=L: e.tensor_tensor(out=L[:, 34:35], in0=L[:, 8:9], in1=L[:, 25:26], op=ALU.subtract), reads=[R_lg[j]], writes=[R_lg[j]])
                        k.op("act", lambda e, L=L: e.activation(out=L[:, 35:36], in_=L[:, 34:35], func=AF.Sigmoid), reads=[R_lg[j]], writes=[R_lg[j]])
                        k.op("dve", lambda e, L=L: e.tensor_scalar(out=L[:, 36:37], in0=L[:, 35:36], scalar1=-1.0, scalar2=1.0, op0=ALU.mult, op1=ALU.add), reads=[R_lg[j]], writes=[R_lg[j]])
                        k.op("dve", lambda e, L=L: e.tensor_scalar(out=L[:, 9:17], in0=L[:, 9:17], scalar1=L[:, 35:36], scalar2=None, op0=ALU.mult), reads=[R_lg[j]], writes=[R_lg[j]])
                        k.op("dve", lambda e, L=L: e.scalar_tensor_tensor(out=L[:, 9:17], in0=L[:, 26:34], scalar=L[:, 36:37], in1=L[:, 9:17], op0=ALU.mult, op1=ALU.add), reads=[R_lg[j]], writes=[R_lg[j]])
                        for ex in range(NE):
                            i2 = m % 2; m += 1
                            k.op("pool", lambda e, L=L, i2=i2, ex=ex: e.tensor_copy(out=ge[i2][:], in_=L[:, 9 + ex:10 + ex].to_broadcast([128, 128])), reads=[R_lg[j]], writes=[R_ge[i2]])
                            pg, rg = ps_next()
                            k.op("pe", lambda e, pg=pg, i2=i2: e.matmul(pg[:, 0:128], ge[i2][:], ident[:], start=True, stop=True), reads=[R_ge[i2], R_id], writes=[rg])
                            k.op("act", lambda e, pg=pg, j=j, ex=ex: e.activation(out=gb[j][:, ex, :], in_=pg[:, 0:128], func=AF.Copy), reads=[rg], writes=[R_gb[j]])
                        k.dma("sp", GB[b][:, :, t0:t0 + 128], gb[j][:], reads=[R_gb[j]], writes=[R_GB[b]])
            k.barrier()

        def stage_ffn(l, last):
            moe = (l % 2 == 1)
            i_w = l // 2
            F = DFE if moe else DFF
            GW = 512 if moe else 256
            ng = F // GW
            nfc = GW // 128
            tstart = LC if last else 0
            with ExitStack() as s1:
                hT = sb("ff_h", [128, 8, S], BF16, s1); R_h = Res()
                yacc = sb("ff_y", [128, 8, S], F32, s1); R_ya = [Res() for _ in range(9)]
                w1 = [sb("ff_w1%d" % i, [128, 8, GW], BF16, s1) for i in range(2)]
                w3 = [sb("ff_w3%d" % i, [128, 8, GW], BF16, s1) for i in range(2)]
                w2 = [sb("ff_w2%d" % i, [128, nfc, D], BF16, s1) for i in range(2)]
                R_wg = [Res(), Res()]
                sg = [sb("ff_s%d" % i, [128, 256], F32, s1) for i in range(2)]; R_sg = [Res(), Res()]
                ac = [sb("ff_a%d" % i, [128, nfc, 256], BF16, s1) for i in range(2)]; R_ac = [Res(), Res()]
                gt = [sb("ff_g%d" % i, [128, 256], F32, s1) for i in range(2)]; R_gt = [Res(), Res()]
                xb = [sb("ff_x%d" % i, [128, 8, 256], F32, s1) for i in range(2)]; R_xb = [Res(), Res()]
                nw = 0; na_ = 0; ns = 0; ngt = 0
                for b in range(NB):
                    k.dma("sp", hT[:, :, tstart:S], H2T[b][:, :, tstart:S], reads=[R_H2T[b]], writes=[R_h])
                    first = True
                    for ex in range(NE if moe else 1):
                        for g in range(ng):
                            jw = nw % 2; nw += 1
                            if moe:
                                s_w1, s_w3, s_w2 = I["moe_w1"][i_w, ex], I["moe_w3"][i_w, ex], I["moe_w2"][i_w, ex]
                            else:
                                s_w1, s_w3, s_w2 = I["ffn_w1"][i_w], I["ffn_w3"][i_w], I["ffn_w2"][i_w]
                            k.dma("pool", w1[jw][:], s_w1[:, :, g * GW:(g + 1) * GW], writes=[R_wg[jw]])
                            k.dma("pool", w3[jw][:], s_w3[:, :, g * GW:(g + 1) * GW], writes=[R_wg[jw]])
                            k.dma("pool", w2[jw][:], s_w2[:, g * nfc:(g + 1) * nfc, :], writes=[R_wg[jw]])
                            def phase_H(tb, ja, jw=jw, ex=ex, g=g):
                                nonlocal ns, ngt
                                t0 = tb * 256
                                if moe:
                                    jg = ngt % 2; ngt += 1
                                    k.dma("sp", gt[jg][:], GB[b][:, ex, t0:t0 + 256], reads=[R_GB[b]], writes=[R_gt[jg]])
                                for fc in range(nfc):
                                    ph, rh = ps_next()
                                    for kc in range(8):
                                        k.op("pe", lambda e, ph=ph, fc=fc, kc=kc, t0=t0: e.matmul(ph[:, 0:256], w1[jw][:, kc, fc * 128:(fc + 1) * 128], hT[:, kc, t0:t0 + 256],
                                             start=(kc == 0), stop=(kc == 7)), reads=[R_wg[jw], R_h], writes=[rh])
                                    for kc in range(8):
                                        k.op("pe", lambda e, ph=ph, fc=fc, kc=kc, t0=t0: e.matmul(ph[:, 256:512], w3[jw][:, kc, fc * 128:(fc + 1) * 128], hT[:, kc, t0:t0 + 256],
                                             start=(kc == 0), stop=(kc == 7)), reads=[R_wg[jw], R_h], writes=[rh])
                                    js = ns % 2; ns += 1
                                    k.op("act", lambda e, ph=ph, js=js: e.activation(out=sg[js][:], in_=ph[:, 0:256], func=AF.Silu), reads=[rh], writes=[R_sg[js]])
                                    if moe:
                                        k.op("pool", lambda e, js=js, jg=jg: e.tensor_tensor(out=sg[js][:], in0=sg[js][:], in1=gt[jg][:], op=ALU.mult), reads=[R_sg[js], R_gt[jg]], writes=[R_sg[js]])
                                    k.op("dve", lambda e, ph=ph, js=js, ja=ja, fc=fc: e.tensor_tensor(out=ac[ja][:, fc, :], in0=sg[js][:], in1=ph[:, 256:512], op=ALU.mult),
                                         reads=[R_sg[js], rh], writes=[R_ac[ja]])

                            def phase_O(tb, ja, jw=jw, first=first):
                                t0 = tb * 256
                                for ocp in range(4):
                                    po, ro = ps_next()
                                    for half in range(2):
                                        oc = 2 * ocp + half
                                        for fc in range(nfc):
                                            k.op("pe", lambda e, po=po, fc=fc, oc=oc, half=half: e.matmul(po[:, half * 256:(half + 1) * 256], w2[jw][:, fc, oc * 128:(oc + 1) * 128], ac[ja][:, fc, :],
                                                 start=(fc == 0), stop=(fc == nfc - 1)), reads=[R_wg[jw], R_ac[ja]], writes=[ro])
                                    yv_ = yacc[:, 2 * ocp:2 * ocp + 2, t0:t0 + 256]
                                    pv_ = po[:, :].rearrange("p (a c) -> p a c", a=2)
                                    if first:
                                        k.op("act", lambda e, yv_=yv_, pv_=pv_: e.activation(out=yv_, in_=pv_, func=AF.Copy), reads=[ro], writes=[R_ya[tb]])
                                    else:
                                        k.op("dve", lambda e, yv_=yv_, pv_=pv_: e.tensor_tensor(out=yv_, in0=yv_, in1=pv_, op=ALU.add), reads=[ro, R_ya[tb]], writes=[R_ya[tb]])

                            prev = None
                            for tb in range(tstart // 256, S // 256):
                                ja = na_ % 2; na_ += 1
                                phase_H(tb, ja)
                                if prev is not None:
                                    phase_O(*prev)
                                prev = (tb, ja)
                            phase_O(*prev)
                            first = False
                    for tb in range(tstart // 256, S // 256):
                        t0 = tb * 256
                        j = tb % 2
                        wq = which_of(b, t0)
                        k.dma("sp", xb[j][:], XT[b][:, :, t0:t0 + 256], reads=[R_XT[b]], writes=[R_xb[j]])
                        for oc in range(8):
                            k.op("dve", lambda e, j=j, oc=oc, t0=t0, wq=wq: e.scalar_tensor_tensor(out=xb[j][:, oc, :], in0=yacc[:, oc, t0:t0 + 256],
                                 scalar=MOD[:, l, 40 + oc, wq:wq + 1], in1=xb[j][:, oc, :], op0=ALU.mult, op1=ALU.add), reads=[R_ya[tb], R_mod, R_xb[j]], writes=[R_xb[j]])
                        k.dma("sp", XT[b][:, :, t0:t0 + 256], xb[j][:], reads=[R_xb[j]], writes=[R_XT[b]])
            k.barrier()

        def stage_final():
            with ExitStack() as s1:
                fg = sb("fn_g", [128, 8], F32, s1); R_fg = Res()
                k.dma("sp", fg[:], I["final_g"][:, :], writes=[R_fg])
                xb = [sb("fx%d" % i, [128, 8, 256], F32, s1) for i in range(2)]; R_xb = [Res(), Res()]
                sq = [sb("fsq%d" % i, [128, 8, 256], F32, s1) for i in range(2)]; R_sq = [Res(), Res()]
                rs = [sb("frs%d" % i, [128, 256], F32, s1) for i in range(2)]; R_rs = [Res(), Res()]
                ob = [sb("fo%d" % i, [128, 8, 256], F32, s1) for i in range(2)]; R_ob = [Res(), Res()]
                n = 0
                outs = []
                for b in range(NB):
                    for t0 in range(LC, S, 256):
                        j = n % 2; n += 1
                        k.dma("sp", xb[j][:], XT[b][:, :, t0:t0 + 256], reads=[R_XT[b]], writes=[R_xb[j]])
                        k.op("act", lambda e, j=j: e.activation(out=sq[j][:], in_=xb[j][:], func=AF.Square), reads=[R_xb[j]], writes=[R_sq[j]])
                        pt, rp = ps_next()
                        for c in range(8):
                            k.op("pe", lambda e, pt=pt, j=j, c=c: e.matmul(pt[:, 0:256], ones[:], sq[j][:, c, :], start=(c == 0), stop=(c == 7)), reads=[R_ones, R_sq[j]], writes=[rp])
                        k.op("act", lambda e, pt=pt, j=j: e.activation(out=rs[j][:], in_=pt[:, 0:256], func=AF.Sqrt, bias=eps_t[:, 0:1], scale=1.0 / D), reads=[rp, R_eps], writes=[R_rs[j]])
                        k.op("dve", lambda e, j=j: e.reciprocal(out=rs[j][:], in_=rs[j][:]), reads=[R_rs[j]], writes=[R_rs[j]])
                        for c in range(8):
                            k.op("dve", lambda e, j=j, c=c: e.scalar_tensor_tensor(out=ob[j][:, c, :], in0=xb[j][:, c, :], scalar=fg[:, c:c + 1], in1=rs[j][:],
                                 op0=ALU.mult, op1=ALU.mult), reads=[R_xb[j], R_fg, R_rs[j]], writes=[R_ob[j]])
                        outs.append(k.dma("sp", out[b, :, :, t0 - LC:t0 - LC + 256], ob[j][:], reads=[R_ob[j]]))
                return outs

        def stage_rwkv(l, need_ctx):
            with ExitStack() as s1:
                NSET = 3
                def T_(name, shape, dt=F32, n=3):
                    return [sb(name + str(i), shape, dt, s1) for i in range(n)], [Res() for _ in range(n)]
                w2 = sb("rw_w2", [128, 512], F32, s1); a2 = sb("rw_a2", [128, 512], F32, s1); g2 = sb("rw_g2", [128, 512], F32, s1)
                w0b = sb("rw_w0b", [64, 2, 512], F32, s1); a0T = sb("rw_a0T", [128, 2, 4], F32, s1)
                kkp = sb("rw_kkp", [128, 4], F32, s1); kap = sb("rw_kap", [128, 4], F32, s1); okap = sb("rw_okap", [128, 4], F32, s1)
                rkp = sb("rw_rkp", [128, 4], F32, s1)
                m2 = sb("rw_m2", [64, 2, 128], F32, s1); m3 = sb("rw_m3", [64, 2, 128], F32, s1); mL = sb("rw_mL", [64, 2, 64], F32, s1)
                bones = sb("rw_bones", [128, 128], F32, s1); sel = sb("rw_sel", [128, 2], F32, s1)
                c1 = sb("rw_c1", [128, 4], F32, s1)
                R_par = Res()
                for dst, src in ((w2, I["rw_w2"][l]), (a2, I["rw_a2"][l]), (g2, I["rw_g2"][l]), (a0T, I["rw_a0T"][l]),
                                 (kkp, I["rw_kk"][l]), (kap, I["rw_ka"][l]), (rkp, I["rw_rk"][l]),
                                 (m2, I["rw_m2"]), (m3, I["rw_m3"]), (mL, I["rw_mL"]), (bones, I["rw_bones"]), (sel, I["rw_sel"]), (c1, I["rw_c1"]),
                                 (w0b, I["rw_w0b"][l])):
                    k.dma("sp", dst[:], src, writes=[R_par])
                k.op("dve", lambda e: e.tensor_scalar(out=okap[:], in0=kap[:], scalar1=-1.0, scalar2=1.0, op0=ALU.mult, op1=ALU.add), reads=[R_par], writes=[R_par])
                Hst_b = [sb("rw_H%d" % i, [128, 4, 64], F32, s1) for i in range(NB)]; R_H_b = [Res() for _ in range(NB)]
                ub, R_ub = T_("rw_u", [128, 15, 64])
                twl, R_twl = T_("rw_twl", [128, 64])
                sgl, R_sgl = T_("rw_sgl", [128, 64], n=2)
                e2a, R_e2a = T_("rw_e2a", [64, 512], n=2)
                e2b, R_e2b = T_("rw_e2b", [64, 512], n=2)
                a_sb, R_a = T_("rw_a", [128, 4, 64])
                a1_sb, R_a1 = T_("rw_a1", [128, 4, 64], n=2)
                kr, R_kr = T_("rw_kr", [128, 4, 64])
                sq, R_sq = T_("rw_sq", [128, 4, 64])
                kk_t, R_kk = T_("rw_kkt", [128, 4, 64])
                ff, R_ff = T_("rw_ff", [128, 4, 64])
                kd, R_kd = T_("rw_kd", [128, 4, 64])
                bb, R_bb = T_("rw_bb", [128, 4, 64])
                EG, R_EG = T_("rw_EG", [128, 4, 2, 64])
                IEG, R_IEG = T_("rw_IEG", [128, 4, 64])
                fm, R_fm = T_("rw_fm", [128, 4, 4, 64])
                tm, R_tm = T_("rw_tm", [64, 4, 4, 128])
                LA, R_LA = T_("rw_LA", [64, 8, 128])
                NBt, R_NB = T_("rw_NB", [64, 8, 128])
                Lm, R_Lm = T_("rw_Lm", [64, 8, 64])
                Ll32, R_Ll32 = T_("rw_Ll32", [64, 8, 64])
                Pb, R_Pb = T_("rw_Pb", [64, 8, 64], BF16)
                Pm, R_Pm = T_("rw_Pm", [64, 8, 64])
                Nl, R_Nl = T_("rw_Nl", [64, 8, 64], BF16, n=4)
                Ll, R_Ll = T_("rw_Ll", [64, 8, 64], BF16, n=4)
                WT, R_WT = T_("rw_WT", [128, 4, 64])
                Xa, R_Xa = T_("rw_Xa", [64, 8, 64])
                Uta, R_Uta = T_("rw_Uta", [64, 8, 64])
                Ua, R_Ua = T_("rw_Ua", [64, 8, 64])
                ysb, R_ysb = T_("rw_ysb", [64, 512])
                htmp, R_htmp = T_("rw_htmp", [128, 4, 64])
                rk_t, R_rk = T_("rw_rkt", [128, 4, 64], n=2)
                rkh, R_rkh = T_("rw_rkh", [64, 8], n=2)
                bon, R_bon = T_("rw_bon", [64, 512], n=2)
                gsb, R_gsb = T_("rw_gsb", [64, 512], n=2)
                n = 0
                nl4 = [0]
                nb_run = NB if RW_DBG["nb"] is None else RW_DBG["nb"]
                for d in range(RW_DBG["nd"]):
                    for b in range(nb_run):
                        k.op("pool", lambda e, b=b: e.memset(Hst_b[b][:], 0.0), writes=[R_H_b[b]])
                    order = list(range(36)) if d == 0 else [3, 2, 1, 0] + list(range(35, 3, -1))
                    if RW_DBG["nch"] is not None:
                        order = order[:RW_DBG["nch"]]
                    last_col = 63 if d == 0 else 0
                    for ch in order:
                      for b in range(nb_run):
                        j = n % NSET; j2 = n % 2; n += 1
                        def chunk_body(b=b, ch=ch, d=d, j=j, j2=j2, last_col=last_col, Hst=Hst_b[b], R_H=R_H_b[b]):
                            if (not need_ctx) and False:
                                pass
                            t0 = ch * 64
                            U_ = ub[j]
                            k.dma("sp", U_[:], UT[b][:, :, t0:t0 + 64], reads=[R_UT[b]], writes=[R_ub[j]])
                            k.op("act", lambda e, j=j, U_=U_: e.activation(out=twl[j][:], in_=U_[:, 12, :], func=AF.Tanh), reads=[R_ub[j]], writes=[R_twl[j]])
                            pt, rp = ps_next()
                            k.op("pe", lambda e, pt=pt, j=j, d=d: e.matmul(pt[0:64, :], twl[j][d * 64:(d + 1) * 64, :], w2[d * 64:(d + 1) * 64, :], start=True, stop=True),
                                 reads=[R_twl[j], R_par], writes=[rp])
                            k.op("dve", lambda e, j2=j2, pt=pt, j=j, d=d: e.tensor_tensor(out=e2a[j2][:], in0=pt[0:64, :], in1=w0b[:, d, :], op=ALU.add), reads=[rp, R_par], writes=[R_e2a[j2]])
                            k.op("act", lambda e, j2=j2, j=j: e.activation(out=e2b[j2][:], in_=e2a[j2][:], func=AF.Exp, scale=-1.0), reads=[R_e2a[j2]], writes=[R_e2b[j2]])
                            k.op("act", lambda e, j2=j2, j=j: e.activation(out=e2a[j2][:], in_=e2b[j2][:], func=AF.Ln, bias=c1[0:64, 0:1], scale=1.0), reads=[R_e2b[j2], R_par], writes=[R_e2a[j2]])
                            k.op("act", lambda e, j2=j2, j=j: e.activation(out=e2b[j2][:], in_=e2a[j2][:], func=AF.Exp, bias=c1[0:64, 1:2], scale=-1.0), reads=[R_e2a[j2], R_par], writes=[R_e2b[j2]])
                            def a_path(dd, dst, R_dst):
                                pa, ra = ps_next()
                                for q in range(4):
                                    k.op("pe", lambda e, pa=pa, q=q, dd=dd, U_=U_: e.matmul(pa[:, q * 64:(q + 1) * 64], a2[dd * 64:(dd + 1) * 64, q * 128:(q + 1) * 128], U_[dd * 64:(dd + 1) * 64, 13, :], start=True, stop=True),
                                         reads=[R_par, R_ub[j]], writes=[ra])
                                for q in range(4):
                                    k.op("act", lambda e, pa=pa, q=q, dd=dd, dst=dst: e.activation(out=dst[:, q, :], in_=pa[:, q * 64:(q + 1) * 64], func=AF.Sigmoid, bias=a0T[:, dd, q:q + 1], scale=1.0),
                                         reads=[ra, R_par], writes=[R_dst])
                            a_path(d, a_sb[j], R_a[j])
                            k.op("dve", lambda e, j=j, U_=U_: e.tensor_tensor(out=kr[j][:], in0=U_[:, 4:8, :], in1=kkp[:, :].unsqueeze(2).to_broadcast([128, 4, 64]), op=ALU.mult), reads=[R_ub[j], R_par], writes=[R_kr[j]])
                            k.op("pool", lambda e, j=j: e.tensor_tensor(out=sq[j][:], in0=kr[j][:], in1=kr[j][:], op=ALU.mult), reads=[R_kr[j]], writes=[R_sq[j]])
                            pn_, rn = ps_next()
                            k.op("pe", lambda e, pn_=pn_, j=j: e.matmul(pn_[:, 0:256], bones[:], sq[j][:].rearrange("p q t -> p (q t)"), start=True, stop=True), reads=[R_par, R_sq[j]], writes=[rn])
                            k.op("act", lambda e, pn_=pn_, j=j: e.activation(out=sq[j][:].rearrange("p q t -> p (q t)"), in_=pn_[:, 0:256], func=AF.Sqrt), reads=[rn], writes=[R_sq[j]])
                            k.op("dve", lambda e, j=j: e.tensor_scalar(out=sq[j][:], in0=sq[j][:], scalar1=1e-12, scalar2=None, op0=ALU.max), reads=[R_sq[j]], writes=[R_sq[j]])
                            k.op("dve", lambda e, j=j: e.reciprocal(out=sq[j][:], in_=sq[j][:]), reads=[R_sq[j]], writes=[R_sq[j]])
                            k.op("dve", lambda e, j=j: e.tensor_tensor(out=kk_t[j][:], in0=kr[j][:], in1=sq[j][:], op=ALU.mult), reads=[R_kr[j], R_sq[j]], writes=[R_kk[j]])
                            k.op("pool", lambda e, j=j: e.tensor_tensor(out=ff[j][:], in0=a_sb[j][:], in1=kap[:, :].unsqueeze(2).to_broadcast([128, 4, 64]), op=ALU.mult), reads=[R_a[j], R_par], writes=[R_ff[j]])
                            k.op("pool", lambda e, j=j: e.tensor_tensor(out=ff[j][:], in0=ff[j][:], in1=okap[:, :].unsqueeze(2).to_broadcast([128, 4, 64]), op=ALU.add), reads=[R_ff[j], R_par], writes=[R_ff[j]])
                            k.op("pool", lambda e, j=j, U_=U_: e.tensor_tensor(out=kd[j][:], in0=U_[:, 4:8, :], in1=ff[j][:], op=ALU.mult), reads=[R_ub[j], R_ff[j]], writes=[R_kd[j]])
                            k.op("pool", lambda e, j=j: e.tensor_tensor(out=bb[j][:], in0=kk_t[j][:], in1=a_sb[j][:], op=ALU.mult), reads=[R_kk[j], R_a[j]], writes=[R_bb[j]])
                            pc, rc = ps_next()
                            for q in range(4):
                                k.op("pe", lambda e, j2=j2, pc=pc, q=q, j=j, d=d: e.matmul(pc[:, q * 128:(q + 1) * 128], e2b[j2][:, q * 128:(q + 1) * 128], m2[:, d, :], start=True, stop=True),
                                     reads=[R_e2b[j2], R_par], writes=[rc])
                            k.op("act", lambda e, pc=pc, j=j: e.activation(out=EG[j][:].rearrange("p q s t -> p (q s t)"), in_=pc[:, :], func=AF.Exp, scale=-1.0), reads=[rc], writes=[R_EG[j]])
                            k.op("act", lambda e, pc=pc, j=j: e.activation(out=IEG[j][:], in_=pc[:, :].rearrange("p (q s t) -> p q s t", q=4, s=2)[:, :, 1, :], func=AF.Exp, scale=1.0), reads=[rc], writes=[R_IEG[j]])
                            F_ = fm[j]
                            k.op("dve", lambda e, j=j, F_=F_: e.tensor_tensor(out=F_[:, :, 0, :], in0=kd[j][:], in1=IEG[j][:], op=ALU.mult), reads=[R_kd[j], R_IEG[j]], writes=[R_fm[j]])
                            k.op("dve", lambda e, j=j, F_=F_: e.tensor_tensor(out=F_[:, :, 1, :], in0=bb[j][:], in1=IEG[j][:], op=ALU.mult), reads=[R_bb[j], R_IEG[j]], writes=[R_fm[j]])
                            k.op("pool", lambda e, j=j, F_=F_: e.tensor_tensor(out=F_[:, :, 2, :], in0=kk_t[j][:], in1=EG[j][:, :, 0, :], op=ALU.mult), reads=[R_kk[j], R_EG[j]], writes=[R_fm[j]])
                            k.op("pool", lambda e, j=j, F_=F_, U_=U_: e.tensor_tensor(out=F_[:, :, 3, :], in0=U_[:, 0:4, :], in1=EG[j][:, :, 1, :], op=ALU.mult), reads=[R_ub[j], R_EG[j]], writes=[R_fm[j]])
                            T_m = tm[j]
                            for kind, (srcf, sc) in enumerate(((lambda q, F_=F_: F_[:, q, 2, :], 1.0), (lambda q, F_=F_: F_[:, q, 0, :], 1.0), (lambda q, F_=F_: F_[:, q, 1, :], -1.0), (lambda q, U_=U_: U_[:, 8 + q, :], 1.0))):
                                ptx, rtx = ps_next()
                                for q in range(4):
                                    k.op("pe", lambda e, ptx=ptx, q=q, srcf=srcf: e.transpose(ptx[0:64, q * 128:(q + 1) * 128], srcf(q), ident[:, :]),
                                         reads=[R_fm[j], R_ub[j], R_id], writes=[rtx])
                                eng = "act" if kind % 2 == 0 else "dve"
                                if eng == "act":
                                    k.op("act", lambda e, ptx=ptx, kind=kind, sc=sc, T_m=T_m: e.activation(out=T_m[:, :, kind, :], in_=ptx[0:64, :].rearrange("p (q c) -> p q c", q=4), func=AF.Copy, scale=sc),
                                         reads=[rtx], writes=[R_tm[j]])
                                else:
                                    k.op("dve", lambda e, ptx=ptx, kind=kind, sc=sc, T_m=T_m: e.tensor_scalar(out=T_m[:, :, kind, :], in0=ptx[0:64, :].rearrange("p (q c) -> p q c", q=4), scalar1=sc, scalar2=None, op0=ALU.mult),
                                         reads=[rtx], writes=[R_tm[j]])
                            if RW_DBG["upto"] < "A2":
                                return
                            if d == 0 and (need_ctx or ch >= 4):
                                a_path(1, a1_sb[j2], R_a1[j2])
                                k.op("dve", lambda e, j2=j2, j=j: e.tensor_tensor(out=a1_sb[j2][:], in0=a1_sb[j2][:], in1=a_sb[j][:], op=ALU.add), reads=[R_a1[j2], R_a[j]], writes=[R_a1[j2]])
                                k.op("dve", lambda e, j2=j2, j=j: e.scalar_tensor_tensor(out=a1_sb[j2][:], in0=a1_sb[j2][:], scalar=0.5, in1=kap[:, :].unsqueeze(2).to_broadcast([128, 4, 64]), op0=ALU.mult, op1=ALU.mult), reads=[R_a1[j2], R_par], writes=[R_a1[j2]])
                                k.op("dve", lambda e, j2=j2, j=j: e.tensor_tensor(out=a1_sb[j2][:], in0=a1_sb[j2][:], in1=okap[:, :].unsqueeze(2).to_broadcast([128, 4, 64]), op=ALU.add), reads=[R_a1[j2], R_par], writes=[R_a1[j2]])
                                k.op("dve", lambda e, j2=j2, j=j, U_=U_: e.tensor_tensor(out=rk_t[j2][:], in0=a1_sb[j2][:], in1=U_[:, 4:8, :], op=ALU.mult), reads=[R_a1[j2], R_ub[j]], writes=[R_rk[j2]])
                                k.op("dve", lambda e, j2=j2, j=j, U_=U_: e.tensor_tensor(out=rk_t[j2][:], in0=rk_t[j2][:], in1=U_[:, 0:4, :], op=ALU.mult), reads=[R_rk[j2], R_ub[j]], writes=[R_rk[j2]])
                                k.op("dve", lambda e, j2=j2, j=j: e.tensor_tensor(out=rk_t[j2][:], in0=rk_t[j2][:], in1=rkp[:, :].unsqueeze(2).to_broadcast([128, 4, 64]), op=ALU.mult), reads=[R_rk[j2], R_par], writes=[R_rk[j2]])
                                pr, rr = ps_next()
                                for q in range(4):
                                    k.op("pe", lambda e, j2=j2, pr=pr, q=q, j=j: e.matmul(pr[0:64, q * 2:(q + 1) * 2], rk_t[j2][:, q, :], sel[:, :], start=True, stop=True), reads=[R_rk[j2], R_par], writes=[rr])
                                k.op("act", lambda e, j2=j2, pr=pr, j=j: e.activation(out=rkh[j2][:], in_=pr[0:64, 0:8], func=AF.Copy), reads=[rr], writes=[R_rkh[j2]])
                                k.op("dve", lambda e, j2=j2, j=j, T_m=T_m: e.tensor_tensor(out=bon[j2][:].rearrange("p (q h v) -> p q h v", q=4, h=2), in0=T_m[:, :, 3, :].rearrange("p q (h v) -> p q h v", h=2),
                                     in1=rkh[j2][:, :].rearrange("p (q h) -> p q h", q=4).unsqueeze(3).to_broadcast([64, 4, 2, 64]), op=ALU.mult), reads=[R_tm[j], R_rkh[j2]], writes=[R_bon[j2]])
                                k.dma("sp", BON[b][t0:t0 + 64, :], bon[j2][:], reads=[R_bon[j2]], writes=[R_BON[b]])
                                k.op("act", lambda e, j2=j2, j=j, U_=U_: e.activation(out=sgl[j2][:], in_=U_[:, 14, :], func=AF.Sigmoid), reads=[R_ub[j]], writes=[R_sgl[j2]])
                                pg, rg = ps_next()
                                k.op("pe", lambda e, j2=j2, pg=pg, j=j: e.matmul(pg[0:64, :], sgl[j2][:], g2[:], start=True, stop=True), reads=[R_sgl[j2], R_par], writes=[rg])
                                k.op("act", lambda e, j2=j2, pg=pg, j=j: e.activation(out=gsb[j2][:], in_=pg[0:64, :], func=AF.Copy), reads=[rg], writes=[R_gsb[j2]])
                                k.dma("sp", GG[b][t0:t0 + 64, :], gsb[j2][:], reads=[R_gsb[j2]], writes=[R_GG[b]])
                            if RW_DBG["upto"] < "B":
                                return
                            def hp(h):
                                return h // 2, (h % 2) * 64
                            hv = lambda t, h2: t[:].rearrange("p (q h) c -> p q h c", q=4, h=2)[:, :, h2, :]
                            for h2 in range(2):
                                p0 = h2 * 64
                                p1, r1 = ps_next()
                                p2, r2 = ps_next()
                                p3, r3 = ps_next()
                                for q in range(4):
                                    k.op("pe", lambda e, p1=p1, q=q, p0=p0, F_=F_: e.matmul(p1[0:64, q * 128:(q + 1) * 128], F_[p0:p0 + 64, q, 0, :], F_[p0:p0 + 64, q, 2:4, :].rearrange("p s t -> p (s t)"), start=True, stop=True),
                                         reads=[R_fm[j]], writes=[r1])
                                    k.op("pe", lambda e, p2=p2, q=q, p0=p0, F_=F_: e.matmul(p2[0:64, q * 128:(q + 1) * 128], F_[p0:p0 + 64, q, 1, :], F_[p0:p0 + 64, q, 2:4, :].rearrange("p s t -> p (s t)"), start=True, stop=True),
                                         reads=[R_fm[j]], writes=[r2])
                                    k.op("pe", lambda e, p3=p3, q=q, p0=p0, F_=F_: e.matmul(p3[0:64, q * 64:(q + 1) * 64], F_[p0:p0 + 64, q, 2, :], F_[p0:p0 + 64, q, 1, :], start=True, stop=True), reads=[R_fm[j]], writes=[r3])
                                k.op("dve", lambda e, p1=p1, h2=h2, j=j, d=d: e.tensor_tensor(out=hv(LA[j], h2), in0=p1[0:64, :].rearrange("p (q c) -> p q c", q=4),
                                     in1=m2[:, d, :].unsqueeze(1).to_broadcast([64, 4, 128]), op=ALU.mult), reads=[r1, R_par], writes=[R_LA[j]])
                                k.op("dve", lambda e, p2=p2, h2=h2, j=j, d=d: e.tensor_tensor(out=hv(NBt[j], h2), in0=p2[0:64, :].rearrange("p (q c) -> p q c", q=4),
                                     in1=m3[:, d, :].unsqueeze(1).to_broadcast([64, 4, 128]), op=ALU.mult), reads=[r2, R_par], writes=[R_NB[j]])
                                k.op("dve", lambda e, p3=p3, h2=h2, j=j, d=d: e.tensor_tensor(out=hv(Lm[j], h2), in0=p3[0:64, 0:256].rearrange("p (q c) -> p q c", q=4),
                                     in1=mL[:, d, :].unsqueeze(1).to_broadcast([64, 4, 64]), op=ALU.mult), reads=[r3, R_par], writes=[R_Lm[j]])
                            if RW_DBG["upto"] < "B0":
                                return
                            k.op("dve", lambda e, j=j: e.scalar_tensor_tensor(out=Pm[j][:], in0=NBt[j][:, :, 0:64], scalar=-1.0, in1=ident[0:64, 0:64].unsqueeze(1).to_broadcast([64, 8, 64]), op0=ALU.mult, op1=ALU.add),
                                 reads=[R_NB[j], R_id], writes=[R_Pm[j]])
                            Ncur = lambda h, j=j: NBt[j][:, h, 0:64]
                            Lcur = lambda h, j=j: Lm[j][:, h, :]
                            R_Nc, R_Lc = R_NB[j], R_Lm[j]
                            nlev = RW_DBG.get("nlev", 5)
                            for lev in range(nlev):
                                i4 = nl4[0] % 4; nl4[0] += 1
                                pL, rL = ps_next()
                                for h in range(8):
                                    k.op("pe", lambda e, pL=pL, h=h, Ncur=Ncur, Lcur=Lcur: e.matmul(pL[0:64, h * 64:(h + 1) * 64], Ncur(h), Lcur(h), start=True, stop=True), reads=[R_Nc, R_Lc], writes=[rL])
                                if lev == 0:
                                    k.op("act", lambda e, pL=pL, j=j: e.activation(out=Ll32[j][:].rearrange("p h c -> p (h c)"), in_=pL[0:64, :], func=AF.Copy), reads=[rL], writes=[R_Ll32[j]])
                                    k.op("pool", lambda e, i4=i4, j=j: e.tensor_copy(out=Ll[i4][:], in_=Ll32[j][:]), reads=[R_Ll32[j]], writes=[R_Ll[i4]])
                                else:
                                    k.op("act", lambda e, pL=pL, i4=i4: e.activation(out=Ll[i4][:].rearrange("p h c -> p (h c)"), in_=pL[0:64, :], func=AF.Copy), reads=[rL], writes=[R_Ll[i4]])
                                if lev < nlev - 1:
                                    pN, rN = ps_next()
                                    for h in range(8):
                                        k.op("pe", lambda e, pN=pN, h=h, Ncur=Ncur, Lcur=Lcur: e.matmul(pN[0:64, h * 64:(h + 1) * 64], Lcur(h), Ncur(h), start=True, stop=True), reads=[R_Nc, R_Lc], writes=[rN])
                                    k.op("dve", lambda e, pN=pN, i4=i4: e.tensor_copy(out=Nl[i4][:].rearrange("p h c -> p (h c)"), in_=pN[0:64, :]), reads=[rN], writes=[R_Nl[i4]])
                                pP, rP = ps_next()
                                for h in range(8):
                                    if lev == 0:
                                        k.op("pe", lambda e, pP=pP, h=h, j=j: e.matmul(pP[0:64, h * 64:(h + 1) * 64], Ll32[j][:, h, :], Pm[j][:, h, :], start=True, stop=True), reads=[R_Ll32[j], R_Pm[j]], writes=[rP])
                                    else:
                                        k.op("pe", lambda e, pP=pP, h=h, i4=i4, j=j: e.matmul(pP[0:64, h * 64:(h + 1) * 64], Ll[i4][:, h, :], Pb[j][:, h, :], start=True, stop=True), reads=[R_Ll[i4], R_Pb[j]], writes=[rP])
                                k.op("dve", lambda e, pP=pP, j=j: e.tensor_tensor(out=Pm[j][:].rearrange("p h c -> p (h c)"), in0=Pm[j][:].rearrange("p h c -> p (h c)"), in1=pP[0:64, :], op=ALU.add), reads=[rP, R_Pm[j]], writes=[R_Pm[j]])
                                if lev < nlev - 1:
                                    k.op("act", lambda e, j=j: e.activation(out=Pb[j][:], in_=Pm[j][:], func=AF.Copy), reads=[R_Pm[j]], writes=[R_Pb[j]])
                                Ncur = lambda h, i4=i4: Nl[i4][:, h, :]
                                Lcur = lambda h, i4=i4: Ll[i4][:, h, :]
                                R_Nc, R_Lc = R_Nl[i4], R_Ll[i4]
                            if RW_DBG["upto"] < "B2":
                                return
                            pW, rW = ps_next()
                            for h in range(8):
                                q, p0 = hp(h)
                                k.op("pe", lambda e, pW=pW, h=h, q=q, j=j, T_m=T_m: e.matmul(pW[:, h * 64:(h + 1) * 64], T_m[:, q, 0, :], Pm[j][:, h, :], start=True, stop=True), reads=[R_tm[j], R_Pm[j]], writes=[rW])
                            for h2 in range(2):
                                k.op("act" if h2 == 0 else "dve",
                                     (lambda e, pW=pW, j=j, h2=h2: e.activation(out=WT[j][h2 * 64:(h2 + 1) * 64, :, :], in_=pW[h2 * 64:(h2 + 1) * 64, :].rearrange("p (q h i) -> p q h i", q=4, h=2)[:, :, h2, :], func=AF.Copy)) if h2 == 0 else
                                     (lambda e, pW=pW, j=j, h2=h2: e.tensor_copy(out=WT[j][h2 * 64:(h2 + 1) * 64, :, :], in_=pW[h2 * 64:(h2 + 1) * 64, :].rearrange("p (q h i) -> p q h i", q=4, h=2)[:, :, h2, :])),
                                     reads=[rW], writes=[R_WT[j]])
                            pX, rX = ps_next()
                            for h in range(8):
                                q, p0 = hp(h)
                                k.op("pe", lambda e, pX=pX, h=h, q=q, p0=p0, j=j, T_m=T_m: e.matmul(pX[0:64, h * 64:(h + 1) * 64], LA[j][:, h, 0:64], T_m[:, q, 3, p0:p0 + 64], start=True, stop=True), reads=[R_LA[j], R_tm[j]], writes=[rX])
                            k.op("act", lambda e, pX=pX, j=j: e.activation(out=Xa[j][:].rearrange("p h c -> p (h c)"), in_=pX[0:64, :], func=AF.Copy), reads=[rX], writes=[R_Xa[j]])
                            pU, rU = ps_next()
                            for h in range(8):
                                k.op("pe", lambda e, pU=pU, h=h, j=j: e.matmul(pU[0:64, h * 64:(h + 1) * 64], Pm[j][:, h, :], Xa[j][:, h, :], start=True, stop=True), reads=[R_Pm[j], R_Xa[j]], writes=[rU])
                            k.op("act", lambda e, pU=pU, j=j: e.activation(out=Uta[j][:].rearrange("p h c -> p (h c)"), in_=pU[0:64, :], func=AF.Copy), reads=[rU], writes=[R_Uta[j]])
                            if RW_DBG["upto"] < "C":
                                return
                            for h2 in range(2):
                                p0 = h2 * 64
                                pS, rS = ps_next()
                                for q in range(4):
                                    k.op("pe", lambda e, Hst=Hst, pS=pS, q=q, p0=p0, j=j: e.matmul(pS[0:64, q * 64:(q + 1) * 64], WT[j][p0:p0 + 64, q, :], Hst[p0:p0 + 64, q, :], start=True, stop=True), reads=[R_WT[j], R_H], writes=[rS])
                                k.op("dve", lambda e, pS=pS, j=j, h2=h2: e.tensor_tensor(out=hv(Ua[j], h2), in0=hv(Uta[j], h2), in1=pS[0:64, 0:256].rearrange("p (q c) -> p q c", q=4), op=ALU.add),
                                     reads=[rS, R_Uta[j]], writes=[R_Ua[j]])
                            if RW_DBG["upto"] < "C1":
                                return
                            pY, rY = ps_next()
                            for h in range(8):
                                q, p0 = hp(h)
                                k.op("pe", lambda e, pY=pY, h=h, q=q, p0=p0, j=j, T_m=T_m: e.matmul(pY[0:64, h * 64:(h + 1) * 64], LA[j][:, h, 64:128], T_m[:, q, 3, p0:p0 + 64], start=True, stop=False), reads=[R_LA[j], R_tm[j]], writes=[rY])
                                k.op("pe", lambda e, pY=pY, h=h, j=j: e.matmul(pY[0:64, h * 64:(h + 1) * 64], NBt[j][:, h, 64:128], Ua[j][:, h, :], start=False, stop=True), reads=[R_NB[j], R_Ua[j]], writes=[rY])
                            k.op("act", lambda e, pY=pY, j=j: e.activation(out=ysb[j][:], in_=pY[0:64, :], func=AF.Copy), reads=[rY], writes=[R_ysb[j]])
                            for h2 in range(2):
                                p0 = h2 * 64
                                pR, rR = ps_next()
                                for q in range(4):
                                    k.op("pe", lambda e, Hst=Hst, pR=pR, q=q, p0=p0, j=j, F_=F_: e.matmul(pR[0:64, q * 64:(q + 1) * 64], F_[p0:p0 + 64, q, 3, :], Hst[p0:p0 + 64, q, :], start=True, stop=True), reads=[R_fm[j], R_H], writes=[rR])
                                yv = lambda t, h2: t[:].rearrange("p (q h c) -> p q h c", q=4, h=2)[:, :, h2, :]
                                k.op("dve", lambda e, pR=pR, j=j, h2=h2, yv=yv: e.tensor_tensor(out=yv(ysb[j], h2), in0=yv(ysb[j], h2), in1=pR[0:64, 0:256].rearrange("p (q c) -> p q c", q=4), op=ALU.add),
                                     reads=[rR, R_ysb[j]], writes=[R_ysb[j]])
                            k.dma("sp", YD[d][b][t0:t0 + 64, :], ysb[j][:], reads=[R_ysb[j]], writes=[R_YD[d][b]])
                            if RW_DBG["upto"] < "C2":
                                return
                            pH, rH = ps_next()
                            for q in range(4):
                                k.op("pe", lambda e, pH=pH, q=q, T_m=T_m: e.matmul(pH[:, q * 128:(q + 1) * 128], T_m[:, q, 1, :], T_m[:, q, 3, :], start=True, stop=False), reads=[R_tm[j]], writes=[rH])
                                k.op("pe", lambda e, pH=pH, q=q, j=j, T_m=T_m: e.matmul(pH[:, q * 128:(q + 1) * 128], T_m[:, q, 2, :], Ua[j][:, 2 * q:2 * q + 2, :].rearrange("p h c -> p (h c)"), start=False, stop=True), reads=[R_tm[j], R_Ua[j]], writes=[rH])
                            for h2 in range(2):
                                ps_ = slice(h2 * 64, (h2 + 1) * 64)
                                k.op("dve", lambda e, Hst=Hst, pH=pH, j=j, h2=h2, ps_=ps_: e.tensor_tensor(out=htmp[j][ps_, :, :], in0=pH[ps_, :].rearrange("p (q h v) -> p q h v", q=4, h=2)[:, :, h2, :], in1=Hst[ps_, :, :], op=ALU.add),
                                     reads=[rH, R_H], writes=[R_htmp[j]])
                                k.op("dve", lambda e, Hst=Hst, j=j, ps_=ps_, last_col=last_col: e.tensor_tensor(out=Hst[ps_, :, :], in0=htmp[j][ps_, :, :], in1=EG[j][ps_, :, 1, last_col:last_col + 1].to_broadcast([64, 4, 64]), op=ALU.mult),
                                     reads=[R_htmp[j], R_EG[j]], writes=[R_H])
                        chunk_body()
            k.barrier()
            if not RW_DBG["readout"]:
                return
            with ExitStack() as s1:
                lnw_ro = sb("ro_lnw", [128, 512], F32, s1); lnb_ro = sb("ro_lnb", [128, 512], F32, s1); c1_ro = sb("ro_c1", [128, 4], F32, s1); R_par_ro = Res()
                k.dma("sp", lnw_ro[:], I["rw_lnw"][l], writes=[R_par_ro]); k.dma("sp", lnb_ro[:], I["rw_lnb"][l], writes=[R_par_ro]); k.dma("sp", c1_ro[:], I["rw_c1"], writes=[R_par_ro])
                def T2(name, shape, dt=F32):
                    return [sb(name + str(i), shape, dt, s1) for i in range(2)], [Res(), Res()]
                y0_ro, R_y0_ro = T2("ro_y0", [128, 512]); y1_ro, R_y1_ro = T2("ro_y1", [128, 512]); bo_ro, R_bo_ro = T2("ro_bo", [128, 512]); gg_ro, R_gg_ro = T2("ro_gg", [128, 512])
                stt__ro, R_stt_ro = T2("ro_st", [128, 32]); yc_ro, R_yc_ro = T2("ro_yc", [128, 512]); sq_ro, R_sq_ro = T2("ro_sq", [128, 512]); ot_ro, R_ot_ro = T2("ro_ot", [128, 4, 128], BF16)
                n = 0
                for b in range(NB):
                    for tt in range(0 if need_ctx else 2, NT):
                        j = n % 2; n += 1
                        t0 = tt * 128
                        k.dma("sp", y0_ro[j][:], YD[0][b][t0:t0 + 128, :], reads=[R_YD[0][b]], writes=[R_y0_ro[j]])
                        k.dma("sp", y1_ro[j][:], YD[1][b][t0:t0 + 128, :], reads=[R_YD[1][b]], writes=[R_y1_ro[j]])
                        k.dma("sp", bo_ro[j][:], BON[b][t0:t0 + 128, :], reads=[R_BON[b]], writes=[R_bo_ro[j]])
                        k.dma("sp", gg_ro[j][:], GG[b][t0:t0 + 128, :], reads=[R_GG[b]], writes=[R_gg_ro[j]])
                        S_ = stt__ro[j]
                        v3 = lambda t: t[:].rearrange("p (h v) -> p h v", h=8)
                        bc = lambda a: a.unsqueeze(2).to_broadcast([128, 8, 64])
                        k.op("dve", lambda e, j=j: e.tensor_tensor(out=y0_ro[j][:], in0=y0_ro[j][:], in1=y1_ro[j][:], op=ALU.add), reads=[R_y0_ro[j], R_y1_ro[j]], writes=[R_y0_ro[j]])
                        k.op("dve", lambda e, j=j, S_=S_: e.tensor_reduce(out=S_[:, 0:8], in_=v3(y0_ro[j]), axis=AX.X, op=ALU.add), reads=[R_y0_ro[j]], writes=[R_stt_ro[j]])
                        k.op("dve", lambda e, S_=S_: e.tensor_scalar(out=S_[:, 8:16], in0=S_[:, 0:8], scalar1=1.0 / 64, scalar2=None, op0=ALU.mult), reads=[R_stt_ro[j]], writes=[R_stt_ro[j]])
                        k.op("dve", lambda e, j=j, S_=S_: e.tensor_tensor(out=v3(yc_ro[j]), in0=v3(y0_ro[j]), in1=bc(S_[:, 8:16]), op=ALU.subtract), reads=[R_y0_ro[j], R_stt_ro[j]], writes=[R_yc_ro[j]])
                        k.op("pool", lambda e, j=j: e.tensor_tensor(out=sq_ro[j][:], in0=yc_ro[j][:], in1=yc_ro[j][:], op=ALU.mult), reads=[R_yc_ro[j]], writes=[R_sq_ro[j]])
                        k.op("dve", lambda e, j=j, S_=S_: e.tensor_reduce(out=S_[:, 16:24], in_=v3(sq_ro[j]), axis=AX.X, op=ALU.add), reads=[R_sq_ro[j]], writes=[R_stt_ro[j]])
                        k.op("act", lambda e, S_=S_: e.activation(out=S_[:, 24:32], in_=S_[:, 16:24], func=AF.Sqrt, bias=c1_ro[:, 2:3], scale=1.0 / 64), reads=[R_stt_ro[j], R_par_ro], writes=[R_stt_ro[j]])
                        k.op("dve", lambda e, S_=S_: e.reciprocal(out=S_[:, 24:32], in_=S_[:, 24:32]), reads=[R_stt_ro[j]], writes=[R_stt_ro[j]])
                        k.op("dve", lambda e, j=j, S_=S_: e.tensor_tensor(out=v3(yc_ro[j]), in0=v3(yc_ro[j]), in1=bc(S_[:, 24:32]), op=ALU.mult), reads=[R_yc_ro[j], R_stt_ro[j]], writes=[R_yc_ro[j]])
                        k.op("pool", lambda e, j=j: e.tensor_tensor(out=yc_ro[j][:], in0=yc_ro[j][:], in1=lnw_ro[:], op=ALU.mult), reads=[R_yc_ro[j], R_par_ro], writes=[R_yc_ro[j]])
                        k.op("pool", lambda e, j=j: e.tensor_tensor(out=yc_ro[j][:], in0=yc_ro[j][:], in1=lnb_ro[:], op=ALU.add), reads=[R_yc_ro[j], R_par_ro], writes=[R_yc_ro[j]])
                        k.op("dve", lambda e, j=j: e.tensor_tensor(out=yc_ro[j][:], in0=yc_ro[j][:], in1=bo_ro[j][:], op=ALU.add), reads=[R_yc_ro[j], R_bo_ro[j]], writes=[R_yc_ro[j]])
                        k.op("dve", lambda e, j=j: e.tensor_tensor(out=yc_ro[j][:], in0=yc_ro[j][:], in1=gg_ro[j][:], op=ALU.mult), reads=[R_yc_ro[j], R_gg_ro[j]], writes=[R_yc_ro[j]])
                        pt, rp = ps_next()
                        for q in range(4):
                            k.op("pe", lambda e, pt=pt, q=q, j=j: e.transpose(pt[:, q * 128:(q + 1) * 128], yc_ro[j][:, q * 128:(q + 1) * 128], ident[:, :]), reads=[R_yc_ro[j], R_id], writes=[rp])
                        k.op("act", lambda e, pt=pt, j=j: e.activation(out=ot_ro[j][:].rearrange("p q t -> p (q t)"), in_=pt[:, :], func=AF.Copy), reads=[rp], writes=[R_ot_ro[j]])
                        k.dma("sp", YT[b][:, 0:4, t0:t0 + 128], ot_ro[j][:], reads=[R_ot_ro[j]], writes=[R_YT[b]])
            k.barrier()

        toks = []
        for l in range(n_layers):
            if stages is None or "norm1" in stages:
                stage_norm(l, 0, 0, HT, R_HT)
            if stages is None or "proj" in stages:
                stage_proj(l)
            last = (l == DEPTH - 1)
            if stages is None or "na" in stages:
                stage_na(l, not last, **na_kw)
            if stages is None or "rwkv" in stages:
                stage_rwkv(l, not last)
            if stages is None or "wout" in stages:
                stage_wout(l, last)
            if stages is None or "ffn" in stages:
                moe = (l % 2 == 1)
                tsel = range(LC, S, 256) if last else None
                if moe:
                    stage_norm(l, 1, 3, H2T, R_H2T, dst32=H2F, R_dst32=R_H2F, tsel=tsel)
                    stage_router(l, last)
                else:
                    stage_norm(l, 1, 3, H2T, R_H2T, tsel=tsel)
                stage_ffn(l, last)
        if stages is None or "final" in stages:
            fin_out = stage_final()
        else:
            fin_out = []

        fin = list(fin_out)
        for r in R_XT + R_HT + R_QK + R_VN + R_UT + R_YT + R_YD[0] + R_YD[1] + R_BON + R_GG:
            if r.w is not None:
                fin.append(r.w)
        k.final_wait("sp", fin)
        k.emit()
        print("insts", k.n_inst, "epochs", k.n_epochs)
    return nc


def _fm(a, nchunk):
    return np.ascontiguousarray(a.reshape(nchunk, 128, -1).transpose(1, 0, 2))


def _build_bfull(rpb):
    L, H = rpb.shape[:2]
    q = np.arange(64)[:, None]; kc = np.arange(64)[None, :]
    lo = np.clip(q - 8, 0, 48)
    valid = (kc >= lo) & (kc < lo + 16)
    idx = np.clip(kc - q + 15, 0, 30)
    g = rpb[:, :, :, idx]
    g = np.where(valid[None, None, None], g, np.float32(NEG)).astype(np.float32)
    return np.ascontiguousarray(g.transpose(0, 3, 1, 2, 4).reshape(L, 64, H, 960))


def _prep_shared(inp):
    m = {}
    m["ada_w"] = np.stack([_fm(inp["ada_w"][l], 8) for l in range(4)])
    m["ada_bT"] = np.stack([inp["ada_b"][l].reshape(48, 128).T.copy() for l in range(4)])
    m["g_mix"] = np.stack([inp["norm_mix_g"][l].reshape(8, 128).T.copy() for l in range(4)])
    m["g_ffn"] = np.stack([inp["norm_ffn_g"][l].reshape(8, 128).T.copy() for l in range(4)])
    m["w_in"] = np.stack([_fm(inp["w_in"][l], 8) for l in range(4)])
    m["mu"] = np.stack([inp["shift_mu"][l].reshape(15, 128).T.copy() for l in range(4)])
    m["ident"] = np.eye(128, dtype=np.float32)
    m["final_g"] = inp["final_g"].reshape(8, 128).T.copy()
    m["bfull"] = _build_bfull(inp["na_rpb"])
    m["w_out"] = np.stack([_fm(inp["w_out"][l], 8) for l in range(4)])
    m["rw_w2"] = inp["w2"].reshape(4, 128, 512); m["rw_a2"] = inp["a2"].reshape(4, 128, 512); m["rw_g2"] = inp["g2"]
    m["rw_a0T"] = np.stack([inp["a0"][l].reshape(2, 4, 128).transpose(2, 0, 1) for l in range(4)])
    c4 = lambda a: np.stack([a[l].reshape(4, 128).T for l in range(4)])
    m["rw_kk"] = c4(inp["k_k"]); m["rw_ka"] = c4(inp["k_a"]); m["rw_rk"] = c4(inp["r_k"].reshape(4, 512))
    m["rw_lnw"] = np.broadcast_to(inp["ln_x_w"][:, None, :], (4, 128, 512)); m["rw_lnb"] = np.broadcast_to(inp["ln_x_b"][:, None, :], (4, 128, 512))
    m["rw_w0b"] = np.broadcast_to(inp["w0"][:, None, :, :], (4, 64, 2, 512))
    idx = np.arange(64)
    strict = [(idx[:, None] < idx[None, :]), (idx[:, None] > idx[None, :])]
    incl = [(idx[:, None] <= idx[None, :]), (idx[:, None] >= idx[None, :])]
    m["rw_m2"] = np.stack([np.concatenate([strict[d], incl[d]], 1) for d in range(2)], 1).astype(np.float32)
    m["rw_m3"] = np.stack([np.concatenate([strict[d].astype(np.float32), -incl[d].astype(np.float32)], 1) for d in range(2)], 1)
    m["rw_mL"] = np.stack([strict[d].T for d in range(2)], 1).astype(np.float32)
    bo = np.zeros((128, 128), np.float32); bo[:64, :64] = 1; bo[64:, 64:] = 1
    m["rw_bones"] = bo
    se = np.zeros((128, 2), np.float32); se[:64, 0] = 1; se[64:, 1] = 1
    m["rw_sel"] = se
    m["rw_c1"] = np.tile(np.array([1.0, -0.5, 64e-5, 1e-12], np.float32)[None], (128, 1))
    m["ffn_w1"] = np.stack([_fm(inp["ffn_w1"][i], 8) for i in range(2)])
    m["ffn_w3"] = np.stack([_fm(inp["ffn_w3"][i], 8) for i in range(2)])
    m["ffn_w2"] = np.stack([_fm(inp["ffn_w2"][i], DFF // 128) for i in range(2)])
    m["router"] = np.stack([_fm(inp["router"][i], 8) for i in range(2)])
    m["moe_w1"] = np.stack([np.stack([_fm(inp["moe_w1"][i, e], 8) for e in range(NE)]) for i in range(2)])
    m["moe_w3"] = np.stack([np.stack([_fm(inp["moe_w3"][i, e], 8) for e in range(NE)]) for i in range(2)])
    m["moe_w2"] = np.stack([np.stack([_fm(inp["moe_w2"][i, e], DFE // 128) for e in range(NE)]) for i in range(2)])
    return {k_: np.ascontiguousarray(v, dtype=np.float32) for k_, v in m.items()}


def _prep_core(inp, core):
    bs = [2 * core, 2 * core + 1]
    m = {}
    xcat = [np.concatenate([inp["ctx"][b], inp["x"][b]], 0) for b in bs]
    m["xT"] = np.stack([_fm(np.ascontiguousarray(xc.T), 8) for xc in xcat]).astype(np.float32)
    cc = np.stack([inp["c"][bs[0]], inp["c"][bs[1]], inp["c_ctx"]], 1)
    m["cT"] = _fm(cc, 8).astype(np.float32)
    return m


def kernel(**inputs):
    inp = {k_: np.asarray(v) for k_, v in inputs.items()}
    n = 8
    shared = _prep_shared(inp)
    in_maps = []
    for core in range(n):
        m = dict(shared)
        m.update(_prep_core(inp, core))
        in_maps.append(m)
    nc = build_program()
    res = run_bass_kernel_spmd(nc, in_maps, core_ids=list(range(n)))
    outs = []
    for core in range(n):
        o = np.asarray(res.results[core]["out"])
        for b in range(NB):
            outs.append(o[b].transpose(1, 0, 2).reshape(D, T).T)
    return np.ascontiguousarray(np.stack(outs, 0)).astype(np.float32)
```

```python
import numpy as np
import concourse.bass as bass
import concourse.mybir as mybir
from concourse.bass_utils import run_bass_kernel_spmd
from contextlib import ExitStack

F32 = mybir.dt.float32
BF16 = mybir.dt.bfloat16
AF = mybir.ActivationFunctionType
ALU = mybir.AluOpType
AX = mybir.AxisListType

SAME_ENGINE_SYNC = True
DMA_RING = {"sp": 16, "act": 4, "pool": 16}
SEM_LIMIT = 60000
LOAD_Q = "pool"
MAX_EPOCHS = 7


class Res:
    __slots__ = ("name", "w", "rd")

    def __init__(self, name=""):
        self.name = name
        self.w = None
        self.rd = []


class K:
    ENG = ("pe", "act", "dve", "pool", "sp")
    CE = ("pe", "act", "dve", "pool")

    def __init__(self, nc, stack):
        self.nc = nc
        self.stack = stack
        self.recs = []
        self.cnt = {e: 0 for e in self.ENG}
        self.slots = []
        self.dq = {}
        for q in ("sp", "act", "pool"):
            idx = []
            for i in range(DMA_RING[q]):
                self.slots.append(0)
                idx.append(len(self.slots) - 1)
            self.dq[q] = {"idx": idx, "n": 0}
        self.waited_e = {e: {} for e in self.ENG}
        self.waited_d = {e: {} for e in self.ENG}
        self.n_inst = 0

    def _need(self, eng, tok, waits, force=False):
        if tok is None:
            return
        if tok[0] == "e":
            _, x, seq = tok
            if x == eng and (not SAME_ENGINE_SYNC or eng == "pe") and not force:
                return
            if self.waited_e[eng].get(x, 0) >= seq:
                return
            self.waited_e[eng][x] = seq
            waits.append(tok)
        else:
            _, si, use = tok
            if self.waited_d[eng].get(si, 0) >= use:
                return
            self.waited_d[eng][si] = use
            waits.append(tok)

    def _deps(self, eng, reads, writes):
        waits = []
        for r in reads:
            self._need(eng, r.w, waits)
        for w in writes:
            if not (w.w is not None and w.w[0] == "e" and w.w[1] == eng):
                self._need(eng, w.w, waits)
            best = {}
            for t in w.rd:
                if t[0] == "e" and t[1] == eng:
                    continue
                key = (t[0], t[1])
                if key not in best or best[key][2] < t[2]:
                    best[key] = t
            for t in best.values():
                self._need(eng, t, waits)
        return waits

    def _commit(self, tok, reads, writes):
        for r in reads:
            if r.rd and r.rd[-1][0] == tok[0] and r.rd[-1][1] == tok[1]:
                r.rd[-1] = tok
            else:
                r.rd.append(tok)
        for w in writes:
            w.w = tok
            w.rd = []

    def op(self, eng, fn, reads=(), writes=()):
        waits = self._deps(eng, reads, writes)
        self.cnt[eng] += 1
        tok = ("e", eng, self.cnt[eng])
        self.recs.append({"eng": eng, "waits": waits, "fn": fn, "tok": tok})
        self._commit(tok, reads, writes)
        self.n_inst += 1
        return tok

    def dma(self, q, out, in_, reads=(), writes=(), **kw):
        if q == "sp" and LOAD_Q != "sp" and "DRAM" not in str(out.space):
            q = LOAD_Q
        waits = self._deps(q, reads, writes)
        d = self.dq[q]
        si = d["idx"][d["n"] % len(d["idx"])]
        d["n"] += 1
        prev = self.slots[si]
        if prev > 0:
            self._need(q, ("d", si, prev), waits)
        self.slots[si] = prev + 1
        tok = ("d", si, prev + 1)
        fn = (lambda e, o=out, i=in_, kw=kw: e.dma_start(out=o, in_=i, **kw))
        self.recs.append({"eng": q, "waits": waits, "fn": fn, "tok": tok})
        self._commit(tok, reads, writes)
        self.n_inst += 1
        return tok

    def _all_waits(self, eng):
        waits = []
        for x in self.CE:
            if self.cnt[x] > 0:
                self._need(eng, ("e", x, self.cnt[x]), waits, force=True)
        for si, v in enumerate(self.slots):
            if v > 0:
                self._need(eng, ("d", si, v), waits)
        return waits

    def barrier(self):
        for eng in self.ENG:
            self.recs.append({"eng": eng, "waits": self._all_waits(eng), "fn": None, "tok": None})

    def final_wait(self, eng, toks):
        waits = []
        for t in toks:
            self._need(eng, t, waits)
        self.recs.append({"eng": eng, "waits": waits, "fn": None, "tok": None})

    def emit(self):
        nc, st = self.nc, self.stack
        recs = self.recs
        needed = set()
        for r in recs:
            for w in r["waits"]:
                if w[0] == "e":
                    needed.add((w[1], w[2]))
        out = []
        ep = 0
        ms = {e: 0 for e in self.CE}
        du = [0] * len(self.slots)
        last_e = {e: 0 for e in self.CE}
        last_d = [0] * len(self.slots)
        val = {}
        pend_last = {}

        def close_epoch():
            nonlocal ep, ms, du
            bw = {}
            for e in self.CE:
                if e in pend_last:
                    rr = out[pend_last[e]]
                    t = rr["tok"]
                    if t not in val:
                        ms[e] += 1
                        val[t] = (ep, ms[e])
                        rr["inc"] = True
            allw = []
            for e in self.CE:
                if e in pend_last:
                    allw.append(out[pend_last[e]]["tok"])
            for si in range(len(self.slots)):
                if du[si] > 0:
                    allw.append(("d", si, last_d[si]))
            for eng in self.ENG:
                out.append({"eng": eng, "waits": list(allw), "fn": None, "tok": None, "ep": ep})
            ep += 1
            ms = {e: 0 for e in self.CE}
            du = [0] * len(self.slots)
            pend_last.clear()

        for r in recs:
            t = r["tok"]
            if t is not None:
                if t[0] == "e":
                    if ms[t[1]] + 2 > SEM_LIMIT:
                        close_epoch()
                else:
                    if (du[t[1]] + 2) * 16 > SEM_LIMIT:
                        close_epoch()
            r = dict(r)
            r["ep"] = ep
            out.append(r)
            if t is not None:
                if t[0] == "e":
                    pend_last[t[1]] = len(out) - 1
                    if (t[1], t[2]) in needed:
                        ms[t[1]] += 1
                        val[t] = (ep, ms[t[1]])
                        r["inc"] = True
                    else:
                        r["inc"] = False
                else:
                    du[t[1]] += 1
                    last_d[t[1]] = t[2]
                    val[t] = (ep, du[t[1]] * 16)
        n_ep = ep + 1
        print("epochs needed", n_ep, "milestones", ms, "dma uses", max(du))
        assert n_ep <= MAX_EPOCHS, "too many epochs %d" % n_ep
        self.n_epochs = n_ep
        esem = [{e: st.enter_context(nc.semaphore("c%d_%s" % (i, e))) for e in self.CE} for i in range(n_ep)]
        dsem = [[st.enter_context(nc.semaphore("d%d_%d" % (i, j))) for j in range(len(self.slots))] for i in range(n_ep)]
        streams = {e: [] for e in self.ENG}
        for r in out:
            streams[r["eng"]].append(r)

        with nc.Block() as block:
            def run(engname, e):
                for ep_ in range(1, n_ep):
                    if engname in self.CE:
                        e.sem_clear(esem[ep_][engname])
                    if engname in self.dq:
                        for si in self.dq[engname]["idx"]:
                            e.sem_clear(dsem[ep_][si])
                for r in streams[engname]:
                    for w in r["waits"]:
                        v = val.get(w)
                        if v is None or v[0] != r["ep"]:
                            continue
                        if w[0] == "e":
                            e.wait_ge(esem[v[0]][w[1]], v[1])
                        else:
                            e.wait_ge(dsem[v[0]][w[1]], v[1])
                    if r["fn"] is None:
                        continue
                    ins = r["fn"](e)
                    t = r["tok"]
                    if t[0] == "e":
                        if r.get("inc"):
                            ins.then_inc(esem[r["ep"]][t[1]], 1)
                    else:
                        ins.then_inc(dsem[r["ep"]][t[1]], 16)

            @block.sync
            def _(e):
                run("sp", e)

            @block.tensor
            def _(e):
                run("pe", e)

            @block.vector
            def _(e):
                run("dve", e)

            @block.scalar
            def _(e):
                run("act", e)

            @block.gpsimd
            def _(e):
                run("pool", e)


D = 1024
DEPTH = 4
NB = 2
LC = 256
T = 2048
S = LC + T
NT = S // 128
D_IN = 3456
DFF = 2816
DFE = 3584
NE = 8
NEG = -30000.0

DEBUG_OUT = []
RW_DBG = {"nch": None, "upto": "Z", "nb": None, "nd": 2, "readout": True}
N_LAYERS_RUN = DEPTH


def build_program(n_layers=DEPTH, stages=None, debug=(), na_kw={}):
    nc = bass.Bass("TRN2", target_bir_lowering=False)
    st = ExitStack()
    with st:
        k = K(nc, st)

        def din(name, shape, dt=F32):
            return nc.dram_tensor(name, list(shape), dt, kind="ExternalInput").ap()

        def dscr(name, shape, dt=F32):
            kind = "ExternalOutput" if name in debug else "Internal"
            return nc.dram_tensor(name, list(shape), dt, kind=kind).ap()

        I = {}
        I["xT"] = din("xT", [NB, 128, 8, S])
        I["cT"] = din("cT", [128, 8, 3])
        I["ada_w"] = din("ada_w", [DEPTH, 128, 8, 6 * D])
        I["ada_bT"] = din("ada_bT", [DEPTH, 128, 48])
        I["g_mix"] = din("g_mix", [DEPTH, 128, 8])
        I["g_ffn"] = din("g_ffn", [DEPTH, 128, 8])
        I["w_in"] = din("w_in", [DEPTH, 128, 8, D_IN])
        I["mu"] = din("mu", [DEPTH, 128, 15])
        I["ident"] = din("ident", [128, 128])
        I["final_g"] = din("final_g", [128, 8])
        I["bfull"] = din("bfull", [DEPTH, 64, 8, 960])
        I["w_out"] = din("w_out", [DEPTH, 128, 8, D])
        I["rw_w2"] = din("rw_w2", [DEPTH, 128, 512]); I["rw_a2"] = din("rw_a2", [DEPTH, 128, 512]); I["rw_g2"] = din("rw_g2", [DEPTH, 128, 512])
        I["rw_a0T"] = din("rw_a0T", [DEPTH, 128, 2, 4]); I["rw_kk"] = din("rw_kk", [DEPTH, 128, 4]); I["rw_ka"] = din("rw_ka", [DEPTH, 128, 4]); I["rw_rk"] = din("rw_rk", [DEPTH, 128, 4])
        I["rw_lnw"] = din("rw_lnw", [DEPTH, 128, 512]); I["rw_lnb"] = din("rw_lnb", [DEPTH, 128, 512]); I["rw_w0b"] = din("rw_w0b", [DEPTH, 64, 2, 512])
        I["rw_m2"] = din("rw_m2", [64, 2, 128]); I["rw_m3"] = din("rw_m3", [64, 2, 128]); I["rw_mL"] = din("rw_mL", [64, 2, 64])
        I["rw_bones"] = din("rw_bones", [128, 128]); I["rw_sel"] = din("rw_sel", [128, 2]); I["rw_c1"] = din("rw_c1", [128, 4])
        I["ffn_w1"] = din("ffn_w1", [2, 128, 8, DFF]); I["ffn_w3"] = din("ffn_w3", [2, 128, 8, DFF]); I["ffn_w2"] = din("ffn_w2", [2, 128, DFF // 128, D])
        I["router"] = din("router", [2, 128, 8, NE])
        I["moe_w1"] = din("moe_w1", [2, NE, 128, 8, DFE]); I["moe_w3"] = din("moe_w3", [2, NE, 128, 8, DFE]); I["moe_w2"] = din("moe_w2", [2, NE, 128, DFE // 128, D])
        out = nc.dram_tensor("out", [NB, 128, 8, T], F32, kind="ExternalOutput").ap()

        XT = [dscr("xt%d" % b, [128, 8, S]) for b in range(NB)]
        HT = [dscr("ht%d" % b, [128, 8, S], BF16) for b in range(NB)]
        QT = [dscr("qt%d" % b, [128, 4, S], BF16) for b in range(NB)]
        KT = [dscr("kt%d" % b, [128, 4, S], BF16) for b in range(NB)]
        VN = [dscr("vn%d" % b, [S, 512], BF16) for b in range(NB)]
        UT = [dscr("ut%d" % b, [128, 15, S]) for b in range(NB)]
        R_XT = [Res() for _ in range(NB)]
        R_HT = [Res() for _ in range(NB)]
        R_QK = [Res() for _ in range(NB)]
        R_VN = [Res() for _ in range(NB)]
        R_UT = [Res() for _ in range(NB)]
        YT = [dscr("yt%d" % b, [128, 8, S], BF16) for b in range(NB)]
        H2T = [dscr("h2t%d" % b, [128, 8, S], BF16) for b in range(NB)]
        H2F = [dscr("h2f%d" % b, [128, 8, S]) for b in range(NB)]
        GB = [dscr("gb%d" % b, [128, NE, S]) for b in range(NB)]
        YD = [[dscr("yd%d_%d" % (d, b), [S, 512]) for b in range(NB)] for d in range(2)]
        R_YD = [[Res() for b in range(NB)] for d in range(2)]
        BON = [dscr("bon%d" % b, [S, 512]) for b in range(NB)]; R_BON = [Res() for _ in range(NB)]
        GG = [dscr("gg%d" % b, [S, 512]) for b in range(NB)]; R_GG = [Res() for _ in range(NB)]
        R_H2T = [Res() for _ in range(NB)]; R_H2F = [Res() for _ in range(NB)]; R_GB = [Res() for _ in range(NB)]
        R_YT = [Res() for _ in range(NB)]

        uid = [0]

        def sb(name, shape, dt=F32, stack=st):
            uid[0] += 1
            return stack.enter_context(nc.sbuf_tensor("%s_%d" % (name, uid[0]), list(shape), dt))

        ident = sb("ident_sb", [128, 128]); R_id = Res()
        ones = sb("ones_sb", [128, 128]); R_ones = Res()
        MOD = sb("mod_sb", [128, DEPTH, 48, 3]); R_mod = Res()
        GS = sb("gs_sb", [128, DEPTH, 2, 8, 3]); R_gs = Res()
        gmix = sb("gmix_sb", [128, DEPTH, 8]); gffn = sb("gffn_sb", [128, DEPTH, 8]); R_g = Res()
        eps_t = sb("eps_sb", [128, 1]); R_eps = Res()
        psum = [st.enter_context(nc.psum_tensor("ps%d" % i, [128, 512], F32)) for i in range(8)]
        R_ps = [Res() for _ in range(8)]
        pctr = [0]

        def ps_next():
            i = pctr[0] % 8
            pctr[0] += 1
            return psum[i], R_ps[i]

        k.dma("sp", ident[:], I["ident"][:, :], writes=[R_id])
        ident_bf = sb("identbf_sb", [128, 128], BF16)
        k.op("dve", lambda e: e.tensor_copy(out=ident_bf[:], in_=ident[:]), reads=[R_id], writes=[R_id])
        k.op("dve", lambda e: e.memset(ones[:], 1.0), writes=[R_ones])
        k.op("dve", lambda e: e.memset(eps_t[:], 1e-6), writes=[R_eps])
        for l in range(DEPTH):
            k.dma("sp", gmix[:, l, :], I["g_mix"][l], writes=[R_g])
            k.dma("sp", gffn[:, l, :], I["g_ffn"][l], writes=[R_g])

        with ExitStack() as s1:
            cT = sb("cT_sb", [128, 8, 3], F32, s1); R_c = Res()
            sT = sb("sT", [128, 8, 3], F32, s1); R_s = Res()
            abT = sb("abT", [128, 48], F32, s1); R_ab = Res()
            aw = [sb("aw%d" % i, [128, 8, 512], F32, s1) for i in range(2)]
            R_aw = [Res(), Res()]
            xcp = [sb("xcp%d" % i, [128, 8, 256], F32, s1) for i in range(2)]
            R_xcp = [Res(), Res()]
            k.dma("sp", cT[:], I["cT"][:, :, :], writes=[R_c])
            k.op("act", lambda e: e.activation(out=sT[:], in_=cT[:], func=AF.Silu), reads=[R_c], writes=[R_s])
            n = 0
            for b in range(NB):
                for t0 in range(0, S, 256):
                    j = n % 2; n += 1
                    k.dma("sp", xcp[j][:], I["xT"][b, :, :, t0:t0 + 256], writes=[R_xcp[j]])
                    k.dma("sp", XT[b][:, :, t0:t0 + 256], xcp[j][:], reads=[R_xcp[j]], writes=[R_XT[b]])
            n = 0
            for l in range(n_layers):
                k.dma("sp", abT[:], I["ada_bT"][l], writes=[R_ab])
                for cc in range(12):
                    j = n % 2; n += 1
                    k.dma("sp", aw[j][:], I["ada_w"][l, :, :, cc * 512:(cc + 1) * 512], writes=[R_aw[j]])
                    pt, rp = ps_next()
                    for c4 in range(4):
                        for kc in range(8):
                            k.op("pe", lambda e, pt=pt, j=j, c4=c4, kc=kc: e.matmul(
                                pt[:, c4 * 3:c4 * 3 + 3], aw[j][:, kc, c4 * 128:(c4 + 1) * 128], sT[:, kc, :],
                                start=(kc == 0), stop=(kc == 7)), reads=[R_aw[j], R_s], writes=[rp])
                    k.op("dve", lambda e, pt=pt, l=l, cc=cc: e.tensor_tensor(
                        out=MOD[:, l, cc * 4:(cc + 1) * 4, :], in0=pt[:, 0:12].rearrange("p (a b) -> p a b", b=3),
                        in1=abT[:, cc * 4:(cc + 1) * 4].unsqueeze(2).to_broadcast([128, 4, 3]), op=ALU.add),
                        reads=[rp, R_ab], writes=[R_mod])
                for which, (gt, mi) in enumerate(((gmix, 1), (gffn, 4))):
                    k.op("dve", lambda e, l=l, which=which, gt=gt, mi=mi: e.scalar_tensor_tensor(
                        out=GS[:, l, which, :, :], in0=MOD[:, l, mi * 8:(mi + 1) * 8, :], scalar=1.0,
                        in1=gt[:, l, :].unsqueeze(2).to_broadcast([128, 8, 3]), op0=ALU.add, op1=ALU.mult),
                        reads=[R_mod, R_g], writes=[R_gs])
        k.barrier()

        def which_of(b, t0):
            return 2 if t0 < LC else b

        def stage_norm(l, which, shift_idx, dst, R_dst, gsel=None, dst32=None, R_dst32=None, bsel=range(NB), tsel=None):
            with ExitStack() as s1:
                xb = [sb("nx%d" % i, [128, 8, 256], F32, s1) for i in range(2)]; R_xb = [Res(), Res()]
                sq = [sb("nsq%d" % i, [128, 8, 256], F32, s1) for i in range(2)]; R_sq = [Res(), Res()]
                rs = [sb("nrs%d" % i, [128, 256], F32, s1) for i in range(2)]; R_rs = [Res(), Res()]
                tm = [sb("ntm%d" % i, [128, 8, 256], F32, s1) for i in range(2)]; R_tm = [Res(), Res()]
                hb = [sb("nhb%d" % i, [128, 8, 256], BF16, s1) for i in range(2)]; R_hb = [Res(), Res()]
                n = 0
                for b in bsel:
                    for t0 in (tsel if tsel is not None else range(0, S, 256)):
                        j = n % 2; n += 1
                        w = which_of(b, t0)
                        k.dma("sp", xb[j][:], XT[b][:, :, t0:t0 + 256], reads=[R_XT[b]], writes=[R_xb[j]])
                        k.op("act", lambda e, j=j: e.activation(out=sq[j][:], in_=xb[j][:], func=AF.Square), reads=[R_xb[j]], writes=[R_sq[j]])
                        pt, rp = ps_next()
                        for c in range(8):
                            k.op("pe", lambda e, pt=pt, j=j, c=c: e.matmul(pt[:, 0:256], ones[:], sq[j][:, c, :], start=(c == 0), stop=(c == 7)),
                                 reads=[R_ones, R_sq[j]], writes=[rp])
                        k.op("act", lambda e, pt=pt, j=j: e.activation(out=rs[j][:], in_=pt[:, 0:256], func=AF.Sqrt, bias=eps_t[:, 0:1], scale=1.0 / D),
                             reads=[rp, R_eps], writes=[R_rs[j]])
                        k.op("dve", lambda e, j=j: e.reciprocal(out=rs[j][:], in_=rs[j][:]), reads=[R_rs[j]], writes=[R_rs[j]])
                        for c in range(8):
                            k.op("dve", lambda e, j=j, c=c, w=w: e.scalar_tensor_tensor(
                                out=tm[j][:, c, :], in0=xb[j][:, c, :], scalar=GS[:, l, which, c, w:w + 1], in1=rs[j][:],
                                op0=ALU.mult, op1=ALU.mult), reads=[R_xb[j], R_gs, R_rs[j]], writes=[R_tm[j]])
                            k.op("act", lambda e, j=j, c=c, w=w: e.activation(
                                out=hb[j][:, c, :], in_=tm[j][:, c, :], func=AF.Identity, bias=MOD[:, l, shift_idx * 8 + c, w:w + 1], scale=1.0),
                                reads=[R_tm[j], R_mod], writes=[R_hb[j]])
                            if dst32 is not None:
                                k.op("pool", lambda e, j=j, c=c, w=w: e.tensor_scalar(
                                    out=tm[j][:, c, :], in0=tm[j][:, c, :], scalar1=MOD[:, l, shift_idx * 8 + c, w:w + 1], scalar2=None, op0=ALU.add),
                                    reads=[R_tm[j], R_mod, R_hb[j]], writes=[R_tm[j]])
                        k.dma("sp", dst[b][:, :, t0:t0 + 256], hb[j][:], reads=[R_hb[j]], writes=[R_dst[b]])
                        if dst32 is not None:
                            k.dma("sp", dst32[b][:, :, t0:t0 + 256], tm[j][:], reads=[R_tm[j]], writes=[R_dst32[b]])
            k.barrier()

        def stage_proj(l):
            with ExitStack() as s1:
                w = sb("pw", [128, 8, D_IN], BF16, s1); R_w = Res()
                mu = sb("pmu", [128, 15], F32, s1); R_mu = Res()
                mu1 = sb("pmu1", [128, 15], F32, s1)
                muh = sb("pmuh", [128, 15], F32, s1)
                hb = [sb("ph%d" % i, [128, 8, 258], BF16, s1) for i in range(2)]; R_hb = [Res(), Res()]
                hs = [sb("phs%d" % i, [128, 8, 256], BF16, s1) for i in range(2)]; R_hs = [Res(), Res()]
                oq = [sb("poq%d" % i, [128, 8, 256], BF16, s1) for i in range(2)]; R_oq = [Res(), Res()]
                ov = [sb("pov%d" % i, [128, 2, 512], BF16, s1) for i in range(2)]; R_ov = [Res(), Res()]
                ou = [sb("pou%d" % i, [128, 15, 256], F32, s1) for i in range(2)]; R_ou = [Res(), Res()]
                t2 = [sb("pt2%d" % i, [128, 256], F32, s1) for i in range(2)]; R_t2 = [Res(), Res()]
                for kc in range(8):
                    k.dma("pool", w[:, kc, :], I["w_in"][l, :, kc, :], writes=[R_w])
                k.dma("sp", mu[:], I["mu"][l], writes=[R_mu])
                k.op("dve", lambda e: e.tensor_scalar(out=mu1[:], in0=mu[:], scalar1=-1.0, scalar2=1.0, op0=ALU.mult, op1=ALU.add), reads=[R_mu], writes=[R_mu])
                k.op("dve", lambda e: e.tensor_scalar(out=muh[:], in0=mu[:], scalar1=0.5, scalar2=None, op0=ALU.mult), reads=[R_mu], writes=[R_mu])
                n = 0
                for b in range(NB):
                    for t0 in range(0, S, 256):
                        j = n % 2; n += 1
                        seg0, seg1 = (0, LC) if t0 < LC else (LC, S)
                        lo, hi = max(t0 - 1, seg0), min(t0 + 257, seg1)
                        if lo == t0 or hi == t0 + 256:
                            k.op("dve", lambda e, j=j: e.memset(hb[j][:], 0.0), writes=[R_hb[j]])
                        k.dma("sp", hb[j][:, :, lo - (t0 - 1):hi - (t0 - 1)], HT[b][:, :, lo:hi], reads=[R_HT[b]], writes=[R_hb[j]])
                        k.op("dve", lambda e, j=j: e.tensor_tensor(out=hs[j][:], in0=hb[j][:, :, 0:256], in1=hb[j][:, :, 2:258], op=ALU.add),
                             reads=[R_hb[j]], writes=[R_hs[j]])
                        for cj in range(8):
                            pt, rp = ps_next()
                            for kc in range(8):
                                k.op("pe", lambda e, pt=pt, j=j, cj=cj, kc=kc: e.matmul(pt[:, 0:256], w[:, kc, cj * 128:(cj + 1) * 128], hb[j][:, kc, 1:257],
                                     start=(kc == 0), stop=(kc == 7)), reads=[R_w, R_hb[j]], writes=[rp])
                            k.op("act", lambda e, pt=pt, j=j, cj=cj: e.activation(out=oq[j][:, cj, :], in_=pt[:, 0:256], func=AF.Copy), reads=[rp], writes=[R_oq[j]])
                        k.dma("sp", QT[b][:, :, t0:t0 + 256], oq[j][:, 0:4, :], reads=[R_oq[j]], writes=[R_QK[b]])
                        k.dma("sp", KT[b][:, :, t0:t0 + 256], oq[j][:, 4:8, :], reads=[R_oq[j]], writes=[R_QK[b]])
                        for tt in range(2):
                            pt, rp = ps_next()
                            for kc in range(8):
                                k.op("pe", lambda e, pt=pt, j=j, tt=tt, kc=kc: e.matmul(pt[:, :], hb[j][:, kc, 1 + tt * 128:1 + (tt + 1) * 128], w[:, kc, 1024:1536],
                                     start=(kc == 0), stop=(kc == 7)), reads=[R_w, R_hb[j]], writes=[rp])
                            k.op("act", lambda e, pt=pt, j=j, tt=tt: e.activation(out=ov[j][:, tt, :], in_=pt[:, :], func=AF.Copy), reads=[rp], writes=[R_ov[j]])
                        k.dma("sp", VN[b][t0:t0 + 256, :].rearrange("(a p) c -> p a c", p=128), ov[j][:], reads=[R_ov[j]], writes=[R_VN[b]])
                        for cj in range(15):
                            c0 = 1536 + cj * 128
                            pa, ra = ps_next()
                            for kc in range(8):
                                k.op("pe", lambda e, pa=pa, j=j, c0=c0, kc=kc: e.matmul(pa[:, 0:256], w[:, kc, c0:c0 + 128], hb[j][:, kc, 1:257],
                                     start=(kc == 0), stop=(kc == 7)), reads=[R_w, R_hb[j]], writes=[ra])
                            pb, rb = ps_next()
                            for kc in range(8):
                                k.op("pe", lambda e, pb=pb, j=j, c0=c0, kc=kc: e.matmul(pb[:, 0:256], w[:, kc, c0:c0 + 128], hs[j][:, kc, :],
                                     start=(kc == 0), stop=(kc == 7)), reads=[R_w, R_hs[j]], writes=[rb])
                            k.op("act", lambda e, pb=pb, j=j, cj=cj: e.activation(out=t2[j][:], in_=pb[:, 0:256], func=AF.Copy, scale=muh[:, cj:cj + 1]),
                                 reads=[rb, R_mu], writes=[R_t2[j]])
                            k.op("dve", lambda e, pa=pa, j=j, cj=cj: e.scalar_tensor_tensor(out=ou[j][:, cj, :], in0=pa[:, 0:256], scalar=mu1[:, cj:cj + 1], in1=t2[j][:],
                                 op0=ALU.mult, op1=ALU.add), reads=[ra, R_mu, R_t2[j]], writes=[R_ou[j]])
                        k.dma("sp", UT[b][:, :, t0:t0 + 256], ou[j][:], reads=[R_ou[j]], writes=[R_UT[b]])
            k.barrier()

        def stage_na(l, need_ctx, hsel=range(8), bsel=range(NB)):
            NBUF = 3
            with ExitStack() as s1:
                bias = sb("na_bias", [128, 8, 960], F32, s1); R_bias = Res()
                k.dma("sp", bias[0:64], I["bfull"][l], writes=[R_bias])
                k.dma("sp", bias[64:128], I["bfull"][l], writes=[R_bias])
                qh = [sb("na_q%d" % i, [64, S], BF16, s1) for i in range(2)]
                kh = [sb("na_k%d" % i, [64, S], BF16, s1) for i in range(2)]
                VE = [sb("na_ve%d" % i, [128, 18, 64], BF16, s1) for i in range(2)]
                VO = [sb("na_vo%d" % i, [128, 18, 64], BF16, s1) for i in range(2)]
                R_in = [Res(), Res()]
                ytok = sb("na_ytok", [128, NT, 512], BF16, s1); R_ytok = Res()
                yfm = [sb("na_yfm%d" % i, [128, 4, 128], BF16, s1) for i in range(2)]; R_yfm = [Res(), Res()]
                ssb = [sb("na_s%d" % i, [128, 832], F32, s1) for i in range(NBUF)]; R_s = [Res() for _ in range(NBUF)]
                pbf = [sb("na_p%d" % i, [128, 832], BF16, s1) for i in range(NBUF)]; R_p = [Res() for _ in range(NBUF)]
                pT = [sb("na_pT%d" % i, [128, 896], BF16, s1) for i in range(NBUF)]; R_pT = [Res() for _ in range(NBUF)]
                st4 = [sb("na_st%d" % i, [128, 4], F32, s1) for i in range(NBUF)]; R_st = [Res() for _ in range(NBUF)]
                n = 0
                u = 0
                for b in bsel:
                    if not need_ctx:
                        pass
                    for h in hsel:
                        j = n % 2; n += 1
                        p0 = (h % 2) * 64
                        k.dma("sp", qh[j][:], QT[b][p0:p0 + 64, h // 2, :], reads=[R_QK[b]], writes=[R_in[j]])
                        k.dma("sp", kh[j][:], KT[b][p0:p0 + 64, h // 2, :], reads=[R_QK[b]], writes=[R_in[j]])
                        k.dma("sp", VE[j][:], VN[b][:, h * 64:(h + 1) * 64].rearrange("(a p) d -> p a d", p=128), reads=[R_VN[b]], writes=[R_in[j]])
                        k.dma("sp", VO[j][:, 0:17, :], VN[b][64:64 + 17 * 128, h * 64:(h + 1) * 64].rearrange("(a p) d -> p a d", p=128), reads=[R_VN[b]], writes=[R_in[j]])
                        k.dma("sp", VO[j][0:64, 17, :], VN[b][S - 64:S, h * 64:(h + 1) * 64], reads=[R_VN[b]], writes=[R_in[j]])
                        units = [("lat", r) for r in range(0, 32, 2)] + ([("ctx", 0), ("ctx", 1)] if need_ctx else [])
                        for kind, r in units:
                            i = u % NBUF; u += 1
                            b1, rb1 = ps_next()
                            if kind == "lat":
                                r0A = min(max(r - 4, 0), 24); r0B = min(max(r - 3, 0), 24)
                                R0 = min(r0A, 23)
                                tq = LC + r * 64; w0 = LC + R0 * 64
                                tile_i = tq // 128
                                b2, rb2 = ps_next()
                                k.op("pe", lambda e, b1=b1, j=j, tq=tq: e.matmul(b1[:, 0:256], qh[j][:, tq:tq + 128], kh[j][:, 0:256], start=True, stop=True), reads=[R_in[j]], writes=[rb1])
                                k.op("pe", lambda e, b1=b1, j=j, tq=tq, w0=w0: e.matmul(b1[:, 256:512], qh[j][:, tq:tq + 128], kh[j][:, w0:w0 + 256], start=True, stop=True), reads=[R_in[j]], writes=[rb1])
                                k.op("pe", lambda e, b2=b2, j=j, tq=tq, w0=w0: e.matmul(b2[:, 0:320], qh[j][:, tq:tq + 128], kh[j][:, w0 + 256:w0 + 576], start=True, stop=True), reads=[R_in[j]], writes=[rb2])
                                k.op("act", lambda e, b1=b1, i=i: e.activation(out=ssb[i][:, 0:256], in_=b1[:, 0:256], func=AF.Copy, scale=0.125), reads=[rb1], writes=[R_s[i]])
                                for half, (rq, r0X) in enumerate(((r, r0A), (r + 1, r0B))):
                                    sX = r0X - R0
                                    hp_ = slice(half * 64, (half + 1) * 64)
                                    bs0 = (r0X - rq + 7) * 64
                                    n1 = 256 - sX * 64
                                    k.op("dve", lambda e, b1=b1, i=i, h=h, hp_=hp_, sX=sX, bs0=bs0, n1=n1: e.scalar_tensor_tensor(
                                        out=ssb[i][hp_, 256 + sX * 64:512], in0=b1[hp_, 256 + sX * 64:512], scalar=0.125, in1=bias[hp_, h, bs0:bs0 + n1], op0=ALU.mult, op1=ALU.add),
                                        reads=[rb1, R_bias], writes=[R_s[i]])
                                    n2 = 512 - n1
                                    k.op("dve", lambda e, b2=b2, i=i, h=h, hp_=hp_, bs0=bs0, n1=n1, n2=n2: e.scalar_tensor_tensor(
                                        out=ssb[i][hp_, 512:512 + n2], in0=b2[hp_, 0:n2], scalar=0.125, in1=bias[hp_, h, bs0 + n1:bs0 + 512], op0=ALU.mult, op1=ALU.add),
                                        reads=[rb2, R_bias], writes=[R_s[i]])
                                    ng0 = 256 + (512 if sX == 0 else 0)
                                    k.op("pool", lambda e, i=i, hp_=hp_, ng0=ng0: e.memset(ssb[i][hp_, ng0:ng0 + 64], NEG), writes=[R_s[i]])
                                W = 832
                            else:
                                tile_i = r
                                k.op("pe", lambda e, b1=b1, j=j, r=r: e.matmul(b1[:, 0:256], qh[j][:, r * 128:(r + 1) * 128], kh[j][:, 0:256], start=True, stop=True), reads=[R_in[j]], writes=[rb1])
                                k.op("act", lambda e, b1=b1, i=i: e.activation(out=ssb[i][:, 0:256], in_=b1[:, 0:256], func=AF.Copy, scale=0.125), reads=[rb1], writes=[R_s[i]])
                                W = 256
                            k.op("dve", lambda e, i=i, W=W: e.tensor_reduce(out=st4[i][:, 0:1], in_=ssb[i][:, 0:W], axis=AX.X, op=ALU.max), reads=[R_s[i]], writes=[R_st[i]])
                            k.op("dve", lambda e, i=i: e.tensor_scalar(out=st4[i][:, 1:2], in0=st4[i][:, 0:1], scalar1=-1.0, scalar2=None, op0=ALU.mult), reads=[R_st[i]], writes=[R_st[i]])
                            k.op("act", lambda e, i=i, W=W: e.activation(out=pbf[i][:, 0:W], in_=ssb[i][:, 0:W], func=AF.Exp, bias=st4[i][:, 1:2], scale=1.0, accum_out=st4[i][:, 2:3]),
                                 reads=[R_s[i], R_st[i]], writes=[R_p[i], R_st[i]])
                            k.op("dve", lambda e, i=i: e.reciprocal(out=st4[i][:, 3:4], in_=st4[i][:, 2:3]), reads=[R_st[i]], writes=[R_st[i]])
                            pc, rc = ps_next()
                            pcb = pc[:].bitcast(BF16)
                            nch = (W + 127) // 128
                            for c in range(nch):
                                kw = min(128, W - c * 128)
                                k.op("pe", lambda e, pcb=pcb, i=i, c=c, kw=kw: e.transpose(pcb[0:kw, c * 128:(c + 1) * 128], pbf[i][:, c * 128:c * 128 + kw], ident_bf[:, :]),
                                     reads=[R_p[i], R_id], writes=[rc])
                            k.op("dve" if kind == "lat" else "act",
                                 (lambda e, pcb=pcb, i=i, nch=nch: e.tensor_copy(out=pT[i][:, 0:nch * 128], in_=pcb[:, 0:nch * 128])) if kind == "lat" else
                                 (lambda e, pcb=pcb, i=i, nch=nch: e.activation(out=pT[i][:, 0:nch * 128], in_=pcb[:, 0:nch * 128], func=AF.Copy)),
                                 reads=[rc], writes=[R_pT[i]])
                            pd, rd = ps_next()
                            for c in range(nch):
                                kw = min(128, W - c * 128)
                                if c < 2:
                                    vt = VE[j][:, c, :]
                                else:
                                    even = (R0 % 2 == 0)
                                    base_t = (w0 // 128) if even else ((w0 - 64) // 128)
                                    vt = (VE[j] if even else VO[j])[0:kw, base_t + (c - 2), :]
                                k.op("pe", lambda e, pd=pd, i=i, c=c, kw=kw, vt=vt, nch=nch: e.matmul(pd[:, 0:64], pT[i][0:kw, c * 128:(c + 1) * 128], vt, start=(c == 0), stop=(c == nch - 1)),
                                     reads=[R_in[j], R_pT[i]], writes=[rd])
                            k.op("act", lambda e, pd=pd, i=i, tile_i=tile_i, h=h: e.activation(out=ytok[:, tile_i, h * 64:(h + 1) * 64], in_=pd[:, 0:64], func=AF.Copy, scale=st4[i][:, 3:4]),
                                 reads=[rd, R_st[i]], writes=[R_ytok])
                    for tt in range(0 if need_ctx else 2, NT):
                        jj = tt % 2
                        pt, rp = ps_next()
                        ptb = pt[:].bitcast(BF16)
                        for q in range(4):
                            k.op("pe", lambda e, ptb=ptb, q=q, tt=tt: e.transpose(ptb[:, q * 128:(q + 1) * 128], ytok[:, tt, q * 128:(q + 1) * 128], ident_bf[:, :]), reads=[R_ytok, R_id], writes=[rp])
                        k.op("act", lambda e, ptb=ptb, jj=jj: e.activation(out=yfm[jj][:].rearrange("p q t -> p (q t)"), in_=ptb[:, 0:512], func=AF.Copy), reads=[rp], writes=[R_yfm[jj]])
                        k.dma("sp", YT[b][:, 4:8, tt * 128:(tt + 1) * 128], yfm[jj][:], reads=[R_yfm[jj]], writes=[R_YT[b]])
            k.barrier()

        def stage_wout(l, last):
            with ExitStack() as s1:
                w = sb("wo_w", [128, 8, D], BF16, s1); R_w = Res()
                for kc in range(8):
                    k.dma("pool", w[:, kc, :], I["w_out"][l, :, kc, :], writes=[R_w])
                yb = [sb("wo_y%d" % i, [128, 8, 256], BF16, s1) for i in range(2)]; R_yb = [Res(), Res()]
                xb = [sb("wo_x%d" % i, [128, 8, 256], F32, s1) for i in range(2)]; R_xb = [Res(), Res()]
                n = 0
                for b in range(NB):
                    for t0 in range(LC if last else 0, S, 256):
                        j = n % 2; n += 1
                        wq = which_of(b, t0)
                        k.dma("sp", yb[j][:], YT[b][:, :, t0:t0 + 256], reads=[R_YT[b]], writes=[R_yb[j]])
                        k.dma("sp", xb[j][:], XT[b][:, :, t0:t0 + 256], reads=[R_XT[b]], writes=[R_xb[j]])
                        for oc in range(8):
                            pt, rp = ps_next()
                            for kc in range(8):
                                k.op("pe", lambda e, pt=pt, j=j, oc=oc, kc=kc: e.matmul(pt[:, 0:256], w[:, kc, oc * 128:(oc + 1) * 128], yb[j][:, kc, :],
                                     start=(kc == 0), stop=(kc == 7)), reads=[R_w, R_yb[j]], writes=[rp])
                            k.op("dve", lambda e, pt=pt, j=j, oc=oc, wq=wq: e.scalar_tensor_tensor(out=xb[j][:, oc, :], in0=pt[:, 0:256],
                                 scalar=MOD[:, l, 16 + oc, wq:wq + 1], in1=xb[j][:, oc, :], op0=ALU.mult, op1=ALU.add), reads=[rp, R_mod, R_xb[j]], writes=[R_xb[j]])
                        k.dma("sp", XT[b][:, :, t0:t0 + 256], xb[j][:], reads=[R_xb[j]], writes=[R_XT[b]])
            k.barrier()

        def stage_router(l, last):
            i_moe = l // 2
            with ExitStack() as s1:
                rw = sb("rt_w", [128, 8, NE], F32, s1); R_rw = Res()
                k.dma("sp", rw[:], I["router"][i_moe], writes=[R_rw])
                hb = [sb("rt_h%d" % i, [128, 8, 128], F32, s1) for i in range(2)]; R_hb = [Res(), Res()]
                lg = [sb("rt_l%d" % i, [128, 40], F32, s1) for i in range(2)]; R_lg = [Res(), Res()]
                ge = [sb("rt_ge%d" % i, [128, 128], F32, s1) for i in range(2)]; R_ge = [Res(), Res()]
                gb = [sb("rt_gb%d" % i, [128, NE, 128], F32, s1) for i in range(2)]; R_gb = [Res(), Res()]
                n = 0; m = 0
                for b in range(NB):
                    for tt in range(2 if last else 0, NT):
                        j = n % 2; n += 1
                        t0 = tt * 128
                        k.dma("sp", hb[j][:], H2F[b][:, :, t0:t0 + 128], reads=[R_H2F[b]], writes=[R_hb[j]])
                        pt, rp = ps_next()
                        for kc in range(8):
                            k.op("pe", lambda e, pt=pt, j=j, kc=kc: e.matmul(pt[:, 0:NE], hb[j][:, kc, :], rw[:, kc, :], start=(kc == 0), stop=(kc == 7)),
                                 reads=[R_hb[j], R_rw], writes=[rp])
                        L = lg[j]
                        k.op("dve", lambda e, pt=pt, L=L: e.tensor_copy(out=L[:, 0:8], in_=pt[:, 0:8]), reads=[rp], writes=[R_lg[j]])
                        k.op("dve", lambda e, L=L: e.tensor_reduce(out=L[:, 8:9], in_=L[:, 0:8], axis=AX.X, op=ALU.max), reads=[R_lg[j]], writes=[R_lg[j]])
                        k.op("dve", lambda e, L=L: e.tensor_scalar(out=L[:, 9:17], in0=L[:, 0:8], scalar1=L[:, 8:9], scalar2=None, op0=ALU.is_ge), reads=[R_lg[j]], writes=[R_lg[j]])
                        k.op("dve", lambda e, L=L: e.scalar_tensor_tensor(out=L[:, 17:25], in0=L[:, 9:17], scalar=-1e30, in1=L[:, 0:8], op0=ALU.mult, op1=ALU.add), reads=[R_lg[j]], writes=[R_lg[j]])
                        k.op("dve", lambda e, L=L: e.tensor_reduce(out=L[:, 25:26], in_=L[:, 17:25], axis=AX.X, op=ALU.max), reads=[R_lg[j]], writes=[R_lg[j]])
                        k.op("dve", lambda e, L=L: e.tensor_scalar(out=L[:, 26:34], in0=L[:, 17:25], scalar1=L[:, 25:26], scalar2=None, op0=ALU.is_ge), reads=[R_lg[j]], writes=[R_lg[j]])
                        k.op("dve", lambda e, L=L: e.tensor_tensor(out=L[:, 34:35], in0=L[:, 8:9], in1=L[:, 25:26], op=ALU.subtract), reads=[R_lg[j]], writes=[R_lg[j]])
                        k.op("act", lambda e, L=L: e.activation(out=L[:, 35:36], in_=L[:, 34:35], func=AF.Sigmoid), reads=[R_lg[j]], writes=[R_lg[j]])
                        k.op("dve", lambda e, L=L: e.tensor_scalar(out=L[:, 36:37], in0=L[:, 35:36], scalar1=-1.0, scalar2=1.0, op0=ALU.mult, op1=ALU.add), reads=[R_lg[j]], writes=[R_lg[j]])
                        k.op("dve", lambda e, L=L: e.tensor_scalar(out=L[:, 9:17], in0=L[:, 9:17], scalar1=L[:, 35:36], scalar2=None, op0=ALU.mult), reads=[R_lg[j]], writes=[R_lg[j]])
                        k.op("dve", lambda e, L=L: e.scalar_tensor_tensor(out=L[:, 9:17], in0=L[:, 26:34], scalar=L[:, 36:37], in1=L[:, 9:17], op0=ALU.mult, op1=ALU.add), reads=[R_lg[j]], writes=[R_lg[j]])
                        for ex in range(NE):
                            i2 = m % 2; m += 1
                            k.op("pool", lambda e, L=L, i2=i2, ex=ex: e.tensor_copy(out=ge[i2][:], in_=L[:, 9 + ex:10 + ex].to_broadcast([128, 128])), reads=[R_lg[j]], writes=[R_ge[i2]])
                            pg, rg = ps_next()
                            k.op("pe", lambda e, pg=pg, i2=i2: e.matmul(pg[:, 0:128], ge[i2][:], ident[:], start=True, stop=True), reads=[R_ge[i2], R_id], writes=[rg])
                            k.op("act", lambda e, pg=pg, j=j, ex=ex: e.activation(out=gb[j][:, ex, :], in_=pg[:, 0:128], func=AF.Copy), reads=[rg], writes=[R_gb[j]])
                        k.dma("sp", GB[b][:, :, t0:t0 + 128], gb[j][:], reads=[R_gb[j]], writes=[R_GB[b]])
            k.barrier()

        def stage_ffn(l, last):
            moe = (l % 2 == 1)
            i_w = l // 2
            F = DFE if moe else DFF
            GW = 512 if moe else 256
            ng = F // GW
            nfc = GW // 128
            tstart = LC if last else 0
            with ExitStack() as s1:
                hT = sb("ff_h", [128, 8, S], BF16, s1); R_h = Res()
                yacc = sb("ff_y", [128, 8, S], F32, s1); R_ya = [Res() for _ in range(9)]
                w1 = [sb("ff_w1%d" % i, [128, 8, GW], BF16, s1) for i in range(2)]
                w3 = [sb("ff_w3%d" % i, [128, 8, GW], BF16, s1) for i in range(2)]
                w2 = [sb("ff_w2%d" % i, [128, nfc, D], BF16, s1) for i in range(2)]
                R_wg = [Res(), Res()]
                sg = [sb("ff_s%d" % i, [128, 256], F32, s1) for i in range(2)]; R_sg = [Res(), Res()]
                ac = [sb("ff_a%d" % i, [128, nfc, 256], BF16, s1) for i in range(2)]; R_ac = [Res(), Res()]
                gt = [sb("ff_g%d" % i, [128, 256], F32, s1) for i in range(2)]; R_gt = [Res(), Res()]
                xb = [sb("ff_x%d" % i, [128, 8, 256], F32, s1) for i in range(2)]; R_xb = [Res(), Res()]
                nw = 0; na_ = 0; ns = 0; ngt = 0
                for b in range(NB):
                    k.dma("sp", hT[:, :, tstart:S], H2T[b][:, :, tstart:S], reads=[R_H2T[b]], writes=[R_h])
                    first = True
                    for ex in range(NE if moe else 1):
                        for g in range(ng):
                            jw = nw % 2; nw += 1
                            if moe:
                                s_w1, s_w3, s_w2 = I["moe_w1"][i_w, ex], I["moe_w3"][i_w, ex], I["moe_w2"][i_w, ex]
                            else:
                                s_w1, s_w3, s_w2 = I["ffn_w1"][i_w], I["ffn_w3"][i_w], I["ffn_w2"][i_w]
                            k.dma("pool", w1[jw][:], s_w1[:, :, g * GW:(g + 1) * GW], writes=[R_wg[jw]])
                            k.dma("pool", w3[jw][:], s_w3[:, :, g * GW:(g + 1) * GW], writes=[R_wg[jw]])
                            k.dma("pool", w2[jw][:], s_w2[:, g * nfc:(g + 1) * nfc, :], writes=[R_wg[jw]])
                            def phase_H(tb, ja, jw=jw, ex=ex, g=g):
                                nonlocal ns, ngt
                                t0 = tb * 256
                                if moe:
                                    jg = ngt % 2; ngt += 1
                                    k.dma("sp", gt[jg][:], GB[b][:, ex, t0:t0 + 256], reads=[R_GB[b]], writes=[R_gt[jg]])
                                for fc in range(nfc):
                                    ph, rh = ps_next()
                                    for kc in range(8):
                                        k.op("pe", lambda e, ph=ph, fc=fc, kc=kc, t0=t0: e.matmul(ph[:, 0:256], w1[jw][:, kc, fc * 128:(fc + 1) * 128], hT[:, kc, t0:t0 + 256],
                                             start=(kc == 0), stop=(kc == 7)), reads=[R_wg[jw], R_h], writes=[rh])
                                    for kc in range(8):
                                        k.op("pe", lambda e, ph=ph, fc=fc, kc=kc, t0=t0: e.matmul(ph[:, 256:512], w3[jw][:, kc, fc * 128:(fc + 1) * 128], hT[:, kc, t0:t0 + 256],
                                             start=(kc == 0), stop=(kc == 7)), reads=[R_wg[jw], R_h], writes=[rh])
                                    js = ns % 2; ns += 1
                                    k.op("act", lambda e, ph=ph, js=js: e.activation(out=sg[js][:], in_=ph[:, 0:256], func=AF.Silu), reads=[rh], writes=[R_sg[js]])
                                    if moe:
                                        k.op("pool", lambda e, js=js, jg=jg: e.tensor_tensor(out=sg[js][:], in0=sg[js][:], in1=gt[jg][:], op=ALU.mult), reads=[R_sg[js], R_gt[jg]], writes=[R_sg[js]])
                                    k.op("dve", lambda e, ph=ph, js=js, ja=ja, fc=fc: e.tensor_tensor(out=ac[ja][:, fc, :], in0=sg[js][:], in1=ph[:, 256:512], op=ALU.mult),
                                         reads=[R_sg[js], rh], writes=[R_ac[ja]])

                            def phase_O(tb, ja, jw=jw, first=first):
                                t0 = tb * 256
                                for ocp in range(4):
                                    po, ro = ps_next()
                                    for half in range(2):
                                        oc = 2 * ocp + half
                                        for fc in range(nfc):
                                            k.op("pe", lambda e, po=po, fc=fc, oc=oc, half=half: e.matmul(po[:, half * 256:(half + 1) * 256], w2[jw][:, fc, oc * 128:(oc + 1) * 128], ac[ja][:, fc, :],
                                                 start=(fc == 0), stop=(fc == nfc - 1)), reads=[R_wg[jw], R_ac[ja]], writes=[ro])
                                    yv_ = yacc[:, 2 * ocp:2 * ocp + 2, t0:t0 + 256]
                                    pv_ = po[:, :].rearrange("p (a c) -> p a c", a=2)
                                    if first:
                                        k.op("act", lambda e, yv_=yv_, pv_=pv_: e.activation(out=yv_, in_=pv_, func=AF.Copy), reads=[ro], writes=[R_ya[tb]])
                                    else:
                                        k.op("dve", lambda e, yv_=yv_, pv_=pv_: e.tensor_tensor(out=yv_, in0=yv_, in1=pv_, op=ALU.add), reads=[ro, R_ya[tb]], writes=[R_ya[tb]])

                            prev = None
                            for tb in range(tstart // 256, S // 256):
                                ja = na_ % 2; na_ += 1
                                phase_H(tb, ja)
                                if prev is not None:
                                    phase_O(*prev)
                                prev = (tb, ja)
                            phase_O(*prev)
                            first = False
                    for tb in range(tstart // 256, S // 256):
                        t0 = tb * 256
                        j = tb % 2
                        wq = which_of(b, t0)
                        k.dma("sp", xb[j][:], XT[b][:, :, t0:t0 + 256], reads=[R_XT[b]], writes=[R_xb[j]])
                        for oc in range(8):
                            k.op("dve", lambda e, j=j, oc=oc, t0=t0, wq=wq: e.scalar_tensor_tensor(out=xb[j][:, oc, :], in0=yacc[:, oc, t0:t0 + 256],
                                 scalar=MOD[:, l, 40 + oc, wq:wq + 1], in1=xb[j][:, oc, :], op0=ALU.mult, op1=ALU.add), reads=[R_ya[tb], R_mod, R_xb[j]], writes=[R_xb[j]])
                        k.dma("sp", XT[b][:, :, t0:t0 + 256], xb[j][:], reads=[R_xb[j]], writes=[R_XT[b]])
            k.barrier()

        def stage_final():
            with ExitStack() as s1:
                fg = sb("fn_g", [128, 8], F32, s1); R_fg = Res()
                k.dma("sp", fg[:], I["final_g"][:, :], writes=[R_fg])
                xb = [sb("fx%d" % i, [128, 8, 256], F32, s1) for i in range(2)]; R_xb = [Res(), Res()]
                sq = [sb("fsq%d" % i, [128, 8, 256], F32, s1) for i in range(2)]; R_sq = [Res(), Res()]
                rs = [sb("frs%d" % i, [128, 256], F32, s1) for i in range(2)]; R_rs = [Res(), Res()]
                ob = [sb("fo%d" % i, [128, 8, 256], F32, s1) for i in range(2)]; R_ob = [Res(), Res()]
                n = 0
                outs = []
                for b in range(NB):
                    for t0 in range(LC, S, 256):
                        j = n % 2; n += 1
                        k.dma("sp", xb[j][:], XT[b][:, :, t0:t0 + 256], reads=[R_XT[b]], writes=[R_xb[j]])
                        k.op("act", lambda e, j=j: e.activation(out=sq[j][:], in_=xb[j][:], func=AF.Square), reads=[R_xb[j]], writes=[R_sq[j]])
                        pt, rp = ps_next()
                        for c in range(8):
                            k.op("pe", lambda e, pt=pt, j=j, c=c: e.matmul(pt[:, 0:256], ones[:], sq[j][:, c, :], start=(c == 0), stop=(c == 7)), reads=[R_ones, R_sq[j]], writes=[rp])
                        k.op("act", lambda e, pt=pt, j=j: e.activation(out=rs[j][:], in_=pt[:, 0:256], func=AF.Sqrt, bias=eps_t[:, 0:1], scale=1.0 / D), reads=[rp, R_eps], writes=[R_rs[j]])
                        k.op("dve", lambda e, j=j: e.reciprocal(out=rs[j][:], in_=rs[j][:]), reads=[R_rs[j]], writes=[R_rs[j]])
                        for c in range(8):
                            k.op("dve", lambda e, j=j, c=c: e.scalar_tensor_tensor(out=ob[j][:, c, :], in0=xb[j][:, c, :], scalar=fg[:, c:c + 1], in1=rs[j][:],
                                 op0=ALU.mult, op1=ALU.mult), reads=[R_xb[j], R_fg, R_rs[j]], writes=[R_ob[j]])
                        outs.append(k.dma("sp", out[b, :, :, t0 - LC:t0 - LC + 256], ob[j][:], reads=[R_ob[j]]))
                return outs

        def stage_rwkv(l, need_ctx):
            with ExitStack() as s1:
                NSET = 3
                def T_(name, shape, dt=F32, n=3):
                    return [sb(name + str(i), shape, dt, s1) for i in range(n)], [Res() for _ in range(n)]
                w2 = sb("rw_w2", [128, 512], F32, s1); a2 = sb("rw_a2", [128, 512], F32, s1); g2 = sb("rw_g2", [128, 512], F32, s1)
                w0b = sb("rw_w0b", [64, 2, 512], F32, s1); a0T = sb("rw_a0T", [128, 2, 4], F32, s1)
                kkp = sb("rw_kkp", [128, 4], F32, s1); kap = sb("rw_kap", [128, 4], F32, s1); okap = sb("rw_okap", [128, 4], F32, s1)
                rkp = sb("rw_rkp", [128, 4], F32, s1)
                m2 = sb("rw_m2", [64, 2, 128], F32, s1); m3 = sb("rw_m3", [64, 2, 128], F32, s1); mL = sb("rw_mL", [64, 2, 64], F32, s1)
                bones = sb("rw_bones", [128, 128], F32, s1); sel = sb("rw_sel", [128, 2], F32, s1)
                c1 = sb("rw_c1", [128, 4], F32, s1)
                R_par = Res()
                for dst, src in ((w2, I["rw_w2"][l]), (a2, I["rw_a2"][l]), (g2, I["rw_g2"][l]), (a0T, I["rw_a0T"][l]),
                                 (kkp, I["rw_kk"][l]), (kap, I["rw_ka"][l]), (rkp, I["rw_rk"][l]),
                                 (m2, I["rw_m2"]), (m3, I["rw_m3"]), (mL, I["rw_mL"]), (bones, I["rw_bones"]), (sel, I["rw_sel"]), (c1, I["rw_c1"]),
                                 (w0b, I["rw_w0b"][l])):
                    k.dma("sp", dst[:], src, writes=[R_par])
                k.op("dve", lambda e: e.tensor_scalar(out=okap[:], in0=kap[:], scalar1=-1.0, scalar2=1.0, op0=ALU.mult, op1=ALU.add), reads=[R_par], writes=[R_par])
                Hst_b = [sb("rw_H%d" % i, [128, 4, 64], F32, s1) for i in range(NB)]; R_H_b = [Res() for _ in range(NB)]
                ub, R_ub = T_("rw_u", [128, 15, 64])
                twl, R_twl = T_("rw_twl", [128, 64])
                sgl, R_sgl = T_("rw_sgl", [128, 64], n=2)
                e2a, R_e2a = T_("rw_e2a", [64, 512], n=2)
                e2b, R_e2b = T_("rw_e2b", [64, 512], n=2)
                a_sb, R_a = T_("rw_a", [128, 4, 64])
                a1_sb, R_a1 = T_("rw_a1", [128, 4, 64], n=2)
                kr, R_kr = T_("rw_kr", [128, 4, 64])
                sq, R_sq = T_("rw_sq", [128, 4, 64])
                kk_t, R_kk = T_("rw_kkt", [128, 4, 64])
                ff, R_ff = T_("rw_ff", [128, 4, 64])
                kd, R_kd = T_("rw_kd", [128, 4, 64])
                bb, R_bb = T_("rw_bb", [128, 4, 64])
                EG, R_EG = T_("rw_EG", [128, 4, 2, 64])
                IEG, R_IEG = T_("rw_IEG", [128, 4, 64])
                fm, R_fm = T_("rw_fm", [128, 4, 4, 64])
                tm, R_tm = T_("rw_tm", [64, 4, 4, 128])
                LA, R_LA = T_("rw_LA", [64, 8, 128])
                NBt, R_NB = T_("rw_NB", [64, 8, 128])
                Lm, R_Lm = T_("rw_Lm", [64, 8, 64])
                Ll32, R_Ll32 = T_("rw_Ll32", [64, 8, 64])
                Pb, R_Pb = T_("rw_Pb", [64, 8, 64], BF16)
                Pm, R_Pm = T_("rw_Pm", [64, 8, 64])
                Nl, R_Nl = T_("rw_Nl", [64, 8, 64], BF16, n=4)
                Ll, R_Ll = T_("rw_Ll", [64, 8, 64], BF16, n=4)
                WT, R_WT = T_("rw_WT", [128, 4, 64])
                Xa, R_Xa = T_("rw_Xa", [64, 8, 64])
                Uta, R_Uta = T_("rw_Uta", [64, 8, 64])
                Ua, R_Ua = T_("rw_Ua", [64, 8, 64])
                ysb, R_ysb = T_("rw_ysb", [64, 512])
                htmp, R_htmp = T_("rw_htmp", [128, 4, 64])
                rk_t, R_rk = T_("rw_rkt", [128, 4, 64], n=2)
                rkh, R_rkh = T_("rw_rkh", [64, 8], n=2)
                bon, R_bon = T_("rw_bon", [64, 512], n=2)
                gsb, R_gsb = T_("rw_gsb", [64, 512], n=2)
                n = 0
                nl4 = [0]
                nb_run = NB if RW_DBG["nb"] is None else RW_DBG["nb"]
                for d in range(RW_DBG["nd"]):
                    for b in range(nb_run):
                        k.op("pool", lambda e, b=b: e.memset(Hst_b[b][:], 0.0), writes=[R_H_b[b]])
                    order = list(range(36)) if d == 0 else [3, 2, 1, 0] + list(range(35, 3, -1))
                    if RW_DBG["nch"] is not None:
                        order = order[:RW_DBG["nch"]]
                    last_col = 63 if d == 0 else 0
                    for ch in order:
                      for b in range(nb_run):
                        j = n % NSET; j2 = n % 2; n += 1
                        def chunk_body(b=b, ch=ch, d=d, j=j, j2=j2, last_col=last_col, Hst=Hst_b[b], R_H=R_H_b[b]):
                            if (not need_ctx) and False:
                                pass
                            t0 = ch * 64
                            U_ = ub[j]
                            k.dma("sp", U_[:], UT[b][:, :, t0:t0 + 64], reads=[R_UT[b]], writes=[R_ub[j]])
                            k.op("act", lambda e, j=j, U_=U_: e.activation(out=twl[j][:], in_=U_[:, 12, :], func=AF.Tanh), reads=[R_ub[j]], writes=[R_twl[j]])
                            pt, rp = ps_next()
                            k.op("pe", lambda e, pt=pt, j=j, d=d: e.matmul(pt[0:64, :], twl[j][d * 64:(d + 1) * 64, :], w2[d * 64:(d + 1) * 64, :], start=True, stop=True),
                                 reads=[R_twl[j], R_par], writes=[rp])
                            k.op("dve", lambda e, j2=j2, pt=pt, j=j, d=d: e.tensor_tensor(out=e2a[j2][:], in0=pt[0:64, :], in1=w0b[:, d, :], op=ALU.add), reads=[rp, R_par], writes=[R_e2a[j2]])
                            k.op("act", lambda e, j2=j2, j=j: e.activation(out=e2b[j2][:], in_=e2a[j2][:], func=AF.Exp, scale=-1.0), reads=[R_e2a[j2]], writes=[R_e2b[j2]])
                            k.op("act", lambda e, j2=j2, j=j: e.activation(out=e2a[j2][:], in_=e2b[j2][:], func=AF.Ln, bias=c1[0:64, 0:1], scale=1.0), reads=[R_e2b[j2], R_par], writes=[R_e2a[j2]])
                            k.op("act", lambda e, j2=j2, j=j: e.activation(out=e2b[j2][:], in_=e2a[j2][:], func=AF.Exp, bias=c1[0:64, 1:2], scale=-1.0), reads=[R_e2a[j2], R_par], writes=[R_e2b[j2]])
                            def a_path(dd, dst, R_dst):
                                pa, ra = ps_next()
                                for q in range(4):
                                    k.op("pe", lambda e, pa=pa, q=q, dd=dd, U_=U_: e.matmul(pa[:, q * 64:(q + 1) * 64], a2[dd * 64:(dd + 1) * 64, q * 128:(q + 1) * 128], U_[dd * 64:(dd + 1) * 64, 13, :], start=True, stop=True),
                                         reads=[R_par, R_ub[j]], writes=[ra])
                                for q in range(4):
                                    k.op("act", lambda e, pa=pa, q=q, dd=dd, dst=dst: e.activation(out=dst[:, q, :], in_=pa[:, q * 64:(q + 1) * 64], func=AF.Sigmoid, bias=a0T[:, dd, q:q + 1], scale=1.0),
                                         reads=[ra, R_par], writes=[R_dst])
                            a_path(d, a_sb[j], R_a[j])
                            k.op("dve", lambda e, j=j, U_=U_: e.tensor_tensor(out=kr[j][:], in0=U_[:, 4:8, :], in1=kkp[:, :].unsqueeze(2).to_broadcast([128, 4, 64]), op=ALU.mult), reads=[R_ub[j], R_par], writes=[R_kr[j]])
                            k.op("pool", lambda e, j=j: e.tensor_tensor(out=sq[j][:], in0=kr[j][:], in1=kr[j][:], op=ALU.mult), reads=[R_kr[j]], writes=[R_sq[j]])
                            pn_, rn = ps_next()
                            k.op("pe", lambda e, pn_=pn_, j=j: e.matmul(pn_[:, 0:256], bones[:], sq[j][:].rearrange("p q t -> p (q t)"), start=True, stop=True), reads=[R_par, R_sq[j]], writes=[rn])
                            k.op("act", lambda e, pn_=pn_, j=j: e.activation(out=sq[j][:].rearrange("p q t -> p (q t)"), in_=pn_[:, 0:256], func=AF.Sqrt), reads=[rn], writes=[R_sq[j]])
                            k.op("dve", lambda e, j=j: e.tensor_scalar(out=sq[j][:], in0=sq[j][:], scalar1=1e-12, scalar2=None, op0=ALU.max), reads=[R_sq[j]], writes=[R_sq[j]])
                            k.op("dve", lambda e, j=j: e.reciprocal(out=sq[j][:], in_=sq[j][:]), reads=[R_sq[j]], writes=[R_sq[j]])
                            k.op("dve", lambda e, j=j: e.tensor_tensor(out=kk_t[j][:], in0=kr[j][:], in1=sq[j][:], op=ALU.mult), reads=[R_kr[j], R_sq[j]], writes=[R_kk[j]])
                            k.op("pool", lambda e, j=j: e.tensor_tensor(out=ff[j][:], in0=a_sb[j][:], in1=kap[:, :].unsqueeze(2).to_broadcast([128, 4, 64]), op=ALU.mult), reads=[R_a[j], R_par], writes=[R_ff[j]])
                            k.op("pool", lambda e, j=j: e.tensor_tensor(out=ff[j][:], in0=ff[j][:], in1=okap[:, :].unsqueeze(2).to_broadcast([128, 4, 64]), op=ALU.add), reads=[R_ff[j], R_par], writes=[R_ff[j]])
                            k.op("pool", lambda e, j=j, U_=U_: e.tensor_tensor(out=kd[j][:], in0=U_[:, 4:8, :], in1=ff[j][:], op=ALU.mult), reads=[R_ub[j], R_ff[j]], writes=[R_kd[j]])
                            k.op("pool", lambda e, j=j: e.tensor_tensor(out=bb[j][:], in0=kk_t[j][:], in1=a_sb[j][:], op=ALU.mult), reads=[R_kk[j], R_a[j]], writes=[R_bb[j]])
                            pc, rc = ps_next()
                            for q in range(4):
                                k.op("pe", lambda e, j2=j2, pc=pc, q=q, j=j, d=d: e.matmul(pc[:, q * 128:(q + 1) * 128], e2b[j2][:, q * 128:(q + 1) * 128], m2[:, d, :], start=True, stop=True),
                                     reads=[R_e2b[j2], R_par], writes=[rc])
                            k.op("act", lambda e, pc=pc, j=j: e.activation(out=EG[j][:].rearrange("p q s t -> p (q s t)"), in_=pc[:, :], func=AF.Exp, scale=-1.0), reads=[rc], writes=[R_EG[j]])
                            k.op("act", lambda e, pc=pc, j=j: e.activation(out=IEG[j][:], in_=pc[:, :].rearrange("p (q s t) -> p q s t", q=4, s=2)[:, :, 1, :], func=AF.Exp, scale=1.0), reads=[rc], writes=[R_IEG[j]])
                            F_ = fm[j]
                            k.op("dve", lambda e, j=j, F_=F_: e.tensor_tensor(out=F_[:, :, 0, :], in0=kd[j][:], in1=IEG[j][:], op=ALU.mult), reads=[R_kd[j], R_IEG[j]], writes=[R_fm[j]])
                            k.op("dve", lambda e, j=j, F_=F_: e.tensor_tensor(out=F_[:, :, 1, :], in0=bb[j][:], in1=IEG[j][:], op=ALU.mult), reads=[R_bb[j], R_IEG[j]], writes=[R_fm[j]])
                            k.op("pool", lambda e, j=j, F_=F_: e.tensor_tensor(out=F_[:, :, 2, :], in0=kk_t[j][:], in1=EG[j][:, :, 0, :], op=ALU.mult), reads=[R_kk[j], R_EG[j]], writes=[R_fm[j]])
                            k.op("pool", lambda e, j=j, F_=F_, U_=U_: e.tensor_tensor(out=F_[:, :, 3, :], in0=U_[:, 0:4, :], in1=EG[j][:, :, 1, :], op=ALU.mult), reads=[R_ub[j], R_EG[j]], writes=[R_fm[j]])
                            T_m = tm[j]
                            for kind, (srcf, sc) in enumerate(((lambda q, F_=F_: F_[:, q, 2, :], 1.0), (lambda q, F_=F_: F_[:, q, 0, :], 1.0), (lambda q, F_=F_: F_[:, q, 1, :], -1.0), (lambda q, U_=U_: U_[:, 8 + q, :], 1.0))):
                                ptx, rtx = ps_next()
                                for q in range(4):
                                    k.op("pe", lambda e, ptx=ptx, q=q, srcf=srcf: e.transpose(ptx[0:64, q * 128:(q + 1) * 128], srcf(q), ident[:, :]),
                                         reads=[R_fm[j], R_ub[j], R_id], writes=[rtx])
                                eng = "act" if kind % 2 == 0 else "dve"
                                if eng == "act":
                                    k.op("act", lambda e, ptx=ptx, kind=kind, sc=sc, T_m=T_m: e.activation(out=T_m[:, :, kind, :], in_=ptx[0:64, :].rearrange("p (q c) -> p q c", q=4), func=AF.Copy, scale=sc),
                                         reads=[rtx], writes=[R_tm[j]])
                                else:
                                    k.op("dve", lambda e, ptx=ptx, kind=kind, sc=sc, T_m=T_m: e.tensor_scalar(out=T_m[:, :, kind, :], in0=ptx[0:64, :].rearrange("p (q c) -> p q c", q=4), scalar1=sc, scalar2=None, op0=ALU.mult),
                                         reads=[rtx], writes=[R_tm[j]])
                            if RW_DBG["upto"] < "A2":
                                return
                            if d == 0 and (need_ctx or ch >= 4):
                                a_path(1, a1_sb[j2], R_a1[j2])
                                k.op("dve", lambda e, j2=j2, j=j: e.tensor_tensor(out=a1_sb[j2][:], in0=a1_sb[j2][:], in1=a_sb[j][:], op=ALU.add), reads=[R_a1[j2], R_a[j]], writes=[R_a1[j2]])
                                k.op("dve", lambda e, j2=j2, j=j: e.scalar_tensor_tensor(out=a1_sb[j2][:], in0=a1_sb[j2][:], scalar=0.5, in1=kap[:, :].unsqueeze(2).to_broadcast([128, 4, 64]), op0=ALU.mult, op1=ALU.mult), reads=[R_a1[j2], R_par], writes=[R_a1[j2]])
                                k.op("dve", lambda e, j2=j2, j=j: e.tensor_tensor(out=a1_sb[j2][:], in0=a1_sb[j2][:], in1=okap[:, :].unsqueeze(2).to_broadcast([128, 4, 64]), op=ALU.add), reads=[R_a1[j2], R_par], writes=[R_a1[j2]])
                                k.op("dve", lambda e, j2=j2, j=j, U_=U_: e.tensor_tensor(out=rk_t[j2][:], in0=a1_sb[j2][:], in1=U_[:, 4:8, :], op=ALU.mult), reads=[R_a1[j2], R_ub[j]], writes=[R_rk[j2]])
                                k.op("dve", lambda e, j2=j2, j=j, U_=U_: e.tensor_tensor(out=rk_t[j2][:], in0=rk_t[j2][:], in1=U_[:, 0:4, :], op=ALU.mult), reads=[R_rk[j2], R_ub[j]], writes=[R_rk[j2]])
                                k.op("dve", lambda e, j2=j2, j=j: e.tensor_tensor(out=rk_t[j2][:], in0=rk_t[j2][:], in1=rkp[:, :].unsqueeze(2).to_broadcast([128, 4, 64]), op=ALU.mult), reads=[R_rk[j2], R_par], writes=[R_rk[j2]])
                                pr, rr = ps_next()
                                for q in range(4):
                                    k.op("pe", lambda e, j2=j2, pr=pr, q=q, j=j: e.matmul(pr[0:64, q * 2:(q + 1) * 2], rk_t[j2][:, q, :], sel[:, :], start=True, stop=True), reads=[R_rk[j2], R_par], writes=[rr])
                                k.op("act", lambda e, j2=j2, pr=pr, j=j: e.activation(out=rkh[j2][:], in_=pr[0:64, 0:8], func=AF.Copy), reads=[rr], writes=[R_rkh[j2]])
                                k.op("dve", lambda e, j2=j2, j=j, T_m=T_m: e.tensor_tensor(out=bon[j2][:].rearrange("p (q h v) -> p q h v", q=4, h=2), in0=T_m[:, :, 3, :].rearrange("p q (h v) -> p q h v", h=2),
                                     in1=rkh[j2][:, :].rearrange("p (q h) -> p q h", q=4).unsqueeze(3).to_broadcast([64, 4, 2, 64]), op=ALU.mult), reads=[R_tm[j], R_rkh[j2]], writes=[R_bon[j2]])
                                k.dma("sp", BON[b][t0:t0 + 64, :], bon[j2][:], reads=[R_bon[j2]], writes=[R_BON[b]])
                                k.op("act", lambda e, j2=j2, j=j, U_=U_: e.activation(out=sgl[j2][:], in_=U_[:, 14, :], func=AF.Sigmoid), reads=[R_ub[j]], writes=[R_sgl[j2]])
                                pg, rg = ps_next()
                                k.op("pe", lambda e, j2=j2, pg=pg, j=j: e.matmul(pg[0:64, :], sgl[j2][:], g2[:], start=True, stop=True), reads=[R_sgl[j2], R_par], writes=[rg])
                                k.op("act", lambda e, j2=j2, pg=pg, j=j: e.activation(out=gsb[j2][:], in_=pg[0:64, :], func=AF.Copy), reads=[rg], writes=[R_gsb[j2]])
                                k.dma("sp", GG[b][t0:t0 + 64, :], gsb[j2][:], reads=[R_gsb[j2]], writes=[R_GG[b]])
                            if RW_DBG["upto"] < "B":
                                return
                            def hp(h):
                                return h // 2, (h % 2) * 64
                            hv = lambda t, h2: t[:].rearrange("p (q h) c -> p q h c", q=4, h=2)[:, :, h2, :]
                            for h2 in range(2):
                                p0 = h2 * 64
                                p1, r1 = ps_next()
                                p2, r2 = ps_next()
                                p3, r3 = ps_next()
                                for q in range(4):
                                    k.op("pe", lambda e, p1=p1, q=q, p0=p0, F_=F_: e.matmul(p1[0:64, q * 128:(q + 1) * 128], F_[p0:p0 + 64, q, 0, :], F_[p0:p0 + 64, q, 2:4, :].rearrange("p s t -> p (s t)"), start=True, stop=True),
                                         reads=[R_fm[j]], writes=[r1])
                                    k.op("pe", lambda e, p2=p2, q=q, p0=p0, F_=F_: e.matmul(p2[0:64, q * 128:(q + 1) * 128], F_[p0:p0 + 64, q, 1, :], F_[p0:p0 + 64, q, 2:4, :].rearrange("p s t -> p (s t)"), start=True, stop=True),
                                         reads=[R_fm[j]], writes=[r2])
                                    k.op("pe", lambda e, p3=p3, q=q, p0=p0, F_=F_: e.matmul(p3[0:64, q * 64:(q + 1) * 64], F_[p0:p0 + 64, q, 2, :], F_[p0:p0 + 64, q, 1, :], start=True, stop=True), reads=[R_fm[j]], writes=[r3])
                                k.op("dve", lambda e, p1=p1, h2=h2, j=j, d=d: e.tensor_tensor(out=hv(LA[j], h2), in0=p1[0:64, :].rearrange("p (q c) -> p q c", q=4),
                                     in1=m2[:, d, :].unsqueeze(1).to_broadcast([64, 4, 128]), op=ALU.mult), reads=[r1, R_par], writes=[R_LA[j]])
                                k.op("dve", lambda e, p2=p2, h2=h2, j=j, d=d: e.tensor_tensor(out=hv(NBt[j], h2), in0=p2[0:64, :].rearrange("p (q c) -> p q c", q=4),
                                     in1=m3[:, d, :].unsqueeze(1).to_broadcast([64, 4, 128]), op=ALU.mult), reads=[r2, R_par], writes=[R_NB[j]])
                                k.op("dve", lambda e, p3=p3, h2=h2, j=j, d=d: e.tensor_tensor(out=hv(Lm[j], h2), in0=p3[0:64, 0:256].rearrange("p (q c) -> p q c", q=4),
                                     in1=mL[:, d, :].unsqueeze(1).to_broadcast([64, 4, 64]), op=ALU.mult), reads=[r3, R_par], writes=[R_Lm[j]])
                            if RW_DBG["upto"] < "B0":
                                return
                            k.op("dve", lambda e, j=j: e.scalar_tensor_tensor(out=Pm[j][:], in0=NBt[j][:, :, 0:64], scalar=-1.0, in1=ident[0:64, 0:64].unsqueeze(1).to_broadcast([64, 8, 64]), op0=ALU.mult, op1=ALU.add),
                                 reads=[R_NB[j], R_id], writes=[R_Pm[j]])
                            Ncur = lambda h, j=j: NBt[j][:, h, 0:64]
                            Lcur = lambda h, j=j: Lm[j][:, h, :]
                            R_Nc, R_Lc = R_NB[j], R_Lm[j]
                            nlev = RW_DBG.get("nlev", 5)
                            for lev in range(nlev):
                                i4 = nl4[0] % 4; nl4[0] += 1
                                pL, rL = ps_next()
                                for h in range(8):
                                    k.op("pe", lambda e, pL=pL, h=h, Ncur=Ncur, Lcur=Lcur: e.matmul(pL[0:64, h * 64:(h + 1) * 64], Ncur(h), Lcur(h), start=True, stop=True), reads=[R_Nc, R_Lc], writes=[rL])
                                if lev == 0:
                                    k.op("act", lambda e, pL=pL, j=j: e.activation(out=Ll32[j][:].rearrange("p h c -> p (h c)"), in_=pL[0:64, :], func=AF.Copy), reads=[rL], writes=[R_Ll32[j]])
                                    k.op("pool", lambda e, i4=i4, j=j: e.tensor_copy(out=Ll[i4][:], in_=Ll32[j][:]), reads=[R_Ll32[j]], writes=[R_Ll[i4]])
                                else:
                                    k.op("act", lambda e, pL=pL, i4=i4: e.activation(out=Ll[i4][:].rearrange("p h c -> p (h c)"), in_=pL[0:64, :], func=AF.Copy), reads=[rL], writes=[R_Ll[i4]])
                                if lev < nlev - 1:
                                    pN, rN = ps_next()
                                    for h in range(8):
                                        k.op("pe", lambda e, pN=pN, h=h, Ncur=Ncur, Lcur=Lcur: e.matmul(pN[0:64, h * 64:(h + 1) * 64], Lcur(h), Ncur(h), start=True, stop=True), reads=[R_Nc, R_Lc], writes=[rN])
                                    k.op("dve", lambda e, pN=pN, i4=i4: e.tensor_copy(out=Nl[i4][:].rearrange("p h c -> p (h c)"), in_=pN[0:64, :]), reads=[rN], writes=[R_Nl[i4]])
                                pP, rP = ps_next()
                                for h in range(8):
                                    if lev == 0:
                                        k.op("pe", lambda e, pP=pP, h=h, j=j: e.matmul(pP[0:64, h * 64:(h + 1) * 64], Ll32[j][:, h, :], Pm[j][:, h, :], start=True, stop=True), reads=[R_Ll32[j], R_Pm[j]], writes=[rP])
                                    else:
                                        k.op("pe", lambda e, pP=pP, h=h, i4=i4, j=j: e.matmul(pP[0:64, h * 64:(h + 1) * 64], Ll[i4][:, h, :], Pb[j][:, h, :], start=True, stop=True), reads=[R_Ll[i4], R_Pb[j]], writes=[rP])
                                k.op("dve", lambda e, pP=pP, j=j: e.tensor_tensor(out=Pm[j][:].rearrange("p h c -> p (h c)"), in0=Pm[j][:].rearrange("p h c -> p (h c)"), in1=pP[0:64, :], op=ALU.add), reads=[rP, R_Pm[j]], writes=[R_Pm[j]])
                                if lev < nlev - 1:
                                    k.op("act", lambda e, j=j: e.activation(out=Pb[j][:], in_=Pm[j][:], func=AF.Copy), reads=[R_Pm[j]], writes=[R_Pb[j]])
                                Ncur = lambda h, i4=i4: Nl[i4][:, h, :]
                                Lcur = lambda h, i4=i4: Ll[i4][:, h, :]
                                R_Nc, R_Lc = R_Nl[i4], R_Ll[i4]
                            if RW_DBG["upto"] < "B2":
                                return
                            pW, rW = ps_next()
                            for h in range(8):
                                q, p0 = hp(h)
                                k.op("pe", lambda e, pW=pW, h=h, q=q, j=j, T_m=T_m: e.matmul(pW[:, h * 64:(h + 1) * 64], T_m[:, q, 0, :], Pm[j][:, h, :], start=True, stop=True), reads=[R_tm[j], R_Pm[j]], writes=[rW])
                            for h2 in range(2):
                                k.op("act" if h2 == 0 else "dve",
                                     (lambda e, pW=pW, j=j, h2=h2: e.activation(out=WT[j][h2 * 64:(h2 + 1) * 64, :, :], in_=pW[h2 * 64:(h2 + 1) * 64, :].rearrange("p (q h i) -> p q h i", q=4, h=2)[:, :, h2, :], func=AF.Copy)) if h2 == 0 else
                                     (lambda e, pW=pW, j=j, h2=h2: e.tensor_copy(out=WT[j][h2 * 64:(h2 + 1) * 64, :, :], in_=pW[h2 * 64:(h2 + 1) * 64, :].rearrange("p (q h i) -> p q h i", q=4, h=2)[:, :, h2, :])),
                                     reads=[rW], writes=[R_WT[j]])
                            pX, rX = ps_next()
                            for h in range(8):
                                q, p0 = hp(h)
                                k.op("pe", lambda e, pX=pX, h=h, q=q, p0=p0, j=j, T_m=T_m: e.matmul(pX[0:64, h * 64:(h + 1) * 64], LA[j][:, h, 0:64], T_m[:, q, 3, p0:p0 + 64], start=True, stop=True), reads=[R_LA[j], R_tm[j]], writes=[rX])
                            k.op("act", lambda e, pX=pX, j=j: e.activation(out=Xa[j][:].rearrange("p h c -> p (h c)"), in_=pX[0:64, :], func=AF.Copy), reads=[rX], writes=[R_Xa[j]])
                            pU, rU = ps_next()
                            for h in range(8):
                                k.op("pe", lambda e, pU=pU, h=h, j=j: e.matmul(pU[0:64, h * 64:(h + 1) * 64], Pm[j][:, h, :], Xa[j][:, h, :], start=True, stop=True), reads=[R_Pm[j], R_Xa[j]], writes=[rU])
                            k.op("act", lambda e, pU=pU, j=j: e.activation(out=Uta[j][:].rearrange("p h c -> p (h c)"), in_=pU[0:64, :], func=AF.Copy), reads=[rU], writes=[R_Uta[j]])
                            if RW_DBG["upto"] < "C":
                                return
                            for h2 in range(2):
                                p0 = h2 * 64
                                pS, rS = ps_next()
                                for q in range(4):
                                    k.op("pe", lambda e, Hst=Hst, pS=pS, q=q, p0=p0, j=j: e.matmul(pS[0:64, q * 64:(q + 1) * 64], WT[j][p0:p0 + 64, q, :], Hst[p0:p0 + 64, q, :], start=True, stop=True), reads=[R_WT[j], R_H], writes=[rS])
                                k.op("dve", lambda e, pS=pS, j=j, h2=h2: e.tensor_tensor(out=hv(Ua[j], h2), in0=hv(Uta[j], h2), in1=pS[0:64, 0:256].rearrange("p (q c) -> p q c", q=4), op=ALU.add),
                                     reads=[rS, R_Uta[j]], writes=[R_Ua[j]])
                            if RW_DBG["upto"] < "C1":
                                return
                            pY, rY = ps_next()
                            for h in range(8):
                                q, p0 = hp(h)
                                k.op("pe", lambda e, pY=pY, h=h, q=q, p0=p0, j=j, T_m=T_m: e.matmul(pY[0:64, h * 64:(h + 1) * 64], LA[j][:, h, 64:128], T_m[:, q, 3, p0:p0 + 64], start=True, stop=False), reads=[R_LA[j], R_tm[j]], writes=[rY])
                                k.op("pe", lambda e, pY=pY, h=h, j=j: e.matmul(pY[0:64, h * 64:(h + 1) * 64], NBt[j][:, h, 64:128], Ua[j][:, h, :], start=False, stop=True), reads=[R_NB[j], R_Ua[j]], writes=[rY])
                            k.op("act", lambda e, pY=pY, j=j: e.activation(out=ysb[j][:], in_=pY[0:64, :], func=AF.Copy), reads=[rY], writes=[R_ysb[j]])
                            for h2 in range(2):
                                p0 = h2 * 64
                                pR, rR = ps_next()
                                for q in range(4):
                                    k.op("pe", lambda e, Hst=Hst, pR=pR, q=q, p0=p0, j=j, F_=F_: e.matmul(pR[0:64, q * 64:(q + 1) * 64], F_[p0:p0 + 64, q, 3, :], Hst[p0:p0 + 64, q, :], start=True, stop=True), reads=[R_fm[j], R_H], writes=[rR])
                                yv = lambda t, h2: t[:].rearrange("p (q h c) -> p q h c", q=4, h=2)[:, :, h2, :]
                                k.op("dve", lambda e, pR=pR, j=j, h2=h2, yv=yv: e.tensor_tensor(out=yv(ysb[j], h2), in0=yv(ysb[j], h2), in1=pR[0:64, 0:256].rearrange("p (q c) -> p q c", q=4), op=ALU.add),
                                     reads=[rR, R_ysb[j]], writes=[R_ysb[j]])
                            k.dma("sp", YD[d][b][t0:t0 + 64, :], ysb[j][:], reads=[R_ysb[j]], writes=[R_YD[d][b]])
                            if RW_DBG["upto"] < "C2":
                                return
                            pH, rH = ps_next()
                            for q in range(4):
                                k.op("pe", lambda e, pH=pH, q=q, T_m=T_m: e.matmul(pH[:, q * 128:(q + 1) * 128], T_m[:, q, 1, :], T_m[:, q, 3, :], start=True, stop=False), reads=[R_tm[j]], writes=[rH])
                                k.op("pe", lambda e, pH=pH, q=q, j=j, T_m=T_m: e.matmul(pH[:, q * 128:(q + 1) * 128], T_m[:, q, 2, :], Ua[j][:, 2 * q:2 * q + 2, :].rearrange("p h c -> p (h c)"), start=False, stop=True), reads=[R_tm[j], R_Ua[j]], writes=[rH])
                            for h2 in range(2):
                                ps_ = slice(h2 * 64, (h2 + 1) * 64)
                                k.op("dve", lambda e, Hst=Hst, pH=pH, j=j, h2=h2, ps_=ps_: e.tensor_tensor(out=htmp[j][ps_, :, :], in0=pH[ps_, :].rearrange("p (q h v) -> p q h v", q=4, h=2)[:, :, h2, :], in1=Hst[ps_, :, :], op=ALU.add),
                                     reads=[rH, R_H], writes=[R_htmp[j]])
                                k.op("dve", lambda e, Hst=Hst, j=j, ps_=ps_, last_col=last_col: e.tensor_tensor(out=Hst[ps_, :, :], in0=htmp[j][ps_, :, :], in1=EG[j][ps_, :, 1, last_col:last_col + 1].to_broadcast([64, 4, 64]), op=ALU.mult),
                                     reads=[R_htmp[j], R_EG[j]], writes=[R_H])
                        chunk_body()
            k.barrier()
            if not RW_DBG["readout"]:
                return
            with ExitStack() as s1:
                lnw_ro = sb("ro_lnw", [128, 512], F32, s1); lnb_ro = sb("ro_lnb", [128, 512], F32, s1); c1_ro = sb("ro_c1", [128, 4], F32, s1); R_par_ro = Res()
                k.dma("sp", lnw_ro[:], I["rw_lnw"][l], writes=[R_par_ro]); k.dma("sp", lnb_ro[:], I["rw_lnb"][l], writes=[R_par_ro]); k.dma("sp", c1_ro[:], I["rw_c1"], writes=[R_par_ro])
                def T2(name, shape, dt=F32):
                    return [sb(name + str(i), shape, dt, s1) for i in range(2)], [Res(), Res()]
                y0_ro, R_y0_ro = T2("ro_y0", [128, 512]); y1_ro, R_y1_ro = T2("ro_y1", [128, 512]); bo_ro, R_bo_ro = T2("ro_bo", [128, 512]); gg_ro, R_gg_ro = T2("ro_gg", [128, 512])
                stt__ro, R_stt_ro = T2("ro_st", [128, 32]); yc_ro, R_yc_ro = T2("ro_yc", [128, 512]); sq_ro, R_sq_ro = T2("ro_sq", [128, 512]); ot_ro, R_ot_ro = T2("ro_ot", [128, 4, 128], BF16)
                n = 0
                for b in range(NB):
                    for tt in range(0 if need_ctx else 2, NT):
                        j = n % 2; n += 1
                        t0 = tt * 128
                        k.dma("sp", y0_ro[j][:], YD[0][b][t0:t0 + 128, :], reads=[R_YD[0][b]], writes=[R_y0_ro[j]])
                        k.dma("sp", y1_ro[j][:], YD[1][b][t0:t0 + 128, :], reads=[R_YD[1][b]], writes=[R_y1_ro[j]])
                        k.dma("sp", bo_ro[j][:], BON[b][t0:t0 + 128, :], reads=[R_BON[b]], writes=[R_bo_ro[j]])
                        k.dma("sp", gg_ro[j][:], GG[b][t0:t0 + 128, :], reads=[R_GG[b]], writes=[R_gg_ro[j]])
                        S_ = stt__ro[j]
                        v3 = lambda t: t[:].rearrange("p (h v) -> p h v", h=8)
                        bc = lambda a: a.unsqueeze(2).to_broadcast([128, 8, 64])
                        k.op("dve", lambda e, j=j: e.tensor_tensor(out=y0_ro[j][:], in0=y0_ro[j][:], in1=y1_ro[j][:], op=ALU.add), reads=[R_y0_ro[j], R_y1_ro[j]], writes=[R_y0_ro[j]])
                        k.op("dve", lambda e, j=j, S_=S_: e.tensor_reduce(out=S_[:, 0:8], in_=v3(y0_ro[j]), axis=AX.X, op=ALU.add), reads=[R_y0_ro[j]], writes=[R_stt_ro[j]])
                        k.op("dve", lambda e, S_=S_: e.tensor_scalar(out=S_[:, 8:16], in0=S_[:, 0:8], scalar1=1.0 / 64, scalar2=None, op0=ALU.mult), reads=[R_stt_ro[j]], writes=[R_stt_ro[j]])
                        k.op("dve", lambda e, j=j, S_=S_: e.tensor_tensor(out=v3(yc_ro[j]), in0=v3(y0_ro[j]), in1=bc(S_[:, 8:16]), op=ALU.subtract), reads=[R_y0_ro[j], R_stt_ro[j]], writes=[R_yc_ro[j]])
                        k.op("pool", lambda e, j=j: e.tensor_tensor(out=sq_ro[j][:], in0=yc_ro[j][:], in1=yc_ro[j][:], op=ALU.mult), reads=[R_yc_ro[j]], writes=[R_sq_ro[j]])
                        k.op("dve", lambda e, j=j, S_=S_: e.tensor_reduce(out=S_[:, 16:24], in_=v3(sq_ro[j]), axis=AX.X, op=ALU.add), reads=[R_sq_ro[j]], writes=[R_stt_ro[j]])
                        k.op("act", lambda e, S_=S_: e.activation(out=S_[:, 24:32], in_=S_[:, 16:24], func=AF.Sqrt, bias=c1_ro[:, 2:3], scale=1.0 / 64), reads=[R_stt_ro[j], R_par_ro], writes=[R_stt_ro[j]])
                        k.op("dve", lambda e, S_=S_: e.reciprocal(out=S_[:, 24:32], in_=S_[:, 24:32]), reads=[R_stt_ro[j]], writes=[R_stt_ro[j]])
                        k.op("dve", lambda e, j=j, S_=S_: e.tensor_tensor(out=v3(yc_ro[j]), in0=v3(yc_ro[j]), in1=bc(S_[:, 24:32]), op=ALU.mult), reads=[R_yc_ro[j], R_stt_ro[j]], writes=[R_yc_ro[j]])
                        k.op("pool", lambda e, j=j: e.tensor_tensor(out=yc_ro[j][:], in0=yc_ro[j][:], in1=lnw_ro[:], op=ALU.mult), reads=[R_yc_ro[j], R_par_ro], writes=[R_yc_ro[j]])
                        k.op("pool", lambda e, j=j: e.tensor_tensor(out=yc_ro[j][:], in0=yc_ro[j][:], in1=lnb_ro[:], op=ALU.add), reads=[R_yc_ro[j], R_par_ro], writes=[R_yc_ro[j]])
                        k.op("dve", lambda e, j=j: e.tensor_tensor(out=yc_ro[j][:], in0=yc_ro[j][:], in1=bo_ro[j][:], op=ALU.add), reads=[R_yc_ro[j], R_bo_ro[j]], writes=[R_yc_ro[j]])
                        k.op("dve", lambda e, j=j: e.tensor_tensor(out=yc_ro[j][:], in0=yc_ro[j][:], in1=gg_ro[j][:], op=ALU.mult), reads=[R_yc_ro[j], R_gg_ro[j]], writes=[R_yc_ro[j]])
                        pt, rp = ps_next()
                        for q in range(4):
                            k.op("pe", lambda e, pt=pt, q=q, j=j: e.transpose(pt[:, q * 128:(q + 1) * 128], yc_ro[j][:, q * 128:(q + 1) * 128], ident[:, :]), reads=[R_yc_ro[j], R_id], writes=[rp])
                        k.op("act", lambda e, pt=pt, j=j: e.activation(out=ot_ro[j][:].rearrange("p q t -> p (q t)"), in_=pt[:, :], func=AF.Copy), reads=[rp], writes=[R_ot_ro[j]])
                        k.dma("sp", YT[b][:, 0:4, t0:t0 + 128], ot_ro[j][:], reads=[R_ot_ro[j]], writes=[R_YT[b]])
            k.barrier()

        toks = []
        for l in range(n_layers):
            if stages is None or "norm1" in stages:
                stage_norm(l, 0, 0, HT, R_HT)
            if stages is None or "proj" in stages:
                stage_proj(l)
            last = (l == DEPTH - 1)
            if stages is None or "na" in stages:
                stage_na(l, not last, **na_kw)
            if stages is None or "rwkv" in stages:
                stage_rwkv(l, not last)
            if stages is None or "wout" in stages:
                stage_wout(l, last)
            if stages is None or "ffn" in stages:
                moe = (l % 2 == 1)
                tsel = range(LC, S, 256) if last else None
                if moe:
                    stage_norm(l, 1, 3, H2T, R_H2T, dst32=H2F, R_dst32=R_H2F, tsel=tsel)
                    stage_router(l, last)
                else:
                    stage_norm(l, 1, 3, H2T, R_H2T, tsel=tsel)
                stage_ffn(l, last)
        if stages is None or "final" in stages:
            fin_out = stage_final()
        else:
            fin_out = []

        fin = list(fin_out)
        for r in R_XT + R_HT + R_QK + R_VN + R_UT + R_YT + R_YD[0] + R_YD[1] + R_BON + R_GG:
            if r.w is not None:
                fin.append(r.w)
        k.final_wait("sp", fin)
        k.emit()
        print("insts", k.n_inst, "epochs", k.n_epochs)
    return nc


def _fm(a, nchunk):
    return np.ascontiguousarray(a.reshape(nchunk, 128, -1).transpose(1, 0, 2))


def _build_bfull(rpb):
    L, H = rpb.shape[:2]
    q = np.arange(64)[:, None]; kc = np.arange(64)[None, :]
    lo = np.clip(q - 8, 0, 48)
    valid = (kc >= lo) & (kc < lo + 16)
    idx = np.clip(kc - q + 15, 0, 30)
    g = rpb[:, :, :, idx]
    g = np.where(valid[None, None, None], g, np.float32(NEG)).astype(np.float32)
    return np.ascontiguousarray(g.transpose(0, 3, 1, 2, 4).reshape(L, 64, H, 960))


def _prep_shared(inp):
    m = {}
    m["ada_w"] = np.stack([_fm(inp["ada_w"][l], 8) for l in range(4)])
    m["ada_bT"] = np.stack([inp["ada_b"][l].reshape(48, 128).T.copy() for l in range(4)])
    m["g_mix"] = np.stack([inp["norm_mix_g"][l].reshape(8, 128).T.copy() for l in range(4)])
    m["g_ffn"] = np.stack([inp["norm_ffn_g"][l].reshape(8, 128).T.copy() for l in range(4)])
    m["w_in"] = np.stack([_fm(inp["w_in"][l], 8) for l in range(4)])
    m["mu"] = np.stack([inp["shift_mu"][l].reshape(15, 128).T.copy() for l in range(4)])
    m["ident"] = np.eye(128, dtype=np.float32)
    m["final_g"] = inp["final_g"].reshape(8, 128).T.copy()
    m["bfull"] = _build_bfull(inp["na_rpb"])
    m["w_out"] = np.stack([_fm(inp["w_out"][l], 8) for l in range(4)])
    m["rw_w2"] = inp["w2"].reshape(4, 128, 512); m["rw_a2"] = inp["a2"].reshape(4, 128, 512); m["rw_g2"] = inp["g2"]
    m["rw_a0T"] = np.stack([inp["a0"][l].reshape(2, 4, 128).transpose(2, 0, 1) for l in range(4)])
    c4 = lambda a: np.stack([a[l].reshape(4, 128).T for l in range(4)])
    m["rw_kk"] = c4(inp["k_k"]); m["rw_ka"] = c4(inp["k_a"]); m["rw_rk"] = c4(inp["r_k"].reshape(4, 512))
    m["rw_lnw"] = np.broadcast_to(inp["ln_x_w"][:, None, :], (4, 128, 512)); m["rw_lnb"] = np.broadcast_to(inp["ln_x_b"][:, None, :], (4, 128, 512))
    m["rw_w0b"] = np.broadcast_to(inp["w0"][:, None, :, :], (4, 64, 2, 512))
    idx = np.arange(64)
    strict = [(idx[:, None] < idx[None, :]), (idx[:, None] > idx[None, :])]
    incl = [(idx[:, None] <= idx[None, :]), (idx[:, None] >= idx[None, :])]
    m["rw_m2"] = np.stack([np.concatenate([strict[d], incl[d]], 1) for d in range(2)], 1).astype(np.float32)
    m["rw_m3"] = np.stack([np.concatenate([strict[d].astype(np.float32), -incl[d].astype(np.float32)], 1) for d in range(2)], 1)
    m["rw_mL"] = np.stack([strict[d].T for d in range(2)], 1).astype(np.float32)
    bo = np.zeros((128, 128), np.float32); bo[:64, :64] = 1; bo[64:, 64:] = 1
    m["rw_bones"] = bo
    se = np.zeros((128, 2), np.float32); se[:64, 0] = 1; se[64:, 1] = 1
    m["rw_sel"] = se
    m["rw_c1"] = np.tile(np.array([1.0, -0.5, 64e-5, 1e-12], np.float32)[None], (128, 1))
    m["ffn_w1"] = np.stack([_fm(inp["ffn_w1"][i], 8) for i in range(2)])
    m["ffn_w3"] = np.stack([_fm(inp["ffn_w3"][i], 8) for i in range(2)])
    m["ffn_w2"] = np.stack([_fm(inp["ffn_w2"][i], DFF // 128) for i in range(2)])
    m["router"] = np.stack([_fm(inp["router"][i], 8) for i in range(2)])
    m["moe_w1"] = np.stack([np.stack([_fm(inp["moe_w1"][i, e], 8) for e in range(NE)]) for i in range(2)])
    m["moe_w3"] = np.stack([np.stack([_fm(inp["moe_w3"][i, e], 8) for e in range(NE)]) for i in range(2)])
    m["moe_w2"] = np.stack([np.stack([_fm(inp["moe_w2"][i, e], DFE // 128) for e in range(NE)]) for i in range(2)])
    return {k_: np.ascontiguousarray(v, dtype=np.float32) for k_, v in m.items()}


def _prep_core(inp, core):
    bs = [2 * core, 2 * core + 1]
    m = {}
    xcat = [np.concatenate([inp["ctx"][b], inp["x"][b]], 0) for b in bs]
    m["xT"] = np.stack([_fm(np.ascontiguousarray(xc.T), 8) for xc in xcat]).astype(np.float32)
    cc = np.stack([inp["c"][bs[0]], inp["c"][bs[1]], inp["c_ctx"]], 1)
    m["cT"] = _fm(cc, 8).astype(np.float32)
    return m


def kernel(**inputs):
    inp = {k_: np.asarray(v) for k_, v in inputs.items()}
    n = 8
    shared = _prep_shared(inp)
    in_maps = []
    for core in range(n):
        m = dict(shared)
        m.update(_prep_core(inp, core))
        in_maps.append(m)
    nc = build_program()
    res = run_bass_kernel_spmd(nc, in_maps, core_ids=list(range(n)))
    outs = []
    for core in range(n):
        o = np.asarray(res.results[core]["out"])
        for b in range(NB):
            outs.append(o[b].transpose(1, 0, 2).reshape(D, T).T)
    return np.ascontiguousarray(np.stack(outs, 0)).astype(np.float32)
```
